# Optimizing a Trainium2 kernel written in Bass

```python
import functools
import jax, jax.numpy as jnp
from jax import lax
import numpy as np

D_MODEL = 1024
BATCH = 16
SEQ = 4096
DEPTH = 1
DEC_BATCH = 128
DEC_SEQ = 1
PAST_LEN = 8192
PAGE_SIZE = 128

ATT_GROUPS = ((128, 1), (512, 4), (2048, 16))
N_GROUPS = 3
ATT_HEADS = 8
ATT_HEAD_DIM = 64
ATT_WIDTH = N_GROUPS * ATT_HEADS * ATT_HEAD_DIM
ATT_OUT = ATT_HEADS * ATT_HEAD_DIM
HG_HEADS = 8
HG_DK = 128
HG_DV = D_MODEL // HG_HEADS
HG_CHUNK = 64
PEER_HEADS = 8
PEER_KEYS = 128
PEER_EXPERTS = PEER_KEYS * PEER_KEYS
PEER_TOPK = 16
PEER_QDIM = 256
PEER_HALF = PEER_QDIM // 2
PEER_BLOCK = 256
EPS = 1e-6
IN_SPLITS = (ATT_WIDTH, ATT_WIDTH, ATT_WIDTH, HG_HEADS * HG_DK, HG_HEADS * HG_DK, HG_HEADS * HG_DV, HG_HEADS * HG_DV, D_MODEL, D_MODEL)
IN_WIDTH = sum(IN_SPLITS)
IN_OFFSETS = tuple(int(o) for o in np.cumsum(IN_SPLITS)[:-1])

kernel_name = 'hybrid_dilswa_hgrn2_peer_step'


def rms_norm(x, w):
    xf = x.astype(jnp.float32)
    y = xf * lax.rsqrt(jnp.mean(xf * xf, axis=-1, keepdims=True) + EPS)
    return (y * w.astype(jnp.float32)).astype(x.dtype)


def dilated_window_prompt(q, k, v, window, dilation):
    B, S, H, E = q.shape
    span = window // dilation
    blk = span
    unit = dilation * blk
    s_pad = -(-S // unit) * unit
    nb = s_pad // unit

    def to_blocks(t):
        t = jnp.pad(t, ((0, 0), (0, s_pad - S), (0, 0), (0, 0)))
        t = t.reshape(B, nb * blk, dilation, H, E).transpose(0, 2, 1, 3, 4)
        return t.reshape(B, dilation, nb, blk, H, E)

    def with_prev(t):
        prev = jnp.pad(t, ((0, 0), (0, 0), (1, 0), (0, 0), (0, 0), (0, 0)))[:, :, :-1]
        return jnp.concatenate([prev, t], axis=3)

    qb = to_blocks(q)
    kk = with_prev(to_blocks(k))
    vv = with_prev(to_blocks(v))
    s = jnp.einsum('brnqhe,brnkhe->brnqhk', qb, kk, preferred_element_type=jnp.float32)
    qi = jnp.arange(blk)[:, None]
    ki = jnp.arange(2 * blk)[None, :]
    delta = qi + blk - ki
    band = (delta >= 0) & (delta <= span)
    valid = band[None] & ((jnp.arange(nb)[:, None, None] > 0) | (ki[None] >= blk))
    s = jnp.where(valid[None, None, :, :, None, :], s, -jnp.inf)
    m = jnp.max(s, axis=-1, keepdims=True)
    p = jnp.exp(s - m)
    l = jnp.sum(p, axis=-1)
    o = jnp.einsum('brnqhk,brnkhe->brnqhe', p, vv, preferred_element_type=jnp.float32) / l[..., None]
    lse = m[..., 0] + jnp.log(l)
    o = o.reshape(B, dilation, nb * blk, H, E).transpose(0, 2, 1, 3, 4).reshape(B, s_pad, H, E)[:, :S]
    lse = lse.reshape(B, dilation, nb * blk, H).transpose(0, 2, 1, 3).reshape(B, s_pad, H)[:, :S]
    return o, lse


def dilated_window_decode(cache_kv, q, k, v, window, dilation):
    L = cache_kv.shape[1]
    T = q.shape[1]
    keys = jnp.concatenate([cache_kv[:, :, 0].astype(k.dtype), k], axis=1)
    vals = jnp.concatenate([cache_kv[:, :, 1].astype(v.dtype), v], axis=1)
    n_taps = window // dilation + 1
    idx = L + jnp.arange(T)[:, None] - dilation * jnp.arange(n_taps)[None, :]
    valid = idx >= 0
    idx = jnp.maximum(idx, 0)
    kg = keys[:, idx]
    vg = vals[:, idx]
    s = jnp.einsum('bthe,btmhe->bthm', q, kg, preferred_element_type=jnp.float32)
    s = jnp.where(valid[None, :, None, :], s, -jnp.inf)
    m = jnp.max(s, axis=-1, keepdims=True)
    p = jnp.exp(s - m)
    l = jnp.sum(p, axis=-1)
    o = jnp.einsum('bthm,btmhe->bthe', p, vg, preferred_element_type=jnp.float32) / l[..., None]
    return o, m[..., 0] + jnp.log(l)


def merge_groups(outs, lses):
    w = jax.nn.softmax(jnp.stack(lses, 0), axis=0)
    o = jnp.einsum('gbth,gbthe->bthe', w, jnp.stack(outs, 0))
    return o.reshape(o.shape[0], o.shape[1], ATT_OUT)


def attend_prompt(qa, ka, va):
    T = qa.shape[1]
    outs, lses, rows = [], [], []
    for g, (win, dil) in enumerate(ATT_GROUPS):
        o, lse = dilated_window_prompt(qa[:, :, g], ka[:, :, g], va[:, :, g], win, dil)
        outs.append(o)
        lses.append(lse)
        rows.append(jnp.stack([ka[:, :, g], va[:, :, g]], axis=2)[:, max(T - win, 0):])
    return merge_groups(outs, lses), rows


def attend_sample(caches, qa, ka, va):
    outs, lses, rows = [], [], []
    for g, (win, dil) in enumerate(ATT_GROUPS):
        o, lse = dilated_window_decode(caches[g], qa[:, :, g], ka[:, :, g], va[:, :, g], win, dil)
        outs.append(o)
        lses.append(lse)
        rows.append(jnp.stack([ka[:, :, g], va[:, :, g]], axis=2))
    return merge_groups(outs, lses), rows


def hgrn2_chunk(state, q, k, v, logf):
    C = q.shape[1]
    b = jnp.cumsum(logf, axis=1)
    causal = jnp.tril(jnp.ones((C, C), bool))
    diff = b[:, :, None] - b[:, None, :]
    decay = jnp.exp(jnp.where(causal[None, :, :, None, None], diff, -jnp.inf))
    attn = jnp.einsum('bthk,bshk,btshk->bhts', q, k, decay)
    o = jnp.einsum('bthk,bhkv->bthv', q * jnp.exp(b), state) + jnp.einsum('bhts,bshv->bthv', attn, v)
    b_last = b[:, -1]
    new_state = jnp.exp(b_last)[..., None] * state + jnp.einsum('bshk,bshv->bhkv', k * jnp.exp(b_last[:, None] - b), v)
    return new_state, o


def hgrn2_prompt(q, k, v, logf):
    B, S, H, _ = q.shape
    nc = S // HG_CHUNK

    def chunks(t):
        return t.reshape(B, nc, HG_CHUNK, *t.shape[2:]).swapaxes(0, 1)

    s0 = jnp.zeros((B, H, HG_DK, HG_DV), jnp.float32)
    s_fin, o = lax.scan(lambda st, inp: hgrn2_chunk(st, *inp), s0, (chunks(q), chunks(k), chunks(v), chunks(logf)))
    return o.swapaxes(0, 1).reshape(B, S, H, HG_DV), s_fin


def hgrn2_step(state, q, k, v, logf):
    new_state, o = hgrn2_chunk(state.astype(jnp.float32), q, k, v, logf)
    return o, new_state


def peer_ffn(x, w_q, subkeys, u_tab, v_tab):
    N, D = x.shape
    nb = -(-N // PEER_BLOCK)
    xp = jnp.pad(x, ((0, nb * PEER_BLOCK - N), (0, 0))).reshape(nb, PEER_BLOCK, D)

    def block(xb):
        q = (xb @ w_q).reshape(PEER_BLOCK, PEER_HEADS, 2, PEER_HALF)
        s = jnp.einsum('nhpe,pke->nhpk', q, subkeys, preferred_element_type=jnp.float32)
        s1, i1 = lax.top_k(s[:, :, 0], PEER_TOPK)
        s2, i2 = lax.top_k(s[:, :, 1], PEER_TOPK)
        cand = (s1[..., :, None] + s2[..., None, :]).reshape(PEER_BLOCK, PEER_HEADS, PEER_TOPK * PEER_TOPK)
        cidx = (i1[..., :, None] * PEER_KEYS + i2[..., None, :]).reshape(PEER_BLOCK, PEER_HEADS, PEER_TOPK * PEER_TOPK)
        top, pos = lax.top_k(cand, PEER_TOPK)
        idx = jnp.take_along_axis(cidx, pos, axis=-1)
        g = jax.nn.softmax(top, axis=-1)
        act = jax.nn.gelu(jnp.einsum('nd,nhkd->nhk', xb, u_tab[idx], preferred_element_type=jnp.float32))
        return jnp.einsum('nhk,nhkd->nd', (g * act).astype(xb.dtype), v_tab[idx])

    return lax.map(block, xp).reshape(nb * PEER_BLOCK, D)[:N]


def run_layer(x, c, attend, recur, w_ada, b_ada, norm1_w, norm2_w, w_in, q_norm_w, k_norm_w, lb, hg_norm_w, w_br_a, w_br_b, w_o, w_peer_q, peer_subkeys, peer_u, peer_v):
    B, T, D = x.shape
    mods = jax.nn.silu(c) @ w_ada + b_ada
    sh1, sc1, g1, sh2, sc2, g2 = [m[:, None, :] for m in jnp.split(mods, 6, axis=-1)]
    h = rms_norm(x, norm1_w) * (1 + sc1) + sh1
    qa, ka, va, qh, fh, ih, gh, ga, gb = jnp.split(h @ w_in, IN_OFFSETS, axis=-1)
    att_shape = (B, T, N_GROUPS, ATT_HEADS, ATT_HEAD_DIM)
    qa = rms_norm(qa.reshape(att_shape), q_norm_w) * (ATT_HEAD_DIM ** -0.5)
    ka = rms_norm(ka.reshape(att_shape), k_norm_w)
    va = va.reshape(att_shape)
    att, kv_rows = attend(qa, ka, va)
    att = att.astype(x.dtype)
    f = lb + (1 - lb) * jax.nn.sigmoid(fh.astype(jnp.float32))
    hg_k_shape = (B, T, HG_HEADS, HG_DK)
    q_hg = jax.nn.silu(qh.astype(jnp.float32)).reshape(hg_k_shape)
    k_hg = (1 - f).reshape(hg_k_shape)
    logf = jnp.log(f).reshape(hg_k_shape)
    v_hg = ih.astype(jnp.float32).reshape(B, T, HG_HEADS, HG_DV)
    o_hg, hg_state = recur(q_hg, k_hg, v_hg, logf)
    o_hg = rms_norm(o_hg, hg_norm_w) * jax.nn.silu(gh.astype(jnp.float32).reshape(B, T, HG_HEADS, HG_DV))
    hg = o_hg.reshape(B, T, HG_HEADS * HG_DV).astype(x.dtype)
    y = jax.nn.sigmoid(ga) * (att @ w_br_a) + jax.nn.sigmoid(gb) * (hg @ w_br_b)
    x = x + g1 * (y @ w_o)
    h2 = rms_norm(x, norm2_w) * (1 + sc2) + sh2
    x = x + g2 * peer_ffn(h2.reshape(B * T, D), w_peer_q, peer_subkeys, peer_u, peer_v).reshape(B, T, D)
    return x, kv_rows, hg_state


def setup_inputs(seed: int = 0) -> dict:
    key = jax.random.key(seed)
    ks = iter(jax.random.split(key, 32))

    def nrm(shape, scale=1.0):
        return jax.random.normal(next(ks), shape, jnp.float32) * scale

    def gain(shape):
        return 1.0 + 0.02 * jax.random.normal(next(ks), shape, jnp.float32)

    kv_tail = (2, ATT_HEADS, ATT_HEAD_DIM)
    return {
        'x_prompt': nrm((BATCH, SEQ, D_MODEL)),
        'x_sample': nrm((DEC_BATCH, DEC_SEQ, D_MODEL)),
        'cache_kv_w128': nrm((DEPTH, DEC_BATCH, min(ATT_GROUPS[0][0], PAST_LEN)) + kv_tail),
        'cache_kv_w512': nrm((DEPTH, DEC_BATCH, min(ATT_GROUPS[1][0], PAST_LEN)) + kv_tail),
        'cache_kv_w2048': nrm((DEPTH, DEC_BATCH, min(ATT_GROUPS[2][0], PAST_LEN)) + kv_tail),
        'state_hgrn': nrm((DEPTH, DEC_BATCH, HG_HEADS, HG_DK, HG_DV), 0.5),
        'c_prompt': nrm((BATCH, D_MODEL)),
        'c_sample': nrm((DEC_BATCH, D_MODEL)),
        'w_ada': nrm((DEPTH, D_MODEL, 6 * D_MODEL), 0.5 * D_MODEL ** -0.5),
        'b_ada': nrm((DEPTH, 6 * D_MODEL), 0.02),
        'norm1_w': gain((DEPTH, D_MODEL)),
        'norm2_w': gain((DEPTH, D_MODEL)),
        'w_in': nrm((DEPTH, D_MODEL, IN_WIDTH), D_MODEL ** -0.5),
        'q_norm_w': gain((DEPTH, ATT_HEAD_DIM)),
        'k_norm_w': gain((DEPTH, ATT_HEAD_DIM)),
        'hg_lb_logits': nrm((DEPTH + 1, HG_HEADS * HG_DK)),
        'hg_norm_w': gain((DEPTH, HG_DV)),
        'w_br_a': nrm((DEPTH, ATT_OUT, D_MODEL), ATT_OUT ** -0.5),
        'w_br_b': nrm((DEPTH, HG_HEADS * HG_DV, D_MODEL), (HG_HEADS * HG_DV) ** -0.5),
        'w_o': nrm((DEPTH, D_MODEL, D_MODEL), D_MODEL ** -0.5),
        'w_peer_q': nrm((DEPTH, D_MODEL, PEER_HEADS * PEER_QDIM), D_MODEL ** -0.5),
        'peer_subkeys': nrm((DEPTH, 2, PEER_KEYS, PEER_HALF), PEER_HALF ** -0.5),
        'peer_u': nrm((DEPTH, PEER_EXPERTS, D_MODEL), D_MODEL ** -0.5),
        'peer_v': nrm((DEPTH, PEER_EXPERTS, D_MODEL), PEER_HEADS ** -0.5),
    }


def reference(x_prompt, x_sample, cache_kv_w128, cache_kv_w512, cache_kv_w2048, state_hgrn, c_prompt, c_sample, w_ada, b_ada, norm1_w, norm2_w, w_in, q_norm_w, k_norm_w, hg_lb_logits, hg_norm_w, w_br_a, w_br_b, w_o, w_peer_q, peer_subkeys, peer_u, peer_v):
    lower_bounds = jnp.cumsum(jax.nn.softmax(hg_lb_logits.astype(jnp.float32), axis=0), axis=0)
    y_prompt, y_sample = x_prompt, x_sample
    kv_p = [[] for _ in range(N_GROUPS)]
    kv_s = [[] for _ in range(N_GROUPS)]
    hg_p, hg_s = [], []
    for layer in range(DEPTH):
        weights = (w_ada[layer], b_ada[layer], norm1_w[layer], norm2_w[layer], w_in[layer], q_norm_w[layer], k_norm_w[layer], lower_bounds[layer], hg_norm_w[layer], w_br_a[layer], w_br_b[layer], w_o[layer], w_peer_q[layer], peer_subkeys[layer], peer_u[layer], peer_v[layer])
        y_prompt, rows_p, st_p = run_layer(y_prompt, c_prompt, attend_prompt, hgrn2_prompt, *weights)
        caches = (cache_kv_w128[layer], cache_kv_w512[layer], cache_kv_w2048[layer])
        y_sample, rows_s, st_s = run_layer(y_sample, c_sample, functools.partial(attend_sample, caches), functools.partial(hgrn2_step, state_hgrn[layer]), *weights)
        for g in range(N_GROUPS):
            kv_p[g].append(rows_p[g])
            kv_s[g].append(rows_s[g])
        hg_p.append(st_p)
        hg_s.append(st_s)
    kv128_p = jnp.stack(kv_p[0])
    kv512_p = jnp.stack(kv_p[1])
    kv2048_p = jnp.stack(kv_p[2])
    hgrn_p = jnp.stack(hg_p)
    kv128_s = jnp.stack(kv_s[0])
    kv512_s = jnp.stack(kv_s[1])
    kv2048_s = jnp.stack(kv_s[2])
    hgrn_s = jnp.stack(hg_s)
    return (y_prompt, y_sample, kv128_p, kv512_p, kv2048_p, hgrn_p, kv128_s, kv512_s, kv2048_s, hgrn_s)
```

```python
import contextlib
import numpy as np
import concourse.bass as bass
import concourse.mybir as mybir
from concourse.bass_utils import run_bass_kernel_spmd

F32 = mybir.dt.float32
BF16 = mybir.dt.bfloat16
I32 = mybir.dt.int32
U32 = mybir.dt.uint32
ALU = mybir.AluOpType
AF = mybir.ActivationFunctionType
AX = mybir.AxisListType

NCORES = 8
D = 1024
SEQ = 4096
NSEQ = 2
NS = 16
INW = 10752
GROUPS = ((128, 1), (512, 4), (2048, 16))
EPS = 1e-6
NT = SEQ // 128
NROWS = NSEQ + NS
DEBUG = False


class Res:
    __slots__ = ("w", "rs")

    def __init__(self):
        self.w = {}
        self.rs = {}


class K:
    def __init__(self, nc, es):
        self.nc = nc
        self.es = es
        self.eng = {"pe": nc.tensor, "act": nc.scalar, "dve": nc.vector, "pool": nc.gpsimd, "sp": nc.sync}
        self.sem = {}
        self.cnt = {}
        for e in ("pe", "act", "dve", "pool"):
            self.sem[e] = es.enter_context(nc.semaphore("c_" + e))
            self.cnt[e] = 0
        self.known = {e: {} for e in self.eng}
        self.dsem = {}
        self.dpos = {}
        for q, n in (("sp", 24), ("pool", 16), ("act", 8)):
            self.dsem[q] = [[es.enter_context(nc.semaphore("d_%s%d" % (q, i))), 0] for i in range(n)]
            self.dpos[q] = 0
        self.deferred = []

    def _wait(self, e, tok):
        s, v = tok
        if v <= 0:
            return
        if e == "pe" and s is self.sem["pe"]:
            return
        kn = self.known[e]
        if kn.get(id(s), 0) >= v:
            return
        self.eng[e].wait_ge(s, v)
        kn[id(s)] = v

    def _deps(self, e, reads, writes):
        for r in reads:
            for tok in r.w.values():
                self._wait(e, tok)
        for r in writes:
            for tok in r.w.values():
                self._wait(e, tok)
            for tok in r.rs.values():
                self._wait(e, tok)

    def _mark(self, tok, reads, writes, accum):
        s, v = tok
        for r in reads:
            r.rs[id(s)] = tok
        for r in writes:
            r.w = {id(s): tok}
            r.rs = {}
        for r in accum:
            r.w[id(s)] = tok

    def op(self, e, fn, reads=(), writes=(), accum=()):
        self._deps(e, reads, writes)
        ins = fn()
        self.cnt[e] += 1
        ins.then_inc(self.sem[e], 1)
        self._mark((self.sem[e], self.cnt[e]), reads, writes, accum)

    def dma(self, q, out, in_, reads=(), writes=(), accum=(), indirect=None):
        self._deps(q, reads, writes)
        slot = self.dsem[q][self.dpos[q] % len(self.dsem[q])]
        self.dpos[q] += 1
        self._wait(q, (slot[0], slot[1]))
        if indirect is not None:
            ins = self.eng[q].indirect_dma_start(out=out, out_offset=None, in_=in_, in_offset=indirect)
        else:
            ins = self.eng[q].dma_start(out=out, in_=in_)
        slot[1] += 16
        ins.then_inc(slot[0], 16)
        self._mark((slot[0], slot[1]), reads, writes, accum)

    def barrier(self):
        self.flush()
        toks = [(self.sem[e], self.cnt[e]) for e in self.sem]
        for q in self.dsem:
            toks += [(s, v) for s, v in self.dsem[q]]
        for e in self.eng:
            for tok in toks:
                if e != "pe" or tok[0] is not self.sem["pe"]:
                    self._wait(e, tok)

    def defer(self, fn):
        self.deferred.append(fn)

    def flush(self):
        d, self.deferred = self.deferred, []
        for fn in d:
            fn()

    def finish(self):
        self.flush()
        for q in self.dsem:
            for s, v in self.dsem[q]:
                self._wait("sp", (s, v))


def build_program(stop_after=99):
    nc = bass.Bass("TRN2", target_bir_lowering=False)
    es = contextlib.ExitStack()
    with es:
        _emit(nc, es, stop_after)
    return nc


def _emit(nc, es, stop_after):
    k = K(nc, es)

    def din(name, shape, dt=F32):
        return nc.dram_tensor(name, list(shape), dt, kind="ExternalInput").ap()

    def dout(name, shape, dt=F32):
        return nc.dram_tensor(name, list(shape), dt, kind="ExternalOutput").ap()

    def dscr(name, shape, dt):
        return nc.dram_tensor(name, list(shape), dt, kind="Internal").ap()

    def sb(name, shape, dt):
        return es.enter_context(nc.sbuf_tensor(name, list(shape), dt))

    xp = din("xp", [NSEQ, SEQ, D])
    xs = din("xs", [NS, D])
    cT = din("cT", [D, NROWS])
    ck = [din("ck%d" % g, [NS, 128, 2, 512]) for g in range(3)]
    st_in = din("st_in", [NS, 8, 128, 128])
    w_ada = din("w_ada", [D, 6 * D])
    b_ada = din("b_ada", [1, 6 * D])
    norm1_w = din("norm1_w", [1, D])
    norm2_w = din("norm2_w", [1, D])
    w_in = din("w_in", [D, INW])
    qk_w = din("qk_w", [2, 512])
    lb_log = din("lb_log", [2, D])
    hgn_w = din("hgn_w", [1, D])
    w_br_a = din("w_br_a", [512, D])
    w_br_b = din("w_br_b", [D, D])
    w_o = din("w_o", [D, D])
    w_pq = din("w_pq", [D, 2048])
    skT = din("skT", [2, 128, 128])
    peer_u = din("peer_u", [16384, D])
    peer_v = din("peer_v", [16384, D])
    cst = din("cst", [128, 128 * 8])
    selc = din("selc", [NS, 2064])

    y_p = dout("y_p", [NSEQ, SEQ, D])
    y_s = dout("y_s", [NS, D])
    kv_p = [dout("kv%d_p" % g, [NSEQ, GROUPS[g][0], 2, 512]) for g in range(3)]
    hg_p = dout("hg_p", [NSEQ, 8, 128, 128])
    kv_s = [dout("kv%d_s" % g, [NS, 2, 512]) for g in range(3)]
    hg_s = dout("hg_s", [NS, 8, 128, 128])
    dbg = dout("dbg", [2, 10, 128, D]) if DEBUG else None

    MODS = dscr("MODS", [NROWS, 6 * D], F32)
    NTOK = NSEQ * SEQ + 128
    QS = dscr("QS", [NTOK, 3, 512], BF16)
    KS = dscr("KS", [NTOK, 3, 512], BF16)
    VS = dscr("VS", [NTOK, 3, 520], BF16)

    cst_f = sb("cst_f", [128, 1024], F32)
    ident_b = sb("ident_b", [128, 128], BF16)
    r_cst = Res()
    k.dma("sp", cst_f[:], cst, writes=[r_cst])
    r_idb = Res()
    k.op("dve", lambda: nc.vector.tensor_copy(ident_b[:], cst_f[:, 0:128]), reads=[r_cst], writes=[r_idb])
    ident_f = cst_f[:, 0:128]

    psf = [es.enter_context(nc.psum_tensor("psf%d" % i, [128, 512], F32)) for i in range(7)]
    r_psf = [Res() for _ in range(7)]
    psb = es.enter_context(nc.psum_tensor("psb", [128, 1024], BF16))
    r_psb = Res()

    def rstd_from_ss(P, ss, n_elem, tmp):
        t, r = ss
        k.op("dve", lambda: nc.vector.tensor_scalar(t, t, 1.0 / n_elem, EPS, ALU.mult, ALU.add), writes=[r])
        k.op("act", lambda: nc.scalar.activation(t, t, AF.Sqrt), writes=[r])
        k.op("dve", lambda: nc.vector.reciprocal(t, t), writes=[r])

    with contextlib.ExitStack() as ph:
        def psb_(name, shape, dt):
            return ph.enter_context(nc.sbuf_tensor(name, list(shape), dt))
        cT_f = psb_("cT_f", [128, 8, NROWS], F32)
        cT_b = psb_("cT_b", [128, 8, NROWS], BF16)
        r_cT = Res()
        k.dma("sp", cT_f[:], cT.rearrange("(kc p) n -> p kc n", p=128), writes=[r_cT])
        k.op("act", lambda: nc.scalar.activation(cT_b[:], cT_f[:], AF.Silu), reads=[r_cT], writes=[r_cT])
        wa = [psb_("wa%d" % i, [128, 8, 512], BF16) for i in range(2)]
        r_wa = [Res(), Res()]
        ba = [psb_("ba%d" % i, [NROWS, 512], F32) for i in range(2)]
        r_ba = [Res(), Res()]
        mo = [psb_("mo%d" % i, [NROWS, 512], F32) for i in range(2)]
        r_mo = [Res(), Res()]
        r_MODS = Res()
        for c in range(12):
            i = c % 2
            cs = slice(c * 512, (c + 1) * 512)
            k.dma("pool", wa[i][:], w_ada[:, cs].rearrange("(kc p) n -> p kc n", p=128), writes=[r_wa[i]])
            k.dma("sp", ba[i][:], b_ada[:, cs].partition_broadcast(NROWS), writes=[r_ba[i]])
            for kc in range(8):
                k.op("pe", lambda kc=kc: nc.tensor.matmul(psf[i][0:NROWS, :], cT_b[:, kc, :], wa[i][:, kc, :],
                                                          start=(kc == 0), stop=(kc == 7)),
                     reads=[r_cT, r_wa[i]], writes=[r_psf[i]])
            k.op("dve", lambda: nc.vector.tensor_tensor(mo[i][:], psf[i][0:NROWS, :], ba[i][:], ALU.add),
                 reads=[r_ba[i]], writes=[r_psf[i], r_mo[i]])
            k.dma("sp", MODS[:, cs], mo[i][:], reads=[r_mo[i]], accum=[r_MODS])
        k.barrier()
    if stop_after <= 0:
        k.finish()
        return

    def tok0(s):
        return s * SEQ

    NTOKX = NSEQ * SEQ + 128
    GG = dscr("GG", [NTOKX, 3072], BF16)
    HGS = dscr("HGS", [NTOKX, D], BF16)
    OACC = dscr("OACC", [3, NTOKX, 520], F32)
    UV = dscr("UV", [16384, 2 * D], BF16)
    r_uv = Res()
    r_scr = Res()
    r_gg = Res()
    r_hgs = Res()
    r_oacc = Res()
    bank_ctr = [0]

    nbank_rot = [7]

    def nb():
        i = bank_ctr[0] % nbank_rot[0]
        bank_ctr[0] += 1
        return psf[i], r_psf[i]

    maskc_b = sb("maskc_b", [128, 4, 128], BF16)
    maskp_b = sb("maskp_b", [128, 4, 128], BF16)
    r_mask = Res()
    for hh in range(4):
        k.op("dve", lambda hh=hh: nc.vector.tensor_copy(maskc_b[:, hh, :], cst_f[:, 128:256]), reads=[r_cst], writes=[r_mask])
        k.op("dve", lambda hh=hh: nc.vector.tensor_copy(maskp_b[:, hh, :], cst_f[:, 256:384]), reads=[r_cst], writes=[r_mask])
    L1 = cst_f[:, 384:512]
    L3 = cst_f[:, 512:640]
    R4 = cst_f[:, 640:644]
    MA = cst_f[:, 768:896]

    def proj(P, hT, ts_, W, c0, ncol512, r_hT, r_W):
        out = []
        for j in range(ncol512):
            b_, rb = nb()
            for kc in range(8):
                k.op("pe", lambda kc=kc: nc.tensor.matmul(b_[0:P, :], hT[:, kc, ts_], W[:, kc, c0 + j * 512:c0 + (j + 1) * 512],
                                                          start=(kc == 0), stop=(kc == 7)),
                     reads=[r_hT, r_W], writes=[rb])
            out.append((b_, rb))
        return out

    with contextlib.ExitStack() as seqscope:
        hT = seqscope.enter_context(nc.sbuf_tensor("hT", [128, 8, SEQ], BF16))
        r_hT = Res()
        for s in range(3):
            P = 128 if s < 2 else NS
            ntile = NT if s < 2 else 1

            def xsrc(t):
                return xp[s, t * 128:(t + 1) * 128, :] if s < 2 else xs[:, :]

            with contextlib.ExitStack() as ph:
                def psb_(name, shape, dt):
                    return ph.enter_context(nc.sbuf_tensor(name + "_s%d" % s, list(shape), dt))
                S1 = psb_("S1", [128, D], F32)
                SH1 = psb_("SH1", [128, D], F32)
                n1w = psb_("n1w", [128, D], F32)
                r_S1 = Res()
                r_n1w = Res()
                k.dma("sp", n1w[:], norm1_w.partition_broadcast(128), writes=[r_n1w])
                qkw = psb_("qkw", [128, 2, 512], F32)
                r_qkw = Res()
                k.dma("sp", qkw[:, 0, :], qk_w[0:1, :].partition_broadcast(128), writes=[r_qkw])
                k.dma("sp", qkw[:, 1, :], qk_w[1:2, :].partition_broadcast(128), writes=[r_qkw])
                k.op("dve", lambda: nc.vector.tensor_scalar(qkw[:, 0, :], qkw[:, 0, :], 0.125, None, ALU.mult), writes=[r_qkw])
                xt = [psb_("xt%d" % i, [128, D], F32) for i in range(2)]
                r_xt = [Res(), Res()]
                xm = psb_("xm", [128, D], F32)
                xb = psb_("xb", [128, D], BF16)
                r_xm = Res()
                r_xb = Res()
                junk = psb_("junk", [128, D], F32)
                r_junk = Res()
                ss1 = psb_("ss1", [128, 1], F32)
                r_ss1 = Res()
                Wg = psb_("Wg", [128, 8, 1536], BF16)
                r_Wg = Res()
                ss8 = psb_("ss8", [128, 8], F32)
                r_ss8 = Res()
                qn_b = [psb_("qn_b%d" % i, [128, 512], BF16) for i in range(2)]
                r_qn = [Res(), Res()]
                kn32 = [psb_("kn32%d" % i, [128, 512], F32) for i in range(2)]
                r_kn32 = [Res(), Res()]
                kn_b = [psb_("kn_b%d" % i, [128, 512], BF16) for i in range(2)]
                r_knb = [Res(), Res()]
                v32 = [psb_("v32%d" % i, [128, 512], F32) for i in range(2)]
                r_v32 = [Res(), Res()]
                vaug = [psb_("vaug%d" % i, [128, 8, 65], BF16) for i in range(2)]
                r_vaug = [Res(), Res()]
                for i in range(2):
                    k.op("pool", lambda i=i: nc.gpsimd.memset(vaug[i][:], 1.0), writes=[r_vaug[i]])

                if s < 2:
                    k.dma("sp", SH1[:], MODS[s:s + 1, 0:D].partition_broadcast(128), reads=[r_MODS], writes=[r_S1])
                    k.dma("sp", S1[:], MODS[s:s + 1, D:2 * D].partition_broadcast(128), reads=[r_MODS], writes=[r_S1])
                else:
                    k.dma("sp", SH1[0:NS, :], MODS[2:2 + NS, 0:D], reads=[r_MODS], writes=[r_S1])
                    k.dma("sp", S1[0:NS, :], MODS[2:2 + NS, D:2 * D], reads=[r_MODS], writes=[r_S1])
                k.op("dve", lambda: nc.vector.scalar_tensor_tensor(S1[0:P, :], S1[0:P, :], 1.0, n1w[0:P, :], ALU.add, ALU.mult),
                     reads=[r_n1w], writes=[r_S1])

                k.dma("sp", xt[0][0:P, :], xsrc(0), writes=[r_xt[0]])
                for t in range(ntile):
                    i = t % 2
                    if t + 1 < ntile:
                        k.dma("sp", xt[1 - i][0:P, :], xsrc(t + 1), writes=[r_xt[1 - i]])
                    k.op("act", lambda: nc.scalar.activation(junk[0:P, :], xt[i][0:P, :], AF.Square, accum_out=ss1[0:P, :]),
                         reads=[r_xt[i]], writes=[r_junk, r_ss1])
                    rstd_from_ss(P, (ss1[0:P, :], r_ss1), D, None)
                    k.op("dve", lambda: nc.vector.scalar_tensor_tensor(xm[0:P, :], xt[i][0:P, :], ss1[0:P, 0:1], S1[0:P, :],
                                                                       ALU.mult, ALU.mult),
                         reads=[r_xt[i], r_ss1, r_S1], writes=[r_xm])
                    k.op("dve", lambda: nc.vector.tensor_tensor(xb[0:P, :], xm[0:P, :], SH1[0:P, :], ALU.add),
                         reads=[r_xm, r_S1], writes=[r_xb])
                    for kc in range(8):
                        k.op("pe", lambda kc=kc: nc.tensor.transpose(psb[:, kc * 128:kc * 128 + P], xb[0:P, kc * 128:(kc + 1) * 128],
                                                                     ident_b[0:P, 0:P]),
                             reads=[r_xb, r_idb], writes=[r_psb])
                    k.op("act", lambda: nc.scalar.copy(hT[:, :, t * 128:t * 128 + P],
                                                       psb[:].rearrange("p (kc n) -> p kc n", kc=8)[:, :, 0:P]),
                         writes=[r_psb, r_hT])

                for g in range(3):
                    win = GROUPS[g][0]
                    for part in range(3):
                        c0 = part * 1536 + g * 512
                        k.dma("pool", Wg[:, :, part * 512:(part + 1) * 512],
                              w_in[:, c0:c0 + 512].rearrange("(kc p) n -> p kc n", p=128), writes=[r_Wg])
                    for t in range(ntile):
                        i = t % 2
                        ts_ = slice(t * 128, t * 128 + P)
                        g0 = tok0(s) + t * 128
                        pr = proj(P, hT, ts_, Wg, 0, 3, r_hT, r_Wg)
                        bank = [p_[0] for p_ in pr]
                        rbank = [p_[1] for p_ in pr]
                        for part in range(2):
                            ps_ = bank[part]
                            k.op("act", lambda: nc.scalar.activation(junk[0:P, 0:512], ps_[0:P, :], AF.Square),
                                 writes=[rbank[part], r_junk])
                            k.op("dve", lambda: nc.vector.tensor_reduce(ss8[0:P, :], junk[0:P, 0:512].rearrange("p (h e) -> p h e", h=8),
                                                                        AX.X, ALU.add),
                                 reads=[r_junk], writes=[r_ss8])
                            rstd_from_ss(P, (ss8[0:P, :], r_ss8), 64, None)
                            dst32 = junk if part == 0 else kn32[i]
                            rdst = r_junk if part == 0 else r_kn32[i]
                            k.op("dve", lambda: nc.vector.tensor_tensor(
                                dst32[0:P, 0:512].rearrange("p (h e) -> p h e", h=8),
                                ps_[0:P, :].rearrange("p (h e) -> p h e", h=8),
                                ss8[0:P, :].unsqueeze(2).to_broadcast([P, 8, 64]), ALU.mult),
                                reads=[r_ss8], writes=[rbank[part], rdst])
                            if part == 0:
                                k.op("dve", lambda: nc.vector.tensor_tensor(qn_b[i][0:P, :], junk[0:P, 0:512], qkw[0:P, 0, :], ALU.mult),
                                     reads=[r_junk, r_qkw], writes=[r_qn[i]])
                            else:
                                k.op("dve", lambda: nc.vector.tensor_tensor(kn32[i][0:P, :], kn32[i][0:P, :], qkw[0:P, 1, :], ALU.mult),
                                     reads=[r_qkw], writes=[r_kn32[i]])
                                k.op("pool", lambda: nc.gpsimd.tensor_copy(kn_b[i][0:P, :], kn32[i][0:P, :]),
                                     reads=[r_kn32[i]], writes=[r_knb[i]])
                        k.op("act", lambda: nc.scalar.copy(v32[i][0:P, :], bank[2][0:P, :]), writes=[rbank[2], r_v32[i]])
                        k.op("pool", lambda: nc.gpsimd.tensor_copy(vaug[i][0:P, :, 0:64],
                                                                    v32[i][0:P, :].rearrange("p (h e) -> p h e", h=8)),
                             reads=[r_v32[i]], writes=[r_vaug[i]])
                        k.flush()

                        def stores(i=i, g=g, g0=g0, t=t, P=P, s=s, win=win):
                            k.dma("sp", QS[g0:g0 + P, g, :], qn_b[i][0:P, :], reads=[r_qn[i]], accum=[r_scr])
                            k.dma("sp", KS[g0:g0 + P, g, :], kn_b[i][0:P, :], reads=[r_knb[i]], accum=[r_scr])
                            k.dma("sp", VS[g0:g0 + P, g, :], vaug[i][0:P, :, :].rearrange("p h e -> p (h e)"),
                                  reads=[r_vaug[i]], accum=[r_scr])
                            if s < 2:
                                r0 = t * 128 - (SEQ - win)
                                if r0 >= 0:
                                    k.dma("sp", kv_p[g][s, r0:r0 + 128, 0, :], kn32[i][:], reads=[r_kn32[i]])
                                    k.dma("sp", kv_p[g][s, r0:r0 + 128, 1, :], v32[i][:], reads=[r_v32[i]])
                            else:
                                k.dma("sp", kv_s[g][:, 0, :], kn32[i][0:P, :], reads=[r_kn32[i]])
                                k.dma("sp", kv_s[g][:, 1, :], v32[i][0:P, :], reads=[r_v32[i]])
                        k.defer(stores)
                    k.flush()
                k.barrier()
            if stop_after <= 1:
                continue

            with contextlib.ExitStack() as ph:
                def psb_(name, shape, dt):
                    return ph.enter_context(nc.sbuf_tensor(name + "_s%d" % s, list(shape), dt))
                Wt = psb_("Wt", [128, 8, 3072], BF16)
                r_Wt = Res()
                for j, c0 in enumerate((7680, 8704, 9728)):
                    k.dma("pool", Wt[:, :, j * 1024:(j + 1) * 1024],
                          w_in[:, c0:c0 + 1024].rearrange("(kc p) n -> p kc n", p=128), writes=[r_Wt])
                ggb = [psb_("ggb%d" % i, [128, 3072], BF16) for i in range(2)]
                r_ggb = [Res(), Res()]
                for t in range(ntile):
                    i = t % 2
                    ts_ = slice(t * 128, t * 128 + P)
                    g0 = tok0(s) + t * 128
                    for j in range(6):
                        pr = proj(P, hT, ts_, Wt, j * 512, 1, r_hT, r_Wt)
                        b_, rb = pr[0]
                        fn_ = AF.Silu if j < 2 else AF.Sigmoid
                        k.op("act", lambda: nc.scalar.activation(ggb[i][0:P, j * 512:(j + 1) * 512], b_[0:P, :], fn_),
                             writes=[rb, r_ggb[i]])
                    k.flush()
                    k.defer(lambda i=i, g0=g0, P=P: k.dma("sp", GG[g0:g0 + P, :], ggb[i][0:P, :], reads=[r_ggb[i]], accum=[r_gg]))
                k.barrier()
            if stop_after <= 2:
                continue

            with contextlib.ExitStack() as ph:
                def psb_(name, shape, dt):
                    return ph.enter_context(nc.sbuf_tensor(name + "_s%d" % s, list(shape), dt))
                Wh = psb_("Wh", [128, 8, 3072], BF16)
                r_Wh = Res()
                for j in range(3):
                    c0 = 4608 + j * 1024
                    k.dma("pool", Wh[:, :, j * 1024:(j + 1) * 1024],
                          w_in[:, c0:c0 + 1024].rearrange("(kc p) n -> p kc n", p=128), writes=[r_Wh])
                lbt = psb_("lbt", [128, D], F32)
                omlt = psb_("omlt", [128, D], F32)
                hgw = psb_("hgw", [128, D], F32)
                r_lb = Res()
                k.dma("sp", lbt[:], lb_log[0:1, :].partition_broadcast(128), writes=[r_lb])
                k.dma("sp", omlt[:], lb_log[1:2, :].partition_broadcast(128), writes=[r_lb])
                k.dma("sp", hgw[:], hgn_w.partition_broadcast(128), writes=[r_lb])
                k.op("dve", lambda: nc.vector.tensor_tensor(lbt[:], lbt[:], omlt[:], ALU.subtract), writes=[r_lb])
                k.op("act", lambda: nc.scalar.activation(lbt[:], lbt[:], AF.Sigmoid), writes=[r_lb])
                k.op("dve", lambda: nc.vector.tensor_scalar(omlt[:], lbt[:], -1.0, 1.0, ALU.mult, ALU.add), writes=[r_lb])
                logf = psb_("logf", [128, D], F32)
                kk = psb_("kk", [128, D], F32)
                et = psb_("et", [128, D], F32)
                t2 = psb_("t2", [128, D], F32)
                r_logf, r_kk, r_et, r_t2 = Res(), Res(), Res(), Res()
                kt_b = psb_("kt_b", [128, D], BF16)
                kh_b = psb_("kh_b", [128, D], BF16)
                qt_b = psb_("qt_b", [128, D], BF16)
                v_b = psb_("v_b", [128, D], BF16)
                r_ktb, r_khb, r_qtb, r_vb = Res(), Res(), Res(), Res()
                gt_b = psb_("gt_b", [128, D], BF16)
                r_gtb = Res()
                hg_b = [psb_("hg_b%d" % i, [128, D], BF16) for i in range(2)]
                r_hgb = [Res(), Res()]
                ss8 = psb_("hss8", [128, 8], F32)
                r_ss8 = Res()
                if s < 2:
                    qT = psb_("qT", [128, 8, 128], BF16)
                    kT = psb_("kT", [128, 8, 128], BF16)
                    r_qT, r_kT = Res(), Res()
                    Abd = psb_("Abd", [128, 8, 128], BF16)
                    r_Abd = Res()
                    k.op("pool", lambda: nc.gpsimd.memset(Abd[:], 0.0), writes=[r_Abd])
                    Sm = psb_("Sm", [128, 8, 128], F32)
                    St = psb_("St", [128, 8, 128], F32)
                    Sb = [psb_("Sb%d" % i, [128, 8, 128], BF16) for i in range(2)]
                    r_Sm, r_St = Res(), Res()
                    r_Sb = [Res(), Res()]
                    eb = psb_("eb", [128, 8, 4], F32)
                    r_eb = Res()
                    k.op("pool", lambda: nc.gpsimd.memset(Sm[:], 0.0), writes=[r_Sm])
                    k.op("pool", lambda: nc.gpsimd.memset(Sb[0][:], 0.0), writes=[r_Sb[0]])
                else:
                    selc_sb = psb_("selc_sb", [NS, 2048], F32)
                    k.dma("sp", selc_sb[:], selc[:, 0:2048], writes=[r_cst])
                    fT = psb_("fT", [128, 3, 8, NS], F32)
                    r_fT = Res()
                    v32s = psb_("v32s", [NS, D], F32)
                    r_v32s = Res()
                    QZ = psb_("QZ", [128, 8, NS * NS], F32)
                    r_QZ = Res()
                    k.op("pool", lambda: nc.gpsimd.memset(QZ[:], 0.0), writes=[r_QZ])
                    S0b = [psb_("S0b%d" % i, [128, 8, 128], F32) for i in range(2)]
                    r_S0b = [Res(), Res()]
                    Sn = [psb_("Sn%d" % i, [128, 8, 128], F32) for i in range(2)]
                    r_Sn = [Res(), Res()]

                def hg_epilogue(P, po, i, g0):
                    for hb in range(2):
                        b_, rb = po[hb]
                        k.op("act", lambda: nc.scalar.activation(t2[0:P, hb * 512:(hb + 1) * 512], b_[0:P, :], AF.Square),
                             writes=[rb, r_t2])
                    k.op("dve", lambda: nc.vector.tensor_reduce(ss8[0:P, :], t2[0:P, :].rearrange("p (h e) -> p h e", h=8), AX.X, ALU.add),
                         reads=[r_t2], writes=[r_ss8])
                    rstd_from_ss(P, (ss8[0:P, :], r_ss8), 128, None)
                    for hb in range(2):
                        b_, rb = po[hb]
                        k.op("dve", lambda: nc.vector.tensor_tensor(
                            t2[0:P, hb * 512:(hb + 1) * 512].rearrange("p (h e) -> p h e", h=4),
                            b_[0:P, :].rearrange("p (h e) -> p h e", h=4),
                            ss8[0:P, hb * 4:(hb + 1) * 4].unsqueeze(2).to_broadcast([P, 4, 128]), ALU.mult),
                            reads=[r_ss8], writes=[rb, r_t2])
                    k.op("dve", lambda: nc.vector.tensor_tensor(t2[0:P, :], t2[0:P, :], gt_b[0:P, :], ALU.mult),
                         reads=[r_gtb], writes=[r_t2])
                    k.op("dve", lambda: nc.vector.tensor_tensor(hg_b[i][0:P, :], t2[0:P, :], hgw[0:P, :], ALU.mult),
                         reads=[r_t2, r_lb], writes=[r_hgb[i]])
                    k.flush()
                    k.defer(lambda: k.dma("sp", HGS[g0:g0 + P, :], hg_b[i][0:P, :], reads=[r_hgb[i]], accum=[r_hgs]))

                for t in range(ntile):
                    i = t % 2
                    ts_ = slice(t * 128, t * 128 + P)
                    g0 = tok0(s) + t * 128
                    k.dma("sp", gt_b[0:P, :], GG[g0:g0 + P, 0:D], reads=[r_gg], writes=[r_gtb])
                    pr = proj(P, hT, ts_, Wh, 1024, 2, r_hT, r_Wh)
                    for hb in range(2):
                        b_, rb = pr[hb]
                        k.op("act", lambda: nc.scalar.activation(logf[0:P, hb * 512:(hb + 1) * 512], b_[0:P, :], AF.Sigmoid),
                             writes=[rb, r_logf])
                    k.op("dve", lambda: nc.vector.tensor_tensor(logf[0:P, :], logf[0:P, :], omlt[0:P, :], ALU.mult), reads=[r_lb], writes=[r_logf])
                    k.op("dve", lambda: nc.vector.tensor_tensor(logf[0:P, :], logf[0:P, :], lbt[0:P, :], ALU.add), reads=[r_lb], writes=[r_logf])
                    k.op("dve", lambda: nc.vector.tensor_scalar(kk[0:P, :], logf[0:P, :], -1.0, 1.0, ALU.mult, ALU.add),
                         reads=[r_logf], writes=[r_kk])
                    if s < 2:
                        k.op("act", lambda: nc.scalar.activation(logf[0:P, :], logf[0:P, :], AF.Ln), reads=[r_kk], writes=[r_logf])
                    if s == 2:
                        k.op("pool", lambda: nc.gpsimd.tensor_copy(et[0:P, :], logf[0:P, :]), reads=[r_logf], writes=[r_et])
                        pq = proj(P, hT, ts_, Wh, 0, 2, r_hT, r_Wh)
                        for hb in range(2):
                            b_, rb = pq[hb]
                            k.op("act", lambda: nc.scalar.activation(t2[0:P, hb * 512:(hb + 1) * 512], b_[0:P, :], AF.Silu),
                                 writes=[rb, r_t2])
                        pv = proj(P, hT, ts_, Wh, 2048, 2, r_hT, r_Wh)
                        for hb in range(2):
                            b_, rb = pv[hb]
                            k.op("act", lambda: nc.scalar.copy(v32s[0:P, hb * 512:(hb + 1) * 512], b_[0:P, :]), writes=[rb, r_v32s])
                        for qi, (src_, rs_) in enumerate(((et, r_et), (kk, r_kk), (t2, r_t2))):
                            b_, rb = nb()
                            for h in range(8):
                                k.op("pe", lambda h=h: nc.tensor.transpose(b_[:, h * NS:(h + 1) * NS], src_[0:P, h * 128:(h + 1) * 128],
                                                                            ident_f[0:P, 0:P]),
                                     reads=[rs_, r_cst], writes=[rb])
                            k.op("act", lambda: nc.scalar.copy(fT[:, qi, :, :], b_[:, 0:8 * NS].rearrange("p (h b) -> p h b", h=8)),
                                 writes=[rb, r_fT])
                        k.op("dve", lambda: nc.vector.tensor_copy(QZ[:, :, 0:NS * NS:NS + 1], fT[:, 2, :, :]), reads=[r_fT], writes=[r_QZ])
                        po = [(psf[5], r_psf[5]), (psf[6], r_psf[6])]
                        for b in range(NS):
                            ib = b % 2
                            k.dma("sp", S0b[ib][:], st_in[b].rearrange("h k v -> k h v"), writes=[r_S0b[ib]])
                            pvb = [(psf[2 * ib], r_psf[2 * ib]), (psf[2 * ib + 1], r_psf[2 * ib + 1])]
                            for hb in range(2):
                                b_, rb = pvb[hb]
                                k.op("pe", lambda: nc.tensor.matmul(b_[:, :], selc_sb[0:NS, b * 128:(b + 1) * 128], v32s[0:NS, hb * 512:(hb + 1) * 512],
                                                                    start=True, stop=True),
                                     reads=[r_v32s, r_cst], writes=[rb])
                            k.op("dve", lambda: nc.vector.tensor_tensor(Sn[ib][:], S0b[ib][:],
                                                                        fT[:, 0, :, b:b + 1].to_broadcast([128, 8, 128]), ALU.mult),
                                 reads=[r_S0b[ib], r_fT], writes=[r_Sn[ib]])
                            for hb in range(2):
                                b_, rb = pvb[hb]
                                k.op("dve", lambda: nc.vector.tensor_tensor(
                                    S0b[ib][:, hb * 4:(hb + 1) * 4, :], b_[:, :].rearrange("p (h v) -> p h v", h=4),
                                    fT[:, 1, hb * 4:(hb + 1) * 4, b:b + 1].to_broadcast([128, 4, 128]), ALU.mult),
                                    reads=[r_fT], writes=[rb, r_S0b[ib]])
                            k.op("dve", lambda: nc.vector.tensor_tensor(Sn[ib][:], Sn[ib][:], S0b[ib][:], ALU.add),
                                 reads=[r_S0b[ib]], writes=[r_Sn[ib]])
                            k.dma("sp", hg_s[b].rearrange("h k v -> k h v"), Sn[ib][:], reads=[r_Sn[ib]])
                            for h in range(8):
                                b_, rb = po[h // 4]
                                k.op("pe", lambda h=h: nc.tensor.matmul(b_[0:NS, (h % 4) * 128:(h % 4 + 1) * 128],
                                                                        QZ[:, h, b * NS:(b + 1) * NS], Sn[ib][:, h, :],
                                                                        start=(b == 0 and h % 4 == 0), stop=(b == NS - 1),
                                                                        skip_group_check=True),
                                     reads=[r_QZ, r_Sn[ib]], writes=[rb])
                        hg_epilogue(P, po, i, g0)
                        continue
                    d1 = [nb(), nb()]
                    d3 = [nb(), nb()]
                    for hb in range(2):
                        k.op("pe", lambda: nc.tensor.matmul(d1[hb][0][:, :], L1, logf[:, hb * 512:(hb + 1) * 512], start=True, stop=True),
                             reads=[r_logf, r_cst], writes=[d1[hb][1]])
                        k.op("pe", lambda: nc.tensor.matmul(d3[hb][0][:, :], L3, logf[:, hb * 512:(hb + 1) * 512], start=True, stop=True),
                             reads=[r_logf, r_cst], writes=[d3[hb][1]])
                    for hb in range(2):
                        k.op("act", lambda: nc.scalar.activation(et[:, hb * 512:(hb + 1) * 512], d1[hb][0][:, :], AF.Exp, scale=-1.0),
                             writes=[d1[hb][1], r_et])
                    k.op("dve", lambda: nc.vector.tensor_tensor(kt_b[:], kk[:], et[:], ALU.mult), reads=[r_kk, r_et], writes=[r_ktb])
                    for hb in range(2):
                        k.op("act", lambda: nc.scalar.activation(et[:, hb * 512:(hb + 1) * 512], d3[hb][0][:, :], AF.Exp),
                             writes=[d3[hb][1], r_et])
                    k.op("dve", lambda: nc.vector.tensor_tensor(kh_b[:], kk[:], et[:], ALU.mult), reads=[r_kk, r_et], writes=[r_khb])
                    for hb in range(2):
                        k.op("act", lambda: nc.scalar.activation(et[:, hb * 512:(hb + 1) * 512], d1[hb][0][:, :], AF.Exp),
                             writes=[d1[hb][1], r_et])
                    pq = proj(P, hT, ts_, Wh, 0, 2, r_hT, r_Wh)
                    for hb in range(2):
                        b_, rb = pq[hb]
                        k.op("act", lambda: nc.scalar.activation(t2[:, hb * 512:(hb + 1) * 512], b_[:, :], AF.Silu), writes=[rb, r_t2])
                    k.op("dve", lambda: nc.vector.tensor_tensor(qt_b[:], t2[:], et[:], ALU.mult), reads=[r_t2, r_et], writes=[r_qtb])
                    pv = proj(P, hT, ts_, Wh, 2048, 2, r_hT, r_Wh)
                    for hb in range(2):
                        b_, rb = pv[hb]
                        k.op("act", lambda: nc.scalar.copy(v_b[:, hb * 512:(hb + 1) * 512], b_[:, :]), writes=[rb, r_vb])
                    be, rbe = nb()
                    for h in range(8):
                        k.op("pe", lambda h=h: nc.tensor.matmul(be[:, h * 4:(h + 1) * 4], logf[:, h * 128:(h + 1) * 128], R4,
                                                                start=(h == 0), stop=(h == 7), skip_group_check=True),
                             reads=[r_logf, r_cst], writes=[rbe])
                    k.op("act", lambda: nc.scalar.activation(eb[:], be[:, 0:32].rearrange("p (h c) -> p h c", h=8), AF.Exp),
                         writes=[rbe, r_eb])
                    for src_, rs_, dst_, rd_ in ((qt_b, r_qtb, qT, r_qT), (kt_b, r_ktb, kT, r_kT)):
                        for h in range(8):
                            k.op("pe", lambda h=h: nc.tensor.transpose(psb[:, h * 128:(h + 1) * 128], src_[:, h * 128:(h + 1) * 128], ident_b[:]),
                                 reads=[rs_, r_idb], writes=[r_psb])
                        k.op("act", lambda: nc.scalar.copy(dst_[:], psb[:].rearrange("p (h n) -> p h n", h=8)), writes=[r_psb, rd_])
                    pa = [nb(), nb()]
                    for h in range(8):
                        b_, rb = pa[h // 4]
                        c_ = (h % 4) * 128
                        k.op("pe", lambda h=h: nc.tensor.matmul(b_[0:64, c_:c_ + 64], kT[:, h, 0:64], qT[:, h, 0:64], start=True, stop=True,
                                                                skip_group_check=True),
                             reads=[r_kT, r_qT], writes=[rb])
                        k.op("pe", lambda h=h: nc.tensor.matmul(b_[:, c_ + 64:c_ + 128], kT[:, h, :], qT[:, h, 64:128], start=True, stop=True,
                                                                skip_group_check=True),
                             reads=[r_kT, r_qT], writes=[rb])
                    for hb in range(2):
                        b_, rb = pa[hb]
                        bv = b_[:, :].rearrange("p (h t) -> p h t", h=4)
                        k.op("dve", lambda: nc.vector.tensor_tensor(Abd[0:64, hb * 4:(hb + 1) * 4, 0:64], bv[0:64, :, 0:64],
                                                                    MA[0:64, 0:64].unsqueeze(1).to_broadcast([64, 4, 64]), ALU.mult),
                             reads=[r_cst], writes=[rb, r_Abd])
                        k.op("dve", lambda: nc.vector.tensor_tensor(Abd[64:128, hb * 4:(hb + 1) * 4, 64:128], bv[64:128, :, 64:128],
                                                                    MA[64:128, 64:128].unsqueeze(1).to_broadcast([64, 4, 64]), ALU.mult),
                             reads=[r_cst], writes=[rb, r_Abd])
                    k.op("dve", lambda: nc.vector.tensor_tensor(Sb[0][:], Sm[:], eb[:, :, 2:3].to_broadcast([128, 8, 128]), ALU.mult),
                         reads=[r_Sm, r_eb], writes=[r_Sb[0]])
                    for c in range(2):
                        src_S, rsrc = (Sm, r_Sm) if c == 0 else (St, r_St)
                        dst_S, rdst = (St, r_St) if c == 0 else (Sm, r_Sm)
                        psn = [nb(), nb()]
                        for h in range(8):
                            b_, rb = psn[h // 4]
                            c_ = (h % 4) * 128
                            k.op("pe", lambda h=h: nc.tensor.matmul(b_[:, c_:c_ + 128], kh_b[c * 64:(c + 1) * 64, h * 128:(h + 1) * 128],
                                                                    v_b[c * 64:(c + 1) * 64, h * 128:(h + 1) * 128], start=True, stop=True,
                                                                    skip_group_check=True),
                                 reads=[r_khb, r_vb], writes=[rb])
                        k.op("dve", lambda: nc.vector.tensor_tensor(dst_S[:], src_S[:], eb[:, :, c:c + 1].to_broadcast([128, 8, 128]), ALU.mult),
                             reads=[rsrc, r_eb], writes=[rdst])
                        for hb in range(2):
                            b_, rb = psn[hb]
                            k.op("dve", lambda: nc.vector.tensor_tensor(dst_S[:, hb * 4:(hb + 1) * 4, :], dst_S[:, hb * 4:(hb + 1) * 4, :],
                                                                        b_[:, :].rearrange("p (h v) -> p h v", h=4), ALU.add),
                                 writes=[rb, rdst])
                        if c == 0:
                            k.op("dve", lambda: nc.vector.tensor_tensor(Sb[1][:], St[:], eb[:, :, 3:4].to_broadcast([128, 8, 128]), ALU.mult),
                                 reads=[r_St, r_eb], writes=[r_Sb[1]])
                    po = [nb(), nb()]
                    for h in range(8):
                        b_, rb = po[h // 4]
                        c_ = (h % 4) * 128
                        k.op("pe", lambda h=h: nc.tensor.matmul(b_[:, c_:c_ + 128], Abd[:, h, :], v_b[:, h * 128:(h + 1) * 128],
                                                                start=True, stop=False, skip_group_check=True),
                             reads=[r_Abd, r_vb], writes=[rb])
                        k.op("pe", lambda h=h: nc.tensor.matmul(b_[0:64, c_:c_ + 128], qT[:, h, 0:64], Sb[0][:, h, :],
                                                                start=False, stop=False, skip_group_check=True),
                             reads=[r_qT, r_Sb[0]], writes=[rb])
                        k.op("pe", lambda h=h: nc.tensor.matmul(b_[64:128, c_:c_ + 128], qT[:, h, 64:128], Sb[1][:, h, :],
                                                                start=False, stop=True, skip_group_check=True),
                             reads=[r_qT, r_Sb[1]], writes=[rb])
                    hg_epilogue(P, po, i, g0)
                k.flush()
                if s < 2:
                    k.dma("sp", hg_p[s].rearrange("h k v -> k h v"), Sm[:], reads=[r_Sm])
                k.barrier()
    if stop_after <= 3:
        k.finish()
        return


    with contextlib.ExitStack() as ph:
        def psb_(name, shape, dt):
            return ph.enter_context(nc.sbuf_tensor(name, list(shape), dt))
        qblk = [psb_("qblk%d" % i, [128, 512], BF16) for i in range(2)]
        kblk = [psb_("kblk%d" % i, [128, 512], BF16) for i in range(2)]
        vblk = [psb_("vblk%d" % i, [128, 8, 65], BF16) for i in range(2)]
        r_qblk, r_kblk, r_vblk = [Res(), Res()], [Res(), Res()], [Res(), Res()]
        qTa = psb_("qTa", [128, 4, 128], BF16)
        kTa = [psb_("kTa%d" % i, [128, 4, 128], BF16) for i in range(2)]
        r_qTa = Res()
        r_kTa = [Res(), Res()]
        pT = [psb_("pT%d" % i, [128, 4, 128], BF16) for i in range(4)]
        r_pT = [Res() for _ in range(4)]
        oac = [psb_("oac%d" % i, [128, 520], F32) for i in range(2)]
        r_oac = [Res(), Res()]
        for i in range(2):
            k.op("pool", lambda i=i: nc.gpsimd.memset(qblk[i][:], 0.0), writes=[r_qblk[i]])
            k.op("pool", lambda i=i: nc.gpsimd.memset(kblk[i][:], 0.0), writes=[r_kblk[i]])
            k.op("pool", lambda i=i: nc.gpsimd.memset(vblk[i][:], 1.0), writes=[r_vblk[i]])
        blk_ctr = [0]
        for c_ in range(8):
            rs_ = slice(c_ * 2048, (c_ + 1) * 2048)
            k.dma("pool", UV[rs_, 0:D], peer_u[rs_, :], accum=[r_uv])
            k.dma("pool", UV[rs_, D:2 * D], peer_v[rs_, :], accum=[r_uv])

        def attn_block(load_cur, has_prev, ip, store):
            n_ = blk_ctr[0]
            blk_ctr[0] += 1
            ic = 1 - ip
            load_cur(ic)
            for src_, rs_, dst_, rd_ in ((qblk[ic], r_qblk[ic], qTa, r_qTa), (kblk[ic], r_kblk[ic], kTa[ic], r_kTa[ic])):
                for hp in range(4):
                    k.op("pe", lambda hp=hp: nc.tensor.transpose(psb[:, hp * 128:(hp + 1) * 128], src_[:, hp * 128:(hp + 1) * 128], ident_b[:]),
                         reads=[rs_, r_idb], writes=[r_psb])
                k.op("act", lambda: nc.scalar.copy(dst_[:], psb[:, 0:512].rearrange("p (h n) -> p h n", h=4)), writes=[r_psb, rd_])
            srcs = [(ic, maskc_b)] + ([(ip, maskp_b)] if has_prev else [])
            pts = []
            for si, (ib, msk) in enumerate(srcs):
                for hb in range(2):
                    b_, rb = nb()
                    for hh in range(4):
                        h = 2 * hh + hb
                        po_ = hb * 64
                        k.op("pe", lambda: nc.tensor.matmul(b_[:, hh * 128:(hh + 1) * 128], kTa[ib][po_:po_ + 64, h // 2, :],
                                                            qTa[po_:po_ + 64, h // 2, :], start=True, stop=True, skip_group_check=True),
                             reads=[r_kTa[ib], r_qTa], writes=[rb])
                    pi = si * 2 + hb
                    k.op("act", lambda: nc.scalar.activation(pT[pi][:], b_[:, :].rearrange("p (h n) -> p h n", h=4), AF.Exp),
                         writes=[rb, r_pT[pi]])
                    k.op("pool", lambda: nc.gpsimd.tensor_tensor(pT[pi][:], pT[pi][:], msk[:], ALU.mult), reads=[r_mask], writes=[r_pT[pi]])
                    pts.append((pi, ib))
            io = n_ % 2
            for hb in range(2):
                b_, rb = nb()
                for hh in range(4):
                    h = hb * 4 + hh
                    for si, (ib, msk) in enumerate(srcs):
                        pi = si * 2 + (h % 2)
                        k.op("pe", lambda: nc.tensor.matmul(b_[:, hh * 65:(hh + 1) * 65], pT[pi][:, h // 2, :], vblk[ib][:, h, :],
                                                            start=(si == 0), stop=(si == len(srcs) - 1), skip_group_check=True),
                             reads=[r_pT[pi], r_vblk[ib]], writes=[rb])
                k.op("act", lambda: nc.scalar.copy(oac[io][:, hb * 260:(hb + 1) * 260], b_[:, 0:260]), writes=[rb, r_oac[io]])
            k.flush()
            k.defer(lambda io=io, store=store: store(oac[io], r_oac[io]))
            return ic

        for s in range(2):
            for g in range(3):
                d = GROUPS[g][1]
                nblk = SEQ // (128 * d)

                def view(T, width):
                    return T[tok0(s):tok0(s) + SEQ, g, :].rearrange("(n j dd) c -> dd n j c", dd=d, j=128)
                Qv, Kv, Vv = view(QS, 512), view(KS, 512), view(VS, 520)
                Ov = OACC[g, tok0(s):tok0(s) + SEQ, :].rearrange("(n j dd) c -> dd n j c", dd=d, j=128)
                for r in range(d):
                    ip = 0
                    for n in range(nblk):
                        def load_cur(ic, r=r, n=n, Qv=Qv, Kv=Kv, Vv=Vv):
                            k.dma("sp", qblk[ic][:], Qv[r, n], reads=[r_scr], writes=[r_qblk[ic]])
                            k.dma("sp", kblk[ic][:], Kv[r, n], reads=[r_scr], writes=[r_kblk[ic]])
                            k.dma("sp", vblk[ic][:].rearrange("p h e -> p (h e)"), Vv[r, n], reads=[r_scr], writes=[r_vblk[ic]])

                        def store(o_, ro_, r=r, n=n, Ov=Ov):
                            k.dma("sp", Ov[r, n], o_[:], reads=[ro_], accum=[r_oacc])
                        ip = attn_block(load_cur, n > 0, ip, store)
        for b in range(NS):
            for g in range(3):
                tokb = tok0(2) + b
                ip = 0
                k.dma("pool", kblk[ip][:], ck[g][b, :, 0, :], writes=[r_kblk[ip]])
                k.dma("pool", vblk[ip][:, :, 0:64], ck[g][b, :, 1, :].rearrange("j (h e) -> j h e", h=8), writes=[r_vblk[ip]])
                for hp in range(4):
                    k.op("pe", lambda hp=hp: nc.tensor.transpose(psb[:, hp * 128:(hp + 1) * 128], kblk[ip][:, hp * 128:(hp + 1) * 128], ident_b[:]),
                         reads=[r_kblk[ip], r_idb], writes=[r_psb])
                k.op("act", lambda: nc.scalar.copy(kTa[ip][:], psb[:, 0:512].rearrange("p (h n) -> p h n", h=4)), writes=[r_psb, r_kTa[ip]])

                def load_cur(ic, g=g, tokb=tokb):
                    k.dma("sp", qblk[ic][0:1, :], QS[tokb:tokb + 1, g, :], reads=[r_scr], writes=[r_qblk[ic]])
                    k.dma("sp", kblk[ic][0:1, :], KS[tokb:tokb + 1, g, :], reads=[r_scr], writes=[r_kblk[ic]])
                    k.dma("sp", vblk[ic][0:1, :, :].rearrange("p h e -> p (h e)"), VS[tokb:tokb + 1, g, :], reads=[r_scr], writes=[r_vblk[ic]])

                def store(o_, ro_, g=g, tokb=tokb):
                    k.dma("sp", OACC[g, tokb:tokb + 1, :], o_[0:1, :], reads=[ro_], accum=[r_oacc])
                attn_block(load_cur, True, ip, store)
        k.barrier()
    if stop_after <= 4:
        k.finish()
        return

    with contextlib.ExitStack() as ph:
        def psb_(name, shape, dt):
            return ph.enter_context(nc.sbuf_tensor(name, list(shape), dt))
        Wa = psb_("Wa", [128, 4, D], BF16)
        Wb = psb_("Wb", [128, 8, D], BF16)
        Wo = psb_("Wo", [128, 8, D], BF16)
        Wq = psb_("Wq", [128, 8, 2048], BF16)
        skt = psb_("skt", [128, 2, 128], F32)
        r_W = Res()
        k.dma("pool", Wa[:], w_br_a.rearrange("(kc p) n -> p kc n", p=128), writes=[r_W])
        k.dma("pool", Wb[:], w_br_b.rearrange("(kc p) n -> p kc n", p=128), writes=[r_W])
        k.dma("pool", Wo[:], w_o.rearrange("(kc p) n -> p kc n", p=128), writes=[r_W])
        k.dma("pool", Wq[:], w_pq.rearrange("(kc p) n -> p kc n", p=128), writes=[r_W])
        k.dma("sp", skt[:], skT.rearrange("t e c -> e t c"), writes=[r_W])
        n2w = psb_("n2w", [128, D], F32)
        k.dma("sp", n2w[:], norm2_w.partition_broadcast(128), writes=[r_W])
        iota16 = psb_("iota16", [128, 16], F32)
        k.dma("sp", iota16[:], selc[0:1, 2048:2064].partition_broadcast(128), writes=[r_W])
        G1 = psb_("G1", [128, D], F32)
        S2 = psb_("S2", [128, D], F32)
        SH2 = psb_("SH2", [128, D], F32)
        G2s = [psb_("G2_%d" % i, [128, D], F32) for i in range(2)]
        r_M = Res()
        xin = [psb_("xin0", [128, D], F32)] * 2
        r_xin = [Res()] * 2
        oin = [psb_("oin0", [128, 3, 520], F32)] * 2
        r_oin = [Res()] * 2
        hgin = [psb_("hgin0", [128, D], BF16)] * 2
        r_hgin = [Res()] * 2
        ggin = [psb_("ggin0", [128, 2048], BF16)] * 2
        r_ggin = [Res()] * 2
        rl = psb_("rl", [128, 8], F32)
        r_rl = Res()
        att_b = psb_("att_b", [128, 512], BF16)
        r_attb = Res()
        TT = psb_("TT", [128, 8, 128], BF16)
        r_TT = Res()
        f1 = psb_("f1", [128, D], F32)
        r_f1 = Res()
        yb = psb_("yb", [128, D], BF16)
        r_yb = Res()
        x1s = [psb_("x1_%d" % i, [128, D], F32) for i in range(2)]
        r_x1s = [Res(), Res()]
        h2bs = [psb_("h2b_%d" % i, [128, D], BF16) for i in range(2)]
        r_h2bs = [Res(), Res()]
        ssq = psb_("ssq", [128, 1], F32)
        r_ssq = Res()
        sc = psb_("sc", [128, 16, 128], F32)
        sc2 = psb_("sc2", [128, 16, 128], F32)
        r_sc, r_sc2 = Res(), Res()
        q2T = sc2
        r_q2T = r_sc2
        vals = psb_("vals", [128, 16, 16], F32)
        idxu = psb_("idxu", [128, 16, 16], U32)
        idxf = psb_("idxf", [128, 16, 16], F32)
        r_vals, r_idxu, r_idxf = Res(), Res(), Res()
        cand = psb_("cand", [128, 8, 256], F32)
        cand2 = sc2[:].rearrange("p a b -> p (a b)").rearrange("p (h x) -> p h x", h=8)
        r_cand, r_cand2 = Res(), r_sc2
        tv = psb_("tv", [128, 8, 16], F32)
        posu = psb_("posu", [128, 8, 16], U32)
        pa_u = psb_("pa_u", [128, 8, 16], U32)
        pa_f = psb_("pa_f", [128, 2, 8, 16], F32)
        r_tv, r_posu, r_pau, r_paf = Res(), Res(), Res(), Res()
        oh = cand2
        r_oh = r_sc2
        isel = psb_("isel", [128, 2, 8, 16], F32)
        r_isel = Res()
        eid_f = psb_("eid_f", [128, 128], F32)
        eids = [psb_("eid%d" % i, [128, 128], I32) for i in range(2)]
        r_eids = [Res(), Res()]
        r_eidf = Res()
        gsm = psb_("gsm", [128, 8], F32)
        gats = [psb_("gat%d" % i, [128, 8, 16], F32) for i in range(2)]
        r_gats = [Res(), Res()]
        dots = psb_("dots", [128, 128], F32)
        r_dots = Res()
        coef = psb_("coef", [128, 128], F32)
        r_coef = Res()
        NUV = 6
        uvb = [psb_("uvb%d" % i, [128, 2 * D], BF16) for i in range(NUV)]
        r_uvb = [Res() for _ in range(NUV)]
        r_dotg = [Res() for _ in range(32)]
        r_coefg = [Res() for _ in range(32)]
        dg = [psb_("dg%d" % i, [128, 128], BF16) for i in range(4)]
        r_dg = [Res() for _ in range(4)]
        jb = psb_("jb", [128, D], BF16)
        r_jb = Res()
        yo = [psb_("yo0", [128, D], F32)] * 2
        r_yo = [Res()] * 2

        def transpose_to_TT(P, src, rsrc, nk):
            for kc in range(nk):
                k.op("pe", lambda kc=kc: nc.tensor.transpose(psb[:, kc * 128:kc * 128 + P], src[0:P, kc * 128:(kc + 1) * 128], ident_b[0:P, 0:P]),
                     reads=[rsrc, r_idb], writes=[r_psb])
            k.op("act", lambda: nc.scalar.copy(TT[:, 0:nk, 0:P], psb[:, 0:nk * 128].rearrange("p (kc n) -> p kc n", kc=nk)[:, :, 0:P]),
                 writes=[r_psb, r_TT])

        def mm_tok(P, W, nk, nbank, rW):
            out = []
            for j in range(nbank):
                b_, rb = nb()
                for kc in range(nk):
                    k.op("pe", lambda kc=kc: nc.tensor.matmul(b_[0:P, :], TT[:, kc, 0:P], W[:, kc, j * 512:(j + 1) * 512],
                                                              start=(kc == 0), stop=(kc == nk - 1)),
                         reads=[r_TT, rW], writes=[rb])
                out.append((b_, rb))
            return out

        tiles = [(s, t) for s in range(2) for t in range(NT)] + [(2, 0)]
        nbank_rot[0] = 5
        cur_s = [-1]

        def loads(idx):
            s, t = tiles[idx]
            P = 128 if s < 2 else NS
            i = idx % 2
            g0 = tok0(s) + t * 128
            k.dma("sp", xin[i][0:P, :], xp[s, t * 128:(t + 1) * 128, :] if s < 2 else xs[:, :], writes=[r_xin[i]])
            for g in range(3):
                k.dma("sp", oin[i][0:P, g, :], OACC[g, g0:g0 + P, :], reads=[r_oacc], writes=[r_oin[i]])
            k.dma("sp", hgin[i][0:P, :], HGS[g0:g0 + P, :], reads=[r_hgs], writes=[r_hgin[i]])
            k.dma("sp", ggin[i][0:P, :], GG[g0:g0 + P, 1024:3072], reads=[r_gg], writes=[r_ggin[i]])

        loads(0)
        def front(idx):
            s, t = tiles[idx]
            P = 128 if s < 2 else NS
            i = idx % 2
            pp = idx % 2
            x1, r_x1 = x1s[pp], r_x1s[pp]
            h2b, r_h2b = h2bs[pp], r_h2bs[pp]
            eid, r_eid = eids[pp], r_eids[pp]
            gat, r_gat = gats[pp], r_gats[pp]
            G2 = G2s[s % 2]
            yield
            if s != cur_s[0]:
                cur_s[0] = s
                for dst_, c0 in ((G1, 2 * D), (SH2, 3 * D), (S2, 4 * D), (G2, 5 * D)):
                    if s < 2:
                        k.dma("sp", dst_[:], MODS[s:s + 1, c0:c0 + D].partition_broadcast(128), reads=[r_MODS], writes=[r_M])
                    else:
                        k.dma("sp", dst_[0:NS, :], MODS[2:2 + NS, c0:c0 + D], reads=[r_MODS], writes=[r_M])
                k.op("dve", lambda: nc.vector.scalar_tensor_tensor(S2[0:P, :], S2[0:P, :], 1.0, n2w[0:P, :], ALU.add, ALU.mult),
                     reads=[r_W], writes=[r_M])
            yield
            o0 = oin[i]
            k.op("dve", lambda: nc.vector.tensor_tensor(o0[0:P, 0, :], o0[0:P, 0, :], o0[0:P, 1, :], ALU.add), writes=[r_oin[i]])
            k.op("dve", lambda: nc.vector.tensor_tensor(o0[0:P, 0, :], o0[0:P, 0, :], o0[0:P, 2, :], ALU.add), writes=[r_oin[i]])
            ov = o0[0:P, 0, :].rearrange("p (h e) -> p h e", h=8)
            k.op("dve", lambda: nc.vector.reciprocal(rl[0:P, :], ov[:, :, 64]), reads=[r_oin[i]], writes=[r_rl])
            k.op("dve", lambda: nc.vector.tensor_tensor(att_b[0:P, :].rearrange("p (h e) -> p h e", h=8), ov[:, :, 0:64],
                                                        rl[0:P, :].unsqueeze(2).to_broadcast([P, 8, 64]), ALU.mult),
                 reads=[r_oin[i], r_rl], writes=[r_attb])
            transpose_to_TT(P, att_b, r_attb, 4)
            pA = mm_tok(P, Wa, 4, 2, r_W)
            for hb in range(2):
                b_, rb = pA[hb]
                k.op("dve", lambda: nc.vector.tensor_tensor(f1[0:P, hb * 512:(hb + 1) * 512], b_[0:P, :], ggin[i][0:P, hb * 512:(hb + 1) * 512], ALU.mult),
                     reads=[r_ggin[i]], writes=[rb, r_f1])
            yield
            transpose_to_TT(P, hgin[i], r_hgin[i], 8)
            pB = mm_tok(P, Wb, 8, 2, r_W)
            for hb in range(2):
                b_, rb = pB[hb]
                k.op("dve", lambda: nc.vector.tensor_tensor(x1[0:P, hb * 512:(hb + 1) * 512], b_[0:P, :],
                                                            ggin[i][0:P, 1024 + hb * 512:1024 + (hb + 1) * 512], ALU.mult),
                     reads=[r_ggin[i]], writes=[rb, r_x1])
            k.op("dve", lambda: nc.vector.tensor_tensor(yb[0:P, :], f1[0:P, :], x1[0:P, :], ALU.add), reads=[r_f1, r_x1], writes=[r_yb])
            yield
            transpose_to_TT(P, yb, r_yb, 8)
            pZ = mm_tok(P, Wo, 8, 2, r_W)
            for hb in range(2):
                b_, rb = pZ[hb]
                k.op("dve", lambda: nc.vector.tensor_tensor(x1[0:P, hb * 512:(hb + 1) * 512], b_[0:P, :], G1[0:P, hb * 512:(hb + 1) * 512], ALU.mult),
                     reads=[r_M], writes=[rb, r_x1])
            k.op("dve", lambda: nc.vector.tensor_tensor(x1[0:P, :], x1[0:P, :], xin[i][0:P, :], ALU.add), reads=[r_xin[i]], writes=[r_x1])
            yield
            if DEBUG and (idx == 0 or s == 2):
                k.dma("pool", dbg[0 if idx == 0 else 1, 1, 0:P, :], hgin[i][0:P, :], reads=[r_hgin[i]])
            if idx + 1 < len(tiles):
                loads(idx + 1)
            k.op("act", lambda: nc.scalar.activation(f1[0:P, :], x1[0:P, :], AF.Square, accum_out=ssq[0:P, :]),
                 reads=[r_x1], writes=[r_f1, r_ssq])
            rstd_from_ss(P, (ssq[0:P, :], r_ssq), D, None)
            k.op("dve", lambda: nc.vector.scalar_tensor_tensor(f1[0:P, :], x1[0:P, :], ssq[0:P, 0:1], S2[0:P, :], ALU.mult, ALU.mult),
                 reads=[r_x1, r_ssq, r_M], writes=[r_f1])
            k.op("dve", lambda: nc.vector.tensor_tensor(h2b[0:P, :], f1[0:P, :], SH2[0:P, :], ALU.add), reads=[r_f1, r_M], writes=[r_h2b])
            transpose_to_TT(P, h2b, r_h2b, 8)
            yield
            for qb in range(4):
                yield
                b_, rb = nb()
                for hh in range(4):
                    hp = qb * 4 + hh
                    for kc in range(8):
                        k.op("pe", lambda kc=kc: nc.tensor.matmul(b_[:, hh * 128:hh * 128 + P], Wq[:, kc, hp * 128:(hp + 1) * 128], TT[:, kc, 0:P],
                                                                  start=(kc == 0), stop=(kc == 7), skip_group_check=True),
                             reads=[r_TT, r_W], writes=[rb])
                k.op("act", lambda: nc.scalar.copy(q2T[:, qb * 4:(qb + 1) * 4, 0:P], b_[:, :].rearrange("p (h n) -> p h n", h=4)[:, :, 0:P]),
                     writes=[rb, r_q2T])
            yield
            for qb in range(4):
                b_, rb = nb()
                for hh in range(4):
                    hp = qb * 4 + hh
                    k.op("pe", lambda: nc.tensor.matmul(b_[0:P, hh * 128:(hh + 1) * 128], q2T[:, hp, 0:P], skt[:, hp % 2, :],
                                                        start=True, stop=True, skip_group_check=True),
                         reads=[r_q2T, r_W], writes=[rb])
                k.op("act", lambda: nc.scalar.copy(sc[0:P, qb * 4:(qb + 1) * 4, :], b_[0:P, :].rearrange("p (h n) -> p h n", h=4)),
                     writes=[rb, r_sc])
            yield
            for hp in range(16):
                if hp % 2 == 0:
                    yield
                k.op("dve", lambda: nc.vector.max(vals[0:P, hp, 0:8], sc[0:P, hp, :]), reads=[r_sc], writes=[r_vals])
                k.op("dve", lambda: nc.vector.max_index(idxu[0:P, hp, 0:8], vals[0:P, hp, 0:8], sc[0:P, hp, :]),
                     reads=[r_sc, r_vals], writes=[r_idxu])
                k.op("dve", lambda: nc.vector.match_replace(sc2[0:P, hp, :], vals[0:P, hp, 0:8], sc[0:P, hp, :], -1e30),
                     reads=[r_sc, r_vals], writes=[r_sc2])
                k.op("dve", lambda: nc.vector.max(vals[0:P, hp, 8:16], sc2[0:P, hp, :]), reads=[r_sc2], writes=[r_vals])
                k.op("dve", lambda: nc.vector.max_index(idxu[0:P, hp, 8:16], vals[0:P, hp, 8:16], sc2[0:P, hp, :]),
                     reads=[r_sc2, r_vals], writes=[r_idxu])
            yield
            k.op("dve", lambda: nc.vector.tensor_copy(idxf[0:P], idxu[0:P]), reads=[r_idxu], writes=[r_idxf])
            v4 = vals[0:P].rearrange("p (h two) j -> p h two j", two=2)
            k.op("dve", lambda: nc.vector.tensor_tensor(cand[0:P].rearrange("p h (a b) -> p h a b", a=16),
                                                        v4[:, :, 0, :].unsqueeze(3).to_broadcast([P, 8, 16, 16]),
                                                        v4[:, :, 1, :].unsqueeze(2).to_broadcast([P, 8, 16, 16]), ALU.add),
                 reads=[r_vals], writes=[r_cand])
            for h in range(8):
                yield
                k.op("dve", lambda: nc.vector.max(tv[0:P, h, 0:8], cand[0:P, h, :]), reads=[r_cand], writes=[r_tv])
                k.op("dve", lambda: nc.vector.max_index(posu[0:P, h, 0:8], tv[0:P, h, 0:8], cand[0:P, h, :]),
                     reads=[r_cand, r_tv], writes=[r_posu])
                k.op("dve", lambda: nc.vector.match_replace(cand2[0:P, h, :], tv[0:P, h, 0:8], cand[0:P, h, :], -1e30),
                     reads=[r_cand, r_tv], writes=[r_cand2])
                k.op("dve", lambda: nc.vector.max(tv[0:P, h, 8:16], cand2[0:P, h, :]), reads=[r_cand2], writes=[r_tv])
                k.op("dve", lambda: nc.vector.max_index(posu[0:P, h, 8:16], tv[0:P, h, 8:16], cand2[0:P, h, :]),
                     reads=[r_cand2, r_tv], writes=[r_posu])
            yield
            k.op("dve", lambda: nc.vector.tensor_single_scalar(pa_u[0:P], posu[0:P], 4, ALU.logical_shift_right), reads=[r_posu], writes=[r_pau])
            k.op("dve", lambda: nc.vector.tensor_copy(pa_f[0:P, 0], pa_u[0:P]), reads=[r_pau], writes=[r_paf])
            k.op("dve", lambda: nc.vector.tensor_single_scalar(pa_u[0:P], posu[0:P], 15, ALU.bitwise_and), reads=[r_posu], writes=[r_pau])
            k.op("dve", lambda: nc.vector.tensor_copy(pa_f[0:P, 1], pa_u[0:P]), reads=[r_pau], writes=[r_paf])
            i4 = idxf[0:P].rearrange("p (h two) j -> p h two j", two=2)
            for w_ in range(2):
                yield
                ohv = oh[0:P].rearrange("p h (j a) -> p h j a", j=16)
                k.op("dve", lambda: nc.vector.tensor_tensor(ohv, pa_f[0:P, w_].unsqueeze(3).to_broadcast([P, 8, 16, 16]),
                                                            iota16[0:P, :].unsqueeze(1).unsqueeze(1).to_broadcast([P, 8, 16, 16]), ALU.is_equal),
                     reads=[r_paf, r_W], writes=[r_oh])
                k.op("dve", lambda: nc.vector.tensor_tensor(ohv, ohv, i4[:, :, w_, :].unsqueeze(2).to_broadcast([P, 8, 16, 16]), ALU.mult),
                     reads=[r_idxf], writes=[r_oh])
                k.op("dve", lambda: nc.vector.tensor_reduce(isel[0:P, w_], ohv, AX.X, ALU.add), reads=[r_oh], writes=[r_isel])
            k.op("dve", lambda: nc.vector.scalar_tensor_tensor(eid_f[0:P, :], isel[0:P, 0].rearrange("p h j -> p (h j)"), 128.0,
                                                               isel[0:P, 1].rearrange("p h j -> p (h j)"), ALU.mult, ALU.add),
                 reads=[r_isel], writes=[r_eidf])
            k.op("dve", lambda: nc.vector.tensor_copy(eid[0:P, :], eid_f[0:P, :]), reads=[r_eidf], writes=[r_eid])
            yield
            k.op("dve", lambda: nc.vector.tensor_tensor(gat[0:P], tv[0:P], tv[0:P, :, 0:1].to_broadcast([P, 8, 16]), ALU.subtract),
                 reads=[r_tv], writes=[r_gat])
            k.op("act", lambda: nc.scalar.activation(gat[0:P], gat[0:P], AF.Exp), writes=[r_gat])
            k.op("dve", lambda: nc.vector.tensor_reduce(gsm[0:P, :], gat[0:P], AX.X, ALU.add), reads=[r_gat], writes=[r_rl])
            k.op("dve", lambda: nc.vector.reciprocal(gsm[0:P, :], gsm[0:P, :]), writes=[r_rl])
            k.op("dve", lambda: nc.vector.tensor_tensor(gat[0:P], gat[0:P], gsm[0:P, :].unsqueeze(2).to_broadcast([P, 8, 16]), ALU.mult),
                 reads=[r_rl], writes=[r_gat])
        def gather(idx, gen):
            s, t = tiles[idx]
            P = 128 if s < 2 else NS
            i = idx % 2
            pp = idx % 2
            x1, r_x1 = x1s[pp], r_x1s[pp]
            h2b, r_h2b = h2bs[pp], r_h2bs[pp]
            eid, r_eid = eids[pp], r_eids[pp]
            gat, r_gat = gats[pp], r_gats[pp]
            G2 = G2s[s % 2]
            py = [(psf[5], r_psf[5]), (psf[6], r_psf[6])]
            gatf = gat[0:P].rearrange("p h j -> p (h j)")
            for grp in range(32):
                gs = slice(grp * 4, grp * 4 + 4)
                rd, rc = r_dotg[grp], r_coefg[grp]
                for j in range(grp * 4, grp * 4 + 4):
                    bi = j % NUV
                    k.dma("pool", uvb[bi][0:P, :], UV, reads=[r_eid, r_uv], writes=[r_uvb[bi]],
                          indirect=bass.IndirectOffsetOnAxis(ap=eid[0:P, j:j + 1], axis=0))
                    k.op("dve", lambda: nc.vector.scalar_tensor_tensor(jb[0:P, :], uvb[bi][0:P, 0:D], 1.0, h2b[0:P, :], ALU.mult, ALU.mult,
                                                                       accum_out=dots[0:P, j:j + 1]),
                         reads=[r_uvb[bi], r_h2b], writes=[r_jb, rd])
                k.op("dve", lambda: nc.vector.tensor_tensor(coef[0:P, gs], dots[0:P, gs], dots[0:P, gs], ALU.mult), reads=[rd], writes=[rc])
                k.op("dve", lambda: nc.vector.tensor_scalar(coef[0:P, gs], coef[0:P, gs], 0.044715, 1.0, ALU.mult, ALU.add), writes=[rc])
                k.op("dve", lambda: nc.vector.tensor_tensor(coef[0:P, gs], coef[0:P, gs], dots[0:P, gs], ALU.mult), reads=[rd], writes=[rc])
                k.op("act", lambda: nc.scalar.activation(coef[0:P, gs], coef[0:P, gs], AF.Sigmoid, scale=1.5957691216057308), writes=[rc])
                k.op("dve", lambda: nc.vector.tensor_tensor(coef[0:P, gs], coef[0:P, gs], dots[0:P, gs], ALU.mult), reads=[rd], writes=[rc])
                k.op("dve", lambda: nc.vector.tensor_tensor(coef[0:P, gs], coef[0:P, gs], gatf[:, gs], ALU.mult), reads=[r_gat], writes=[rc])
                for j in range(grp * 4, grp * 4 + 4):
                    bi = j % NUV
                    di_ = j % 4
                    k.op("act", lambda: nc.scalar.mul(dg[di_][0:P, 0:P], ident_b[0:P, 0:P], coef[0:P, j:j + 1]),
                         reads=[rc, r_idb], writes=[r_dg[di_]])
                    for hb in range(2):
                        b_, rb = py[hb]
                        k.op("pe", lambda: nc.tensor.matmul(b_[0:P, :], dg[di_][0:P, 0:P], uvb[bi][0:P, D + hb * 512:D + (hb + 1) * 512],
                                                            start=(j == 0), stop=(j == 127)),
                             reads=[r_dg[di_], r_uvb[bi]], writes=[rb])
                if gen is not None:
                    for _ in range(2):
                        next(gen, None)
            io = idx % 2
            for hb in range(2):
                b_, rb = py[hb]
                k.op("dve", lambda: nc.vector.tensor_tensor(yo[io][0:P, hb * 512:(hb + 1) * 512], b_[0:P, :], G2[0:P, hb * 512:(hb + 1) * 512], ALU.mult),
                     reads=[r_M], writes=[rb, r_yo[io]])
            k.op("dve", lambda: nc.vector.tensor_tensor(yo[io][0:P, :], yo[io][0:P, :], x1[0:P, :], ALU.add), reads=[r_x1], writes=[r_yo[io]])
            if DEBUG and (idx == 0 or s == 2):
                di = 0 if idx == 0 else 1
                k.dma("pool", dbg[di, 0, 0:P, 0:512], att_b[0:P, :], reads=[r_attb])
                k.dma("pool", dbg[di, 2, 0:P, :], yb[0:P, :], reads=[r_yb])
                k.dma("sp", dbg[di, 3, 0:P, :], x1[0:P, :], reads=[r_x1])
                k.dma("pool", dbg[di, 4, 0:P, :], h2b[0:P, :], reads=[r_h2b])
                k.dma("sp", dbg[di, 5, 0:P, 0:128], coef[0:P, :], reads=r_coefg)
                k.dma("sp", dbg[di, 6, 0:P, 0:128], eid_f[0:P, :], reads=[r_eidf])
                k.dma("sp", dbg[di, 7, 0:P, 0:128], dots[0:P, :], reads=r_dotg)
                k.dma("sp", dbg[di, 8, 0:P, 0:128], gat[0:P].rearrange("p h j -> p (h j)"), reads=[r_gat])
                k.dma("sp", dbg[di, 9, 0:P, 0:256], vals[0:P].rearrange("p a b -> p (a b)"), reads=[r_vals])
            k.dma("sp", y_p[s, t * 128:(t + 1) * 128, :] if s < 2 else y_s[:, :], yo[io][0:P, :], reads=[r_yo[io]])
        g_ = front(0)
        for _ in g_:
            pass
        for idx in range(len(tiles)):
            g_ = front(idx + 1) if idx + 1 < len(tiles) else None
            gather(idx, g_)
            if g_ is not None:
                for _ in g_:
                    pass
        k.flush()
    k.finish()


def _consts():
    c = np.zeros((128, 1024), np.float32)
    j = np.arange(128)[:, None]
    i = np.arange(128)[None, :]
    c[:, 0:128] = np.eye(128, dtype=np.float32)
    c[:, 128:256] = (j <= i)
    c[:, 256:384] = (j >= i)
    same = (j // 64) == (i // 64)
    c[:, 384:512] = same * ((j <= i).astype(np.float32) - ((j % 64) <= 31).astype(np.float32))
    c[:, 512:640] = same * (j > i)
    s_ = np.arange(128)
    c[:, 640] = s_ < 64
    c[:, 641] = s_ >= 64
    c[:, 642] = (s_ < 64) & (s_ % 64 <= 31)
    c[:, 643] = (s_ >= 64) & (s_ % 64 <= 31)
    c[:, 768:896] = same * (j <= i)
    return c


def _selc():
    c = np.zeros((NS, 2064), np.float32)
    for b in range(NS):
        c[b, b * 128:(b + 1) * 128] = 1.0
    c[:, 2048:2064] = np.arange(16, dtype=np.float32)[None, :]
    return c


_CACHE = {}


def kernel(x_prompt, x_sample, cache_kv_w128, cache_kv_w512, cache_kv_w2048, state_hgrn, c_prompt, c_sample,
           w_ada, b_ada, norm1_w, norm2_w, w_in, q_norm_w, k_norm_w, hg_lb_logits, hg_norm_w, w_br_a, w_br_b,
           w_o, w_peer_q, peer_subkeys, peer_u, peer_v, _stop_after=99):
    f = lambda a: np.ascontiguousarray(np.asarray(a, dtype=np.float32))
    key = ("nc", _stop_after)
    if key not in _CACHE:
        _CACHE[key] = build_program(_stop_after)
    nc = _CACHE[key]
    caches = [f(cache_kv_w128)[0], f(cache_kv_w512)[0], f(cache_kv_w2048)[0]]
    shared = {
        "w_ada": f(w_ada)[0], "b_ada": f(b_ada).reshape(1, -1), "norm1_w": f(norm1_w).reshape(1, -1),
        "norm2_w": f(norm2_w).reshape(1, -1), "w_in": f(w_in)[0],
        "qk_w": np.ascontiguousarray(np.stack([np.tile(f(q_norm_w)[0], 8), np.tile(f(k_norm_w)[0], 8)])),
        "lb_log": f(hg_lb_logits), "hgn_w": np.ascontiguousarray(np.tile(f(hg_norm_w)[0], 8).reshape(1, -1)),
        "w_br_a": f(w_br_a)[0], "w_br_b": f(w_br_b)[0], "w_o": f(w_o)[0], "w_pq": f(w_peer_q)[0],
        "skT": np.ascontiguousarray(f(peer_subkeys)[0].transpose(0, 2, 1)),
        "peer_u": f(peer_u)[0], "peer_v": f(peer_v)[0], "cst": _consts(), "selc": _selc(),
    }
    xpf, xsf = f(x_prompt), f(x_sample)
    cp, cs = f(c_prompt), f(c_sample)
    st = f(state_hgrn)[0]
    in_maps = []
    for c in range(NCORES):
        m = dict(shared)
        m["xp"] = xpf[c * NSEQ:(c + 1) * NSEQ]
        m["xs"] = np.ascontiguousarray(xsf[c * NS:(c + 1) * NS, 0, :])
        m["cT"] = np.ascontiguousarray(np.concatenate([cp[c * NSEQ:(c + 1) * NSEQ], cs[c * NS:(c + 1) * NS]], 0).T)
        for g in range(3):
            m["ck%d" % g] = np.ascontiguousarray(
                caches[g][c * NS:(c + 1) * NS, 0::GROUPS[g][1]][:, :128].reshape(NS, 128, 2, 512))
        m["st_in"] = st[c * NS:(c + 1) * NS]
        in_maps.append(m)
    res = run_bass_kernel_spmd(nc, in_maps, core_ids=list(range(NCORES)))
    R = res.results
    cat = lambda n: np.concatenate([np.asarray(r[n]) for r in R], 0)
    y_prompt = cat("y_p")
    y_sample = cat("y_s").reshape(NCORES * NS, 1, D)
    outs = [y_prompt, y_sample]
    for g in range(3):
        outs.append(cat("kv%d_p" % g).reshape(1, NCORES * NSEQ, GROUPS[g][0], 2, 8, 64))
    outs.append(cat("hg_p")[None])
    for g in range(3):
        outs.append(cat("kv%d_s" % g).reshape(1, NCORES * NS, 1, 2, 8, 64))
    outs.append(cat("hg_s")[None])
    if DEBUG:
        global _DBG
        _DBG = np.asarray(R[0]["dbg"])
    return tuple(np.ascontiguousarray(o, dtype=np.float32) for o in outs)
```

```python
import contextlib
import numpy as np
import concourse.bass as bass
import concourse.mybir as mybir
from concourse.bass_utils import run_bass_kernel_spmd

F32 = mybir.dt.float32
BF16 = mybir.dt.bfloat16
I32 = mybir.dt.int32
U32 = mybir.dt.uint32
ALU = mybir.AluOpType
AF = mybir.ActivationFunctionType
AX = mybir.AxisListType

NCORES = 8
D = 1024
SEQ = 4096
NSEQ = 2
NS = 16
INW = 10752
GROUPS = ((128, 1), (512, 4), (2048, 16))
EPS = 1e-6
NT = SEQ // 128
NROWS = NSEQ + NS
DEBUG = False


class Res:
    __slots__ = ("w", "rs")

    def __init__(self):
        self.w = {}
        self.rs = {}


class K:
    def __init__(self, nc, es):
        self.nc = nc
        self.es = es
        self.eng = {"pe": nc.tensor, "act": nc.scalar, "dve": nc.vector, "pool": nc.gpsimd, "sp": nc.sync}
        self.sem = {}
        self.cnt = {}
        for e in ("pe", "act", "dve", "pool"):
            self.sem[e] = es.enter_context(nc.semaphore("c_" + e))
            self.cnt[e] = 0
        self.known = {e: {} for e in self.eng}
        self.dsem = {}
        self.dpos = {}
        for q, n in (("sp", 24), ("pool", 16), ("act", 8)):
            self.dsem[q] = [[es.enter_context(nc.semaphore("d_%s%d" % (q, i))), 0] for i in range(n)]
            self.dpos[q] = 0
        self.deferred = []

    def _wait(self, e, tok):
        s, v = tok
        if v <= 0:
            return
        if e == "pe" and s is self.sem["pe"]:
            return
        kn = self.known[e]
        if kn.get(id(s), 0) >= v:
            return
        self.eng[e].wait_ge(s, v)
        kn[id(s)] = v

    def _deps(self, e, reads, writes):
        for r in reads:
            for tok in r.w.values():
                self._wait(e, tok)
        for r in writes:
            for tok in r.w.values():
                self._wait(e, tok)
            for tok in r.rs.values():
                self._wait(e, tok)

    def _mark(self, tok, reads, writes, accum):
        s, v = tok
        for r in reads:
            r.rs[id(s)] = tok
        for r in writes:
            r.w = {id(s): tok}
            r.rs = {}
        for r in accum:
            r.w[id(s)] = tok

    def op(self, e, fn, reads=(), writes=(), accum=()):
        self._deps(e, reads, writes)
        ins = fn()
        self.cnt[e] += 1
        ins.then_inc(self.sem[e], 1)
        self._mark((self.sem[e], self.cnt[e]), reads, writes, accum)

    def dma(self, q, out, in_, reads=(), writes=(), accum=(), indirect=None):
        self._deps(q, reads, writes)
        slot = self.dsem[q][self.dpos[q] % len(self.dsem[q])]
        self.dpos[q] += 1
        self._wait(q, (slot[0], slot[1]))
        if indirect is not None:
            ins = self.eng[q].indirect_dma_start(out=out, out_offset=None, in_=in_, in_offset=indirect)
        else:
            ins = self.eng[q].dma_start(out=out, in_=in_)
        slot[1] += 16
        ins.then_inc(slot[0], 16)
        self._mark((slot[0], slot[1]), reads, writes, accum)

    def barrier(self):
        self.flush()
        toks = [(self.sem[e], self.cnt[e]) for e in self.sem]
        for q in self.dsem:
            toks += [(s, v) for s, v in self.dsem[q]]
        for e in self.eng:
            for tok in toks:
                if e != "pe" or tok[0] is not self.sem["pe"]:
                    self._wait(e, tok)

    def defer(self, fn):
        self.deferred.append(fn)

    def flush(self):
        d, self.deferred = self.deferred, []
        for fn in d:
            fn()

    def finish(self):
        self.flush()
        for q in self.dsem:
            for s, v in self.dsem[q]:
                self._wait("sp", (s, v))


def build_program(stop_after=99):
    nc = bass.Bass("TRN2", target_bir_lowering=False)
    es = contextlib.ExitStack()
    with es:
        _emit(nc, es, stop_after)
    return nc


def _emit(nc, es, stop_after):
    k = K(nc, es)

    def din(name, shape, dt=F32):
        return nc.dram_tensor(name, list(shape), dt, kind="ExternalInput").ap()

    def dout(name, shape, dt=F32):
        return nc.dram_tensor(name, list(shape), dt, kind="ExternalOutput").ap()

    def dscr(name, shape, dt):
        return nc.dram_tensor(name, list(shape), dt, kind="Internal").ap()

    def sb(name, shape, dt):
        return es.enter_context(nc.sbuf_tensor(name, list(shape), dt))

    xp = din("xp", [NSEQ, SEQ, D])
    xs = din("xs", [NS, D])
    cT = din("cT", [D, NROWS])
    ck = [din("ck%d" % g, [NS, 128, 2, 512]) for g in range(3)]
    st_in = din("st_in", [NS, 8, 128, 128])
    w_ada = din("w_ada", [D, 6 * D])
    b_ada = din("b_ada", [1, 6 * D])
    norm1_w = din("norm1_w", [1, D])
    norm2_w = din("norm2_w", [1, D])
    w_in = din("w_in", [D, INW])
    qk_w = din("qk_w", [2, 512])
    lb_log = din("lb_log", [2, D])
    hgn_w = din("hgn_w", [1, D])
    w_br_a = din("w_br_a", [512, D])
    w_br_b = din("w_br_b", [D, D])
    w_o = din("w_o", [D, D])
    w_pq = din("w_pq", [D, 2048])
    skT = din("skT", [2, 128, 128])
    peer_u = din("peer_u", [16384, D])
    peer_v = din("peer_v", [16384, D])
    cst = din("cst", [128, 128 * 8])
    selc = din("selc", [NS, 2064])

    y_p = dout("y_p", [NSEQ, SEQ, D])
    y_s = dout("y_s", [NS, D])
    kv_p = [dout("kv%d_p" % g, [NSEQ, GROUPS[g][0], 2, 512]) for g in range(3)]
    hg_p = dout("hg_p", [NSEQ, 8, 128, 128])
    kv_s = [dout("kv%d_s" % g, [NS, 2, 512]) for g in range(3)]
    hg_s = dout("hg_s", [NS, 8, 128, 128])
    dbg = dout("dbg", [2, 10, 128, D]) if DEBUG else None

    MODS = dscr("MODS", [NROWS, 6 * D], F32)
    NTOK = NSEQ * SEQ + 128
    QS = dscr("QS", [NTOK, 3, 512], BF16)
    KS = dscr("KS", [NTOK, 3, 512], BF16)
    VS = dscr("VS", [NTOK, 3, 520], BF16)

    cst_f = sb("cst_f", [128, 1024], F32)
    ident_b = sb("ident_b", [128, 128], BF16)
    r_cst = Res()
    k.dma("sp", cst_f[:], cst, writes=[r_cst])
    r_idb = Res()
    k.op("dve", lambda: nc.vector.tensor_copy(ident_b[:], cst_f[:, 0:128]), reads=[r_cst], writes=[r_idb])
    ident_f = cst_f[:, 0:128]

    psf = [es.enter_context(nc.psum_tensor("psf%d" % i, [128, 512], F32)) for i in range(7)]
    r_psf = [Res() for _ in range(7)]
    psb = es.enter_context(nc.psum_tensor("psb", [128, 1024], BF16))
    r_psb = Res()

    def rstd_from_ss(P, ss, n_elem, tmp):
        t, r = ss
        k.op("dve", lambda: nc.vector.tensor_scalar(t, t, 1.0 / n_elem, EPS, ALU.mult, ALU.add), writes=[r])
        k.op("act", lambda: nc.scalar.activation(t, t, AF.Sqrt), writes=[r])
        k.op("dve", lambda: nc.vector.reciprocal(t, t), writes=[r])

    with contextlib.ExitStack() as ph:
        def psb_(name, shape, dt):
            return ph.enter_context(nc.sbuf_tensor(name, list(shape), dt))
        cT_f = psb_("cT_f", [128, 8, NROWS], F32)
        cT_b = psb_("cT_b", [128, 8, NROWS], BF16)
        r_cT = Res()
        k.dma("sp", cT_f[:], cT.rearrange("(kc p) n -> p kc n", p=128), writes=[r_cT])
        k.op("act", lambda: nc.scalar.activation(cT_b[:], cT_f[:], AF.Silu), reads=[r_cT], writes=[r_cT])
        wa = [psb_("wa%d" % i, [128, 8, 512], BF16) for i in range(2)]
        r_wa = [Res(), Res()]
        ba = [psb_("ba%d" % i, [NROWS, 512], F32) for i in range(2)]
        r_ba = [Res(), Res()]
        mo = [psb_("mo%d" % i, [NROWS, 512], F32) for i in range(2)]
        r_mo = [Res(), Res()]
        r_MODS = Res()
        for c in range(12):
            i = c % 2
            cs = slice(c * 512, (c + 1) * 512)
            k.dma("pool", wa[i][:], w_ada[:, cs].rearrange("(kc p) n -> p kc n", p=128), writes=[r_wa[i]])
            k.dma("sp", ba[i][:], b_ada[:, cs].partition_broadcast(NROWS), writes=[r_ba[i]])
            for kc in range(8):
                k.op("pe", lambda kc=kc: nc.tensor.matmul(psf[i][0:NROWS, :], cT_b[:, kc, :], wa[i][:, kc, :],
                                                          start=(kc == 0), stop=(kc == 7)),
                     reads=[r_cT, r_wa[i]], writes=[r_psf[i]])
            k.op("dve", lambda: nc.vector.tensor_tensor(mo[i][:], psf[i][0:NROWS, :], ba[i][:], ALU.add),
                 reads=[r_ba[i]], writes=[r_psf[i], r_mo[i]])
            k.dma("sp", MODS[:, cs], mo[i][:], reads=[r_mo[i]], accum=[r_MODS])
        k.barrier()
    if stop_after <= 0:
        k.finish()
        return

    def tok0(s):
        return s * SEQ

    NTOKX = NSEQ * SEQ + 128
    GG = dscr("GG", [NTOKX, 3072], BF16)
    HGS = dscr("HGS", [NTOKX, D], BF16)
    OACC = dscr("OACC", [3, NTOKX, 520], F32)
    UV = dscr("UV", [16384, 2 * D], BF16)
    r_uv = Res()
    r_scr = Res()
    r_gg = Res()
    r_hgs = Res()
    r_oacc = Res()
    bank_ctr = [0]

    def nb():
        i = bank_ctr[0] % 7
        bank_ctr[0] += 1
        return psf[i], r_psf[i]

    maskc_b = sb("maskc_b", [128, 4, 128], BF16)
    maskp_b = sb("maskp_b", [128, 4, 128], BF16)
    r_mask = Res()
    for hh in range(4):
        k.op("dve", lambda hh=hh: nc.vector.tensor_copy(maskc_b[:, hh, :], cst_f[:, 128:256]), reads=[r_cst], writes=[r_mask])
        k.op("dve", lambda hh=hh: nc.vector.tensor_copy(maskp_b[:, hh, :], cst_f[:, 256:384]), reads=[r_cst], writes=[r_mask])
    L1 = cst_f[:, 384:512]
    L3 = cst_f[:, 512:640]
    R4 = cst_f[:, 640:644]
    MA = cst_f[:, 768:896]

    def proj(P, hT, ts_, W, c0, ncol512, r_hT, r_W):
        out = []
        for j in range(ncol512):
            b_, rb = nb()
            for kc in range(8):
                k.op("pe", lambda kc=kc: nc.tensor.matmul(b_[0:P, :], hT[:, kc, ts_], W[:, kc, c0 + j * 512:c0 + (j + 1) * 512],
                                                          start=(kc == 0), stop=(kc == 7)),
                     reads=[r_hT, r_W], writes=[rb])
            out.append((b_, rb))
        return out

    with contextlib.ExitStack() as seqscope:
        hT = seqscope.enter_context(nc.sbuf_tensor("hT", [128, 8, SEQ], BF16))
        r_hT = Res()
        for s in range(3):
            P = 128 if s < 2 else NS
            ntile = NT if s < 2 else 1

            def xsrc(t):
                return xp[s, t * 128:(t + 1) * 128, :] if s < 2 else xs[:, :]

            with contextlib.ExitStack() as ph:
                def psb_(name, shape, dt):
                    return ph.enter_context(nc.sbuf_tensor(name + "_s%d" % s, list(shape), dt))
                S1 = psb_("S1", [128, D], F32)
                SH1 = psb_("SH1", [128, D], F32)
                n1w = psb_("n1w", [128, D], F32)
                r_S1 = Res()
                r_n1w = Res()
                k.dma("sp", n1w[:], norm1_w.partition_broadcast(128), writes=[r_n1w])
                qkw = psb_("qkw", [128, 2, 512], F32)
                r_qkw = Res()
                k.dma("sp", qkw[:, 0, :], qk_w[0:1, :].partition_broadcast(128), writes=[r_qkw])
                k.dma("sp", qkw[:, 1, :], qk_w[1:2, :].partition_broadcast(128), writes=[r_qkw])
                k.op("dve", lambda: nc.vector.tensor_scalar(qkw[:, 0, :], qkw[:, 0, :], 0.125, None, ALU.mult), writes=[r_qkw])
                xt = [psb_("xt%d" % i, [128, D], F32) for i in range(2)]
                r_xt = [Res(), Res()]
                xm = psb_("xm", [128, D], F32)
                xb = psb_("xb", [128, D], BF16)
                r_xm = Res()
                r_xb = Res()
                junk = psb_("junk", [128, D], F32)
                r_junk = Res()
                ss1 = psb_("ss1", [128, 1], F32)
                r_ss1 = Res()
                Wg = psb_("Wg", [128, 8, 1536], BF16)
                r_Wg = Res()
                ss8 = psb_("ss8", [128, 8], F32)
                r_ss8 = Res()
                qn_b = [psb_("qn_b%d" % i, [128, 512], BF16) for i in range(2)]
                r_qn = [Res(), Res()]
                kn32 = [psb_("kn32%d" % i, [128, 512], F32) for i in range(2)]
                r_kn32 = [Res(), Res()]
                kn_b = [psb_("kn_b%d" % i, [128, 512], BF16) for i in range(2)]
                r_knb = [Res(), Res()]
                v32 = [psb_("v32%d" % i, [128, 512], F32) for i in range(2)]
                r_v32 = [Res(), Res()]
                vaug = [psb_("vaug%d" % i, [128, 8, 65], BF16) for i in range(2)]
                r_vaug = [Res(), Res()]
                for i in range(2):
                    k.op("pool", lambda i=i: nc.gpsimd.memset(vaug[i][:], 1.0), writes=[r_vaug[i]])

                if s < 2:
                    k.dma("sp", SH1[:], MODS[s:s + 1, 0:D].partition_broadcast(128), reads=[r_MODS], writes=[r_S1])
                    k.dma("sp", S1[:], MODS[s:s + 1, D:2 * D].partition_broadcast(128), reads=[r_MODS], writes=[r_S1])
                else:
                    k.dma("sp", SH1[0:NS, :], MODS[2:2 + NS, 0:D], reads=[r_MODS], writes=[r_S1])
                    k.dma("sp", S1[0:NS, :], MODS[2:2 + NS, D:2 * D], reads=[r_MODS], writes=[r_S1])
                k.op("dve", lambda: nc.vector.scalar_tensor_tensor(S1[0:P, :], S1[0:P, :], 1.0, n1w[0:P, :], ALU.add, ALU.mult),
                     reads=[r_n1w], writes=[r_S1])

                k.dma("sp", xt[0][0:P, :], xsrc(0), writes=[r_xt[0]])
                for t in range(ntile):
                    i = t % 2
                    if t + 1 < ntile:
                        k.dma("sp", xt[1 - i][0:P, :], xsrc(t + 1), writes=[r_xt[1 - i]])
                    k.op("act", lambda: nc.scalar.activation(junk[0:P, :], xt[i][0:P, :], AF.Square, accum_out=ss1[0:P, :]),
                         reads=[r_xt[i]], writes=[r_junk, r_ss1])
                    rstd_from_ss(P, (ss1[0:P, :], r_ss1), D, None)
                    k.op("dve", lambda: nc.vector.scalar_tensor_tensor(xm[0:P, :], xt[i][0:P, :], ss1[0:P, 0:1], S1[0:P, :],
                                                                       ALU.mult, ALU.mult),
                         reads=[r_xt[i], r_ss1, r_S1], writes=[r_xm])
                    k.op("dve", lambda: nc.vector.tensor_tensor(xb[0:P, :], xm[0:P, :], SH1[0:P, :], ALU.add),
                         reads=[r_xm, r_S1], writes=[r_xb])
                    for kc in range(8):
                        k.op("pe", lambda kc=kc: nc.tensor.transpose(psb[:, kc * 128:kc * 128 + P], xb[0:P, kc * 128:(kc + 1) * 128],
                                                                     ident_b[0:P, 0:P]),
                             reads=[r_xb, r_idb], writes=[r_psb])
                    k.op("act", lambda: nc.scalar.copy(hT[:, :, t * 128:t * 128 + P],
                                                       psb[:].rearrange("p (kc n) -> p kc n", kc=8)[:, :, 0:P]),
                         writes=[r_psb, r_hT])

                for g in range(3):
                    win = GROUPS[g][0]
                    for part in range(3):
                        c0 = part * 1536 + g * 512
                        k.dma("pool", Wg[:, :, part * 512:(part + 1) * 512],
                              w_in[:, c0:c0 + 512].rearrange("(kc p) n -> p kc n", p=128), writes=[r_Wg])
                    for t in range(ntile):
                        i = t % 2
                        ts_ = slice(t * 128, t * 128 + P)
                        g0 = tok0(s) + t * 128
                        pr = proj(P, hT, ts_, Wg, 0, 3, r_hT, r_Wg)
                        bank = [p_[0] for p_ in pr]
                        rbank = [p_[1] for p_ in pr]
                        for part in range(2):
                            ps_ = bank[part]
                            k.op("act", lambda: nc.scalar.activation(junk[0:P, 0:512], ps_[0:P, :], AF.Square),
                                 writes=[rbank[part], r_junk])
                            k.op("dve", lambda: nc.vector.tensor_reduce(ss8[0:P, :], junk[0:P, 0:512].rearrange("p (h e) -> p h e", h=8),
                                                                        AX.X, ALU.add),
                                 reads=[r_junk], writes=[r_ss8])
                            rstd_from_ss(P, (ss8[0:P, :], r_ss8), 64, None)
                            dst32 = junk if part == 0 else kn32[i]
                            rdst = r_junk if part == 0 else r_kn32[i]
                            k.op("dve", lambda: nc.vector.tensor_tensor(
                                dst32[0:P, 0:512].rearrange("p (h e) -> p h e", h=8),
                                ps_[0:P, :].rearrange("p (h e) -> p h e", h=8),
                                ss8[0:P, :].unsqueeze(2).to_broadcast([P, 8, 64]), ALU.mult),
                                reads=[r_ss8], writes=[rbank[part], rdst])
                            if part == 0:
                                k.op("dve", lambda: nc.vector.tensor_tensor(qn_b[i][0:P, :], junk[0:P, 0:512], qkw[0:P, 0, :], ALU.mult),
                                     reads=[r_junk, r_qkw], writes=[r_qn[i]])
                            else:
                                k.op("dve", lambda: nc.vector.tensor_tensor(kn32[i][0:P, :], kn32[i][0:P, :], qkw[0:P, 1, :], ALU.mult),
                                     reads=[r_qkw], writes=[r_kn32[i]])
                                k.op("pool", lambda: nc.gpsimd.tensor_copy(kn_b[i][0:P, :], kn32[i][0:P, :]),
                                     reads=[r_kn32[i]], writes=[r_knb[i]])
                        k.op("act", lambda: nc.scalar.copy(v32[i][0:P, :], bank[2][0:P, :]), writes=[rbank[2], r_v32[i]])
                        k.op("pool", lambda: nc.gpsimd.tensor_copy(vaug[i][0:P, :, 0:64],
                                                                    v32[i][0:P, :].rearrange("p (h e) -> p h e", h=8)),
                             reads=[r_v32[i]], writes=[r_vaug[i]])
                        k.flush()

                        def stores(i=i, g=g, g0=g0, t=t, P=P, s=s, win=win):
                            k.dma("sp", QS[g0:g0 + P, g, :], qn_b[i][0:P, :], reads=[r_qn[i]], accum=[r_scr])
                            k.dma("sp", KS[g0:g0 + P, g, :], kn_b[i][0:P, :], reads=[r_knb[i]], accum=[r_scr])
                            k.dma("sp", VS[g0:g0 + P, g, :], vaug[i][0:P, :, :].rearrange("p h e -> p (h e)"),
                                  reads=[r_vaug[i]], accum=[r_scr])
                            if s < 2:
                                r0 = t * 128 - (SEQ - win)
                                if r0 >= 0:
                                    k.dma("sp", kv_p[g][s, r0:r0 + 128, 0, :], kn32[i][:], reads=[r_kn32[i]])
                                    k.dma("sp", kv_p[g][s, r0:r0 + 128, 1, :], v32[i][:], reads=[r_v32[i]])
                            else:
                                k.dma("sp", kv_s[g][:, 0, :], kn32[i][0:P, :], reads=[r_kn32[i]])
                                k.dma("sp", kv_s[g][:, 1, :], v32[i][0:P, :], reads=[r_v32[i]])
                        k.defer(stores)
                    k.flush()
                k.barrier()
            if stop_after <= 1:
                continue

            with contextlib.ExitStack() as ph:
                def psb_(name, shape, dt):
                    return ph.enter_context(nc.sbuf_tensor(name + "_s%d" % s, list(shape), dt))
                Wt = psb_("Wt", [128, 8, 3072], BF16)
                r_Wt = Res()
                for j, c0 in enumerate((7680, 8704, 9728)):
                    k.dma("pool", Wt[:, :, j * 1024:(j + 1) * 1024],
                          w_in[:, c0:c0 + 1024].rearrange("(kc p) n -> p kc n", p=128), writes=[r_Wt])
                ggb = [psb_("ggb%d" % i, [128, 3072], BF16) for i in range(2)]
                r_ggb = [Res(), Res()]
                for t in range(ntile):
                    i = t % 2
                    ts_ = slice(t * 128, t * 128 + P)
                    g0 = tok0(s) + t * 128
                    for j in range(6):
                        pr = proj(P, hT, ts_, Wt, j * 512, 1, r_hT, r_Wt)
                        b_, rb = pr[0]
                        fn_ = AF.Silu if j < 2 else AF.Sigmoid
                        k.op("act", lambda: nc.scalar.activation(ggb[i][0:P, j * 512:(j + 1) * 512], b_[0:P, :], fn_),
                             writes=[rb, r_ggb[i]])
                    k.flush()
                    k.defer(lambda i=i, g0=g0, P=P: k.dma("sp", GG[g0:g0 + P, :], ggb[i][0:P, :], reads=[r_ggb[i]], accum=[r_gg]))
                k.barrier()
            if stop_after <= 2:
                continue

            with contextlib.ExitStack() as ph:
                def psb_(name, shape, dt):
                    return ph.enter_context(nc.sbuf_tensor(name + "_s%d" % s, list(shape), dt))
                Wh = psb_("Wh", [128, 8, 3072], BF16)
                r_Wh = Res()
                for j in range(3):
                    c0 = 4608 + j * 1024
                    k.dma("pool", Wh[:, :, j * 1024:(j + 1) * 1024],
                          w_in[:, c0:c0 + 1024].rearrange("(kc p) n -> p kc n", p=128), writes=[r_Wh])
                lbt = psb_("lbt", [128, D], F32)
                omlt = psb_("omlt", [128, D], F32)
                hgw = psb_("hgw", [128, D], F32)
                r_lb = Res()
                k.dma("sp", lbt[:], lb_log[0:1, :].partition_broadcast(128), writes=[r_lb])
                k.dma("sp", omlt[:], lb_log[1:2, :].partition_broadcast(128), writes=[r_lb])
                k.dma("sp", hgw[:], hgn_w.partition_broadcast(128), writes=[r_lb])
                k.op("dve", lambda: nc.vector.tensor_tensor(lbt[:], lbt[:], omlt[:], ALU.subtract), writes=[r_lb])
                k.op("act", lambda: nc.scalar.activation(lbt[:], lbt[:], AF.Sigmoid), writes=[r_lb])
                k.op("dve", lambda: nc.vector.tensor_scalar(omlt[:], lbt[:], -1.0, 1.0, ALU.mult, ALU.add), writes=[r_lb])
                logf = psb_("logf", [128, D], F32)
                kk = psb_("kk", [128, D], F32)
                et = psb_("et", [128, D], F32)
                t2 = psb_("t2", [128, D], F32)
                r_logf, r_kk, r_et, r_t2 = Res(), Res(), Res(), Res()
                kt_b = psb_("kt_b", [128, D], BF16)
                kh_b = psb_("kh_b", [128, D], BF16)
                qt_b = psb_("qt_b", [128, D], BF16)
                v_b = psb_("v_b", [128, D], BF16)
                r_ktb, r_khb, r_qtb, r_vb = Res(), Res(), Res(), Res()
                gt_b = psb_("gt_b", [128, D], BF16)
                r_gtb = Res()
                hg_b = [psb_("hg_b%d" % i, [128, D], BF16) for i in range(2)]
                r_hgb = [Res(), Res()]
                ss8 = psb_("hss8", [128, 8], F32)
                r_ss8 = Res()
                if s < 2:
                    qT = psb_("qT", [128, 8, 128], BF16)
                    kT = psb_("kT", [128, 8, 128], BF16)
                    r_qT, r_kT = Res(), Res()
                    Abd = psb_("Abd", [128, 8, 128], BF16)
                    r_Abd = Res()
                    k.op("pool", lambda: nc.gpsimd.memset(Abd[:], 0.0), writes=[r_Abd])
                    Sm = psb_("Sm", [128, 8, 128], F32)
                    St = psb_("St", [128, 8, 128], F32)
                    Sb = [psb_("Sb%d" % i, [128, 8, 128], BF16) for i in range(2)]
                    r_Sm, r_St = Res(), Res()
                    r_Sb = [Res(), Res()]
                    eb = psb_("eb", [128, 8, 4], F32)
                    r_eb = Res()
                    k.op("pool", lambda: nc.gpsimd.memset(Sm[:], 0.0), writes=[r_Sm])
                    k.op("pool", lambda: nc.gpsimd.memset(Sb[0][:], 0.0), writes=[r_Sb[0]])
                else:
                    selc_sb = psb_("selc_sb", [NS, 2048], F32)
                    k.dma("sp", selc_sb[:], selc[:, 0:2048], writes=[r_cst])
                    fT = psb_("fT", [128, 3, 8, NS], F32)
                    r_fT = Res()
                    v32s = psb_("v32s", [NS, D], F32)
                    r_v32s = Res()
                    QZ = psb_("QZ", [128, 8, NS * NS], F32)
                    r_QZ = Res()
                    k.op("pool", lambda: nc.gpsimd.memset(QZ[:], 0.0), writes=[r_QZ])
                    S0b = [psb_("S0b%d" % i, [128, 8, 128], F32) for i in range(2)]
                    r_S0b = [Res(), Res()]
                    Sn = [psb_("Sn%d" % i, [128, 8, 128], F32) for i in range(2)]
                    r_Sn = [Res(), Res()]

                def hg_epilogue(P, po, i, g0):
                    for hb in range(2):
                        b_, rb = po[hb]
                        k.op("act", lambda: nc.scalar.activation(t2[0:P, hb * 512:(hb + 1) * 512], b_[0:P, :], AF.Square),
                             writes=[rb, r_t2])
                    k.op("dve", lambda: nc.vector.tensor_reduce(ss8[0:P, :], t2[0:P, :].rearrange("p (h e) -> p h e", h=8), AX.X, ALU.add),
                         reads=[r_t2], writes=[r_ss8])
                    rstd_from_ss(P, (ss8[0:P, :], r_ss8), 128, None)
                    for hb in range(2):
                        b_, rb = po[hb]
                        k.op("dve", lambda: nc.vector.tensor_tensor(
                            t2[0:P, hb * 512:(hb + 1) * 512].rearrange("p (h e) -> p h e", h=4),
                            b_[0:P, :].rearrange("p (h e) -> p h e", h=4),
                            ss8[0:P, hb * 4:(hb + 1) * 4].unsqueeze(2).to_broadcast([P, 4, 128]), ALU.mult),
                            reads=[r_ss8], writes=[rb, r_t2])
                    k.op("dve", lambda: nc.vector.tensor_tensor(t2[0:P, :], t2[0:P, :], gt_b[0:P, :], ALU.mult),
                         reads=[r_gtb], writes=[r_t2])
                    k.op("dve", lambda: nc.vector.tensor_tensor(hg_b[i][0:P, :], t2[0:P, :], hgw[0:P, :], ALU.mult),
                         reads=[r_t2, r_lb], writes=[r_hgb[i]])
                    k.flush()
                    k.defer(lambda: k.dma("sp", HGS[g0:g0 + P, :], hg_b[i][0:P, :], reads=[r_hgb[i]], accum=[r_hgs]))

                for t in range(ntile):
                    i = t % 2
                    ts_ = slice(t * 128, t * 128 + P)
                    g0 = tok0(s) + t * 128
                    k.dma("sp", gt_b[0:P, :], GG[g0:g0 + P, 0:D], reads=[r_gg], writes=[r_gtb])
                    pr = proj(P, hT, ts_, Wh, 1024, 2, r_hT, r_Wh)
                    for hb in range(2):
                        b_, rb = pr[hb]
                        k.op("act", lambda: nc.scalar.activation(logf[0:P, hb * 512:(hb + 1) * 512], b_[0:P, :], AF.Sigmoid),
                             writes=[rb, r_logf])
                    k.op("dve", lambda: nc.vector.tensor_tensor(logf[0:P, :], logf[0:P, :], omlt[0:P, :], ALU.mult), reads=[r_lb], writes=[r_logf])
                    k.op("dve", lambda: nc.vector.tensor_tensor(logf[0:P, :], logf[0:P, :], lbt[0:P, :], ALU.add), reads=[r_lb], writes=[r_logf])
                    k.op("dve", lambda: nc.vector.tensor_scalar(kk[0:P, :], logf[0:P, :], -1.0, 1.0, ALU.mult, ALU.add),
                         reads=[r_logf], writes=[r_kk])
                    if s < 2:
                        k.op("act", lambda: nc.scalar.activation(logf[0:P, :], logf[0:P, :], AF.Ln), reads=[r_kk], writes=[r_logf])
                    if s == 2:
                        k.op("pool", lambda: nc.gpsimd.tensor_copy(et[0:P, :], logf[0:P, :]), reads=[r_logf], writes=[r_et])
                        pq = proj(P, hT, ts_, Wh, 0, 2, r_hT, r_Wh)
                        for hb in range(2):
                            b_, rb = pq[hb]
                            k.op("act", lambda: nc.scalar.activation(t2[0:P, hb * 512:(hb + 1) * 512], b_[0:P, :], AF.Silu),
                                 writes=[rb, r_t2])
                        pv = proj(P, hT, ts_, Wh, 2048, 2, r_hT, r_Wh)
                        for hb in range(2):
                            b_, rb = pv[hb]
                            k.op("act", lambda: nc.scalar.copy(v32s[0:P, hb * 512:(hb + 1) * 512], b_[0:P, :]), writes=[rb, r_v32s])
                        for qi, (src_, rs_) in enumerate(((et, r_et), (kk, r_kk), (t2, r_t2))):
                            b_, rb = nb()
                            for h in range(8):
                                k.op("pe", lambda h=h: nc.tensor.transpose(b_[:, h * NS:(h + 1) * NS], src_[0:P, h * 128:(h + 1) * 128],
                                                                            ident_f[0:P, 0:P]),
                                     reads=[rs_, r_cst], writes=[rb])
                            k.op("act", lambda: nc.scalar.copy(fT[:, qi, :, :], b_[:, 0:8 * NS].rearrange("p (h b) -> p h b", h=8)),
                                 writes=[rb, r_fT])
                        k.op("dve", lambda: nc.vector.tensor_copy(QZ[:, :, 0:NS * NS:NS + 1], fT[:, 2, :, :]), reads=[r_fT], writes=[r_QZ])
                        po = [(psf[5], r_psf[5]), (psf[6], r_psf[6])]
                        for b in range(NS):
                            ib = b % 2
                            k.dma("sp", S0b[ib][:], st_in[b].rearrange("h k v -> k h v"), writes=[r_S0b[ib]])
                            pvb = [(psf[2 * ib], r_psf[2 * ib]), (psf[2 * ib + 1], r_psf[2 * ib + 1])]
                            for hb in range(2):
                                b_, rb = pvb[hb]
                                k.op("pe", lambda: nc.tensor.matmul(b_[:, :], selc_sb[0:NS, b * 128:(b + 1) * 128], v32s[0:NS, hb * 512:(hb + 1) * 512],
                                                                    start=True, stop=True),
                                     reads=[r_v32s, r_cst], writes=[rb])
                            k.op("dve", lambda: nc.vector.tensor_tensor(Sn[ib][:], S0b[ib][:],
                                                                        fT[:, 0, :, b:b + 1].to_broadcast([128, 8, 128]), ALU.mult),
                                 reads=[r_S0b[ib], r_fT], writes=[r_Sn[ib]])
                            for hb in range(2):
                                b_, rb = pvb[hb]
                                k.op("dve", lambda: nc.vector.tensor_tensor(
                                    S0b[ib][:, hb * 4:(hb + 1) * 4, :], b_[:, :].rearrange("p (h v) -> p h v", h=4),
                                    fT[:, 1, hb * 4:(hb + 1) * 4, b:b + 1].to_broadcast([128, 4, 128]), ALU.mult),
                                    reads=[r_fT], writes=[rb, r_S0b[ib]])
                            k.op("dve", lambda: nc.vector.tensor_tensor(Sn[ib][:], Sn[ib][:], S0b[ib][:], ALU.add),
                                 reads=[r_S0b[ib]], writes=[r_Sn[ib]])
                            k.dma("sp", hg_s[b].rearrange("h k v -> k h v"), Sn[ib][:], reads=[r_Sn[ib]])
                            for h in range(8):
                                b_, rb = po[h // 4]
                                k.op("pe", lambda h=h: nc.tensor.matmul(b_[0:NS, (h % 4) * 128:(h % 4 + 1) * 128],
                                                                        QZ[:, h, b * NS:(b + 1) * NS], Sn[ib][:, h, :],
                                                                        start=(b == 0 and h % 4 == 0), stop=(b == NS - 1),
                                                                        skip_group_check=True),
                                     reads=[r_QZ, r_Sn[ib]], writes=[rb])
                        hg_epilogue(P, po, i, g0)
                        continue
                    d1 = [nb(), nb()]
                    d3 = [nb(), nb()]
                    for hb in range(2):
                        k.op("pe", lambda: nc.tensor.matmul(d1[hb][0][:, :], L1, logf[:, hb * 512:(hb + 1) * 512], start=True, stop=True),
                             reads=[r_logf, r_cst], writes=[d1[hb][1]])
                        k.op("pe", lambda: nc.tensor.matmul(d3[hb][0][:, :], L3, logf[:, hb * 512:(hb + 1) * 512], start=True, stop=True),
                             reads=[r_logf, r_cst], writes=[d3[hb][1]])
                    for hb in range(2):
                        k.op("act", lambda: nc.scalar.activation(et[:, hb * 512:(hb + 1) * 512], d1[hb][0][:, :], AF.Exp, scale=-1.0),
                             writes=[d1[hb][1], r_et])
                    k.op("dve", lambda: nc.vector.tensor_tensor(kt_b[:], kk[:], et[:], ALU.mult), reads=[r_kk, r_et], writes=[r_ktb])
                    for hb in range(2):
                        k.op("act", lambda: nc.scalar.activation(et[:, hb * 512:(hb + 1) * 512], d3[hb][0][:, :], AF.Exp),
                             writes=[d3[hb][1], r_et])
                    k.op("dve", lambda: nc.vector.tensor_tensor(kh_b[:], kk[:], et[:], ALU.mult), reads=[r_kk, r_et], writes=[r_khb])
                    for hb in range(2):
                        k.op("act", lambda: nc.scalar.activation(et[:, hb * 512:(hb + 1) * 512], d1[hb][0][:, :], AF.Exp),
                             writes=[d1[hb][1], r_et])
                    pq = proj(P, hT, ts_, Wh, 0, 2, r_hT, r_Wh)
                    for hb in range(2):
                        b_, rb = pq[hb]
                        k.op("act", lambda: nc.scalar.activation(t2[:, hb * 512:(hb + 1) * 512], b_[:, :], AF.Silu), writes=[rb, r_t2])
                    k.op("dve", lambda: nc.vector.tensor_tensor(qt_b[:], t2[:], et[:], ALU.mult), reads=[r_t2, r_et], writes=[r_qtb])
                    pv = proj(P, hT, ts_, Wh, 2048, 2, r_hT, r_Wh)
                    for hb in range(2):
                        b_, rb = pv[hb]
                        k.op("act", lambda: nc.scalar.copy(v_b[:, hb * 512:(hb + 1) * 512], b_[:, :]), writes=[rb, r_vb])
                    be, rbe = nb()
                    for h in range(8):
                        k.op("pe", lambda h=h: nc.tensor.matmul(be[:, h * 4:(h + 1) * 4], logf[:, h * 128:(h + 1) * 128], R4,
                                                                start=(h == 0), stop=(h == 7), skip_group_check=True),
                             reads=[r_logf, r_cst], writes=[rbe])
                    k.op("act", lambda: nc.scalar.activation(eb[:], be[:, 0:32].rearrange("p (h c) -> p h c", h=8), AF.Exp),
                         writes=[rbe, r_eb])
                    for src_, rs_, dst_, rd_ in ((qt_b, r_qtb, qT, r_qT), (kt_b, r_ktb, kT, r_kT)):
                        for h in range(8):
                            k.op("pe", lambda h=h: nc.tensor.transpose(psb[:, h * 128:(h + 1) * 128], src_[:, h * 128:(h + 1) * 128], ident_b[:]),
                                 reads=[rs_, r_idb], writes=[r_psb])
                        k.op("act", lambda: nc.scalar.copy(dst_[:], psb[:].rearrange("p (h n) -> p h n", h=8)), writes=[r_psb, rd_])
                    pa = [nb(), nb()]
                    for h in range(8):
                        b_, rb = pa[h // 4]
                        c_ = (h % 4) * 128
                        k.op("pe", lambda h=h: nc.tensor.matmul(b_[0:64, c_:c_ + 64], kT[:, h, 0:64], qT[:, h, 0:64], start=True, stop=True,
                                                                skip_group_check=True),
                             reads=[r_kT, r_qT], writes=[rb])
                        k.op("pe", lambda h=h: nc.tensor.matmul(b_[:, c_ + 64:c_ + 128], kT[:, h, :], qT[:, h, 64:128], start=True, stop=True,
                                                                skip_group_check=True),
                             reads=[r_kT, r_qT], writes=[rb])
                    for hb in range(2):
                        b_, rb = pa[hb]
                        bv = b_[:, :].rearrange("p (h t) -> p h t", h=4)
                        k.op("dve", lambda: nc.vector.tensor_tensor(Abd[0:64, hb * 4:(hb + 1) * 4, 0:64], bv[0:64, :, 0:64],
                                                                    MA[0:64, 0:64].unsqueeze(1).to_broadcast([64, 4, 64]), ALU.mult),
                             reads=[r_cst], writes=[rb, r_Abd])
                        k.op("dve", lambda: nc.vector.tensor_tensor(Abd[64:128, hb * 4:(hb + 1) * 4, 64:128], bv[64:128, :, 64:128],
                                                                    MA[64:128, 64:128].unsqueeze(1).to_broadcast([64, 4, 64]), ALU.mult),
                             reads=[r_cst], writes=[rb, r_Abd])
                    k.op("dve", lambda: nc.vector.tensor_tensor(Sb[0][:], Sm[:], eb[:, :, 2:3].to_broadcast([128, 8, 128]), ALU.mult),
                         reads=[r_Sm, r_eb], writes=[r_Sb[0]])
                    for c in range(2):
                        src_S, rsrc = (Sm, r_Sm) if c == 0 else (St, r_St)
                        dst_S, rdst = (St, r_St) if c == 0 else (Sm, r_Sm)
                        psn = [nb(), nb()]
                        for h in range(8):
                            b_, rb = psn[h // 4]
                            c_ = (h % 4) * 128
                            k.op("pe", lambda h=h: nc.tensor.matmul(b_[:, c_:c_ + 128], kh_b[c * 64:(c + 1) * 64, h * 128:(h + 1) * 128],
                                                                    v_b[c * 64:(c + 1) * 64, h * 128:(h + 1) * 128], start=True, stop=True,
                                                                    skip_group_check=True),
                                 reads=[r_khb, r_vb], writes=[rb])
                        k.op("dve", lambda: nc.vector.tensor_tensor(dst_S[:], src_S[:], eb[:, :, c:c + 1].to_broadcast([128, 8, 128]), ALU.mult),
                             reads=[rsrc, r_eb], writes=[rdst])
                        for hb in range(2):
                            b_, rb = psn[hb]
                            k.op("dve", lambda: nc.vector.tensor_tensor(dst_S[:, hb * 4:(hb + 1) * 4, :], dst_S[:, hb * 4:(hb + 1) * 4, :],
                                                                        b_[:, :].rearrange("p (h v) -> p h v", h=4), ALU.add),
                                 writes=[rb, rdst])
                        if c == 0:
                            k.op("dve", lambda: nc.vector.tensor_tensor(Sb[1][:], St[:], eb[:, :, 3:4].to_broadcast([128, 8, 128]), ALU.mult),
                                 reads=[r_St, r_eb], writes=[r_Sb[1]])
                    po = [nb(), nb()]
                    for h in range(8):
                        b_, rb = po[h // 4]
                        c_ = (h % 4) * 128
                        k.op("pe", lambda h=h: nc.tensor.matmul(b_[:, c_:c_ + 128], Abd[:, h, :], v_b[:, h * 128:(h + 1) * 128],
                                                                start=True, stop=False, skip_group_check=True),
                             reads=[r_Abd, r_vb], writes=[rb])
                        k.op("pe", lambda h=h: nc.tensor.matmul(b_[0:64, c_:c_ + 128], qT[:, h, 0:64], Sb[0][:, h, :],
                                                                start=False, stop=False, skip_group_check=True),
                             reads=[r_qT, r_Sb[0]], writes=[rb])
                        k.op("pe", lambda h=h: nc.tensor.matmul(b_[64:128, c_:c_ + 128], qT[:, h, 64:128], Sb[1][:, h, :],
                                                                start=False, stop=True, skip_group_check=True),
                             reads=[r_qT, r_Sb[1]], writes=[rb])
                    hg_epilogue(P, po, i, g0)
                k.flush()
                if s < 2:
                    k.dma("sp", hg_p[s].rearrange("h k v -> k h v"), Sm[:], reads=[r_Sm])
                k.barrier()
    if stop_after <= 3:
        k.finish()
        return


    with contextlib.ExitStack() as ph:
        def psb_(name, shape, dt):
            return ph.enter_context(nc.sbuf_tensor(name, list(shape), dt))
        qblk = [psb_("qblk%d" % i, [128, 512], BF16) for i in range(2)]
        kblk = [psb_("kblk%d" % i, [128, 512], BF16) for i in range(2)]
        vblk = [psb_("vblk%d" % i, [128, 8, 65], BF16) for i in range(2)]
        r_qblk, r_kblk, r_vblk = [Res(), Res()], [Res(), Res()], [Res(), Res()]
        qTa = psb_("qTa", [128, 4, 128], BF16)
        kTa = [psb_("kTa%d" % i, [128, 4, 128], BF16) for i in range(2)]
        r_qTa = Res()
        r_kTa = [Res(), Res()]
        pT = [psb_("pT%d" % i, [128, 4, 128], BF16) for i in range(4)]
        r_pT = [Res() for _ in range(4)]
        oac = [psb_("oac%d" % i, [128, 520], F32) for i in range(2)]
        r_oac = [Res(), Res()]
        for i in range(2):
            k.op("pool", lambda i=i: nc.gpsimd.memset(qblk[i][:], 0.0), writes=[r_qblk[i]])
            k.op("pool", lambda i=i: nc.gpsimd.memset(kblk[i][:], 0.0), writes=[r_kblk[i]])
            k.op("pool", lambda i=i: nc.gpsimd.memset(vblk[i][:], 1.0), writes=[r_vblk[i]])
        blk_ctr = [0]
        for c_ in range(8):
            rs_ = slice(c_ * 2048, (c_ + 1) * 2048)
            k.dma("pool", UV[rs_, 0:D], peer_u[rs_, :], accum=[r_uv])
            k.dma("pool", UV[rs_, D:2 * D], peer_v[rs_, :], accum=[r_uv])

        def attn_block(load_cur, has_prev, ip, store):
            n_ = blk_ctr[0]
            blk_ctr[0] += 1
            ic = 1 - ip
            load_cur(ic)
            for src_, rs_, dst_, rd_ in ((qblk[ic], r_qblk[ic], qTa, r_qTa), (kblk[ic], r_kblk[ic], kTa[ic], r_kTa[ic])):
                for hp in range(4):
                    k.op("pe", lambda hp=hp: nc.tensor.transpose(psb[:, hp * 128:(hp + 1) * 128], src_[:, hp * 128:(hp + 1) * 128], ident_b[:]),
                         reads=[rs_, r_idb], writes=[r_psb])
                k.op("act", lambda: nc.scalar.copy(dst_[:], psb[:, 0:512].rearrange("p (h n) -> p h n", h=4)), writes=[r_psb, rd_])
            srcs = [(ic, maskc_b)] + ([(ip, maskp_b)] if has_prev else [])
            pts = []
            for si, (ib, msk) in enumerate(srcs):
                for hb in range(2):
                    b_, rb = nb()
                    for hh in range(4):
                        h = 2 * hh + hb
                        po_ = hb * 64
                        k.op("pe", lambda: nc.tensor.matmul(b_[:, hh * 128:(hh + 1) * 128], kTa[ib][po_:po_ + 64, h // 2, :],
                                                            qTa[po_:po_ + 64, h // 2, :], start=True, stop=True, skip_group_check=True),
                             reads=[r_kTa[ib], r_qTa], writes=[rb])
                    pi = si * 2 + hb
                    k.op("act", lambda: nc.scalar.activation(pT[pi][:], b_[:, :].rearrange("p (h n) -> p h n", h=4), AF.Exp),
                         writes=[rb, r_pT[pi]])
                    k.op("pool", lambda: nc.gpsimd.tensor_tensor(pT[pi][:], pT[pi][:], msk[:], ALU.mult), reads=[r_mask], writes=[r_pT[pi]])
                    pts.append((pi, ib))
            io = n_ % 2
            for hb in range(2):
                b_, rb = nb()
                for hh in range(4):
                    h = hb * 4 + hh
                    for si, (ib, msk) in enumerate(srcs):
                        pi = si * 2 + (h % 2)
                        k.op("pe", lambda: nc.tensor.matmul(b_[:, hh * 65:(hh + 1) * 65], pT[pi][:, h // 2, :], vblk[ib][:, h, :],
                                                            start=(si == 0), stop=(si == len(srcs) - 1), skip_group_check=True),
                             reads=[r_pT[pi], r_vblk[ib]], writes=[rb])
                k.op("act", lambda: nc.scalar.copy(oac[io][:, hb * 260:(hb + 1) * 260], b_[:, 0:260]), writes=[rb, r_oac[io]])
            k.flush()
            k.defer(lambda io=io, store=store: store(oac[io], r_oac[io]))
            return ic

        for s in range(2):
            for g in range(3):
                d = GROUPS[g][1]
                nblk = SEQ // (128 * d)

                def view(T, width):
                    return T[tok0(s):tok0(s) + SEQ, g, :].rearrange("(n j dd) c -> dd n j c", dd=d, j=128)
                Qv, Kv, Vv = view(QS, 512), view(KS, 512), view(VS, 520)
                Ov = OACC[g, tok0(s):tok0(s) + SEQ, :].rearrange("(n j dd) c -> dd n j c", dd=d, j=128)
                for r in range(d):
                    ip = 0
                    for n in range(nblk):
                        def load_cur(ic, r=r, n=n, Qv=Qv, Kv=Kv, Vv=Vv):
                            k.dma("sp", qblk[ic][:], Qv[r, n], reads=[r_scr], writes=[r_qblk[ic]])
                            k.dma("sp", kblk[ic][:], Kv[r, n], reads=[r_scr], writes=[r_kblk[ic]])
                            k.dma("sp", vblk[ic][:].rearrange("p h e -> p (h e)"), Vv[r, n], reads=[r_scr], writes=[r_vblk[ic]])

                        def store(o_, ro_, r=r, n=n, Ov=Ov):
                            k.dma("sp", Ov[r, n], o_[:], reads=[ro_], accum=[r_oacc])
                        ip = attn_block(load_cur, n > 0, ip, store)
        for b in range(NS):
            for g in range(3):
                tokb = tok0(2) + b
                ip = 0
                k.dma("pool", kblk[ip][:], ck[g][b, :, 0, :], writes=[r_kblk[ip]])
                k.dma("pool", vblk[ip][:, :, 0:64], ck[g][b, :, 1, :].rearrange("j (h e) -> j h e", h=8), writes=[r_vblk[ip]])
                for hp in range(4):
                    k.op("pe", lambda hp=hp: nc.tensor.transpose(psb[:, hp * 128:(hp + 1) * 128], kblk[ip][:, hp * 128:(hp + 1) * 128], ident_b[:]),
                         reads=[r_kblk[ip], r_idb], writes=[r_psb])
                k.op("act", lambda: nc.scalar.copy(kTa[ip][:], psb[:, 0:512].rearrange("p (h n) -> p h n", h=4)), writes=[r_psb, r_kTa[ip]])

                def load_cur(ic, g=g, tokb=tokb):
                    k.dma("sp", qblk[ic][0:1, :], QS[tokb:tokb + 1, g, :], reads=[r_scr], writes=[r_qblk[ic]])
                    k.dma("sp", kblk[ic][0:1, :], KS[tokb:tokb + 1, g, :], reads=[r_scr], writes=[r_kblk[ic]])
                    k.dma("sp", vblk[ic][0:1, :, :].rearrange("p h e -> p (h e)"), VS[tokb:tokb + 1, g, :], reads=[r_scr], writes=[r_vblk[ic]])

                def store(o_, ro_, g=g, tokb=tokb):
                    k.dma("sp", OACC[g, tokb:tokb + 1, :], o_[0:1, :], reads=[ro_], accum=[r_oacc])
                attn_block(load_cur, True, ip, store)
        k.barrier()
    if stop_after <= 4:
        k.finish()
        return

    with contextlib.ExitStack() as ph:
        def psb_(name, shape, dt):
            return ph.enter_context(nc.sbuf_tensor(name, list(shape), dt))
        Wa = psb_("Wa", [128, 4, D], BF16)
        Wb = psb_("Wb", [128, 8, D], BF16)
        Wo = psb_("Wo", [128, 8, D], BF16)
        Wq = psb_("Wq", [128, 8, 2048], BF16)
        skt = psb_("skt", [128, 2, 128], F32)
        r_W = Res()
        k.dma("pool", Wa[:], w_br_a.rearrange("(kc p) n -> p kc n", p=128), writes=[r_W])
        k.dma("pool", Wb[:], w_br_b.rearrange("(kc p) n -> p kc n", p=128), writes=[r_W])
        k.dma("pool", Wo[:], w_o.rearrange("(kc p) n -> p kc n", p=128), writes=[r_W])
        k.dma("pool", Wq[:], w_pq.rearrange("(kc p) n -> p kc n", p=128), writes=[r_W])
        k.dma("sp", skt[:], skT.rearrange("t e c -> e t c"), writes=[r_W])
        n2w = psb_("n2w", [128, D], F32)
        k.dma("sp", n2w[:], norm2_w.partition_broadcast(128), writes=[r_W])
        iota16 = psb_("iota16", [128, 16], F32)
        k.dma("sp", iota16[:], selc[0:1, 2048:2064].partition_broadcast(128), writes=[r_W])
        G1 = psb_("G1", [128, D], F32)
        S2 = psb_("S2", [128, D], F32)
        SH2 = psb_("SH2", [128, D], F32)
        G2 = psb_("G2", [128, D], F32)
        r_M = Res()
        xin = [psb_("xin0", [128, D], F32)] * 2
        r_xin = [Res()] * 2
        oin = [psb_("oin0", [128, 3, 520], F32)] * 2
        r_oin = [Res()] * 2
        hgin = [psb_("hgin0", [128, D], BF16)] * 2
        r_hgin = [Res()] * 2
        ggin = [psb_("ggin0", [128, 2048], BF16)] * 2
        r_ggin = [Res()] * 2
        rl = psb_("rl", [128, 8], F32)
        r_rl = Res()
        att_b = psb_("att_b", [128, 512], BF16)
        r_attb = Res()
        TT = psb_("TT", [128, 8, 128], BF16)
        r_TT = Res()
        f1 = psb_("f1", [128, D], F32)
        r_f1 = Res()
        yb = psb_("yb", [128, D], BF16)
        r_yb = Res()
        x1 = psb_("x1", [128, D], F32)
        r_x1 = Res()
        h2b = psb_("h2b", [128, D], BF16)
        r_h2b = Res()
        ssq = psb_("ssq", [128, 1], F32)
        r_ssq = Res()
        arena = psb_("arena", [128, 6144], F32)
        sc = arena[:, 0:2048].rearrange("p (a b) -> p a b", a=16)
        sc2 = arena[:, 2048:4096].rearrange("p (a b) -> p a b", a=16)
        r_sc, r_sc2 = Res(), Res()
        q2T = sc2
        r_q2T = r_sc2
        vals = psb_("vals", [128, 16, 16], F32)
        idxu = psb_("idxu", [128, 16, 16], U32)
        idxf = psb_("idxf", [128, 16, 16], F32)
        r_vals, r_idxu, r_idxf = Res(), Res(), Res()
        cand = arena[:, 4096:6144].rearrange("p (h x) -> p h x", h=8)
        cand2 = arena[:, 2048:4096].rearrange("p (h x) -> p h x", h=8)
        r_cand, r_cand2 = Res(), r_sc2
        tv = psb_("tv", [128, 8, 16], F32)
        posu = psb_("posu", [128, 8, 16], U32)
        pa_u = psb_("pa_u", [128, 8, 16], U32)
        pa_f = psb_("pa_f", [128, 2, 8, 16], F32)
        r_tv, r_posu, r_pau, r_paf = Res(), Res(), Res(), Res()
        oh = cand2
        r_oh = r_sc2
        isel = psb_("isel", [128, 2, 8, 16], F32)
        r_isel = Res()
        eid_f = psb_("eid_f", [128, 128], F32)
        eid = psb_("eid", [128, 128], I32)
        r_eid = Res()
        gsm = psb_("gsm", [128, 8], F32)
        gat = psb_("gat", [128, 8, 16], F32)
        r_gat = Res()
        dots = psb_("dots", [128, 128], F32)
        r_dots = Res()
        coef = psb_("coef", [128, 128], F32)
        r_coef = Res()
        NUV = 12
        arena_b = arena.bitcast(BF16)
        uvb = [psb_("uvb%d" % i, [128, 2 * D], BF16) for i in range(8)] + [arena_b[:, i * 2048:(i + 1) * 2048] for i in range(4)]
        r_uvb = [Res() for _ in range(NUV)]
        gather_end = {}
        r_dotg = [Res() for _ in range(32)]
        r_coefg = [Res() for _ in range(32)]
        dg = [psb_("dg%d" % i, [128, 128], BF16) for i in range(4)]
        r_dg = [Res() for _ in range(4)]
        jb = psb_("jb", [128, D], BF16)
        r_jb = Res()
        yo = [psb_("yo0", [128, D], F32)] * 2
        r_yo = [Res()] * 2

        def transpose_to_TT(P, src, rsrc, nk):
            for kc in range(nk):
                k.op("pe", lambda kc=kc: nc.tensor.transpose(psb[:, kc * 128:kc * 128 + P], src[0:P, kc * 128:(kc + 1) * 128], ident_b[0:P, 0:P]),
                     reads=[rsrc, r_idb], writes=[r_psb])
            k.op("act", lambda: nc.scalar.copy(TT[:, 0:nk, 0:P], psb[:, 0:nk * 128].rearrange("p (kc n) -> p kc n", kc=nk)[:, :, 0:P]),
                 writes=[r_psb, r_TT])

        def mm_tok(P, W, nk, nbank, rW):
            out = []
            for j in range(nbank):
                b_, rb = nb()
                for kc in range(nk):
                    k.op("pe", lambda kc=kc: nc.tensor.matmul(b_[0:P, :], TT[:, kc, 0:P], W[:, kc, j * 512:(j + 1) * 512],
                                                              start=(kc == 0), stop=(kc == nk - 1)),
                         reads=[r_TT, rW], writes=[rb])
                out.append((b_, rb))
            return out

        tiles = [(s, t) for s in range(2) for t in range(NT)] + [(2, 0)]
        cur_s = [-1]

        def loads(idx):
            s, t = tiles[idx]
            P = 128 if s < 2 else NS
            i = idx % 2
            g0 = tok0(s) + t * 128
            k.dma("sp", xin[i][0:P, :], xp[s, t * 128:(t + 1) * 128, :] if s < 2 else xs[:, :], writes=[r_xin[i]])
            for g in range(3):
                k.dma("sp", oin[i][0:P, g, :], OACC[g, g0:g0 + P, :], reads=[r_oacc], writes=[r_oin[i]])
            k.dma("sp", hgin[i][0:P, :], HGS[g0:g0 + P, :], reads=[r_hgs], writes=[r_hgin[i]])
            k.dma("sp", ggin[i][0:P, :], GG[g0:g0 + P, 1024:3072], reads=[r_gg], writes=[r_ggin[i]])

        loads(0)
        for idx, (s, t) in enumerate(tiles):
            P = 128 if s < 2 else NS
            i = idx % 2
            if s != cur_s[0]:
                cur_s[0] = s
                for dst_, c0 in ((G1, 2 * D), (SH2, 3 * D), (S2, 4 * D), (G2, 5 * D)):
                    if s < 2:
                        k.dma("sp", dst_[:], MODS[s:s + 1, c0:c0 + D].partition_broadcast(128), reads=[r_MODS], writes=[r_M])
                    else:
                        k.dma("sp", dst_[0:NS, :], MODS[2:2 + NS, c0:c0 + D], reads=[r_MODS], writes=[r_M])
                k.op("dve", lambda: nc.vector.scalar_tensor_tensor(S2[0:P, :], S2[0:P, :], 1.0, n2w[0:P, :], ALU.add, ALU.mult),
                     reads=[r_W], writes=[r_M])
            o0 = oin[i]
            k.op("dve", lambda: nc.vector.tensor_tensor(o0[0:P, 0, :], o0[0:P, 0, :], o0[0:P, 1, :], ALU.add), writes=[r_oin[i]])
            k.op("dve", lambda: nc.vector.tensor_tensor(o0[0:P, 0, :], o0[0:P, 0, :], o0[0:P, 2, :], ALU.add), writes=[r_oin[i]])
            ov = o0[0:P, 0, :].rearrange("p (h e) -> p h e", h=8)
            k.op("dve", lambda: nc.vector.reciprocal(rl[0:P, :], ov[:, :, 64]), reads=[r_oin[i]], writes=[r_rl])
            k.op("dve", lambda: nc.vector.tensor_tensor(att_b[0:P, :].rearrange("p (h e) -> p h e", h=8), ov[:, :, 0:64],
                                                        rl[0:P, :].unsqueeze(2).to_broadcast([P, 8, 64]), ALU.mult),
                 reads=[r_oin[i], r_rl], writes=[r_attb])
            transpose_to_TT(P, att_b, r_attb, 4)
            pA = mm_tok(P, Wa, 4, 2, r_W)
            for hb in range(2):
                b_, rb = pA[hb]
                k.op("dve", lambda: nc.vector.tensor_tensor(f1[0:P, hb * 512:(hb + 1) * 512], b_[0:P, :], ggin[i][0:P, hb * 512:(hb + 1) * 512], ALU.mult),
                     reads=[r_ggin[i]], writes=[rb, r_f1])
            transpose_to_TT(P, hgin[i], r_hgin[i], 8)
            pB = mm_tok(P, Wb, 8, 2, r_W)
            for hb in range(2):
                b_, rb = pB[hb]
                k.op("dve", lambda: nc.vector.tensor_tensor(x1[0:P, hb * 512:(hb + 1) * 512], b_[0:P, :],
                                                            ggin[i][0:P, 1024 + hb * 512:1024 + (hb + 1) * 512], ALU.mult),
                     reads=[r_ggin[i]], writes=[rb, r_x1])
            k.op("dve", lambda: nc.vector.tensor_tensor(yb[0:P, :], f1[0:P, :], x1[0:P, :], ALU.add), reads=[r_f1, r_x1], writes=[r_yb])
            transpose_to_TT(P, yb, r_yb, 8)
            pZ = mm_tok(P, Wo, 8, 2, r_W)
            for hb in range(2):
                b_, rb = pZ[hb]
                k.op("dve", lambda: nc.vector.tensor_tensor(x1[0:P, hb * 512:(hb + 1) * 512], b_[0:P, :], G1[0:P, hb * 512:(hb + 1) * 512], ALU.mult),
                     reads=[r_M], writes=[rb, r_x1])
            k.op("dve", lambda: nc.vector.tensor_tensor(x1[0:P, :], x1[0:P, :], xin[i][0:P, :], ALU.add), reads=[r_xin[i]], writes=[r_x1])
            if DEBUG and (idx == 0 or s == 2):
                k.dma("pool", dbg[0 if idx == 0 else 1, 1, 0:P, :], hgin[i][0:P, :], reads=[r_hgin[i]])
            if idx + 1 < len(tiles):
                loads(idx + 1)
            k.op("act", lambda: nc.scalar.activation(f1[0:P, :], x1[0:P, :], AF.Square, accum_out=ssq[0:P, :]),
                 reads=[r_x1], writes=[r_f1, r_ssq])
            rstd_from_ss(P, (ssq[0:P, :], r_ssq), D, None)
            k.op("dve", lambda: nc.vector.scalar_tensor_tensor(f1[0:P, :], x1[0:P, :], ssq[0:P, 0:1], S2[0:P, :], ALU.mult, ALU.mult),
                 reads=[r_x1, r_ssq, r_M], writes=[r_f1])
            k.op("dve", lambda: nc.vector.tensor_tensor(h2b[0:P, :], f1[0:P, :], SH2[0:P, :], ALU.add), reads=[r_f1, r_M], writes=[r_h2b])
            transpose_to_TT(P, h2b, r_h2b, 8)
            for e1 in ("pe", "act", "dve"):
                for e2 in ("pe", "act", "dve"):
                    if e1 != e2 and gather_end:
                        k._wait(e1, (k.sem[e2], gather_end[e2]))
            for qb in range(4):
                b_, rb = nb()
                for hh in range(4):
                    hp = qb * 4 + hh
                    for kc in range(8):
                        k.op("pe", lambda kc=kc: nc.tensor.matmul(b_[:, hh * 128:hh * 128 + P], Wq[:, kc, hp * 128:(hp + 1) * 128], TT[:, kc, 0:P],
                                                                  start=(kc == 0), stop=(kc == 7), skip_group_check=True),
                             reads=[r_TT, r_W], writes=[rb])
                k.op("act", lambda: nc.scalar.copy(q2T[:, qb * 4:(qb + 1) * 4, 0:P], b_[:, :].rearrange("p (h n) -> p h n", h=4)[:, :, 0:P]),
                     writes=[rb, r_q2T])
            for qb in range(4):
                b_, rb = nb()
                for hh in range(4):
                    hp = qb * 4 + hh
                    k.op("pe", lambda: nc.tensor.matmul(b_[0:P, hh * 128:(hh + 1) * 128], q2T[:, hp, 0:P], skt[:, hp % 2, :],
                                                        start=True, stop=True, skip_group_check=True),
                         reads=[r_q2T, r_W], writes=[rb])
                k.op("act", lambda: nc.scalar.copy(sc[0:P, qb * 4:(qb + 1) * 4, :], b_[0:P, :].rearrange("p (h n) -> p h n", h=4)),
                     writes=[rb, r_sc])
            for hp in range(16):
                k.op("dve", lambda: nc.vector.max(vals[0:P, hp, 0:8], sc[0:P, hp, :]), reads=[r_sc], writes=[r_vals])
                k.op("dve", lambda: nc.vector.max_index(idxu[0:P, hp, 0:8], vals[0:P, hp, 0:8], sc[0:P, hp, :]),
                     reads=[r_sc, r_vals], writes=[r_idxu])
                k.op("dve", lambda: nc.vector.match_replace(sc2[0:P, hp, :], vals[0:P, hp, 0:8], sc[0:P, hp, :], -1e30),
                     reads=[r_sc, r_vals], writes=[r_sc2])
                k.op("dve", lambda: nc.vector.max(vals[0:P, hp, 8:16], sc2[0:P, hp, :]), reads=[r_sc2], writes=[r_vals])
                k.op("dve", lambda: nc.vector.max_index(idxu[0:P, hp, 8:16], vals[0:P, hp, 8:16], sc2[0:P, hp, :]),
                     reads=[r_sc2, r_vals], writes=[r_idxu])
            k.op("dve", lambda: nc.vector.tensor_copy(idxf[0:P], idxu[0:P]), reads=[r_idxu], writes=[r_idxf])
            v4 = vals[0:P].rearrange("p (h two) j -> p h two j", two=2)
            k.op("dve", lambda: nc.vector.tensor_tensor(cand[0:P].rearrange("p h (a b) -> p h a b", a=16),
                                                        v4[:, :, 0, :].unsqueeze(3).to_broadcast([P, 8, 16, 16]),
                                                        v4[:, :, 1, :].unsqueeze(2).to_broadcast([P, 8, 16, 16]), ALU.add),
                 reads=[r_vals], writes=[r_cand])
            for h in range(8):
                k.op("dve", lambda: nc.vector.max(tv[0:P, h, 0:8], cand[0:P, h, :]), reads=[r_cand], writes=[r_tv])
                k.op("dve", lambda: nc.vector.max_index(posu[0:P, h, 0:8], tv[0:P, h, 0:8], cand[0:P, h, :]),
                     reads=[r_cand, r_tv], writes=[r_posu])
                k.op("dve", lambda: nc.vector.match_replace(cand2[0:P, h, :], tv[0:P, h, 0:8], cand[0:P, h, :], -1e30),
                     reads=[r_cand, r_tv], writes=[r_cand2])
                k.op("dve", lambda: nc.vector.max(tv[0:P, h, 8:16], cand2[0:P, h, :]), reads=[r_cand2], writes=[r_tv])
                k.op("dve", lambda: nc.vector.max_index(posu[0:P, h, 8:16], tv[0:P, h, 8:16], cand2[0:P, h, :]),
                     reads=[r_cand2, r_tv], writes=[r_posu])
            k.op("dve", lambda: nc.vector.tensor_single_scalar(pa_u[0:P], posu[0:P], 4, ALU.logical_shift_right), reads=[r_posu], writes=[r_pau])
            k.op("dve", lambda: nc.vector.tensor_copy(pa_f[0:P, 0], pa_u[0:P]), reads=[r_pau], writes=[r_paf])
            k.op("dve", lambda: nc.vector.tensor_single_scalar(pa_u[0:P], posu[0:P], 15, ALU.bitwise_and), reads=[r_posu], writes=[r_pau])
            k.op("dve", lambda: nc.vector.tensor_copy(pa_f[0:P, 1], pa_u[0:P]), reads=[r_pau], writes=[r_paf])
            i4 = idxf[0:P].rearrange("p (h two) j -> p h two j", two=2)
            for w_ in range(2):
                ohv = oh[0:P].rearrange("p h (j a) -> p h j a", j=16)
                k.op("dve", lambda: nc.vector.tensor_tensor(ohv, pa_f[0:P, w_].unsqueeze(3).to_broadcast([P, 8, 16, 16]),
                                                            iota16[0:P, :].unsqueeze(1).unsqueeze(1).to_broadcast([P, 8, 16, 16]), ALU.is_equal),
                     reads=[r_paf, r_W], writes=[r_oh])
                k.op("dve", lambda: nc.vector.tensor_tensor(ohv, ohv, i4[:, :, w_, :].unsqueeze(2).to_broadcast([P, 8, 16, 16]), ALU.mult),
                     reads=[r_idxf], writes=[r_oh])
                k.op("dve", lambda: nc.vector.tensor_reduce(isel[0:P, w_], ohv, AX.X, ALU.add), reads=[r_oh], writes=[r_isel])
            k.op("dve", lambda: nc.vector.scalar_tensor_tensor(eid_f[0:P, :], isel[0:P, 0].rearrange("p h j -> p (h j)"), 128.0,
                                                               isel[0:P, 1].rearrange("p h j -> p (h j)"), ALU.mult, ALU.add),
                 reads=[r_isel], writes=[r_eid])
            k.op("dve", lambda: nc.vector.tensor_copy(eid[0:P, :], eid_f[0:P, :]), writes=[r_eid])
            k.op("dve", lambda: nc.vector.tensor_tensor(gat[0:P], tv[0:P], tv[0:P, :, 0:1].to_broadcast([P, 8, 16]), ALU.subtract),
                 reads=[r_tv], writes=[r_gat])
            k.op("act", lambda: nc.scalar.activation(gat[0:P], gat[0:P], AF.Exp), writes=[r_gat])
            k.op("dve", lambda: nc.vector.tensor_reduce(gsm[0:P, :], gat[0:P], AX.X, ALU.add), reads=[r_gat], writes=[r_rl])
            k.op("dve", lambda: nc.vector.reciprocal(gsm[0:P, :], gsm[0:P, :]), writes=[r_rl])
            k.op("dve", lambda: nc.vector.tensor_tensor(gat[0:P], gat[0:P], gsm[0:P, :].unsqueeze(2).to_broadcast([P, 8, 16]), ALU.mult),
                 reads=[r_rl], writes=[r_gat])
            py = [nb(), nb()]
            gatf = gat[0:P].rearrange("p h j -> p (h j)")
            for e2 in ("pe", "act", "dve"):
                k._wait("pool", (k.sem[e2], k.cnt[e2]))

            def issue_gathers(grp):
                for j in range(grp * 4, grp * 4 + 4):
                    bi = j % NUV
                    k.dma("pool", uvb[bi][0:P, :], UV, reads=[r_eid, r_uv], writes=[r_uvb[bi]],
                          indirect=bass.IndirectOffsetOnAxis(ap=eid[0:P, j:j + 1], axis=0))

            def do_dots(grp):
                for j in range(grp * 4, grp * 4 + 4):
                    bi = j % NUV
                    k.op("dve", lambda: nc.vector.scalar_tensor_tensor(jb[0:P, :], uvb[bi][0:P, 0:D], 1.0, h2b[0:P, :], ALU.mult, ALU.mult,
                                                                       accum_out=dots[0:P, j:j + 1]),
                         reads=[r_uvb[bi], r_h2b], writes=[r_dotg[grp]])

            def do_coef(grp):
                gs = slice(grp * 4, grp * 4 + 4)
                rd, rc = r_dotg[grp], r_coefg[grp]
                k.op("dve", lambda: nc.vector.tensor_tensor(coef[0:P, gs], dots[0:P, gs], dots[0:P, gs], ALU.mult), reads=[rd], writes=[rc])
                k.op("dve", lambda: nc.vector.tensor_scalar(coef[0:P, gs], coef[0:P, gs], 0.044715, 1.0, ALU.mult, ALU.add), writes=[rc])
                k.op("dve", lambda: nc.vector.tensor_tensor(coef[0:P, gs], coef[0:P, gs], dots[0:P, gs], ALU.mult), reads=[rd], writes=[rc])
                k.op("act", lambda: nc.scalar.activation(coef[0:P, gs], coef[0:P, gs], AF.Sigmoid, scale=1.5957691216057308), writes=[rc])
                k.op("dve", lambda: nc.vector.tensor_tensor(coef[0:P, gs], coef[0:P, gs], dots[0:P, gs], ALU.mult), reads=[rd], writes=[rc])
                k.op("dve", lambda: nc.vector.tensor_tensor(coef[0:P, gs], coef[0:P, gs], gatf[:, gs], ALU.mult), reads=[r_gat], writes=[rc])

            def do_mm(grp):
                rc = r_coefg[grp]
                for j in range(grp * 4, grp * 4 + 4):
                    bi = j % NUV
                    di_ = j % 4
                    k.op("act", lambda: nc.scalar.mul(dg[di_][0:P, 0:P], ident_b[0:P, 0:P], coef[0:P, j:j + 1]),
                         reads=[rc, r_idb], writes=[r_dg[di_]])
                    for hb in range(2):
                        b_, rb = py[hb]
                        k.op("pe", lambda: nc.tensor.matmul(b_[0:P, :], dg[di_][0:P, 0:P], uvb[bi][0:P, D + hb * 512:D + (hb + 1) * 512],
                                                            start=(j == 0), stop=(j == 127)),
                             reads=[r_dg[di_], r_uvb[bi]], writes=[rb])

            issue_gathers(0)
            for grp in range(32):
                if grp + 1 < 32:
                    issue_gathers(grp + 1)
                do_dots(grp)
                if grp >= 1:
                    do_coef(grp - 1)
                    do_mm(grp - 1)
            do_coef(31)
            do_mm(31)
            for e2 in ("pe", "act", "dve"):
                gather_end[e2] = k.cnt[e2]
            io = idx % 2
            for hb in range(2):
                b_, rb = py[hb]
                k.op("dve", lambda: nc.vector.tensor_tensor(yo[io][0:P, hb * 512:(hb + 1) * 512], b_[0:P, :], G2[0:P, hb * 512:(hb + 1) * 512], ALU.mult),
                     reads=[r_M], writes=[rb, r_yo[io]])
            k.op("dve", lambda: nc.vector.tensor_tensor(yo[io][0:P, :], yo[io][0:P, :], x1[0:P, :], ALU.add), reads=[r_x1], writes=[r_yo[io]])
            if DEBUG and (idx == 0 or s == 2):
                di = 0 if idx == 0 else 1
                k.dma("pool", dbg[di, 0, 0:P, 0:512], att_b[0:P, :], reads=[r_attb])
                k.dma("pool", dbg[di, 2, 0:P, :], yb[0:P, :], reads=[r_yb])
                k.dma("sp", dbg[di, 3, 0:P, :], x1[0:P, :], reads=[r_x1])
                k.dma("pool", dbg[di, 4, 0:P, :], h2b[0:P, :], reads=[r_h2b])
                k.dma("sp", dbg[di, 5, 0:P, 0:128], coef[0:P, :], reads=r_coefg)
                k.dma("sp", dbg[di, 6, 0:P, 0:128], eid_f[0:P, :], reads=[r_eid])
                k.dma("sp", dbg[di, 7, 0:P, 0:128], dots[0:P, :], reads=r_dotg)
                k.dma("sp", dbg[di, 8, 0:P, 0:128], gat[0:P].rearrange("p h j -> p (h j)"), reads=[r_gat])
                k.dma("sp", dbg[di, 9, 0:P, 0:256], vals[0:P].rearrange("p a b -> p (a b)"), reads=[r_vals])
            k.dma("sp", y_p[s, t * 128:(t + 1) * 128, :] if s < 2 else y_s[:, :], yo[io][0:P, :], reads=[r_yo[io]])
        k.flush()
    k.finish()


def _consts():
    c = np.zeros((128, 1024), np.float32)
    j = np.arange(128)[:, None]
    i = np.arange(128)[None, :]
    c[:, 0:128] = np.eye(128, dtype=np.float32)
    c[:, 128:256] = (j <= i)
    c[:, 256:384] = (j >= i)
    same = (j // 64) == (i // 64)
    c[:, 384:512] = same * ((j <= i).astype(np.float32) - ((j % 64) <= 31).astype(np.float32))
    c[:, 512:640] = same * (j > i)
    s_ = np.arange(128)
    c[:, 640] = s_ < 64
    c[:, 641] = s_ >= 64
    c[:, 642] = (s_ < 64) & (s_ % 64 <= 31)
    c[:, 643] = (s_ >= 64) & (s_ % 64 <= 31)
    c[:, 768:896] = same * (j <= i)
    return c


def _selc():
    c = np.zeros((NS, 2064), np.float32)
    for b in range(NS):
        c[b, b * 128:(b + 1) * 128] = 1.0
    c[:, 2048:2064] = np.arange(16, dtype=np.float32)[None, :]
    return c


_CACHE = {}


def kernel(x_prompt, x_sample, cache_kv_w128, cache_kv_w512, cache_kv_w2048, state_hgrn, c_prompt, c_sample,
           w_ada, b_ada, norm1_w, norm2_w, w_in, q_norm_w, k_norm_w, hg_lb_logits, hg_norm_w, w_br_a, w_br_b,
           w_o, w_peer_q, peer_subkeys, peer_u, peer_v, _stop_after=99):
    f = lambda a: np.ascontiguousarray(np.asarray(a, dtype=np.float32))
    key = ("nc", _stop_after)
    if key not in _CACHE:
        _CACHE[key] = build_program(_stop_after)
    nc = _CACHE[key]
    caches = [f(cache_kv_w128)[0], f(cache_kv_w512)[0], f(cache_kv_w2048)[0]]
    shared = {
        "w_ada": f(w_ada)[0], "b_ada": f(b_ada).reshape(1, -1), "norm1_w": f(norm1_w).reshape(1, -1),
        "norm2_w": f(norm2_w).reshape(1, -1), "w_in": f(w_in)[0],
        "qk_w": np.ascontiguousarray(np.stack([np.tile(f(q_norm_w)[0], 8), np.tile(f(k_norm_w)[0], 8)])),
        "lb_log": f(hg_lb_logits), "hgn_w": np.ascontiguousarray(np.tile(f(hg_norm_w)[0], 8).reshape(1, -1)),
        "w_br_a": f(w_br_a)[0], "w_br_b": f(w_br_b)[0], "w_o": f(w_o)[0], "w_pq": f(w_peer_q)[0],
        "skT": np.ascontiguousarray(f(peer_subkeys)[0].transpose(0, 2, 1)),
        "peer_u": f(peer_u)[0], "peer_v": f(peer_v)[0], "cst": _consts(), "selc": _selc(),
    }
    xpf, xsf = f(x_prompt), f(x_sample)
    cp, cs = f(c_prompt), f(c_sample)
    st = f(state_hgrn)[0]
    in_maps = []
    for c in range(NCORES):
        m = dict(shared)
        m["xp"] = xpf[c * NSEQ:(c + 1) * NSEQ]
        m["xs"] = np.ascontiguousarray(xsf[c * NS:(c + 1) * NS, 0, :])
        m["cT"] = np.ascontiguousarray(np.concatenate([cp[c * NSEQ:(c + 1) * NSEQ], cs[c * NS:(c + 1) * NS]], 0).T)
        for g in range(3):
            m["ck%d" % g] = np.ascontiguousarray(
                caches[g][c * NS:(c + 1) * NS, 0::GROUPS[g][1]][:, :128].reshape(NS, 128, 2, 512))
        m["st_in"] = st[c * NS:(c + 1) * NS]
        in_maps.append(m)
    res = run_bass_kernel_spmd(nc, in_maps, core_ids=list(range(NCORES)))
    R = res.results
    cat = lambda n: np.concatenate([np.asarray(r[n]) for r in R], 0)
    y_prompt = cat("y_p")
    y_sample = cat("y_s").reshape(NCORES * NS, 1, D)
    outs = [y_prompt, y_sample]
    for g in range(3):
        outs.append(cat("kv%d_p" % g).reshape(1, NCORES * NSEQ, GROUPS[g][0], 2, 8, 64))
    outs.append(cat("hg_p")[None])
    for g in range(3):
        outs.append(cat("kv%d_s" % g).reshape(1, NCORES * NS, 1, 2, 8, 64))
    outs.append(cat("hg_s")[None])
    if DEBUG:
        global _DBG
        _DBG = np.asarray(R[0]["dbg"])
    return tuple(np.ascontiguousarray(o, dtype=np.float32) for o in outs)
```

```python
import contextlib
import numpy as np
import concourse.bass as bass
import concourse.mybir as mybir
from concourse.bass_utils import run_bass_kernel_spmd

F32 = mybir.dt.float32
BF16 = mybir.dt.bfloat16
I32 = mybir.dt.int32
U32 = mybir.dt.uint32
ALU = mybir.AluOpType
AF = mybir.ActivationFunctionType
AX = mybir.AxisListType

NCORES = 8
D = 1024
SEQ = 4096
NSEQ = 2
NS = 16
INW = 10752
GROUPS = ((128, 1), (512, 4), (2048, 16))
EPS = 1e-6
NT = SEQ // 128
NROWS = NSEQ + NS
DEBUG = False


class Res:
    __slots__ = ("w", "rs")

    def __init__(self):
        self.w = {}
        self.rs = {}


class K:
    def __init__(self, nc, es):
        self.nc = nc
        self.es = es
        self.eng = {"pe": nc.tensor, "act": nc.scalar, "dve": nc.vector, "pool": nc.gpsimd, "sp": nc.sync}
        self.sem = {}
        self.cnt = {}
        for e in ("pe", "act", "dve", "pool"):
            self.sem[e] = es.enter_context(nc.semaphore("c_" + e))
            self.cnt[e] = 0
        self.known = {e: {} for e in self.eng}
        self.dsem = {}
        self.dpos = {}
        for q, n in (("sp", 24), ("pool", 16), ("act", 8)):
            self.dsem[q] = [[es.enter_context(nc.semaphore("d_%s%d" % (q, i))), 0] for i in range(n)]
            self.dpos[q] = 0
        self.deferred = []

    def _wait(self, e, tok):
        s, v = tok
        if v <= 0:
            return
        if e == "pe" and s is self.sem["pe"]:
            return
        kn = self.known[e]
        if kn.get(id(s), 0) >= v:
            return
        self.eng[e].wait_ge(s, v)
        kn[id(s)] = v

    def _deps(self, e, reads, writes):
        for r in reads:
            for tok in r.w.values():
                self._wait(e, tok)
        for r in writes:
            for tok in r.w.values():
                self._wait(e, tok)
            for tok in r.rs.values():
                self._wait(e, tok)

    def _mark(self, tok, reads, writes, accum):
        s, v = tok
        for r in reads:
            r.rs[id(s)] = tok
        for r in writes:
            r.w = {id(s): tok}
            r.rs = {}
        for r in accum:
            r.w[id(s)] = tok

    def op(self, e, fn, reads=(), writes=(), accum=()):
        self._deps(e, reads, writes)
        ins = fn()
        self.cnt[e] += 1
        ins.then_inc(self.sem[e], 1)
        self._mark((self.sem[e], self.cnt[e]), reads, writes, accum)

    def dma(self, q, out, in_, reads=(), writes=(), accum=(), indirect=None):
        self._deps(q, reads, writes)
        slot = self.dsem[q][self.dpos[q] % len(self.dsem[q])]
        self.dpos[q] += 1
        self._wait(q, (slot[0], slot[1]))
        if indirect is not None:
            ins = self.eng[q].indirect_dma_start(out=out, out_offset=None, in_=in_, in_offset=indirect)
        else:
            ins = self.eng[q].dma_start(out=out, in_=in_)
        slot[1] += 16
        ins.then_inc(slot[0], 16)
        self._mark((slot[0], slot[1]), reads, writes, accum)

    def barrier(self):
        self.flush()
        toks = [(self.sem[e], self.cnt[e]) for e in self.sem]
        for q in self.dsem:
            toks += [(s, v) for s, v in self.dsem[q]]
        for e in self.eng:
            for tok in toks:
                if e != "pe" or tok[0] is not self.sem["pe"]:
                    self._wait(e, tok)

    def defer(self, fn):
        self.deferred.append(fn)

    def flush(self):
        d, self.deferred = self.deferred, []
        for fn in d:
            fn()

    def finish(self):
        self.flush()
        for q in self.dsem:
            for s, v in self.dsem[q]:
                self._wait("sp", (s, v))


def build_program(stop_after=99):
    nc = bass.Bass("TRN2", target_bir_lowering=False)
    es = contextlib.ExitStack()
    with es:
        _emit(nc, es, stop_after)
    return nc


def _emit(nc, es, stop_after):
    k = K(nc, es)

    def din(name, shape, dt=F32):
        return nc.dram_tensor(name, list(shape), dt, kind="ExternalInput").ap()

    def dout(name, shape, dt=F32):
        return nc.dram_tensor(name, list(shape), dt, kind="ExternalOutput").ap()

    def dscr(name, shape, dt):
        return nc.dram_tensor(name, list(shape), dt, kind="Internal").ap()

    def sb(name, shape, dt):
        return es.enter_context(nc.sbuf_tensor(name, list(shape), dt))

    xp = din("xp", [NSEQ, SEQ, D])
    xs = din("xs", [NS, D])
    cT = din("cT", [D, NROWS])
    ck = [din("ck%d" % g, [NS, 128, 2, 512]) for g in range(3)]
    st_in = din("st_in", [NS, 8, 128, 128])
    w_ada = din("w_ada", [D, 6 * D])
    b_ada = din("b_ada", [1, 6 * D])
    norm1_w = din("norm1_w", [1, D])
    norm2_w = din("norm2_w", [1, D])
    w_in = din("w_in", [D, INW])
    qk_w = din("qk_w", [2, 512])
    lb_log = din("lb_log", [2, D])
    hgn_w = din("hgn_w", [1, D])
    w_br_a = din("w_br_a", [512, D])
    w_br_b = din("w_br_b", [D, D])
    w_o = din("w_o", [D, D])
    w_pq = din("w_pq", [D, 2048])
    skT = din("skT", [2, 128, 128])
    peer_u = din("peer_u", [16384, D])
    peer_v = din("peer_v", [16384, D])
    cst = din("cst", [128, 128 * 8])
    selc = din("selc", [NS, 2064])

    y_p = dout("y_p", [NSEQ, SEQ, D])
    y_s = dout("y_s", [NS, D])
    kv_p = [dout("kv%d_p" % g, [NSEQ, GROUPS[g][0], 2, 512]) for g in range(3)]
    hg_p = dout("hg_p", [NSEQ, 8, 128, 128])
    kv_s = [dout("kv%d_s" % g, [NS, 2, 512]) for g in range(3)]
    hg_s = dout("hg_s", [NS, 8, 128, 128])
    dbg = dout("dbg", [2, 10, 128, D]) if DEBUG else None

    MODS = dscr("MODS", [NROWS, 6 * D], F32)
    NTOK = NSEQ * SEQ + 128
    QS = dscr("QS", [NTOK, 3, 512], BF16)
    KS = dscr("KS", [NTOK, 3, 512], BF16)
    VS = dscr("VS", [NTOK, 3, 520], BF16)

    cst_f = sb("cst_f", [128, 1024], F32)
    ident_b = sb("ident_b", [128, 128], BF16)
    r_cst = Res()
    k.dma("sp", cst_f[:], cst, writes=[r_cst])
    r_idb = Res()
    k.op("dve", lambda: nc.vector.tensor_copy(ident_b[:], cst_f[:, 0:128]), reads=[r_cst], writes=[r_idb])
    ident_f = cst_f[:, 0:128]

    psf = [es.enter_context(nc.psum_tensor("psf%d" % i, [128, 512], F32)) for i in range(7)]
    r_psf = [Res() for _ in range(7)]
    psb = es.enter_context(nc.psum_tensor("psb", [128, 1024], BF16))
    r_psb = Res()

    def rstd_from_ss(P, ss, n_elem, tmp):
        t, r = ss
        k.op("dve", lambda: nc.vector.tensor_scalar(t, t, 1.0 / n_elem, EPS, ALU.mult, ALU.add), writes=[r])
        k.op("act", lambda: nc.scalar.activation(t, t, AF.Sqrt), writes=[r])
        k.op("dve", lambda: nc.vector.reciprocal(t, t), writes=[r])

    with contextlib.ExitStack() as ph:
        def psb_(name, shape, dt):
            return ph.enter_context(nc.sbuf_tensor(name, list(shape), dt))
        cT_f = psb_("cT_f", [128, 8, NROWS], F32)
        cT_b = psb_("cT_b", [128, 8, NROWS], BF16)
        r_cT = Res()
        k.dma("sp", cT_f[:], cT.rearrange("(kc p) n -> p kc n", p=128), writes=[r_cT])
        k.op("act", lambda: nc.scalar.activation(cT_b[:], cT_f[:], AF.Silu), reads=[r_cT], writes=[r_cT])
        wa = [psb_("wa%d" % i, [128, 8, 512], BF16) for i in range(2)]
        r_wa = [Res(), Res()]
        ba = [psb_("ba%d" % i, [NROWS, 512], F32) for i in range(2)]
        r_ba = [Res(), Res()]
        mo = [psb_("mo%d" % i, [NROWS, 512], F32) for i in range(2)]
        r_mo = [Res(), Res()]
        r_MODS = Res()
        for c in range(12):
            i = c % 2
            cs = slice(c * 512, (c + 1) * 512)
            k.dma("pool", wa[i][:], w_ada[:, cs].rearrange("(kc p) n -> p kc n", p=128), writes=[r_wa[i]])
            k.dma("sp", ba[i][:], b_ada[:, cs].partition_broadcast(NROWS), writes=[r_ba[i]])
            for kc in range(8):
                k.op("pe", lambda kc=kc: nc.tensor.matmul(psf[i][0:NROWS, :], cT_b[:, kc, :], wa[i][:, kc, :],
                                                          start=(kc == 0), stop=(kc == 7)),
                     reads=[r_cT, r_wa[i]], writes=[r_psf[i]])
            k.op("dve", lambda: nc.vector.tensor_tensor(mo[i][:], psf[i][0:NROWS, :], ba[i][:], ALU.add),
                 reads=[r_ba[i]], writes=[r_psf[i], r_mo[i]])
            k.dma("sp", MODS[:, cs], mo[i][:], reads=[r_mo[i]], accum=[r_MODS])
        k.barrier()
    if stop_after <= 0:
        k.finish()
        return

    def tok0(s):
        return s * SEQ

    NTOKX = NSEQ * SEQ + 128
    GG = dscr("GG", [NTOKX, 3072], BF16)
    HGS = dscr("HGS", [NTOKX, D], BF16)
    OACC = dscr("OACC", [3, NTOKX, 520], F32)
    UV = dscr("UV", [16384, 2 * D], BF16)
    r_uv = Res()
    r_scr = Res()
    r_gg = Res()
    r_hgs = Res()
    r_oacc = Res()
    bank_ctr = [0]

    def nb():
        i = bank_ctr[0] % 7
        bank_ctr[0] += 1
        return psf[i], r_psf[i]

    maskc_b = sb("maskc_b", [128, 4, 128], BF16)
    maskp_b = sb("maskp_b", [128, 4, 128], BF16)
    r_mask = Res()
    for hh in range(4):
        k.op("dve", lambda hh=hh: nc.vector.tensor_copy(maskc_b[:, hh, :], cst_f[:, 128:256]), reads=[r_cst], writes=[r_mask])
        k.op("dve", lambda hh=hh: nc.vector.tensor_copy(maskp_b[:, hh, :], cst_f[:, 256:384]), reads=[r_cst], writes=[r_mask])
    L1 = cst_f[:, 384:512]
    L3 = cst_f[:, 512:640]
    R4 = cst_f[:, 640:644]
    MA = cst_f[:, 768:896]

    def proj(P, hT, ts_, W, c0, ncol512, r_hT, r_W):
        out = []
        for j in range(ncol512):
            b_, rb = nb()
            for kc in range(8):
                k.op("pe", lambda kc=kc: nc.tensor.matmul(b_[0:P, :], hT[:, kc, ts_], W[:, kc, c0 + j * 512:c0 + (j + 1) * 512],
                                                          start=(kc == 0), stop=(kc == 7)),
                     reads=[r_hT, r_W], writes=[rb])
            out.append((b_, rb))
        return out

    with contextlib.ExitStack() as seqscope:
        hT = seqscope.enter_context(nc.sbuf_tensor("hT", [128, 8, SEQ], BF16))
        r_hT = Res()
        for s in range(3):
            P = 128 if s < 2 else NS
            ntile = NT if s < 2 else 1

            def xsrc(t):
                return xp[s, t * 128:(t + 1) * 128, :] if s < 2 else xs[:, :]

            with contextlib.ExitStack() as ph:
                def psb_(name, shape, dt):
                    return ph.enter_context(nc.sbuf_tensor(name + "_s%d" % s, list(shape), dt))
                S1 = psb_("S1", [128, D], F32)
                SH1 = psb_("SH1", [128, D], F32)
                n1w = psb_("n1w", [128, D], F32)
                r_S1 = Res()
                r_n1w = Res()
                k.dma("sp", n1w[:], norm1_w.partition_broadcast(128), writes=[r_n1w])
                qkw = psb_("qkw", [128, 2, 512], F32)
                r_qkw = Res()
                k.dma("sp", qkw[:, 0, :], qk_w[0:1, :].partition_broadcast(128), writes=[r_qkw])
                k.dma("sp", qkw[:, 1, :], qk_w[1:2, :].partition_broadcast(128), writes=[r_qkw])
                k.op("dve", lambda: nc.vector.tensor_scalar(qkw[:, 0, :], qkw[:, 0, :], 0.125, None, ALU.mult), writes=[r_qkw])
                xt = [psb_("xt%d" % i, [128, D], F32) for i in range(2)]
                r_xt = [Res(), Res()]
                xm = psb_("xm", [128, D], F32)
                xb = psb_("xb", [128, D], BF16)
                r_xm = Res()
                r_xb = Res()
                junk = psb_("junk", [128, D], F32)
                r_junk = Res()
                ss1 = psb_("ss1", [128, 1], F32)
                r_ss1 = Res()
                Wg = psb_("Wg", [128, 8, 1536], BF16)
                r_Wg = Res()
                ss8 = psb_("ss8", [128, 8], F32)
                r_ss8 = Res()
                qn_b = [psb_("qn_b%d" % i, [128, 512], BF16) for i in range(2)]
                r_qn = [Res(), Res()]
                kn32 = [psb_("kn32%d" % i, [128, 512], F32) for i in range(2)]
                r_kn32 = [Res(), Res()]
                kn_b = [psb_("kn_b%d" % i, [128, 512], BF16) for i in range(2)]
                r_knb = [Res(), Res()]
                v32 = [psb_("v32%d" % i, [128, 512], F32) for i in range(2)]
                r_v32 = [Res(), Res()]
                vaug = [psb_("vaug%d" % i, [128, 8, 65], BF16) for i in range(2)]
                r_vaug = [Res(), Res()]
                for i in range(2):
                    k.op("pool", lambda i=i: nc.gpsimd.memset(vaug[i][:], 1.0), writes=[r_vaug[i]])

                if s < 2:
                    k.dma("sp", SH1[:], MODS[s:s + 1, 0:D].partition_broadcast(128), reads=[r_MODS], writes=[r_S1])
                    k.dma("sp", S1[:], MODS[s:s + 1, D:2 * D].partition_broadcast(128), reads=[r_MODS], writes=[r_S1])
                else:
                    k.dma("sp", SH1[0:NS, :], MODS[2:2 + NS, 0:D], reads=[r_MODS], writes=[r_S1])
                    k.dma("sp", S1[0:NS, :], MODS[2:2 + NS, D:2 * D], reads=[r_MODS], writes=[r_S1])
                k.op("dve", lambda: nc.vector.scalar_tensor_tensor(S1[0:P, :], S1[0:P, :], 1.0, n1w[0:P, :], ALU.add, ALU.mult),
                     reads=[r_n1w], writes=[r_S1])

                k.dma("sp", xt[0][0:P, :], xsrc(0), writes=[r_xt[0]])
                for t in range(ntile):
                    i = t % 2
                    if t + 1 < ntile:
                        k.dma("sp", xt[1 - i][0:P, :], xsrc(t + 1), writes=[r_xt[1 - i]])
                    k.op("act", lambda: nc.scalar.activation(junk[0:P, :], xt[i][0:P, :], AF.Square, accum_out=ss1[0:P, :]),
                         reads=[r_xt[i]], writes=[r_junk, r_ss1])
                    rstd_from_ss(P, (ss1[0:P, :], r_ss1), D, None)
                    k.op("dve", lambda: nc.vector.scalar_tensor_tensor(xm[0:P, :], xt[i][0:P, :], ss1[0:P, 0:1], S1[0:P, :],
                                                                       ALU.mult, ALU.mult),
                         reads=[r_xt[i], r_ss1, r_S1], writes=[r_xm])
                    k.op("dve", lambda: nc.vector.tensor_tensor(xb[0:P, :], xm[0:P, :], SH1[0:P, :], ALU.add),
                         reads=[r_xm, r_S1], writes=[r_xb])
                    for kc in range(8):
                        k.op("pe", lambda kc=kc: nc.tensor.transpose(psb[:, kc * 128:kc * 128 + P], xb[0:P, kc * 128:(kc + 1) * 128],
                                                                     ident_b[0:P, 0:P]),
                             reads=[r_xb, r_idb], writes=[r_psb])
                    k.op("act", lambda: nc.scalar.copy(hT[:, :, t * 128:t * 128 + P],
                                                       psb[:].rearrange("p (kc n) -> p kc n", kc=8)[:, :, 0:P]),
                         writes=[r_psb, r_hT])

                for g in range(3):
                    win = GROUPS[g][0]
                    for part in range(3):
                        c0 = part * 1536 + g * 512
                        k.dma("pool", Wg[:, :, part * 512:(part + 1) * 512],
                              w_in[:, c0:c0 + 512].rearrange("(kc p) n -> p kc n", p=128), writes=[r_Wg])
                    for t in range(ntile):
                        i = t % 2
                        ts_ = slice(t * 128, t * 128 + P)
                        g0 = tok0(s) + t * 128
                        pr = proj(P, hT, ts_, Wg, 0, 3, r_hT, r_Wg)
                        bank = [p_[0] for p_ in pr]
                        rbank = [p_[1] for p_ in pr]
                        for part in range(2):
                            ps_ = bank[part]
                            k.op("act", lambda: nc.scalar.activation(junk[0:P, 0:512], ps_[0:P, :], AF.Square),
                                 writes=[rbank[part], r_junk])
                            k.op("dve", lambda: nc.vector.tensor_reduce(ss8[0:P, :], junk[0:P, 0:512].rearrange("p (h e) -> p h e", h=8),
                                                                        AX.X, ALU.add),
                                 reads=[r_junk], writes=[r_ss8])
                            rstd_from_ss(P, (ss8[0:P, :], r_ss8), 64, None)
                            dst32 = junk if part == 0 else kn32[i]
                            rdst = r_junk if part == 0 else r_kn32[i]
                            k.op("dve", lambda: nc.vector.tensor_tensor(
                                dst32[0:P, 0:512].rearrange("p (h e) -> p h e", h=8),
                                ps_[0:P, :].rearrange("p (h e) -> p h e", h=8),
                                ss8[0:P, :].unsqueeze(2).to_broadcast([P, 8, 64]), ALU.mult),
                                reads=[r_ss8], writes=[rbank[part], rdst])
                            if part == 0:
                                k.op("dve", lambda: nc.vector.tensor_tensor(qn_b[i][0:P, :], junk[0:P, 0:512], qkw[0:P, 0, :], ALU.mult),
                                     reads=[r_junk, r_qkw], writes=[r_qn[i]])
                            else:
                                k.op("dve", lambda: nc.vector.tensor_tensor(kn32[i][0:P, :], kn32[i][0:P, :], qkw[0:P, 1, :], ALU.mult),
                                     reads=[r_qkw], writes=[r_kn32[i]])
                                k.op("pool", lambda: nc.gpsimd.tensor_copy(kn_b[i][0:P, :], kn32[i][0:P, :]),
                                     reads=[r_kn32[i]], writes=[r_knb[i]])
                        k.op("act", lambda: nc.scalar.copy(v32[i][0:P, :], bank[2][0:P, :]), writes=[rbank[2], r_v32[i]])
                        k.op("pool", lambda: nc.gpsimd.tensor_copy(vaug[i][0:P, :, 0:64],
                                                                    v32[i][0:P, :].rearrange("p (h e) -> p h e", h=8)),
                             reads=[r_v32[i]], writes=[r_vaug[i]])
                        k.flush()

                        def stores(i=i, g=g, g0=g0, t=t, P=P, s=s, win=win):
                            k.dma("sp", QS[g0:g0 + P, g, :], qn_b[i][0:P, :], reads=[r_qn[i]], accum=[r_scr])
                            k.dma("sp", KS[g0:g0 + P, g, :], kn_b[i][0:P, :], reads=[r_knb[i]], accum=[r_scr])
                            k.dma("sp", VS[g0:g0 + P, g, :], vaug[i][0:P, :, :].rearrange("p h e -> p (h e)"),
                                  reads=[r_vaug[i]], accum=[r_scr])
                            if s < 2:
                                r0 = t * 128 - (SEQ - win)
                                if r0 >= 0:
                                    k.dma("sp", kv_p[g][s, r0:r0 + 128, 0, :], kn32[i][:], reads=[r_kn32[i]])
                                    k.dma("sp", kv_p[g][s, r0:r0 + 128, 1, :], v32[i][:], reads=[r_v32[i]])
                            else:
                                k.dma("sp", kv_s[g][:, 0, :], kn32[i][0:P, :], reads=[r_kn32[i]])
                                k.dma("sp", kv_s[g][:, 1, :], v32[i][0:P, :], reads=[r_v32[i]])
                        k.defer(stores)
                    k.flush()
                k.barrier()
            if stop_after <= 1:
                continue

            with contextlib.ExitStack() as ph:
                def psb_(name, shape, dt):
                    return ph.enter_context(nc.sbuf_tensor(name + "_s%d" % s, list(shape), dt))
                Wt = psb_("Wt", [128, 8, 3072], BF16)
                r_Wt = Res()
                for j, c0 in enumerate((7680, 8704, 9728)):
                    k.dma("pool", Wt[:, :, j * 1024:(j + 1) * 1024],
                          w_in[:, c0:c0 + 1024].rearrange("(kc p) n -> p kc n", p=128), writes=[r_Wt])
                ggb = [psb_("ggb%d" % i, [128, 3072], BF16) for i in range(2)]
                r_ggb = [Res(), Res()]
                for t in range(ntile):
                    i = t % 2
                    ts_ = slice(t * 128, t * 128 + P)
                    g0 = tok0(s) + t * 128
                    for j in range(6):
                        pr = proj(P, hT, ts_, Wt, j * 512, 1, r_hT, r_Wt)
                        b_, rb = pr[0]
                        fn_ = AF.Silu if j < 2 else AF.Sigmoid
                        k.op("act", lambda: nc.scalar.activation(ggb[i][0:P, j * 512:(j + 1) * 512], b_[0:P, :], fn_),
                             writes=[rb, r_ggb[i]])
                    k.flush()
                    k.defer(lambda i=i, g0=g0, P=P: k.dma("sp", GG[g0:g0 + P, :], ggb[i][0:P, :], reads=[r_ggb[i]], accum=[r_gg]))
                k.barrier()
            if stop_after <= 2:
                continue

            with contextlib.ExitStack() as ph:
                def psb_(name, shape, dt):
                    return ph.enter_context(nc.sbuf_tensor(name + "_s%d" % s, list(shape), dt))
                Wh = psb_("Wh", [128, 8, 3072], BF16)
                r_Wh = Res()
                for j in range(3):
                    c0 = 4608 + j * 1024
                    k.dma("pool", Wh[:, :, j * 1024:(j + 1) * 1024],
                          w_in[:, c0:c0 + 1024].rearrange("(kc p) n -> p kc n", p=128), writes=[r_Wh])
                lbt = psb_("lbt", [128, D], F32)
                omlt = psb_("omlt", [128, D], F32)
                hgw = psb_("hgw", [128, D], F32)
                r_lb = Res()
                k.dma("sp", lbt[:], lb_log[0:1, :].partition_broadcast(128), writes=[r_lb])
                k.dma("sp", omlt[:], lb_log[1:2, :].partition_broadcast(128), writes=[r_lb])
                k.dma("sp", hgw[:], hgn_w.partition_broadcast(128), writes=[r_lb])
                k.op("dve", lambda: nc.vector.tensor_tensor(lbt[:], lbt[:], omlt[:], ALU.subtract), writes=[r_lb])
                k.op("act", lambda: nc.scalar.activation(lbt[:], lbt[:], AF.Sigmoid), writes=[r_lb])
                k.op("dve", lambda: nc.vector.tensor_scalar(omlt[:], lbt[:], -1.0, 1.0, ALU.mult, ALU.add), writes=[r_lb])
                logf = psb_("logf", [128, D], F32)
                kk = psb_("kk", [128, D], F32)
                et = psb_("et", [128, D], F32)
                t2 = psb_("t2", [128, D], F32)
                r_logf, r_kk, r_et, r_t2 = Res(), Res(), Res(), Res()
                kt_b = psb_("kt_b", [128, D], BF16)
                kh_b = psb_("kh_b", [128, D], BF16)
                qt_b = psb_("qt_b", [128, D], BF16)
                v_b = psb_("v_b", [128, D], BF16)
                r_ktb, r_khb, r_qtb, r_vb = Res(), Res(), Res(), Res()
                gt_b = psb_("gt_b", [128, D], BF16)
                r_gtb = Res()
                hg_b = [psb_("hg_b%d" % i, [128, D], BF16) for i in range(2)]
                r_hgb = [Res(), Res()]
                ss8 = psb_("hss8", [128, 8], F32)
                r_ss8 = Res()
                if s < 2:
                    qT = psb_("qT", [128, 8, 128], BF16)
                    kT = psb_("kT", [128, 8, 128], BF16)
                    r_qT, r_kT = Res(), Res()
                    Abd = psb_("Abd", [128, 8, 128], BF16)
                    r_Abd = Res()
                    k.op("pool", lambda: nc.gpsimd.memset(Abd[:], 0.0), writes=[r_Abd])
                    Sm = psb_("Sm", [128, 8, 128], F32)
                    St = psb_("St", [128, 8, 128], F32)
                    Sb = [psb_("Sb%d" % i, [128, 8, 128], BF16) for i in range(2)]
                    r_Sm, r_St = Res(), Res()
                    r_Sb = [Res(), Res()]
                    eb = psb_("eb", [128, 8, 4], F32)
                    r_eb = Res()
                    k.op("pool", lambda: nc.gpsimd.memset(Sm[:], 0.0), writes=[r_Sm])
                    k.op("pool", lambda: nc.gpsimd.memset(Sb[0][:], 0.0), writes=[r_Sb[0]])
                else:
                    selc_sb = psb_("selc_sb", [NS, 2048], F32)
                    k.dma("sp", selc_sb[:], selc[:, 0:2048], writes=[r_cst])
                    fT = psb_("fT", [128, 3, 8, NS], F32)
                    r_fT = Res()
                    v32s = psb_("v32s", [NS, D], F32)
                    r_v32s = Res()
                    QZ = psb_("QZ", [128, 8, NS * NS], F32)
                    r_QZ = Res()
                    k.op("pool", lambda: nc.gpsimd.memset(QZ[:], 0.0), writes=[r_QZ])
                    S0b = [psb_("S0b%d" % i, [128, 8, 128], F32) for i in range(2)]
                    r_S0b = [Res(), Res()]
                    Sn = [psb_("Sn%d" % i, [128, 8, 128], F32) for i in range(2)]
                    r_Sn = [Res(), Res()]

                def hg_epilogue(P, po, i, g0):
                    for hb in range(2):
                        b_, rb = po[hb]
                        k.op("act", lambda: nc.scalar.activation(t2[0:P, hb * 512:(hb + 1) * 512], b_[0:P, :], AF.Square),
                             writes=[rb, r_t2])
                    k.op("dve", lambda: nc.vector.tensor_reduce(ss8[0:P, :], t2[0:P, :].rearrange("p (h e) -> p h e", h=8), AX.X, ALU.add),
                         reads=[r_t2], writes=[r_ss8])
                    rstd_from_ss(P, (ss8[0:P, :], r_ss8), 128, None)
                    for hb in range(2):
                        b_, rb = po[hb]
                        k.op("dve", lambda: nc.vector.tensor_tensor(
                            t2[0:P, hb * 512:(hb + 1) * 512].rearrange("p (h e) -> p h e", h=4),
                            b_[0:P, :].rearrange("p (h e) -> p h e", h=4),
                            ss8[0:P, hb * 4:(hb + 1) * 4].unsqueeze(2).to_broadcast([P, 4, 128]), ALU.mult),
                            reads=[r_ss8], writes=[rb, r_t2])
                    k.op("dve", lambda: nc.vector.tensor_tensor(t2[0:P, :], t2[0:P, :], gt_b[0:P, :], ALU.mult),
                         reads=[r_gtb], writes=[r_t2])
                    k.op("dve", lambda: nc.vector.tensor_tensor(hg_b[i][0:P, :], t2[0:P, :], hgw[0:P, :], ALU.mult),
                         reads=[r_t2, r_lb], writes=[r_hgb[i]])
                    k.flush()
                    k.defer(lambda: k.dma("sp", HGS[g0:g0 + P, :], hg_b[i][0:P, :], reads=[r_hgb[i]], accum=[r_hgs]))

                for t in range(ntile):
                    i = t % 2
                    ts_ = slice(t * 128, t * 128 + P)
                    g0 = tok0(s) + t * 128
                    k.dma("sp", gt_b[0:P, :], GG[g0:g0 + P, 0:D], reads=[r_gg], writes=[r_gtb])
                    pr = proj(P, hT, ts_, Wh, 1024, 2, r_hT, r_Wh)
                    for hb in range(2):
                        b_, rb = pr[hb]
                        k.op("act", lambda: nc.scalar.activation(logf[0:P, hb * 512:(hb + 1) * 512], b_[0:P, :], AF.Sigmoid),
                             writes=[rb, r_logf])
                    k.op("dve", lambda: nc.vector.tensor_tensor(logf[0:P, :], logf[0:P, :], omlt[0:P, :], ALU.mult), reads=[r_lb], writes=[r_logf])
                    k.op("dve", lambda: nc.vector.tensor_tensor(logf[0:P, :], logf[0:P, :], lbt[0:P, :], ALU.add), reads=[r_lb], writes=[r_logf])
                    k.op("dve", lambda: nc.vector.tensor_scalar(kk[0:P, :], logf[0:P, :], -1.0, 1.0, ALU.mult, ALU.add),
                         reads=[r_logf], writes=[r_kk])
                    if s < 2:
                        k.op("act", lambda: nc.scalar.activation(logf[0:P, :], logf[0:P, :], AF.Ln), reads=[r_kk], writes=[r_logf])
                    if s == 2:
                        k.op("pool", lambda: nc.gpsimd.tensor_copy(et[0:P, :], logf[0:P, :]), reads=[r_logf], writes=[r_et])
                        pq = proj(P, hT, ts_, Wh, 0, 2, r_hT, r_Wh)
                        for hb in range(2):
                            b_, rb = pq[hb]
                            k.op("act", lambda: nc.scalar.activation(t2[0:P, hb * 512:(hb + 1) * 512], b_[0:P, :], AF.Silu),
                                 writes=[rb, r_t2])
                        pv = proj(P, hT, ts_, Wh, 2048, 2, r_hT, r_Wh)
                        for hb in range(2):
                            b_, rb = pv[hb]
                            k.op("act", lambda: nc.scalar.copy(v32s[0:P, hb * 512:(hb + 1) * 512], b_[0:P, :]), writes=[rb, r_v32s])
                        for qi, (src_, rs_) in enumerate(((et, r_et), (kk, r_kk), (t2, r_t2))):
                            b_, rb = nb()
                            for h in range(8):
                                k.op("pe", lambda h=h: nc.tensor.transpose(b_[:, h * NS:(h + 1) * NS], src_[0:P, h * 128:(h + 1) * 128],
                                                                            ident_f[0:P, 0:P]),
                                     reads=[rs_, r_cst], writes=[rb])
                            k.op("act", lambda: nc.scalar.copy(fT[:, qi, :, :], b_[:, 0:8 * NS].rearrange("p (h b) -> p h b", h=8)),
                                 writes=[rb, r_fT])
                        k.op("dve", lambda: nc.vector.tensor_copy(QZ[:, :, 0:NS * NS:NS + 1], fT[:, 2, :, :]), reads=[r_fT], writes=[r_QZ])
                        po = [(psf[5], r_psf[5]), (psf[6], r_psf[6])]
                        for b in range(NS):
                            ib = b % 2
                            k.dma("sp", S0b[ib][:], st_in[b].rearrange("h k v -> k h v"), writes=[r_S0b[ib]])
                            pvb = [(psf[2 * ib], r_psf[2 * ib]), (psf[2 * ib + 1], r_psf[2 * ib + 1])]
                            for hb in range(2):
                                b_, rb = pvb[hb]
                                k.op("pe", lambda: nc.tensor.matmul(b_[:, :], selc_sb[0:NS, b * 128:(b + 1) * 128], v32s[0:NS, hb * 512:(hb + 1) * 512],
                                                                    start=True, stop=True),
                                     reads=[r_v32s, r_cst], writes=[rb])
                            k.op("dve", lambda: nc.vector.tensor_tensor(Sn[ib][:], S0b[ib][:],
                                                                        fT[:, 0, :, b:b + 1].to_broadcast([128, 8, 128]), ALU.mult),
                                 reads=[r_S0b[ib], r_fT], writes=[r_Sn[ib]])
                            for hb in range(2):
                                b_, rb = pvb[hb]
                                k.op("dve", lambda: nc.vector.tensor_tensor(
                                    S0b[ib][:, hb * 4:(hb + 1) * 4, :], b_[:, :].rearrange("p (h v) -> p h v", h=4),
                                    fT[:, 1, hb * 4:(hb + 1) * 4, b:b + 1].to_broadcast([128, 4, 128]), ALU.mult),
                                    reads=[r_fT], writes=[rb, r_S0b[ib]])
                            k.op("dve", lambda: nc.vector.tensor_tensor(Sn[ib][:], Sn[ib][:], S0b[ib][:], ALU.add),
                                 reads=[r_S0b[ib]], writes=[r_Sn[ib]])
                            k.dma("sp", hg_s[b].rearrange("h k v -> k h v"), Sn[ib][:], reads=[r_Sn[ib]])
                            for h in range(8):
                                b_, rb = po[h // 4]
                                k.op("pe", lambda h=h: nc.tensor.matmul(b_[0:NS, (h % 4) * 128:(h % 4 + 1) * 128],
                                                                        QZ[:, h, b * NS:(b + 1) * NS], Sn[ib][:, h, :],
                                                                        start=(b == 0 and h % 4 == 0), stop=(b == NS - 1),
                                                                        skip_group_check=True),
                                     reads=[r_QZ, r_Sn[ib]], writes=[rb])
                        hg_epilogue(P, po, i, g0)
                        continue
                    d1 = [nb(), nb()]
                    d3 = [nb(), nb()]
                    for hb in range(2):
                        k.op("pe", lambda: nc.tensor.matmul(d1[hb][0][:, :], L1, logf[:, hb * 512:(hb + 1) * 512], start=True, stop=True),
                             reads=[r_logf, r_cst], writes=[d1[hb][1]])
                        k.op("pe", lambda: nc.tensor.matmul(d3[hb][0][:, :], L3, logf[:, hb * 512:(hb + 1) * 512], start=True, stop=True),
                             reads=[r_logf, r_cst], writes=[d3[hb][1]])
                    for hb in range(2):
                        k.op("act", lambda: nc.scalar.activation(et[:, hb * 512:(hb + 1) * 512], d1[hb][0][:, :], AF.Exp, scale=-1.0),
                             writes=[d1[hb][1], r_et])
                    k.op("dve", lambda: nc.vector.tensor_tensor(kt_b[:], kk[:], et[:], ALU.mult), reads=[r_kk, r_et], writes=[r_ktb])
                    for hb in range(2):
                        k.op("act", lambda: nc.scalar.activation(et[:, hb * 512:(hb + 1) * 512], d3[hb][0][:, :], AF.Exp),
                             writes=[d3[hb][1], r_et])
                    k.op("dve", lambda: nc.vector.tensor_tensor(kh_b[:], kk[:], et[:], ALU.mult), reads=[r_kk, r_et], writes=[r_khb])
                    for hb in range(2):
                        k.op("act", lambda: nc.scalar.activation(et[:, hb * 512:(hb + 1) * 512], d1[hb][0][:, :], AF.Exp),
                             writes=[d1[hb][1], r_et])
                    pq = proj(P, hT, ts_, Wh, 0, 2, r_hT, r_Wh)
                    for hb in range(2):
                        b_, rb = pq[hb]
                        k.op("act", lambda: nc.scalar.activation(t2[:, hb * 512:(hb + 1) * 512], b_[:, :], AF.Silu), writes=[rb, r_t2])
                    k.op("dve", lambda: nc.vector.tensor_tensor(qt_b[:], t2[:], et[:], ALU.mult), reads=[r_t2, r_et], writes=[r_qtb])
                    pv = proj(P, hT, ts_, Wh, 2048, 2, r_hT, r_Wh)
                    for hb in range(2):
                        b_, rb = pv[hb]
                        k.op("act", lambda: nc.scalar.copy(v_b[:, hb * 512:(hb + 1) * 512], b_[:, :]), writes=[rb, r_vb])
                    be, rbe = nb()
                    for h in range(8):
                        k.op("pe", lambda h=h: nc.tensor.matmul(be[:, h * 4:(h + 1) * 4], logf[:, h * 128:(h + 1) * 128], R4,
                                                                start=(h == 0), stop=(h == 7), skip_group_check=True),
                             reads=[r_logf, r_cst], writes=[rbe])
                    k.op("act", lambda: nc.scalar.activation(eb[:], be[:, 0:32].rearrange("p (h c) -> p h c", h=8), AF.Exp),
                         writes=[rbe, r_eb])
                    for src_, rs_, dst_, rd_ in ((qt_b, r_qtb, qT, r_qT), (kt_b, r_ktb, kT, r_kT)):
                        for h in range(8):
                            k.op("pe", lambda h=h: nc.tensor.transpose(psb[:, h * 128:(h + 1) * 128], src_[:, h * 128:(h + 1) * 128], ident_b[:]),
                                 reads=[rs_, r_idb], writes=[r_psb])
                        k.op("act", lambda: nc.scalar.copy(dst_[:], psb[:].rearrange("p (h n) -> p h n", h=8)), writes=[r_psb, rd_])
                    pa = [nb(), nb()]
                    for h in range(8):
                        b_, rb = pa[h // 4]
                        c_ = (h % 4) * 128
                        k.op("pe", lambda h=h: nc.tensor.matmul(b_[0:64, c_:c_ + 64], kT[:, h, 0:64], qT[:, h, 0:64], start=True, stop=True,
                                                                skip_group_check=True),
                             reads=[r_kT, r_qT], writes=[rb])
                        k.op("pe", lambda h=h: nc.tensor.matmul(b_[:, c_ + 64:c_ + 128], kT[:, h, :], qT[:, h, 64:128], start=True, stop=True,
                                                                skip_group_check=True),
                             reads=[r_kT, r_qT], writes=[rb])
                    for hb in range(2):
                        b_, rb = pa[hb]
                        bv = b_[:, :].rearrange("p (h t) -> p h t", h=4)
                        k.op("dve", lambda: nc.vector.tensor_tensor(Abd[0:64, hb * 4:(hb + 1) * 4, 0:64], bv[0:64, :, 0:64],
                                                                    MA[0:64, 0:64].unsqueeze(1).to_broadcast([64, 4, 64]), ALU.mult),
                             reads=[r_cst], writes=[rb, r_Abd])
                        k.op("dve", lambda: nc.vector.tensor_tensor(Abd[64:128, hb * 4:(hb + 1) * 4, 64:128], bv[64:128, :, 64:128],
                                                                    MA[64:128, 64:128].unsqueeze(1).to_broadcast([64, 4, 64]), ALU.mult),
                             reads=[r_cst], writes=[rb, r_Abd])
                    k.op("dve", lambda: nc.vector.tensor_tensor(Sb[0][:], Sm[:], eb[:, :, 2:3].to_broadcast([128, 8, 128]), ALU.mult),
                         reads=[r_Sm, r_eb], writes=[r_Sb[0]])
                    for c in range(2):
                        src_S, rsrc = (Sm, r_Sm) if c == 0 else (St, r_St)
                        dst_S, rdst = (St, r_St) if c == 0 else (Sm, r_Sm)
                        psn = [nb(), nb()]
                        for h in range(8):
                            b_, rb = psn[h // 4]
                            c_ = (h % 4) * 128
                            k.op("pe", lambda h=h: nc.tensor.matmul(b_[:, c_:c_ + 128], kh_b[c * 64:(c + 1) * 64, h * 128:(h + 1) * 128],
                                                                    v_b[c * 64:(c + 1) * 64, h * 128:(h + 1) * 128], start=True, stop=True,
                                                                    skip_group_check=True),
                                 reads=[r_khb, r_vb], writes=[rb])
                        k.op("dve", lambda: nc.vector.tensor_tensor(dst_S[:], src_S[:], eb[:, :, c:c + 1].to_broadcast([128, 8, 128]), ALU.mult),
                             reads=[rsrc, r_eb], writes=[rdst])
                        for hb in range(2):
                            b_, rb = psn[hb]
                            k.op("dve", lambda: nc.vector.tensor_tensor(dst_S[:, hb * 4:(hb + 1) * 4, :], dst_S[:, hb * 4:(hb + 1) * 4, :],
                                                                        b_[:, :].rearrange("p (h v) -> p h v", h=4), ALU.add),
                                 writes=[rb, rdst])
                        if c == 0:
                            k.op("dve", lambda: nc.vector.tensor_tensor(Sb[1][:], St[:], eb[:, :, 3:4].to_broadcast([128, 8, 128]), ALU.mult),
                                 reads=[r_St, r_eb], writes=[r_Sb[1]])
                    po = [nb(), nb()]
                    for h in range(8):
                        b_, rb = po[h // 4]
                        c_ = (h % 4) * 128
                        k.op("pe", lambda h=h: nc.tensor.matmul(b_[:, c_:c_ + 128], Abd[:, h, :], v_b[:, h * 128:(h + 1) * 128],
                                                                start=True, stop=False, skip_group_check=True),
                             reads=[r_Abd, r_vb], writes=[rb])
                        k.op("pe", lambda h=h: nc.tensor.matmul(b_[0:64, c_:c_ + 128], qT[:, h, 0:64], Sb[0][:, h, :],
                                                                start=False, stop=False, skip_group_check=True),
                             reads=[r_qT, r_Sb[0]], writes=[rb])
                        k.op("pe", lambda h=h: nc.tensor.matmul(b_[64:128, c_:c_ + 128], qT[:, h, 64:128], Sb[1][:, h, :],
                                                                start=False, stop=True, skip_group_check=True),
                             reads=[r_qT, r_Sb[1]], writes=[rb])
                    hg_epilogue(P, po, i, g0)
                k.flush()
                if s < 2:
                    k.dma("sp", hg_p[s].rearrange("h k v -> k h v"), Sm[:], reads=[r_Sm])
                k.barrier()
    if stop_after <= 3:
        k.finish()
        return


    with contextlib.ExitStack() as ph:
        def psb_(name, shape, dt):
            return ph.enter_context(nc.sbuf_tensor(name, list(shape), dt))
        qblk = [psb_("qblk%d" % i, [128, 512], BF16) for i in range(2)]
        kblk = [psb_("kblk%d" % i, [128, 512], BF16) for i in range(2)]
        vblk = [psb_("vblk%d" % i, [128, 8, 65], BF16) for i in range(2)]
        r_qblk, r_kblk, r_vblk = [Res(), Res()], [Res(), Res()], [Res(), Res()]
        qTa = psb_("qTa", [128, 4, 128], BF16)
        kTa = [psb_("kTa%d" % i, [128, 4, 128], BF16) for i in range(2)]
        r_qTa = Res()
        r_kTa = [Res(), Res()]
        pT = [psb_("pT%d" % i, [128, 4, 128], BF16) for i in range(4)]
        r_pT = [Res() for _ in range(4)]
        oac = [psb_("oac%d" % i, [128, 520], F32) for i in range(2)]
        r_oac = [Res(), Res()]
        for i in range(2):
            k.op("pool", lambda i=i: nc.gpsimd.memset(qblk[i][:], 0.0), writes=[r_qblk[i]])
            k.op("pool", lambda i=i: nc.gpsimd.memset(kblk[i][:], 0.0), writes=[r_kblk[i]])
            k.op("pool", lambda i=i: nc.gpsimd.memset(vblk[i][:], 1.0), writes=[r_vblk[i]])
        blk_ctr = [0]
        for c_ in range(8):
            rs_ = slice(c_ * 2048, (c_ + 1) * 2048)
            k.dma("pool", UV[rs_, 0:D], peer_u[rs_, :], accum=[r_uv])
            k.dma("pool", UV[rs_, D:2 * D], peer_v[rs_, :], accum=[r_uv])

        def attn_block(load_cur, has_prev, ip, store):
            n_ = blk_ctr[0]
            blk_ctr[0] += 1
            ic = 1 - ip
            load_cur(ic)
            for src_, rs_, dst_, rd_ in ((qblk[ic], r_qblk[ic], qTa, r_qTa), (kblk[ic], r_kblk[ic], kTa[ic], r_kTa[ic])):
                for hp in range(4):
                    k.op("pe", lambda hp=hp: nc.tensor.transpose(psb[:, hp * 128:(hp + 1) * 128], src_[:, hp * 128:(hp + 1) * 128], ident_b[:]),
                         reads=[rs_, r_idb], writes=[r_psb])
                k.op("act", lambda: nc.scalar.copy(dst_[:], psb[:, 0:512].rearrange("p (h n) -> p h n", h=4)), writes=[r_psb, rd_])
            srcs = [(ic, maskc_b)] + ([(ip, maskp_b)] if has_prev else [])
            pts = []
            for si, (ib, msk) in enumerate(srcs):
                for hb in range(2):
                    b_, rb = nb()
                    for hh in range(4):
                        h = 2 * hh + hb
                        po_ = hb * 64
                        k.op("pe", lambda: nc.tensor.matmul(b_[:, hh * 128:(hh + 1) * 128], kTa[ib][po_:po_ + 64, h // 2, :],
                                                            qTa[po_:po_ + 64, h // 2, :], start=True, stop=True, skip_group_check=True),
                             reads=[r_kTa[ib], r_qTa], writes=[rb])
                    pi = si * 2 + hb
                    k.op("act", lambda: nc.scalar.activation(pT[pi][:], b_[:, :].rearrange("p (h n) -> p h n", h=4), AF.Exp),
                         writes=[rb, r_pT[pi]])
                    k.op("dve", lambda: nc.vector.tensor_tensor(pT[pi][:], pT[pi][:], msk[:], ALU.mult), reads=[r_mask], writes=[r_pT[pi]])
                    pts.append((pi, ib))
            io = n_ % 2
            for hb in range(2):
                b_, rb = nb()
                for hh in range(4):
                    h = hb * 4 + hh
                    for si, (ib, msk) in enumerate(srcs):
                        pi = si * 2 + (h % 2)
                        k.op("pe", lambda: nc.tensor.matmul(b_[:, hh * 65:(hh + 1) * 65], pT[pi][:, h // 2, :], vblk[ib][:, h, :],
                                                            start=(si == 0), stop=(si == len(srcs) - 1), skip_group_check=True),
                             reads=[r_pT[pi], r_vblk[ib]], writes=[rb])
                k.op("act", lambda: nc.scalar.copy(oac[io][:, hb * 260:(hb + 1) * 260], b_[:, 0:260]), writes=[rb, r_oac[io]])
            k.flush()
            k.defer(lambda io=io, store=store: store(oac[io], r_oac[io]))
            return ic

        for s in range(2):
            for g in range(3):
                d = GROUPS[g][1]
                nblk = SEQ // (128 * d)

                def view(T, width):
                    return T[tok0(s):tok0(s) + SEQ, g, :].rearrange("(n j dd) c -> dd n j c", dd=d, j=128)
                Qv, Kv, Vv = view(QS, 512), view(KS, 512), view(VS, 520)
                Ov = OACC[g, tok0(s):tok0(s) + SEQ, :].rearrange("(n j dd) c -> dd n j c", dd=d, j=128)
                for r in range(d):
                    ip = 0
                    for n in range(nblk):
                        def load_cur(ic, r=r, n=n, Qv=Qv, Kv=Kv, Vv=Vv):
                            k.dma("sp", qblk[ic][:], Qv[r, n], reads=[r_scr], writes=[r_qblk[ic]])
                            k.dma("sp", kblk[ic][:], Kv[r, n], reads=[r_scr], writes=[r_kblk[ic]])
                            k.dma("sp", vblk[ic][:].rearrange("p h e -> p (h e)"), Vv[r, n], reads=[r_scr], writes=[r_vblk[ic]])

                        def store(o_, ro_, r=r, n=n, Ov=Ov):
                            k.dma("sp", Ov[r, n], o_[:], reads=[ro_], accum=[r_oacc])
                        ip = attn_block(load_cur, n > 0, ip, store)
        for b in range(NS):
            for g in range(3):
                tokb = tok0(2) + b
                ip = 0
                k.dma("pool", kblk[ip][:], ck[g][b, :, 0, :], writes=[r_kblk[ip]])
                k.dma("pool", vblk[ip][:, :, 0:64], ck[g][b, :, 1, :].rearrange("j (h e) -> j h e", h=8), writes=[r_vblk[ip]])
                for hp in range(4):
                    k.op("pe", lambda hp=hp: nc.tensor.transpose(psb[:, hp * 128:(hp + 1) * 128], kblk[ip][:, hp * 128:(hp + 1) * 128], ident_b[:]),
                         reads=[r_kblk[ip], r_idb], writes=[r_psb])
                k.op("act", lambda: nc.scalar.copy(kTa[ip][:], psb[:, 0:512].rearrange("p (h n) -> p h n", h=4)), writes=[r_psb, r_kTa[ip]])

                def load_cur(ic, g=g, tokb=tokb):
                    k.dma("sp", qblk[ic][0:1, :], QS[tokb:tokb + 1, g, :], reads=[r_scr], writes=[r_qblk[ic]])
                    k.dma("sp", kblk[ic][0:1, :], KS[tokb:tokb + 1, g, :], reads=[r_scr], writes=[r_kblk[ic]])
                    k.dma("sp", vblk[ic][0:1, :, :].rearrange("p h e -> p (h e)"), VS[tokb:tokb + 1, g, :], reads=[r_scr], writes=[r_vblk[ic]])

                def store(o_, ro_, g=g, tokb=tokb):
                    k.dma("sp", OACC[g, tokb:tokb + 1, :], o_[0:1, :], reads=[ro_], accum=[r_oacc])
                attn_block(load_cur, True, ip, store)
        k.barrier()
    if stop_after <= 4:
        k.finish()
        return

    with contextlib.ExitStack() as ph:
        def psb_(name, shape, dt):
            return ph.enter_context(nc.sbuf_tensor(name, list(shape), dt))
        Wa = psb_("Wa", [128, 4, D], BF16)
        Wb = psb_("Wb", [128, 8, D], BF16)
        Wo = psb_("Wo", [128, 8, D], BF16)
        Wq = psb_("Wq", [128, 8, 2048], BF16)
        skt = psb_("skt", [128, 2, 128], F32)
        r_W = Res()
        k.dma("pool", Wa[:], w_br_a.rearrange("(kc p) n -> p kc n", p=128), writes=[r_W])
        k.dma("pool", Wb[:], w_br_b.rearrange("(kc p) n -> p kc n", p=128), writes=[r_W])
        k.dma("pool", Wo[:], w_o.rearrange("(kc p) n -> p kc n", p=128), writes=[r_W])
        k.dma("pool", Wq[:], w_pq.rearrange("(kc p) n -> p kc n", p=128), writes=[r_W])
        k.dma("sp", skt[:], skT.rearrange("t e c -> e t c"), writes=[r_W])
        n2w = psb_("n2w", [128, D], F32)
        k.dma("sp", n2w[:], norm2_w.partition_broadcast(128), writes=[r_W])
        iota16 = psb_("iota16", [128, 16], F32)
        k.dma("sp", iota16[:], selc[0:1, 2048:2064].partition_broadcast(128), writes=[r_W])
        G1 = psb_("G1", [128, D], F32)
        S2 = psb_("S2", [128, D], F32)
        SH2 = psb_("SH2", [128, D], F32)
        G2 = psb_("G2", [128, D], F32)
        r_M = Res()
        xin = [psb_("xin0", [128, D], F32)] * 2
        r_xin = [Res()] * 2
        oin = [psb_("oin0", [128, 3, 520], F32)] * 2
        r_oin = [Res()] * 2
        hgin = [psb_("hgin0", [128, D], BF16)] * 2
        r_hgin = [Res()] * 2
        ggin = [psb_("ggin0", [128, 2048], BF16)] * 2
        r_ggin = [Res()] * 2
        rl = psb_("rl", [128, 8], F32)
        r_rl = Res()
        att_b = psb_("att_b", [128, 512], BF16)
        r_attb = Res()
        TT = psb_("TT", [128, 8, 128], BF16)
        r_TT = Res()
        f1 = psb_("f1", [128, D], F32)
        r_f1 = Res()
        yb = psb_("yb", [128, D], BF16)
        r_yb = Res()
        x1 = psb_("x1", [128, D], F32)
        r_x1 = Res()
        h2b = psb_("h2b", [128, D], BF16)
        r_h2b = Res()
        ssq = psb_("ssq", [128, 1], F32)
        r_ssq = Res()
        sc = psb_("sc", [128, 16, 128], F32)
        sc2 = psb_("sc2", [128, 16, 128], F32)
        r_sc, r_sc2 = Res(), Res()
        q2T = sc2
        r_q2T = r_sc2
        vals = psb_("vals", [128, 16, 16], F32)
        idxu = psb_("idxu", [128, 16, 16], U32)
        idxf = psb_("idxf", [128, 16, 16], F32)
        r_vals, r_idxu, r_idxf = Res(), Res(), Res()
        r_valsH = [Res() for _ in range(16)]
        r_idxuH = [Res() for _ in range(16)]
        r_sc2H = [Res() for _ in range(16)]
        cand = psb_("cand", [128, 8, 256], F32)
        cand2 = sc2[:].rearrange("p a b -> p (a b)").rearrange("p (h x) -> p h x", h=8)
        r_cand, r_cand2 = Res(), r_sc2
        tv = psb_("tv", [128, 8, 16], F32)
        posu = psb_("posu", [128, 8, 16], U32)
        pa_u = psb_("pa_u", [128, 8, 16], U32)
        pa_f = psb_("pa_f", [128, 2, 8, 16], F32)
        r_tv, r_posu, r_pau, r_paf = Res(), Res(), Res(), Res()
        oh = cand2
        r_oh = r_sc2
        isel = psb_("isel", [128, 2, 8, 16], F32)
        r_isel = Res()
        eid_f = psb_("eid_f", [128, 128], F32)
        eid = psb_("eid", [128, 128], I32)
        r_eid = Res()
        gsm = psb_("gsm", [128, 8], F32)
        gat = psb_("gat", [128, 8, 16], F32)
        r_gat = Res()
        dots = psb_("dots", [128, 128], F32)
        r_dots = Res()
        coef = psb_("coef", [128, 128], F32)
        r_coef = Res()
        NUV = 8
        uvb = [psb_("uvb%d" % i, [128, 2 * D], BF16) for i in range(NUV)]
        r_uvb = [Res() for _ in range(NUV)]
        r_dotg = [Res() for _ in range(32)]
        r_coefg = [Res() for _ in range(32)]
        dg = [psb_("dg%d" % i, [128, 128], BF16) for i in range(4)]
        r_dg = [Res() for _ in range(4)]
        jb = psb_("jb", [128, D], BF16)
        r_jb = Res()
        yo = [psb_("yo0", [128, D], F32)] * 2
        r_yo = [Res()] * 2

        def transpose_to_TT(P, src, rsrc, nk):
            for kc in range(nk):
                k.op("pe", lambda kc=kc: nc.tensor.transpose(psb[:, kc * 128:kc * 128 + P], src[0:P, kc * 128:(kc + 1) * 128], ident_b[0:P, 0:P]),
                     reads=[rsrc, r_idb], writes=[r_psb])
            k.op("act", lambda: nc.scalar.copy(TT[:, 0:nk, 0:P], psb[:, 0:nk * 128].rearrange("p (kc n) -> p kc n", kc=nk)[:, :, 0:P]),
                 writes=[r_psb, r_TT])

        def mm_tok(P, W, nk, nbank, rW):
            out = []
            for j in range(nbank):
                b_, rb = nb()
                for kc in range(nk):
                    k.op("pe", lambda kc=kc: nc.tensor.matmul(b_[0:P, :], TT[:, kc, 0:P], W[:, kc, j * 512:(j + 1) * 512],
                                                              start=(kc == 0), stop=(kc == nk - 1)),
                         reads=[r_TT, rW], writes=[rb])
                out.append((b_, rb))
            return out

        tiles = [(s, t) for s in range(2) for t in range(NT)] + [(2, 0)]
        cur_s = [-1]

        def loads(idx):
            s, t = tiles[idx]
            P = 128 if s < 2 else NS
            i = idx % 2
            g0 = tok0(s) + t * 128
            k.dma("sp", xin[i][0:P, :], xp[s, t * 128:(t + 1) * 128, :] if s < 2 else xs[:, :], writes=[r_xin[i]])
            for g in range(3):
                k.dma("sp", oin[i][0:P, g, :], OACC[g, g0:g0 + P, :], reads=[r_oacc], writes=[r_oin[i]])
            k.dma("sp", hgin[i][0:P, :], HGS[g0:g0 + P, :], reads=[r_hgs], writes=[r_hgin[i]])
            k.dma("sp", ggin[i][0:P, :], GG[g0:g0 + P, 1024:3072], reads=[r_gg], writes=[r_ggin[i]])

        loads(0)
        for idx, (s, t) in enumerate(tiles):
            P = 128 if s < 2 else NS
            i = idx % 2
            if s != cur_s[0]:
                cur_s[0] = s
                for dst_, c0 in ((G1, 2 * D), (SH2, 3 * D), (S2, 4 * D), (G2, 5 * D)):
                    if s < 2:
                        k.dma("sp", dst_[:], MODS[s:s + 1, c0:c0 + D].partition_broadcast(128), reads=[r_MODS], writes=[r_M])
                    else:
                        k.dma("sp", dst_[0:NS, :], MODS[2:2 + NS, c0:c0 + D], reads=[r_MODS], writes=[r_M])
                k.op("dve", lambda: nc.vector.scalar_tensor_tensor(S2[0:P, :], S2[0:P, :], 1.0, n2w[0:P, :], ALU.add, ALU.mult),
                     reads=[r_W], writes=[r_M])
            o0 = oin[i]
            k.op("dve", lambda: nc.vector.tensor_tensor(o0[0:P, 0, :], o0[0:P, 0, :], o0[0:P, 1, :], ALU.add), writes=[r_oin[i]])
            k.op("dve", lambda: nc.vector.tensor_tensor(o0[0:P, 0, :], o0[0:P, 0, :], o0[0:P, 2, :], ALU.add), writes=[r_oin[i]])
            ov = o0[0:P, 0, :].rearrange("p (h e) -> p h e", h=8)
            k.op("dve", lambda: nc.vector.reciprocal(rl[0:P, :], ov[:, :, 64]), reads=[r_oin[i]], writes=[r_rl])
            k.op("dve", lambda: nc.vector.tensor_tensor(att_b[0:P, :].rearrange("p (h e) -> p h e", h=8), ov[:, :, 0:64],
                                                        rl[0:P, :].unsqueeze(2).to_broadcast([P, 8, 64]), ALU.mult),
                 reads=[r_oin[i], r_rl], writes=[r_attb])
            transpose_to_TT(P, att_b, r_attb, 4)
            pA = mm_tok(P, Wa, 4, 2, r_W)
            for hb in range(2):
                b_, rb = pA[hb]
                k.op("dve", lambda: nc.vector.tensor_tensor(f1[0:P, hb * 512:(hb + 1) * 512], b_[0:P, :], ggin[i][0:P, hb * 512:(hb + 1) * 512], ALU.mult),
                     reads=[r_ggin[i]], writes=[rb, r_f1])
            transpose_to_TT(P, hgin[i], r_hgin[i], 8)
            pB = mm_tok(P, Wb, 8, 2, r_W)
            for hb in range(2):
                b_, rb = pB[hb]
                k.op("dve", lambda: nc.vector.tensor_tensor(x1[0:P, hb * 512:(hb + 1) * 512], b_[0:P, :],
                                                            ggin[i][0:P, 1024 + hb * 512:1024 + (hb + 1) * 512], ALU.mult),
                     reads=[r_ggin[i]], writes=[rb, r_x1])
            k.op("dve", lambda: nc.vector.tensor_tensor(yb[0:P, :], f1[0:P, :], x1[0:P, :], ALU.add), reads=[r_f1, r_x1], writes=[r_yb])
            transpose_to_TT(P, yb, r_yb, 8)
            pZ = mm_tok(P, Wo, 8, 2, r_W)
            for hb in range(2):
                b_, rb = pZ[hb]
                k.op("dve", lambda: nc.vector.tensor_tensor(x1[0:P, hb * 512:(hb + 1) * 512], b_[0:P, :], G1[0:P, hb * 512:(hb + 1) * 512], ALU.mult),
                     reads=[r_M], writes=[rb, r_x1])
            k.op("dve", lambda: nc.vector.tensor_tensor(x1[0:P, :], x1[0:P, :], xin[i][0:P, :], ALU.add), reads=[r_xin[i]], writes=[r_x1])
            if DEBUG and (idx == 0 or s == 2):
                k.dma("pool", dbg[0 if idx == 0 else 1, 1, 0:P, :], hgin[i][0:P, :], reads=[r_hgin[i]])
            if idx + 1 < len(tiles):
                loads(idx + 1)
            k.op("act", lambda: nc.scalar.activation(f1[0:P, :], x1[0:P, :], AF.Square, accum_out=ssq[0:P, :]),
                 reads=[r_x1], writes=[r_f1, r_ssq])
            rstd_from_ss(P, (ssq[0:P, :], r_ssq), D, None)
            k.op("dve", lambda: nc.vector.scalar_tensor_tensor(f1[0:P, :], x1[0:P, :], ssq[0:P, 0:1], S2[0:P, :], ALU.mult, ALU.mult),
                 reads=[r_x1, r_ssq, r_M], writes=[r_f1])
            k.op("dve", lambda: nc.vector.tensor_tensor(h2b[0:P, :], f1[0:P, :], SH2[0:P, :], ALU.add), reads=[r_f1, r_M], writes=[r_h2b])
            transpose_to_TT(P, h2b, r_h2b, 8)
            for qb in range(4):
                b_, rb = nb()
                for hh in range(4):
                    hp = qb * 4 + hh
                    for kc in range(8):
                        k.op("pe", lambda kc=kc: nc.tensor.matmul(b_[:, hh * 128:hh * 128 + P], Wq[:, kc, hp * 128:(hp + 1) * 128], TT[:, kc, 0:P],
                                                                  start=(kc == 0), stop=(kc == 7), skip_group_check=True),
                             reads=[r_TT, r_W], writes=[rb])
                k.op("act", lambda: nc.scalar.copy(q2T[:, qb * 4:(qb + 1) * 4, 0:P], b_[:, :].rearrange("p (h n) -> p h n", h=4)[:, :, 0:P]),
                     writes=[rb, r_q2T])
            for qb in range(4):
                b_, rb = nb()
                for hh in range(4):
                    hp = qb * 4 + hh
                    k.op("pe", lambda: nc.tensor.matmul(b_[0:P, hh * 128:(hh + 1) * 128], q2T[:, hp, 0:P], skt[:, hp % 2, :],
                                                        start=True, stop=True, skip_group_check=True),
                         reads=[r_q2T, r_W], writes=[rb])
                k.op("act", lambda: nc.scalar.copy(sc[0:P, qb * 4:(qb + 1) * 4, :], b_[0:P, :].rearrange("p (h n) -> p h n", h=4)),
                     writes=[rb, r_sc])
            for hp in range(16):
                rv_, ri_, r2_ = r_valsH[hp], r_idxuH[hp], r_sc2H[hp]
                k.op("dve", lambda: nc.vector.max(vals[0:P, hp, 0:8], sc[0:P, hp, :]), reads=[r_sc, r_sc2], writes=[rv_])
                k.op("dve", lambda: nc.vector.max_index(idxu[0:P, hp, 0:8], vals[0:P, hp, 0:8], sc[0:P, hp, :]),
                     reads=[r_sc, rv_], writes=[ri_])
                k.op("dve", lambda: nc.vector.match_replace(sc2[0:P, hp, :], vals[0:P, hp, 0:8], sc[0:P, hp, :], -1e30),
                     reads=[r_sc, rv_], writes=[r2_])
                k.op("dve", lambda: nc.vector.max(vals[0:P, hp, 8:16], sc2[0:P, hp, :]), reads=[r2_], writes=[rv_])
                k.op("dve", lambda: nc.vector.max_index(idxu[0:P, hp, 8:16], vals[0:P, hp, 8:16], sc2[0:P, hp, :]),
                     reads=[r2_, rv_], writes=[ri_])
            k.op("dve", lambda: nc.vector.tensor_copy(idxf[0:P], idxu[0:P]), reads=r_idxuH, writes=[r_idxf])
            v4 = vals[0:P].rearrange("p (h two) j -> p h two j", two=2)
            k.op("dve", lambda: nc.vector.tensor_tensor(cand[0:P].rearrange("p h (a b) -> p h a b", a=16),
                                                        v4[:, :, 0, :].unsqueeze(3).to_broadcast([P, 8, 16, 16]),
                                                        v4[:, :, 1, :].unsqueeze(2).to_broadcast([P, 8, 16, 16]), ALU.add),
                 reads=r_valsH + r_sc2H, writes=[r_cand, r_sc2])
            for h in range(8):
                k.op("dve", lambda: nc.vector.max(tv[0:P, h, 0:8], cand[0:P, h, :]), reads=[r_cand], writes=[r_tv])
                k.op("dve", lambda: nc.vector.max_index(posu[0:P, h, 0:8], tv[0:P, h, 0:8], cand[0:P, h, :]),
                     reads=[r_cand, r_tv], writes=[r_posu])
                k.op("dve", lambda: nc.vector.match_replace(cand2[0:P, h, :], tv[0:P, h, 0:8], cand[0:P, h, :], -1e30),
                     reads=[r_cand, r_tv], writes=[r_cand2])
                k.op("dve", lambda: nc.vector.max(tv[0:P, h, 8:16], cand2[0:P, h, :]), reads=[r_cand2], writes=[r_tv])
                k.op("dve", lambda: nc.vector.max_index(posu[0:P, h, 8:16], tv[0:P, h, 8:16], cand2[0:P, h, :]),
                     reads=[r_cand2, r_tv], writes=[r_posu])
            k.op("dve", lambda: nc.vector.tensor_single_scalar(pa_u[0:P], posu[0:P], 4, ALU.logical_shift_right), reads=[r_posu], writes=[r_pau])
            k.op("dve", lambda: nc.vector.tensor_copy(pa_f[0:P, 0], pa_u[0:P]), reads=[r_pau], writes=[r_paf])
            k.op("dve", lambda: nc.vector.tensor_single_scalar(pa_u[0:P], posu[0:P], 15, ALU.bitwise_and), reads=[r_posu], writes=[r_pau])
            k.op("dve", lambda: nc.vector.tensor_copy(pa_f[0:P, 1], pa_u[0:P]), reads=[r_pau], writes=[r_paf])
            i4 = idxf[0:P].rearrange("p (h two) j -> p h two j", two=2)
            for w_ in range(2):
                ohv = oh[0:P].rearrange("p h (j a) -> p h j a", j=16)
                k.op("dve", lambda: nc.vector.tensor_tensor(ohv, pa_f[0:P, w_].unsqueeze(3).to_broadcast([P, 8, 16, 16]),
                                                            iota16[0:P, :].unsqueeze(1).unsqueeze(1).to_broadcast([P, 8, 16, 16]), ALU.is_equal),
                     reads=[r_paf, r_W], writes=[r_oh])
                k.op("dve", lambda: nc.vector.tensor_tensor(ohv, ohv, i4[:, :, w_, :].unsqueeze(2).to_broadcast([P, 8, 16, 16]), ALU.mult),
                     reads=[r_idxf], writes=[r_oh])
                k.op("dve", lambda: nc.vector.tensor_reduce(isel[0:P, w_], ohv, AX.X, ALU.add), reads=[r_oh], writes=[r_isel])
            k.op("dve", lambda: nc.vector.scalar_tensor_tensor(eid_f[0:P, :], isel[0:P, 0].rearrange("p h j -> p (h j)"), 128.0,
                                                               isel[0:P, 1].rearrange("p h j -> p (h j)"), ALU.mult, ALU.add),
                 reads=[r_isel], writes=[r_eid])
            k.op("dve", lambda: nc.vector.tensor_copy(eid[0:P, :], eid_f[0:P, :]), writes=[r_eid])
            k.op("dve", lambda: nc.vector.tensor_tensor(gat[0:P], tv[0:P], tv[0:P, :, 0:1].to_broadcast([P, 8, 16]), ALU.subtract),
                 reads=[r_tv], writes=[r_gat])
            k.op("act", lambda: nc.scalar.activation(gat[0:P], gat[0:P], AF.Exp), writes=[r_gat])
            k.op("dve", lambda: nc.vector.tensor_reduce(gsm[0:P, :], gat[0:P], AX.X, ALU.add), reads=[r_gat], writes=[r_rl])
            k.op("dve", lambda: nc.vector.reciprocal(gsm[0:P, :], gsm[0:P, :]), writes=[r_rl])
            k.op("dve", lambda: nc.vector.tensor_tensor(gat[0:P], gat[0:P], gsm[0:P, :].unsqueeze(2).to_broadcast([P, 8, 16]), ALU.mult),
                 reads=[r_rl], writes=[r_gat])
            py = [nb(), nb()]
            gatf = gat[0:P].rearrange("p h j -> p (h j)")
            for grp in range(32):
                gs = slice(grp * 4, grp * 4 + 4)
                rd, rc = r_dotg[grp], r_coefg[grp]
                for j in range(grp * 4, grp * 4 + 4):
                    bi = j % NUV
                    k.dma("pool", uvb[bi][0:P, :], UV, reads=[r_eid, r_uv], writes=[r_uvb[bi]],
                          indirect=bass.IndirectOffsetOnAxis(ap=eid[0:P, j:j + 1], axis=0))
                    k.op("dve", lambda: nc.vector.scalar_tensor_tensor(jb[0:P, :], uvb[bi][0:P, 0:D], 1.0, h2b[0:P, :], ALU.mult, ALU.mult,
                                                                       accum_out=dots[0:P, j:j + 1]),
                         reads=[r_uvb[bi], r_h2b], writes=[r_jb, rd])
                k.op("dve", lambda: nc.vector.tensor_tensor(coef[0:P, gs], dots[0:P, gs], dots[0:P, gs], ALU.mult), reads=[rd], writes=[rc])
                k.op("dve", lambda: nc.vector.tensor_scalar(coef[0:P, gs], coef[0:P, gs], 0.044715, 1.0, ALU.mult, ALU.add), writes=[rc])
                k.op("dve", lambda: nc.vector.tensor_tensor(coef[0:P, gs], coef[0:P, gs], dots[0:P, gs], ALU.mult), reads=[rd], writes=[rc])
                k.op("act", lambda: nc.scalar.activation(coef[0:P, gs], coef[0:P, gs], AF.Sigmoid, scale=1.5957691216057308), writes=[rc])
                k.op("dve", lambda: nc.vector.tensor_tensor(coef[0:P, gs], coef[0:P, gs], dots[0:P, gs], ALU.mult), reads=[rd], writes=[rc])
                k.op("dve", lambda: nc.vector.tensor_tensor(coef[0:P, gs], coef[0:P, gs], gatf[:, gs], ALU.mult), reads=[r_gat], writes=[rc])
                for j in range(grp * 4, grp * 4 + 4):
                    bi = j % NUV
                    di_ = j % 4
                    k.op("act", lambda: nc.scalar.mul(dg[di_][0:P, 0:P], ident_b[0:P, 0:P], coef[0:P, j:j + 1]),
                         reads=[rc, r_idb], writes=[r_dg[di_]])
                    for hb in range(2):
                        b_, rb = py[hb]
                        k.op("pe", lambda: nc.tensor.matmul(b_[0:P, :], dg[di_][0:P, 0:P], uvb[bi][0:P, D + hb * 512:D + (hb + 1) * 512],
                                                            start=(j == 0), stop=(j == 127)),
                             reads=[r_dg[di_], r_uvb[bi]], writes=[rb])
            io = idx % 2
            for hb in range(2):
                b_, rb = py[hb]
                k.op("dve", lambda: nc.vector.tensor_tensor(yo[io][0:P, hb * 512:(hb + 1) * 512], b_[0:P, :], G2[0:P, hb * 512:(hb + 1) * 512], ALU.mult),
                     reads=[r_M], writes=[rb, r_yo[io]])
            k.op("dve", lambda: nc.vector.tensor_tensor(yo[io][0:P, :], yo[io][0:P, :], x1[0:P, :], ALU.add), reads=[r_x1], writes=[r_yo[io]])
            if DEBUG and (idx == 0 or s == 2):
                di = 0 if idx == 0 else 1
                k.dma("pool", dbg[di, 0, 0:P, 0:512], att_b[0:P, :], reads=[r_attb])
                k.dma("pool", dbg[di, 2, 0:P, :], yb[0:P, :], reads=[r_yb])
                k.dma("sp", dbg[di, 3, 0:P, :], x1[0:P, :], reads=[r_x1])
                k.dma("pool", dbg[di, 4, 0:P, :], h2b[0:P, :], reads=[r_h2b])
                k.dma("sp", dbg[di, 5, 0:P, 0:128], coef[0:P, :], reads=r_coefg)
                k.dma("sp", dbg[di, 6, 0:P, 0:128], eid_f[0:P, :], reads=[r_eid])
                k.dma("sp", dbg[di, 7, 0:P, 0:128], dots[0:P, :], reads=r_dotg)
                k.dma("sp", dbg[di, 8, 0:P, 0:128], gat[0:P].rearrange("p h j -> p (h j)"), reads=[r_gat])
                k.dma("sp", dbg[di, 9, 0:P, 0:256], vals[0:P].rearrange("p a b -> p (a b)"), reads=r_valsH)
            k.dma("sp", y_p[s, t * 128:(t + 1) * 128, :] if s < 2 else y_s[:, :], yo[io][0:P, :], reads=[r_yo[io]])
        k.flush()
    k.finish()


def _consts():
    c = np.zeros((128, 1024), np.float32)
    j = np.arange(128)[:, None]
    i = np.arange(128)[None, :]
    c[:, 0:128] = np.eye(128, dtype=np.float32)
    c[:, 128:256] = (j <= i)
    c[:, 256:384] = (j >= i)
    same = (j // 64) == (i // 64)
    c[:, 384:512] = same * ((j <= i).astype(np.float32) - ((j % 64) <= 31).astype(np.float32))
    c[:, 512:640] = same * (j > i)
    s_ = np.arange(128)
    c[:, 640] = s_ < 64
    c[:, 641] = s_ >= 64
    c[:, 642] = (s_ < 64) & (s_ % 64 <= 31)
    c[:, 643] = (s_ >= 64) & (s_ % 64 <= 31)
    c[:, 768:896] = same * (j <= i)
    return c


def _selc():
    c = np.zeros((NS, 2064), np.float32)
    for b in range(NS):
        c[b, b * 128:(b + 1) * 128] = 1.0
    c[:, 2048:2064] = np.arange(16, dtype=np.float32)[None, :]
    return c


_CACHE = {}


def kernel(x_prompt, x_sample, cache_kv_w128, cache_kv_w512, cache_kv_w2048, state_hgrn, c_prompt, c_sample,
           w_ada, b_ada, norm1_w, norm2_w, w_in, q_norm_w, k_norm_w, hg_lb_logits, hg_norm_w, w_br_a, w_br_b,
           w_o, w_peer_q, peer_subkeys, peer_u, peer_v, _stop_after=99):
    f = lambda a: np.ascontiguousarray(np.asarray(a, dtype=np.float32))
    key = ("nc", _stop_after)
    if key not in _CACHE:
        _CACHE[key] = build_program(_stop_after)
    nc = _CACHE[key]
    caches = [f(cache_kv_w128)[0], f(cache_kv_w512)[0], f(cache_kv_w2048)[0]]
    shared = {
        "w_ada": f(w_ada)[0], "b_ada": f(b_ada).reshape(1, -1), "norm1_w": f(norm1_w).reshape(1, -1),
        "norm2_w": f(norm2_w).reshape(1, -1), "w_in": f(w_in)[0],
        "qk_w": np.ascontiguousarray(np.stack([np.tile(f(q_norm_w)[0], 8), np.tile(f(k_norm_w)[0], 8)])),
        "lb_log": f(hg_lb_logits), "hgn_w": np.ascontiguousarray(np.tile(f(hg_norm_w)[0], 8).reshape(1, -1)),
        "w_br_a": f(w_br_a)[0], "w_br_b": f(w_br_b)[0], "w_o": f(w_o)[0], "w_pq": f(w_peer_q)[0],
        "skT": np.ascontiguousarray(f(peer_subkeys)[0].transpose(0, 2, 1)),
        "peer_u": f(peer_u)[0], "peer_v": f(peer_v)[0], "cst": _consts(), "selc": _selc(),
    }
    xpf, xsf = f(x_prompt), f(x_sample)
    cp, cs = f(c_prompt), f(c_sample)
    st = f(state_hgrn)[0]
    in_maps = []
    for c in range(NCORES):
        m = dict(shared)
        m["xp"] = xpf[c * NSEQ:(c + 1) * NSEQ]
        m["xs"] = np.ascontiguousarray(xsf[c * NS:(c + 1) * NS, 0, :])
        m["cT"] = np.ascontiguousarray(np.concatenate([cp[c * NSEQ:(c + 1) * NSEQ], cs[c * NS:(c + 1) * NS]], 0).T)
        for g in range(3):
            m["ck%d" % g] = np.ascontiguousarray(
                caches[g][c * NS:(c + 1) * NS, 0::GROUPS[g][1]][:, :128].reshape(NS, 128, 2, 512))
        m["st_in"] = st[c * NS:(c + 1) * NS]
        in_maps.append(m)
    res = run_bass_kernel_spmd(nc, in_maps, core_ids=list(range(NCORES)))
    R = res.results
    cat = lambda n: np.concatenate([np.asarray(r[n]) for r in R], 0)
    y_prompt = cat("y_p")
    y_sample = cat("y_s").reshape(NCORES * NS, 1, D)
    outs = [y_prompt, y_sample]
    for g in range(3):
        outs.append(cat("kv%d_p" % g).reshape(1, NCORES * NSEQ, GROUPS[g][0], 2, 8, 64))
    outs.append(cat("hg_p")[None])
    for g in range(3):
        outs.append(cat("kv%d_s" % g).reshape(1, NCORES * NS, 1, 2, 8, 64))
    outs.append(cat("hg_s")[None])
    if DEBUG:
        global _DBG
        _DBG = np.asarray(R[0]["dbg"])
    return tuple(np.ascontiguousarray(o, dtype=np.float32) for o in outs)
```

```python
import contextlib
import numpy as np
import concourse.bass as bass
import concourse.mybir as mybir
from concourse.bass_utils import run_bass_kernel_spmd

F32 = mybir.dt.float32
BF16 = mybir.dt.bfloat16
I32 = mybir.dt.int32
U32 = mybir.dt.uint32
ALU = mybir.AluOpType
AF = mybir.ActivationFunctionType
AX = mybir.AxisListType

NCORES = 8
D = 1024
SEQ = 4096
NSEQ = 2
NS = 16
INW = 10752
GROUPS = ((128, 1), (512, 4), (2048, 16))
EPS = 1e-6
NT = SEQ // 128
NROWS = NSEQ + NS
DEBUG = False


class Res:
    __slots__ = ("w", "rs")

    def __init__(self):
        self.w = {}
        self.rs = {}


class K:
    def __init__(self, nc, es):
        self.nc = nc
        self.es = es
        self.eng = {"pe": nc.tensor, "act": nc.scalar, "dve": nc.vector, "pool": nc.gpsimd, "sp": nc.sync}
        self.sem = {}
        self.cnt = {}
        for e in ("pe", "act", "dve", "pool"):
            self.sem[e] = es.enter_context(nc.semaphore("c_" + e))
            self.cnt[e] = 0
        self.known = {e: {} for e in self.eng}
        self.dsem = {}
        self.dpos = {}
        for q, n in (("sp", 24), ("pool", 16), ("act", 8)):
            self.dsem[q] = [[es.enter_context(nc.semaphore("d_%s%d" % (q, i))), 0] for i in range(n)]
            self.dpos[q] = 0
        self.deferred = []

    def _wait(self, e, tok):
        s, v = tok
        if v <= 0:
            return
        if e == "pe" and s is self.sem["pe"]:
            return
        kn = self.known[e]
        if kn.get(id(s), 0) >= v:
            return
        self.eng[e].wait_ge(s, v)
        kn[id(s)] = v

    def _deps(self, e, reads, writes):
        for r in reads:
            for tok in r.w.values():
                self._wait(e, tok)
        for r in writes:
            for tok in r.w.values():
                self._wait(e, tok)
            for tok in r.rs.values():
                self._wait(e, tok)

    def _mark(self, tok, reads, writes, accum):
        s, v = tok
        for r in reads:
            r.rs[id(s)] = tok
        for r in writes:
            r.w = {id(s): tok}
            r.rs = {}
        for r in accum:
            r.w[id(s)] = tok

    def op(self, e, fn, reads=(), writes=(), accum=()):
        self._deps(e, reads, writes)
        ins = fn()
        self.cnt[e] += 1
        ins.then_inc(self.sem[e], 1)
        self._mark((self.sem[e], self.cnt[e]), reads, writes, accum)

    def dma(self, q, out, in_, reads=(), writes=(), accum=(), indirect=None):
        self._deps(q, reads, writes)
        slot = self.dsem[q][self.dpos[q] % len(self.dsem[q])]
        self.dpos[q] += 1
        self._wait(q, (slot[0], slot[1]))
        if indirect is not None:
            ins = self.eng[q].indirect_dma_start(out=out, out_offset=None, in_=in_, in_offset=indirect)
        else:
            ins = self.eng[q].dma_start(out=out, in_=in_)
        slot[1] += 16
        ins.then_inc(slot[0], 16)
        self._mark((slot[0], slot[1]), reads, writes, accum)

    def barrier(self):
        self.flush()
        toks = [(self.sem[e], self.cnt[e]) for e in self.sem]
        for q in self.dsem:
            toks += [(s, v) for s, v in self.dsem[q]]
        for e in self.eng:
            for tok in toks:
                if e != "pe" or tok[0] is not self.sem["pe"]:
                    self._wait(e, tok)

    def defer(self, fn):
        self.deferred.append(fn)

    def flush(self):
        d, self.deferred = self.deferred, []
        for fn in d:
            fn()

    def finish(self):
        self.flush()
        for q in self.dsem:
            for s, v in self.dsem[q]:
                self._wait("sp", (s, v))


def build_program(stop_after=99):
    nc = bass.Bass("TRN2", target_bir_lowering=False)
    es = contextlib.ExitStack()
    with es:
        _emit(nc, es, stop_after)
    return nc


def _emit(nc, es, stop_after):
    k = K(nc, es)

    def din(name, shape, dt=F32):
        return nc.dram_tensor(name, list(shape), dt, kind="ExternalInput").ap()

    def dout(name, shape, dt=F32):
        return nc.dram_tensor(name, list(shape), dt, kind="ExternalOutput").ap()

    def dscr(name, shape, dt):
        return nc.dram_tensor(name, list(shape), dt, kind="Internal").ap()

    def sb(name, shape, dt):
        return es.enter_context(nc.sbuf_tensor(name, list(shape), dt))

    xp = din("xp", [NSEQ, SEQ, D])
    xs = din("xs", [NS, D])
    cT = din("cT", [D, NROWS])
    ck = [din("ck%d" % g, [NS, 128, 2, 512]) for g in range(3)]
    st_in = din("st_in", [NS, 8, 128, 128])
    w_ada = din("w_ada", [D, 6 * D])
    b_ada = din("b_ada", [1, 6 * D])
    norm1_w = din("norm1_w", [1, D])
    norm2_w = din("norm2_w", [1, D])
    w_in = din("w_in", [D, INW])
    qk_w = din("qk_w", [2, 512])
    lb_log = din("lb_log", [2, D])
    hgn_w = din("hgn_w", [1, D])
    w_br_a = din("w_br_a", [512, D])
    w_br_b = din("w_br_b", [D, D])
    w_o = din("w_o", [D, D])
    w_pq = din("w_pq", [D, 2048])
    skT = din("skT", [2, 128, 128])
    peer_u = din("peer_u", [16384, D])
    peer_v = din("peer_v", [16384, D])
    cst = din("cst", [128, 128 * 8])
    selc = din("selc", [NS, 2064])

    y_p = dout("y_p", [NSEQ, SEQ, D])
    y_s = dout("y_s", [NS, D])
    kv_p = [dout("kv%d_p" % g, [NSEQ, GROUPS[g][0], 2, 512]) for g in range(3)]
    hg_p = dout("hg_p", [NSEQ, 8, 128, 128])
    kv_s = [dout("kv%d_s" % g, [NS, 2, 512]) for g in range(3)]
    hg_s = dout("hg_s", [NS, 8, 128, 128])
    dbg = dout("dbg", [2, 10, 128, D]) if DEBUG else None

    MODS = dscr("MODS", [NROWS, 6 * D], F32)
    NTOK = NSEQ * SEQ + 128
    QS = dscr("QS", [NTOK, 3, 512], BF16)
    KS = dscr("KS", [NTOK, 3, 512], BF16)
    VS = dscr("VS", [NTOK, 3, 520], BF16)

    cst_f = sb("cst_f", [128, 1024], F32)
    ident_b = sb("ident_b", [128, 128], BF16)
    r_cst = Res()
    k.dma("sp", cst_f[:], cst, writes=[r_cst])
    r_idb = Res()
    k.op("dve", lambda: nc.vector.tensor_copy(ident_b[:], cst_f[:, 0:128]), reads=[r_cst], writes=[r_idb])
    ident_f = cst_f[:, 0:128]

    psf = [es.enter_context(nc.psum_tensor("psf%d" % i, [128, 512], F32)) for i in range(7)]
    r_psf = [Res() for _ in range(7)]
    psb = es.enter_context(nc.psum_tensor("psb", [128, 1024], BF16))
    r_psb = Res()

    def rstd_from_ss(P, ss, n_elem, tmp):
        t, r = ss
        k.op("dve", lambda: nc.vector.tensor_scalar(t, t, 1.0 / n_elem, EPS, ALU.mult, ALU.add), writes=[r])
        k.op("act", lambda: nc.scalar.activation(t, t, AF.Sqrt), writes=[r])
        k.op("dve", lambda: nc.vector.reciprocal(t, t), writes=[r])

    with contextlib.ExitStack() as ph:
        def psb_(name, shape, dt):
            return ph.enter_context(nc.sbuf_tensor(name, list(shape), dt))
        cT_f = psb_("cT_f", [128, 8, NROWS], F32)
        cT_b = psb_("cT_b", [128, 8, NROWS], BF16)
        r_cT = Res()
        k.dma("sp", cT_f[:], cT.rearrange("(kc p) n -> p kc n", p=128), writes=[r_cT])
        k.op("act", lambda: nc.scalar.activation(cT_b[:], cT_f[:], AF.Silu), reads=[r_cT], writes=[r_cT])
        wa = [psb_("wa%d" % i, [128, 8, 512], BF16) for i in range(2)]
        r_wa = [Res(), Res()]
        ba = [psb_("ba%d" % i, [NROWS, 512], F32) for i in range(2)]
        r_ba = [Res(), Res()]
        mo = [psb_("mo%d" % i, [NROWS, 512], F32) for i in range(2)]
        r_mo = [Res(), Res()]
        r_MODS = Res()
        for c in range(12):
            i = c % 2
            cs = slice(c * 512, (c + 1) * 512)
            k.dma("pool", wa[i][:], w_ada[:, cs].rearrange("(kc p) n -> p kc n", p=128), writes=[r_wa[i]])
            k.dma("sp", ba[i][:], b_ada[:, cs].partition_broadcast(NROWS), writes=[r_ba[i]])
            for kc in range(8):
                k.op("pe", lambda kc=kc: nc.tensor.matmul(psf[i][0:NROWS, :], cT_b[:, kc, :], wa[i][:, kc, :],
                                                          start=(kc == 0), stop=(kc == 7)),
                     reads=[r_cT, r_wa[i]], writes=[r_psf[i]])
            k.op("dve", lambda: nc.vector.tensor_tensor(mo[i][:], psf[i][0:NROWS, :], ba[i][:], ALU.add),
                 reads=[r_ba[i]], writes=[r_psf[i], r_mo[i]])
            k.dma("sp", MODS[:, cs], mo[i][:], reads=[r_mo[i]], accum=[r_MODS])
        k.barrier()
    if stop_after <= 0:
        k.finish()
        return

    def tok0(s):
        return s * SEQ

    NTOKX = NSEQ * SEQ + 128
    GG = dscr("GG", [NTOKX, 3072], BF16)
    HGS = dscr("HGS", [NTOKX, D], BF16)
    OACC = dscr("OACC", [3, NTOKX, 520], F32)
    UV = dscr("UV", [16384, 2 * D], BF16)
    r_uv = Res()
    r_scr = Res()
    r_gg = Res()
    r_hgs = Res()
    r_oacc = Res()
    bank_ctr = [0]

    nbank_rot = [7]

    def nb():
        i = bank_ctr[0] % nbank_rot[0]
        bank_ctr[0] += 1
        return psf[i], r_psf[i]

    maskc_b = sb("maskc_b", [128, 4, 128], BF16)
    maskp_b = sb("maskp_b", [128, 4, 128], BF16)
    r_mask = Res()
    for hh in range(4):
        k.op("dve", lambda hh=hh: nc.vector.tensor_copy(maskc_b[:, hh, :], cst_f[:, 128:256]), reads=[r_cst], writes=[r_mask])
        k.op("dve", lambda hh=hh: nc.vector.tensor_copy(maskp_b[:, hh, :], cst_f[:, 256:384]), reads=[r_cst], writes=[r_mask])
    L1 = cst_f[:, 384:512]
    L3 = cst_f[:, 512:640]
    R4 = cst_f[:, 640:644]
    MA = cst_f[:, 768:896]

    def proj(P, hT, ts_, W, c0, ncol512, r_hT, r_W):
        out = []
        for j in range(ncol512):
            b_, rb = nb()
            for kc in range(8):
                k.op("pe", lambda kc=kc: nc.tensor.matmul(b_[0:P, :], hT[:, kc, ts_], W[:, kc, c0 + j * 512:c0 + (j + 1) * 512],
                                                          start=(kc == 0), stop=(kc == 7)),
                     reads=[r_hT, r_W], writes=[rb])
            out.append((b_, rb))
        return out

    with contextlib.ExitStack() as seqscope:
        hT = seqscope.enter_context(nc.sbuf_tensor("hT", [128, 8, SEQ], BF16))
        r_hT = Res()
        for s in range(3):
            P = 128 if s < 2 else NS
            ntile = NT if s < 2 else 1

            def xsrc(t):
                return xp[s, t * 128:(t + 1) * 128, :] if s < 2 else xs[:, :]

            with contextlib.ExitStack() as ph:
                def psb_(name, shape, dt):
                    return ph.enter_context(nc.sbuf_tensor(name + "_s%d" % s, list(shape), dt))
                S1 = psb_("S1", [128, D], F32)
                SH1 = psb_("SH1", [128, D], F32)
                n1w = psb_("n1w", [128, D], F32)
                r_S1 = Res()
                r_n1w = Res()
                k.dma("sp", n1w[:], norm1_w.partition_broadcast(128), writes=[r_n1w])
                qkw = psb_("qkw", [128, 2, 512], F32)
                r_qkw = Res()
                k.dma("sp", qkw[:, 0, :], qk_w[0:1, :].partition_broadcast(128), writes=[r_qkw])
                k.dma("sp", qkw[:, 1, :], qk_w[1:2, :].partition_broadcast(128), writes=[r_qkw])
                k.op("dve", lambda: nc.vector.tensor_scalar(qkw[:, 0, :], qkw[:, 0, :], 0.125, None, ALU.mult), writes=[r_qkw])
                xt = [psb_("xt%d" % i, [128, D], F32) for i in range(2)]
                r_xt = [Res(), Res()]
                xm = psb_("xm", [128, D], F32)
                xb = psb_("xb", [128, D], BF16)
                r_xm = Res()
                r_xb = Res()
                junk = psb_("junk", [128, D], F32)
                r_junk = Res()
                ss1 = psb_("ss1", [128, 1], F32)
                r_ss1 = Res()
                Wg = psb_("Wg", [128, 8, 1536], BF16)
                r_Wg = Res()
                ss8 = psb_("ss8", [128, 8], F32)
                r_ss8 = Res()
                qn_b = [psb_("qn_b%d" % i, [128, 512], BF16) for i in range(2)]
                r_qn = [Res(), Res()]
                kn32 = [psb_("kn32%d" % i, [128, 512], F32) for i in range(2)]
                r_kn32 = [Res(), Res()]
                kn_b = [psb_("kn_b%d" % i, [128, 512], BF16) for i in range(2)]
                r_knb = [Res(), Res()]
                v32 = [psb_("v32%d" % i, [128, 512], F32) for i in range(2)]
                r_v32 = [Res(), Res()]
                vaug = [psb_("vaug%d" % i, [128, 8, 65], BF16) for i in range(2)]
                r_vaug = [Res(), Res()]
                for i in range(2):
                    k.op("pool", lambda i=i: nc.gpsimd.memset(vaug[i][:], 1.0), writes=[r_vaug[i]])

                if s < 2:
                    k.dma("sp", SH1[:], MODS[s:s + 1, 0:D].partition_broadcast(128), reads=[r_MODS], writes=[r_S1])
                    k.dma("sp", S1[:], MODS[s:s + 1, D:2 * D].partition_broadcast(128), reads=[r_MODS], writes=[r_S1])
                else:
                    k.dma("sp", SH1[0:NS, :], MODS[2:2 + NS, 0:D], reads=[r_MODS], writes=[r_S1])
                    k.dma("sp", S1[0:NS, :], MODS[2:2 + NS, D:2 * D], reads=[r_MODS], writes=[r_S1])
                k.op("dve", lambda: nc.vector.scalar_tensor_tensor(S1[0:P, :], S1[0:P, :], 1.0, n1w[0:P, :], ALU.add, ALU.mult),
                     reads=[r_n1w], writes=[r_S1])

                k.dma("sp", xt[0][0:P, :], xsrc(0), writes=[r_xt[0]])
                for t in range(ntile):
                    i = t % 2
                    if t + 1 < ntile:
                        k.dma("sp", xt[1 - i][0:P, :], xsrc(t + 1), writes=[r_xt[1 - i]])
                    k.op("act", lambda: nc.scalar.activation(junk[0:P, :], xt[i][0:P, :], AF.Square, accum_out=ss1[0:P, :]),
                         reads=[r_xt[i]], writes=[r_junk, r_ss1])
                    rstd_from_ss(P, (ss1[0:P, :], r_ss1), D, None)
                    k.op("dve", lambda: nc.vector.scalar_tensor_tensor(xm[0:P, :], xt[i][0:P, :], ss1[0:P, 0:1], S1[0:P, :],
                                                                       ALU.mult, ALU.mult),
                         reads=[r_xt[i], r_ss1, r_S1], writes=[r_xm])
                    k.op("dve", lambda: nc.vector.tensor_tensor(xb[0:P, :], xm[0:P, :], SH1[0:P, :], ALU.add),
                         reads=[r_xm, r_S1], writes=[r_xb])
                    for kc in range(8):
                        k.op("pe", lambda kc=kc: nc.tensor.transpose(psb[:, kc * 128:kc * 128 + P], xb[0:P, kc * 128:(kc + 1) * 128],
                                                                     ident_b[0:P, 0:P]),
                             reads=[r_xb, r_idb], writes=[r_psb])
                    k.op("act", lambda: nc.scalar.copy(hT[:, :, t * 128:t * 128 + P],
                                                       psb[:].rearrange("p (kc n) -> p kc n", kc=8)[:, :, 0:P]),
                         writes=[r_psb, r_hT])

                for g in range(3):
                    win = GROUPS[g][0]
                    for part in range(3):
                        c0 = part * 1536 + g * 512
                        k.dma("pool", Wg[:, :, part * 512:(part + 1) * 512],
                              w_in[:, c0:c0 + 512].rearrange("(kc p) n -> p kc n", p=128), writes=[r_Wg])
                    for t in range(ntile):
                        i = t % 2
                        ts_ = slice(t * 128, t * 128 + P)
                        g0 = tok0(s) + t * 128
                        pr = proj(P, hT, ts_, Wg, 0, 3, r_hT, r_Wg)
                        bank = [p_[0] for p_ in pr]
                        rbank = [p_[1] for p_ in pr]
                        for part in range(2):
                            ps_ = bank[part]
                            k.op("act", lambda: nc.scalar.activation(junk[0:P, 0:512], ps_[0:P, :], AF.Square),
                                 writes=[rbank[part], r_junk])
                            k.op("dve", lambda: nc.vector.tensor_reduce(ss8[0:P, :], junk[0:P, 0:512].rearrange("p (h e) -> p h e", h=8),
                                                                        AX.X, ALU.add),
                                 reads=[r_junk], writes=[r_ss8])
                            rstd_from_ss(P, (ss8[0:P, :], r_ss8), 64, None)
                            dst32 = junk if part == 0 else kn32[i]
                            rdst = r_junk if part == 0 else r_kn32[i]
                            k.op("dve", lambda: nc.vector.tensor_tensor(
                                dst32[0:P, 0:512].rearrange("p (h e) -> p h e", h=8),
                                ps_[0:P, :].rearrange("p (h e) -> p h e", h=8),
                                ss8[0:P, :].unsqueeze(2).to_broadcast([P, 8, 64]), ALU.mult),
                                reads=[r_ss8], writes=[rbank[part], rdst])
                            if part == 0:
                                k.op("dve", lambda: nc.vector.tensor_tensor(qn_b[i][0:P, :], junk[0:P, 0:512], qkw[0:P, 0, :], ALU.mult),
                                     reads=[r_junk, r_qkw], writes=[r_qn[i]])
                            else:
                                k.op("dve", lambda: nc.vector.tensor_tensor(kn32[i][0:P, :], kn32[i][0:P, :], qkw[0:P, 1, :], ALU.mult),
                                     reads=[r_qkw], writes=[r_kn32[i]])
                                k.op("pool", lambda: nc.gpsimd.tensor_copy(kn_b[i][0:P, :], kn32[i][0:P, :]),
                                     reads=[r_kn32[i]], writes=[r_knb[i]])
                        k.op("act", lambda: nc.scalar.copy(v32[i][0:P, :], bank[2][0:P, :]), writes=[rbank[2], r_v32[i]])
                        k.op("pool", lambda: nc.gpsimd.tensor_copy(vaug[i][0:P, :, 0:64],
                                                                    v32[i][0:P, :].rearrange("p (h e) -> p h e", h=8)),
                             reads=[r_v32[i]], writes=[r_vaug[i]])
                        k.flush()

                        def stores(i=i, g=g, g0=g0, t=t, P=P, s=s, win=win):
                            k.dma("sp", QS[g0:g0 + P, g, :], qn_b[i][0:P, :], reads=[r_qn[i]], accum=[r_scr])
                            k.dma("sp", KS[g0:g0 + P, g, :], kn_b[i][0:P, :], reads=[r_knb[i]], accum=[r_scr])
                            k.dma("sp", VS[g0:g0 + P, g, :], vaug[i][0:P, :, :].rearrange("p h e -> p (h e)"),
                                  reads=[r_vaug[i]], accum=[r_scr])
                            if s < 2:
                                r0 = t * 128 - (SEQ - win)
                                if r0 >= 0:
                                    k.dma("sp", kv_p[g][s, r0:r0 + 128, 0, :], kn32[i][:], reads=[r_kn32[i]])
                                    k.dma("sp", kv_p[g][s, r0:r0 + 128, 1, :], v32[i][:], reads=[r_v32[i]])
                            else:
                                k.dma("sp", kv_s[g][:, 0, :], kn32[i][0:P, :], reads=[r_kn32[i]])
                                k.dma("sp", kv_s[g][:, 1, :], v32[i][0:P, :], reads=[r_v32[i]])
                        k.defer(stores)
                    k.flush()
                k.barrier()
            if stop_after <= 1:
                continue

            with contextlib.ExitStack() as ph:
                def psb_(name, shape, dt):
                    return ph.enter_context(nc.sbuf_tensor(name + "_s%d" % s, list(shape), dt))
                Wt = psb_("Wt", [128, 8, 3072], BF16)
                r_Wt = Res()
                for j, c0 in enumerate((7680, 8704, 9728)):
                    k.dma("pool", Wt[:, :, j * 1024:(j + 1) * 1024],
                          w_in[:, c0:c0 + 1024].rearrange("(kc p) n -> p kc n", p=128), writes=[r_Wt])
                ggb = [psb_("ggb%d" % i, [128, 3072], BF16) for i in range(2)]
                r_ggb = [Res(), Res()]
                for t in range(ntile):
                    i = t % 2
                    ts_ = slice(t * 128, t * 128 + P)
                    g0 = tok0(s) + t * 128
                    for j in range(6):
                        pr = proj(P, hT, ts_, Wt, j * 512, 1, r_hT, r_Wt)
                        b_, rb = pr[0]
                        fn_ = AF.Silu if j < 2 else AF.Sigmoid
                        k.op("act", lambda: nc.scalar.activation(ggb[i][0:P, j * 512:(j + 1) * 512], b_[0:P, :], fn_),
                             writes=[rb, r_ggb[i]])
                    k.flush()
                    k.defer(lambda i=i, g0=g0, P=P: k.dma("sp", GG[g0:g0 + P, :], ggb[i][0:P, :], reads=[r_ggb[i]], accum=[r_gg]))
                k.barrier()
            if stop_after <= 2:
                continue

            with contextlib.ExitStack() as ph:
                def psb_(name, shape, dt):
                    return ph.enter_context(nc.sbuf_tensor(name + "_s%d" % s, list(shape), dt))
                Wh = psb_("Wh", [128, 8, 3072], BF16)
                r_Wh = Res()
                for j in range(3):
                    c0 = 4608 + j * 1024
                    k.dma("pool", Wh[:, :, j * 1024:(j + 1) * 1024],
                          w_in[:, c0:c0 + 1024].rearrange("(kc p) n -> p kc n", p=128), writes=[r_Wh])
                lbt = psb_("lbt", [128, D], F32)
                omlt = psb_("omlt", [128, D], F32)
                hgw = psb_("hgw", [128, D], F32)
                r_lb = Res()
                k.dma("sp", lbt[:], lb_log[0:1, :].partition_broadcast(128), writes=[r_lb])
                k.dma("sp", omlt[:], lb_log[1:2, :].partition_broadcast(128), writes=[r_lb])
                k.dma("sp", hgw[:], hgn_w.partition_broadcast(128), writes=[r_lb])
                k.op("dve", lambda: nc.vector.tensor_tensor(lbt[:], lbt[:], omlt[:], ALU.subtract), writes=[r_lb])
                k.op("act", lambda: nc.scalar.activation(lbt[:], lbt[:], AF.Sigmoid), writes=[r_lb])
                k.op("dve", lambda: nc.vector.tensor_scalar(omlt[:], lbt[:], -1.0, 1.0, ALU.mult, ALU.add), writes=[r_lb])
                logf = psb_("logf", [128, D], F32)
                kk = psb_("kk", [128, D], F32)
                et = psb_("et", [128, D], F32)
                t2 = psb_("t2", [128, D], F32)
                r_logf, r_kk, r_et, r_t2 = Res(), Res(), Res(), Res()
                kt_b = psb_("kt_b", [128, D], BF16)
                kh_b = psb_("kh_b", [128, D], BF16)
                qt_b = psb_("qt_b", [128, D], BF16)
                v_b = psb_("v_b", [128, D], BF16)
                r_ktb, r_khb, r_qtb, r_vb = Res(), Res(), Res(), Res()
                gt_b = psb_("gt_b", [128, D], BF16)
                r_gtb = Res()
                hg_b = [psb_("hg_b%d" % i, [128, D], BF16) for i in range(2)]
                r_hgb = [Res(), Res()]
                ss8 = psb_("hss8", [128, 8], F32)
                r_ss8 = Res()
                if s < 2:
                    qT = psb_("qT", [128, 8, 128], BF16)
                    kT = psb_("kT", [128, 8, 128], BF16)
                    r_qT, r_kT = Res(), Res()
                    Abd = psb_("Abd", [128, 8, 128], BF16)
                    r_Abd = Res()
                    k.op("pool", lambda: nc.gpsimd.memset(Abd[:], 0.0), writes=[r_Abd])
                    Sm = psb_("Sm", [128, 8, 128], F32)
                    St = psb_("St", [128, 8, 128], F32)
                    Sb = [psb_("Sb%d" % i, [128, 8, 128], BF16) for i in range(2)]
                    r_Sm, r_St = Res(), Res()
                    r_Sb = [Res(), Res()]
                    eb = psb_("eb", [128, 8, 4], F32)
                    r_eb = Res()
                    k.op("pool", lambda: nc.gpsimd.memset(Sm[:], 0.0), writes=[r_Sm])
                    k.op("pool", lambda: nc.gpsimd.memset(Sb[0][:], 0.0), writes=[r_Sb[0]])
                else:
                    selc_sb = psb_("selc_sb", [NS, 2048], F32)
                    k.dma("sp", selc_sb[:], selc[:, 0:2048], writes=[r_cst])
                    fT = psb_("fT", [128, 3, 8, NS], F32)
                    r_fT = Res()
                    v32s = psb_("v32s", [NS, D], F32)
                    r_v32s = Res()
                    QZ = psb_("QZ", [128, 8, NS * NS], F32)
                    r_QZ = Res()
                    k.op("pool", lambda: nc.gpsimd.memset(QZ[:], 0.0), writes=[r_QZ])
                    S0b = [psb_("S0b%d" % i, [128, 8, 128], F32) for i in range(2)]
                    r_S0b = [Res(), Res()]
                    Sn = [psb_("Sn%d" % i, [128, 8, 128], F32) for i in range(2)]
                    r_Sn = [Res(), Res()]

                def hg_epilogue(P, po, i, g0):
                    for hb in range(2):
                        b_, rb = po[hb]
                        k.op("act", lambda: nc.scalar.activation(t2[0:P, hb * 512:(hb + 1) * 512], b_[0:P, :], AF.Square),
                             writes=[rb, r_t2])
                    k.op("dve", lambda: nc.vector.tensor_reduce(ss8[0:P, :], t2[0:P, :].rearrange("p (h e) -> p h e", h=8), AX.X, ALU.add),
                         reads=[r_t2], writes=[r_ss8])
                    rstd_from_ss(P, (ss8[0:P, :], r_ss8), 128, None)
                    for hb in range(2):
                        b_, rb = po[hb]
                        k.op("dve", lambda: nc.vector.tensor_tensor(
                            t2[0:P, hb * 512:(hb + 1) * 512].rearrange("p (h e) -> p h e", h=4),
                            b_[0:P, :].rearrange("p (h e) -> p h e", h=4),
                            ss8[0:P, hb * 4:(hb + 1) * 4].unsqueeze(2).to_broadcast([P, 4, 128]), ALU.mult),
                            reads=[r_ss8], writes=[rb, r_t2])
                    k.op("dve", lambda: nc.vector.tensor_tensor(t2[0:P, :], t2[0:P, :], gt_b[0:P, :], ALU.mult),
                         reads=[r_gtb], writes=[r_t2])
                    k.op("dve", lambda: nc.vector.tensor_tensor(hg_b[i][0:P, :], t2[0:P, :], hgw[0:P, :], ALU.mult),
                         reads=[r_t2, r_lb], writes=[r_hgb[i]])
                    k.flush()
                    k.defer(lambda: k.dma("sp", HGS[g0:g0 + P, :], hg_b[i][0:P, :], reads=[r_hgb[i]], accum=[r_hgs]))

                for t in range(ntile):
                    i = t % 2
                    ts_ = slice(t * 128, t * 128 + P)
                    g0 = tok0(s) + t * 128
                    k.dma("sp", gt_b[0:P, :], GG[g0:g0 + P, 0:D], reads=[r_gg], writes=[r_gtb])
                    pr = proj(P, hT, ts_, Wh, 1024, 2, r_hT, r_Wh)
                    for hb in range(2):
                        b_, rb = pr[hb]
                        k.op("act", lambda: nc.scalar.activation(logf[0:P, hb * 512:(hb + 1) * 512], b_[0:P, :], AF.Sigmoid),
                             writes=[rb, r_logf])
                    k.op("dve", lambda: nc.vector.tensor_tensor(logf[0:P, :], logf[0:P, :], omlt[0:P, :], ALU.mult), reads=[r_lb], writes=[r_logf])
                    k.op("dve", lambda: nc.vector.tensor_tensor(logf[0:P, :], logf[0:P, :], lbt[0:P, :], ALU.add), reads=[r_lb], writes=[r_logf])
                    k.op("dve", lambda: nc.vector.tensor_scalar(kk[0:P, :], logf[0:P, :], -1.0, 1.0, ALU.mult, ALU.add),
                         reads=[r_logf], writes=[r_kk])
                    if s < 2:
                        k.op("act", lambda: nc.scalar.activation(logf[0:P, :], logf[0:P, :], AF.Ln), reads=[r_kk], writes=[r_logf])
                    if s == 2:
                        k.op("pool", lambda: nc.gpsimd.tensor_copy(et[0:P, :], logf[0:P, :]), reads=[r_logf], writes=[r_et])
                        pq = proj(P, hT, ts_, Wh, 0, 2, r_hT, r_Wh)
                        for hb in range(2):
                            b_, rb = pq[hb]
                            k.op("act", lambda: nc.scalar.activation(t2[0:P, hb * 512:(hb + 1) * 512], b_[0:P, :], AF.Silu),
                                 writes=[rb, r_t2])
                        pv = proj(P, hT, ts_, Wh, 2048, 2, r_hT, r_Wh)
                        for hb in range(2):
                            b_, rb = pv[hb]
                            k.op("act", lambda: nc.scalar.copy(v32s[0:P, hb * 512:(hb + 1) * 512], b_[0:P, :]), writes=[rb, r_v32s])
                        for qi, (src_, rs_) in enumerate(((et, r_et), (kk, r_kk), (t2, r_t2))):
                            b_, rb = nb()
                            for h in range(8):
                                k.op("pe", lambda h=h: nc.tensor.transpose(b_[:, h * NS:(h + 1) * NS], src_[0:P, h * 128:(h + 1) * 128],
                                                                            ident_f[0:P, 0:P]),
                                     reads=[rs_, r_cst], writes=[rb])
                            k.op("act", lambda: nc.scalar.copy(fT[:, qi, :, :], b_[:, 0:8 * NS].rearrange("p (h b) -> p h b", h=8)),
                                 writes=[rb, r_fT])
                        k.op("dve", lambda: nc.vector.tensor_copy(QZ[:, :, 0:NS * NS:NS + 1], fT[:, 2, :, :]), reads=[r_fT], writes=[r_QZ])
                        po = [(psf[5], r_psf[5]), (psf[6], r_psf[6])]
                        for b in range(NS):
                            ib = b % 2
                            k.dma("sp", S0b[ib][:], st_in[b].rearrange("h k v -> k h v"), writes=[r_S0b[ib]])
                            pvb = [(psf[2 * ib], r_psf[2 * ib]), (psf[2 * ib + 1], r_psf[2 * ib + 1])]
                            for hb in range(2):
                                b_, rb = pvb[hb]
                                k.op("pe", lambda: nc.tensor.matmul(b_[:, :], selc_sb[0:NS, b * 128:(b + 1) * 128], v32s[0:NS, hb * 512:(hb + 1) * 512],
                                                                    start=True, stop=True),
                                     reads=[r_v32s, r_cst], writes=[rb])
                            k.op("dve", lambda: nc.vector.tensor_tensor(Sn[ib][:], S0b[ib][:],
                                                                        fT[:, 0, :, b:b + 1].to_broadcast([128, 8, 128]), ALU.mult),
                                 reads=[r_S0b[ib], r_fT], writes=[r_Sn[ib]])
                            for hb in range(2):
                                b_, rb = pvb[hb]
                                k.op("dve", lambda: nc.vector.tensor_tensor(
                                    S0b[ib][:, hb * 4:(hb + 1) * 4, :], b_[:, :].rearrange("p (h v) -> p h v", h=4),
                                    fT[:, 1, hb * 4:(hb + 1) * 4, b:b + 1].to_broadcast([128, 4, 128]), ALU.mult),
                                    reads=[r_fT], writes=[rb, r_S0b[ib]])
                            k.op("dve", lambda: nc.vector.tensor_tensor(Sn[ib][:], Sn[ib][:], S0b[ib][:], ALU.add),
                                 reads=[r_S0b[ib]], writes=[r_Sn[ib]])
                            k.dma("sp", hg_s[b].rearrange("h k v -> k h v"), Sn[ib][:], reads=[r_Sn[ib]])
                            for h in range(8):
                                b_, rb = po[h // 4]
                                k.op("pe", lambda h=h: nc.tensor.matmul(b_[0:NS, (h % 4) * 128:(h % 4 + 1) * 128],
                                                                        QZ[:, h, b * NS:(b + 1) * NS], Sn[ib][:, h, :],
                                                                        start=(b == 0 and h % 4 == 0), stop=(b == NS - 1),
                                                                        skip_group_check=True),
                                     reads=[r_QZ, r_Sn[ib]], writes=[rb])
                        hg_epilogue(P, po, i, g0)
                        continue
                    d1 = [nb(), nb()]
                    d3 = [nb(), nb()]
                    for hb in range(2):
                        k.op("pe", lambda: nc.tensor.matmul(d1[hb][0][:, :], L1, logf[:, hb * 512:(hb + 1) * 512], start=True, stop=True),
                             reads=[r_logf, r_cst], writes=[d1[hb][1]])
                        k.op("pe", lambda: nc.tensor.matmul(d3[hb][0][:, :], L3, logf[:, hb * 512:(hb + 1) * 512], start=True, stop=True),
                             reads=[r_logf, r_cst], writes=[d3[hb][1]])
                    for hb in range(2):
                        k.op("act", lambda: nc.scalar.activation(et[:, hb * 512:(hb + 1) * 512], d1[hb][0][:, :], AF.Exp, scale=-1.0),
                             writes=[d1[hb][1], r_et])
                    k.op("dve", lambda: nc.vector.tensor_tensor(kt_b[:], kk[:], et[:], ALU.mult), reads=[r_kk, r_et], writes=[r_ktb])
                    for hb in range(2):
                        k.op("act", lambda: nc.scalar.activation(et[:, hb * 512:(hb + 1) * 512], d3[hb][0][:, :], AF.Exp),
                             writes=[d3[hb][1], r_et])
                    k.op("dve", lambda: nc.vector.tensor_tensor(kh_b[:], kk[:], et[:], ALU.mult), reads=[r_kk, r_et], writes=[r_khb])
                    for hb in range(2):
                        k.op("act", lambda: nc.scalar.activation(et[:, hb * 512:(hb + 1) * 512], d1[hb][0][:, :], AF.Exp),
                             writes=[d1[hb][1], r_et])
                    pq = proj(P, hT, ts_, Wh, 0, 2, r_hT, r_Wh)
                    for hb in range(2):
                        b_, rb = pq[hb]
                        k.op("act", lambda: nc.scalar.activation(t2[:, hb * 512:(hb + 1) * 512], b_[:, :], AF.Silu), writes=[rb, r_t2])
                    k.op("dve", lambda: nc.vector.tensor_tensor(qt_b[:], t2[:], et[:], ALU.mult), reads=[r_t2, r_et], writes=[r_qtb])
                    pv = proj(P, hT, ts_, Wh, 2048, 2, r_hT, r_Wh)
                    for hb in range(2):
                        b_, rb = pv[hb]
                        k.op("act", lambda: nc.scalar.copy(v_b[:, hb * 512:(hb + 1) * 512], b_[:, :]), writes=[rb, r_vb])
                    be, rbe = nb()
                    for h in range(8):
                        k.op("pe", lambda h=h: nc.tensor.matmul(be[:, h * 4:(h + 1) * 4], logf[:, h * 128:(h + 1) * 128], R4,
                                                                start=(h == 0), stop=(h == 7), skip_group_check=True),
                             reads=[r_logf, r_cst], writes=[rbe])
                    k.op("act", lambda: nc.scalar.activation(eb[:], be[:, 0:32].rearrange("p (h c) -> p h c", h=8), AF.Exp),
                         writes=[rbe, r_eb])
                    for src_, rs_, dst_, rd_ in ((qt_b, r_qtb, qT, r_qT), (kt_b, r_ktb, kT, r_kT)):
                        for h in range(8):
                            k.op("pe", lambda h=h: nc.tensor.transpose(psb[:, h * 128:(h + 1) * 128], src_[:, h * 128:(h + 1) * 128], ident_b[:]),
                                 reads=[rs_, r_idb], writes=[r_psb])
                        k.op("act", lambda: nc.scalar.copy(dst_[:], psb[:].rearrange("p (h n) -> p h n", h=8)), writes=[r_psb, rd_])
                    pa = [nb(), nb()]
                    for h in range(8):
                        b_, rb = pa[h // 4]
                        c_ = (h % 4) * 128
                        k.op("pe", lambda h=h: nc.tensor.matmul(b_[0:64, c_:c_ + 64], kT[:, h, 0:64], qT[:, h, 0:64], start=True, stop=True,
                                                                skip_group_check=True),
                             reads=[r_kT, r_qT], writes=[rb])
                        k.op("pe", lambda h=h: nc.tensor.matmul(b_[:, c_ + 64:c_ + 128], kT[:, h, :], qT[:, h, 64:128], start=True, stop=True,
                                                                skip_group_check=True),
                             reads=[r_kT, r_qT], writes=[rb])
                    for hb in range(2):
                        b_, rb = pa[hb]
                        bv = b_[:, :].rearrange("p (h t) -> p h t", h=4)
                        k.op("dve", lambda: nc.vector.tensor_tensor(Abd[0:64, hb * 4:(hb + 1) * 4, 0:64], bv[0:64, :, 0:64],
                                                                    MA[0:64, 0:64].unsqueeze(1).to_broadcast([64, 4, 64]), ALU.mult),
                             reads=[r_cst], writes=[rb, r_Abd])
                        k.op("dve", lambda: nc.vector.tensor_tensor(Abd[64:128, hb * 4:(hb + 1) * 4, 64:128], bv[64:128, :, 64:128],
                                                                    MA[64:128, 64:128].unsqueeze(1).to_broadcast([64, 4, 64]), ALU.mult),
                             reads=[r_cst], writes=[rb, r_Abd])
                    k.op("dve", lambda: nc.vector.tensor_tensor(Sb[0][:], Sm[:], eb[:, :, 2:3].to_broadcast([128, 8, 128]), ALU.mult),
                         reads=[r_Sm, r_eb], writes=[r_Sb[0]])
                    for c in range(2):
                        src_S, rsrc = (Sm, r_Sm) if c == 0 else (St, r_St)
                        dst_S, rdst = (St, r_St) if c == 0 else (Sm, r_Sm)
                        psn = [nb(), nb()]
                        for h in range(8):
                            b_, rb = psn[h // 4]
                            c_ = (h % 4) * 128
                            k.op("pe", lambda h=h: nc.tensor.matmul(b_[:, c_:c_ + 128], kh_b[c * 64:(c + 1) * 64, h * 128:(h + 1) * 128],
                                                                    v_b[c * 64:(c + 1) * 64, h * 128:(h + 1) * 128], start=True, stop=True,
                                                                    skip_group_check=True),
                                 reads=[r_khb, r_vb], writes=[rb])
                        k.op("dve", lambda: nc.vector.tensor_tensor(dst_S[:], src_S[:], eb[:, :, c:c + 1].to_broadcast([128, 8, 128]), ALU.mult),
                             reads=[rsrc, r_eb], writes=[rdst])
                        for hb in range(2):
                            b_, rb = psn[hb]
                            k.op("dve", lambda: nc.vector.tensor_tensor(dst_S[:, hb * 4:(hb + 1) * 4, :], dst_S[:, hb * 4:(hb + 1) * 4, :],
                                                                        b_[:, :].rearrange("p (h v) -> p h v", h=4), ALU.add),
                                 writes=[rb, rdst])
                        if c == 0:
                            k.op("dve", lambda: nc.vector.tensor_tensor(Sb[1][:], St[:], eb[:, :, 3:4].to_broadcast([128, 8, 128]), ALU.mult),
                                 reads=[r_St, r_eb], writes=[r_Sb[1]])
                    po = [nb(), nb()]
                    for h in range(8):
                        b_, rb = po[h // 4]
                        c_ = (h % 4) * 128
                        k.op("pe", lambda h=h: nc.tensor.matmul(b_[:, c_:c_ + 128], Abd[:, h, :], v_b[:, h * 128:(h + 1) * 128],
                                                                start=True, stop=False, skip_group_check=True),
                             reads=[r_Abd, r_vb], writes=[rb])
                        k.op("pe", lambda h=h: nc.tensor.matmul(b_[0:64, c_:c_ + 128], qT[:, h, 0:64], Sb[0][:, h, :],
                                                                start=False, stop=False, skip_group_check=True),
                             reads=[r_qT, r_Sb[0]], writes=[rb])
                        k.op("pe", lambda h=h: nc.tensor.matmul(b_[64:128, c_:c_ + 128], qT[:, h, 64:128], Sb[1][:, h, :],
                                                                start=False, stop=True, skip_group_check=True),
                             reads=[r_qT, r_Sb[1]], writes=[rb])
                    hg_epilogue(P, po, i, g0)
                k.flush()
                if s < 2:
                    k.dma("sp", hg_p[s].rearrange("h k v -> k h v"), Sm[:], reads=[r_Sm])
                k.barrier()
    if stop_after <= 3:
        k.finish()
        return


    with contextlib.ExitStack() as ph:
        def psb_(name, shape, dt):
            return ph.enter_context(nc.sbuf_tensor(name, list(shape), dt))
        qblk = [psb_("qblk%d" % i, [128, 512], BF16) for i in range(2)]
        kblk = [psb_("kblk%d" % i, [128, 512], BF16) for i in range(2)]
        vblk = [psb_("vblk%d" % i, [128, 8, 65], BF16) for i in range(2)]
        r_qblk, r_kblk, r_vblk = [Res(), Res()], [Res(), Res()], [Res(), Res()]
        qTa = psb_("qTa", [128, 4, 128], BF16)
        kTa = [psb_("kTa%d" % i, [128, 4, 128], BF16) for i in range(2)]
        r_qTa = Res()
        r_kTa = [Res(), Res()]
        pT = [psb_("pT%d" % i, [128, 4, 128], BF16) for i in range(4)]
        r_pT = [Res() for _ in range(4)]
        oac = [psb_("oac%d" % i, [128, 520], F32) for i in range(2)]
        r_oac = [Res(), Res()]
        for i in range(2):
            k.op("pool", lambda i=i: nc.gpsimd.memset(qblk[i][:], 0.0), writes=[r_qblk[i]])
            k.op("pool", lambda i=i: nc.gpsimd.memset(kblk[i][:], 0.0), writes=[r_kblk[i]])
            k.op("pool", lambda i=i: nc.gpsimd.memset(vblk[i][:], 1.0), writes=[r_vblk[i]])
        blk_ctr = [0]
        for c_ in range(8):
            rs_ = slice(c_ * 2048, (c_ + 1) * 2048)
            k.dma("pool", UV[rs_, 0:D], peer_u[rs_, :], accum=[r_uv])
            k.dma("pool", UV[rs_, D:2 * D], peer_v[rs_, :], accum=[r_uv])

        def attn_block(load_cur, has_prev, ip, store):
            n_ = blk_ctr[0]
            blk_ctr[0] += 1
            ic = 1 - ip
            load_cur(ic)
            for src_, rs_, dst_, rd_ in ((qblk[ic], r_qblk[ic], qTa, r_qTa), (kblk[ic], r_kblk[ic], kTa[ic], r_kTa[ic])):
                for hp in range(4):
                    k.op("pe", lambda hp=hp: nc.tensor.transpose(psb[:, hp * 128:(hp + 1) * 128], src_[:, hp * 128:(hp + 1) * 128], ident_b[:]),
                         reads=[rs_, r_idb], writes=[r_psb])
                k.op("act", lambda: nc.scalar.copy(dst_[:], psb[:, 0:512].rearrange("p (h n) -> p h n", h=4)), writes=[r_psb, rd_])
            srcs = [(ic, maskc_b)] + ([(ip, maskp_b)] if has_prev else [])
            pts = []
            for si, (ib, msk) in enumerate(srcs):
                for hb in range(2):
                    b_, rb = nb()
                    for hh in range(4):
                        h = 2 * hh + hb
                        po_ = hb * 64
                        k.op("pe", lambda: nc.tensor.matmul(b_[:, hh * 128:(hh + 1) * 128], kTa[ib][po_:po_ + 64, h // 2, :],
                                                            qTa[po_:po_ + 64, h // 2, :], start=True, stop=True, skip_group_check=True),
                             reads=[r_kTa[ib], r_qTa], writes=[rb])
                    pi = si * 2 + hb
                    k.op("act", lambda: nc.scalar.activation(pT[pi][:], b_[:, :].rearrange("p (h n) -> p h n", h=4), AF.Exp),
                         writes=[rb, r_pT[pi]])
                    k.op("dve", lambda: nc.vector.tensor_tensor(pT[pi][:], pT[pi][:], msk[:], ALU.mult), reads=[r_mask], writes=[r_pT[pi]])
                    pts.append((pi, ib))
            io = n_ % 2
            for hb in range(2):
                b_, rb = nb()
                for hh in range(4):
                    h = hb * 4 + hh
                    for si, (ib, msk) in enumerate(srcs):
                        pi = si * 2 + (h % 2)
                        k.op("pe", lambda: nc.tensor.matmul(b_[:, hh * 65:(hh + 1) * 65], pT[pi][:, h // 2, :], vblk[ib][:, h, :],
                                                            start=(si == 0), stop=(si == len(srcs) - 1), skip_group_check=True),
                             reads=[r_pT[pi], r_vblk[ib]], writes=[rb])
                k.op("act", lambda: nc.scalar.copy(oac[io][:, hb * 260:(hb + 1) * 260], b_[:, 0:260]), writes=[rb, r_oac[io]])
            k.flush()
            k.defer(lambda io=io, store=store: store(oac[io], r_oac[io]))
            return ic

        for s in range(2):
            for g in range(3):
                d = GROUPS[g][1]
                nblk = SEQ // (128 * d)

                def view(T, width):
                    return T[tok0(s):tok0(s) + SEQ, g, :].rearrange("(n j dd) c -> dd n j c", dd=d, j=128)
                Qv, Kv, Vv = view(QS, 512), view(KS, 512), view(VS, 520)
                Ov = OACC[g, tok0(s):tok0(s) + SEQ, :].rearrange("(n j dd) c -> dd n j c", dd=d, j=128)
                for r in range(d):
                    ip = 0
                    for n in range(nblk):
                        def load_cur(ic, r=r, n=n, Qv=Qv, Kv=Kv, Vv=Vv):
                            k.dma("sp", qblk[ic][:], Qv[r, n], reads=[r_scr], writes=[r_qblk[ic]])
                            k.dma("sp", kblk[ic][:], Kv[r, n], reads=[r_scr], writes=[r_kblk[ic]])
                            k.dma("sp", vblk[ic][:].rearrange("p h e -> p (h e)"), Vv[r, n], reads=[r_scr], writes=[r_vblk[ic]])

                        def store(o_, ro_, r=r, n=n, Ov=Ov):
                            k.dma("sp", Ov[r, n], o_[:], reads=[ro_], accum=[r_oacc])
                        ip = attn_block(load_cur, n > 0, ip, store)
        for b in range(NS):
            for g in range(3):
                tokb = tok0(2) + b
                ip = 0
                k.dma("pool", kblk[ip][:], ck[g][b, :, 0, :], writes=[r_kblk[ip]])
                k.dma("pool", vblk[ip][:, :, 0:64], ck[g][b, :, 1, :].rearrange("j (h e) -> j h e", h=8), writes=[r_vblk[ip]])
                for hp in range(4):
                    k.op("pe", lambda hp=hp: nc.tensor.transpose(psb[:, hp * 128:(hp + 1) * 128], kblk[ip][:, hp * 128:(hp + 1) * 128], ident_b[:]),
                         reads=[r_kblk[ip], r_idb], writes=[r_psb])
                k.op("act", lambda: nc.scalar.copy(kTa[ip][:], psb[:, 0:512].rearrange("p (h n) -> p h n", h=4)), writes=[r_psb, r_kTa[ip]])

                def load_cur(ic, g=g, tokb=tokb):
                    k.dma("sp", qblk[ic][0:1, :], QS[tokb:tokb + 1, g, :], reads=[r_scr], writes=[r_qblk[ic]])
                    k.dma("sp", kblk[ic][0:1, :], KS[tokb:tokb + 1, g, :], reads=[r_scr], writes=[r_kblk[ic]])
                    k.dma("sp", vblk[ic][0:1, :, :].rearrange("p h e -> p (h e)"), VS[tokb:tokb + 1, g, :], reads=[r_scr], writes=[r_vblk[ic]])

                def store(o_, ro_, g=g, tokb=tokb):
                    k.dma("sp", OACC[g, tokb:tokb + 1, :], o_[0:1, :], reads=[ro_], accum=[r_oacc])
                attn_block(load_cur, True, ip, store)
        k.barrier()
    if stop_after <= 4:
        k.finish()
        return

    with contextlib.ExitStack() as ph:
        def psb_(name, shape, dt):
            return ph.enter_context(nc.sbuf_tensor(name, list(shape), dt))
        Wa = psb_("Wa", [128, 4, D], BF16)
        Wb = psb_("Wb", [128, 8, D], BF16)
        Wo = psb_("Wo", [128, 8, D], BF16)
        Wq = psb_("Wq", [128, 8, 2048], BF16)
        skt = psb_("skt", [128, 2, 128], F32)
        r_W = Res()
        k.dma("pool", Wa[:], w_br_a.rearrange("(kc p) n -> p kc n", p=128), writes=[r_W])
        k.dma("pool", Wb[:], w_br_b.rearrange("(kc p) n -> p kc n", p=128), writes=[r_W])
        k.dma("pool", Wo[:], w_o.rearrange("(kc p) n -> p kc n", p=128), writes=[r_W])
        k.dma("pool", Wq[:], w_pq.rearrange("(kc p) n -> p kc n", p=128), writes=[r_W])
        k.dma("sp", skt[:], skT.rearrange("t e c -> e t c"), writes=[r_W])
        n2w = psb_("n2w", [128, D], F32)
        k.dma("sp", n2w[:], norm2_w.partition_broadcast(128), writes=[r_W])
        iota16 = psb_("iota16", [128, 16], F32)
        k.dma("sp", iota16[:], selc[0:1, 2048:2064].partition_broadcast(128), writes=[r_W])
        G1 = psb_("G1", [128, D], F32)
        S2 = psb_("S2", [128, D], F32)
        SH2 = psb_("SH2", [128, D], F32)
        G2s = [psb_("G2_%d" % i, [128, D], F32) for i in range(2)]
        r_M = Res()
        xin = [psb_("xin0", [128, D], F32)] * 2
        r_xin = [Res()] * 2
        oin = [psb_("oin0", [128, 3, 520], F32)] * 2
        r_oin = [Res()] * 2
        hgin = [psb_("hgin0", [128, D], BF16)] * 2
        r_hgin = [Res()] * 2
        ggin = [psb_("ggin0", [128, 2048], BF16)] * 2
        r_ggin = [Res()] * 2
        rl = psb_("rl", [128, 8], F32)
        r_rl = Res()
        att_b = psb_("att_b", [128, 512], BF16)
        r_attb = Res()
        TT = psb_("TT", [128, 8, 128], BF16)
        r_TT = Res()
        f1 = psb_("f1", [128, D], F32)
        r_f1 = Res()
        yb = psb_("yb", [128, D], BF16)
        r_yb = Res()
        x1s = [psb_("x1_%d" % i, [128, D], F32) for i in range(2)]
        r_x1s = [Res(), Res()]
        h2bs = [psb_("h2b_%d" % i, [128, D], BF16) for i in range(2)]
        r_h2bs = [Res(), Res()]
        ssq = psb_("ssq", [128, 1], F32)
        r_ssq = Res()
        sc = psb_("sc", [128, 16, 128], F32)
        sc2 = psb_("sc2", [128, 16, 128], F32)
        r_sc, r_sc2 = Res(), Res()
        q2T = sc2
        r_q2T = r_sc2
        vals = psb_("vals", [128, 16, 16], F32)
        idxu = psb_("idxu", [128, 16, 16], U32)
        idxf = psb_("idxf", [128, 16, 16], F32)
        r_vals, r_idxu, r_idxf = Res(), Res(), Res()
        cand = sc[:].rearrange("p a b -> p (a b)").rearrange("p (h x) -> p h x", h=8)
        cand2 = sc2[:].rearrange("p a b -> p (a b)").rearrange("p (h x) -> p h x", h=8)
        r_cand, r_cand2 = r_sc, r_sc2
        tv = psb_("tv", [128, 8, 16], F32)
        posu = psb_("posu", [128, 8, 16], U32)
        pa_u = psb_("pa_u", [128, 8, 16], U32)
        pa_f = psb_("pa_f", [128, 2, 8, 16], F32)
        r_tv, r_posu, r_pau, r_paf = Res(), Res(), Res(), Res()
        oh = cand2
        r_oh = r_sc2
        isel = psb_("isel", [128, 2, 8, 16], F32)
        r_isel = Res()
        eid_f = psb_("eid_f", [128, 128], F32)
        eids = [psb_("eid%d" % i, [128, 128], I32) for i in range(2)]
        r_eids = [Res(), Res()]
        r_eidf = Res()
        gsm = psb_("gsm", [128, 8], F32)
        gats = [psb_("gat%d" % i, [128, 8, 16], F32) for i in range(2)]
        r_gats = [Res(), Res()]
        dots = psb_("dots", [128, 128], F32)
        r_dots = Res()
        coef = psb_("coef", [128, 128], F32)
        r_coef = Res()
        NUV = 8
        uvb = [psb_("uvb%d" % i, [128, 2 * D], BF16) for i in range(NUV)]
        r_uvb = [Res() for _ in range(NUV)]
        r_dotg = [Res() for _ in range(32)]
        r_coefg = [Res() for _ in range(32)]
        dg = [psb_("dg%d" % i, [128, 128], BF16) for i in range(4)]
        r_dg = [Res() for _ in range(4)]
        jb = psb_("jb", [128, D], BF16)
        r_jb = Res()
        yo = [psb_("yo0", [128, D], F32)] * 2
        r_yo = [Res()] * 2

        def transpose_to_TT(P, src, rsrc, nk):
            yield
            for kc in range(nk):
                k.op("pe", lambda kc=kc: nc.tensor.transpose(psb[:, kc * 128:kc * 128 + P], src[0:P, kc * 128:(kc + 1) * 128], ident_b[0:P, 0:P]),
                     reads=[rsrc, r_idb], writes=[r_psb])
            yield
            k.op("act", lambda: nc.scalar.copy(TT[:, 0:nk, 0:P], psb[:, 0:nk * 128].rearrange("p (kc n) -> p kc n", kc=nk)[:, :, 0:P]),
                 writes=[r_psb, r_TT])
            yield

        def mm_tok(P, W, nk, nbank, rW):
            out = []
            for j in range(nbank):
                b_, rb = nb()
                for kc in range(nk):
                    k.op("pe", lambda kc=kc: nc.tensor.matmul(b_[0:P, :], TT[:, kc, 0:P], W[:, kc, j * 512:(j + 1) * 512],
                                                              start=(kc == 0), stop=(kc == nk - 1)),
                         reads=[r_TT, rW], writes=[rb])
                out.append((b_, rb))
            yield
            return out

        tiles = [(s, t) for s in range(2) for t in range(NT)] + [(2, 0)]
        nbank_rot[0] = 5
        cur_s = [-1]

        def loads(idx):
            s, t = tiles[idx]
            P = 128 if s < 2 else NS
            i = idx % 2
            g0 = tok0(s) + t * 128
            k.dma("sp", xin[i][0:P, :], xp[s, t * 128:(t + 1) * 128, :] if s < 2 else xs[:, :], writes=[r_xin[i]])
            for g in range(3):
                k.dma("sp", oin[i][0:P, g, :], OACC[g, g0:g0 + P, :], reads=[r_oacc], writes=[r_oin[i]])
            k.dma("sp", hgin[i][0:P, :], HGS[g0:g0 + P, :], reads=[r_hgs], writes=[r_hgin[i]])
            k.dma("sp", ggin[i][0:P, :], GG[g0:g0 + P, 1024:3072], reads=[r_gg], writes=[r_ggin[i]])

        loads(0)
        def front(idx):
            s, t = tiles[idx]
            P = 128 if s < 2 else NS
            i = idx % 2
            pp = idx % 2
            x1, r_x1 = x1s[pp], r_x1s[pp]
            h2b, r_h2b = h2bs[pp], r_h2bs[pp]
            eid, r_eid = eids[pp], r_eids[pp]
            gat, r_gat = gats[pp], r_gats[pp]
            G2 = G2s[s % 2]
            yield
            if s != cur_s[0]:
                cur_s[0] = s
                for dst_, c0 in ((G1, 2 * D), (SH2, 3 * D), (S2, 4 * D), (G2, 5 * D)):
                    if s < 2:
                        k.dma("sp", dst_[:], MODS[s:s + 1, c0:c0 + D].partition_broadcast(128), reads=[r_MODS], writes=[r_M])
                    else:
                        k.dma("sp", dst_[0:NS, :], MODS[2:2 + NS, c0:c0 + D], reads=[r_MODS], writes=[r_M])
                k.op("dve", lambda: nc.vector.scalar_tensor_tensor(S2[0:P, :], S2[0:P, :], 1.0, n2w[0:P, :], ALU.add, ALU.mult),
                     reads=[r_W], writes=[r_M])
            yield
            o0 = oin[i]
            k.op("dve", lambda: nc.vector.tensor_tensor(o0[0:P, 0, :], o0[0:P, 0, :], o0[0:P, 1, :], ALU.add), writes=[r_oin[i]])
            k.op("dve", lambda: nc.vector.tensor_tensor(o0[0:P, 0, :], o0[0:P, 0, :], o0[0:P, 2, :], ALU.add), writes=[r_oin[i]])
            ov = o0[0:P, 0, :].rearrange("p (h e) -> p h e", h=8)
            k.op("dve", lambda: nc.vector.reciprocal(rl[0:P, :], ov[:, :, 64]), reads=[r_oin[i]], writes=[r_rl])
            k.op("dve", lambda: nc.vector.tensor_tensor(att_b[0:P, :].rearrange("p (h e) -> p h e", h=8), ov[:, :, 0:64],
                                                        rl[0:P, :].unsqueeze(2).to_broadcast([P, 8, 64]), ALU.mult),
                 reads=[r_oin[i], r_rl], writes=[r_attb])
            yield from transpose_to_TT(P, att_b, r_attb, 4)
            pA = yield from mm_tok(P, Wa, 4, 2, r_W)
            for hb in range(2):
                b_, rb = pA[hb]
                k.op("dve", lambda: nc.vector.tensor_tensor(f1[0:P, hb * 512:(hb + 1) * 512], b_[0:P, :], ggin[i][0:P, hb * 512:(hb + 1) * 512], ALU.mult),
                     reads=[r_ggin[i]], writes=[rb, r_f1])
            yield
            yield from transpose_to_TT(P, hgin[i], r_hgin[i], 8)
            pB = yield from mm_tok(P, Wb, 8, 2, r_W)
            for hb in range(2):
                b_, rb = pB[hb]
                k.op("dve", lambda: nc.vector.tensor_tensor(x1[0:P, hb * 512:(hb + 1) * 512], b_[0:P, :],
                                                            ggin[i][0:P, 1024 + hb * 512:1024 + (hb + 1) * 512], ALU.mult),
                     reads=[r_ggin[i]], writes=[rb, r_x1])
            k.op("dve", lambda: nc.vector.tensor_tensor(yb[0:P, :], f1[0:P, :], x1[0:P, :], ALU.add), reads=[r_f1, r_x1], writes=[r_yb])
            yield
            yield from transpose_to_TT(P, yb, r_yb, 8)
            pZ = yield from mm_tok(P, Wo, 8, 2, r_W)
            for hb in range(2):
                b_, rb = pZ[hb]
                k.op("dve", lambda: nc.vector.tensor_tensor(x1[0:P, hb * 512:(hb + 1) * 512], b_[0:P, :], G1[0:P, hb * 512:(hb + 1) * 512], ALU.mult),
                     reads=[r_M], writes=[rb, r_x1])
            k.op("dve", lambda: nc.vector.tensor_tensor(x1[0:P, :], x1[0:P, :], xin[i][0:P, :], ALU.add), reads=[r_xin[i]], writes=[r_x1])
            yield
            if DEBUG and (idx == 0 or s == 2):
                k.dma("pool", dbg[0 if idx == 0 else 1, 1, 0:P, :], hgin[i][0:P, :], reads=[r_hgin[i]])
            if idx + 1 < len(tiles):
                loads(idx + 1)
            k.op("act", lambda: nc.scalar.activation(f1[0:P, :], x1[0:P, :], AF.Square, accum_out=ssq[0:P, :]),
                 reads=[r_x1], writes=[r_f1, r_ssq])
            yield
            k.op("dve", lambda: nc.vector.tensor_scalar(ssq[0:P, :], ssq[0:P, :], 1.0 / D, EPS, ALU.mult, ALU.add), writes=[r_ssq])
            yield
            k.op("act", lambda: nc.scalar.activation(ssq[0:P, :], ssq[0:P, :], AF.Sqrt), writes=[r_ssq])
            yield
            k.op("dve", lambda: nc.vector.reciprocal(ssq[0:P, :], ssq[0:P, :]), writes=[r_ssq])
            k.op("dve", lambda: nc.vector.scalar_tensor_tensor(f1[0:P, :], x1[0:P, :], ssq[0:P, 0:1], S2[0:P, :], ALU.mult, ALU.mult),
                 reads=[r_x1, r_ssq, r_M], writes=[r_f1])
            k.op("dve", lambda: nc.vector.tensor_tensor(h2b[0:P, :], f1[0:P, :], SH2[0:P, :], ALU.add), reads=[r_f1, r_M], writes=[r_h2b])
            yield from transpose_to_TT(P, h2b, r_h2b, 8)
            yield
            for qb in range(4):
                yield
                b_, rb = nb()
                for hh in range(4):
                    hp = qb * 4 + hh
                    for kc in range(8):
                        k.op("pe", lambda kc=kc: nc.tensor.matmul(b_[:, hh * 128:hh * 128 + P], Wq[:, kc, hp * 128:(hp + 1) * 128], TT[:, kc, 0:P],
                                                                  start=(kc == 0), stop=(kc == 7), skip_group_check=True),
                             reads=[r_TT, r_W], writes=[rb])
                yield
                k.op("act", lambda: nc.scalar.copy(q2T[:, qb * 4:(qb + 1) * 4, 0:P], b_[:, :].rearrange("p (h n) -> p h n", h=4)[:, :, 0:P]),
                     writes=[rb, r_q2T])
            yield
            for qb in range(4):
                b_, rb = nb()
                for hh in range(4):
                    hp = qb * 4 + hh
                    k.op("pe", lambda: nc.tensor.matmul(b_[0:P, hh * 128:(hh + 1) * 128], q2T[:, hp, 0:P], skt[:, hp % 2, :],
                                                        start=True, stop=True, skip_group_check=True),
                         reads=[r_q2T, r_W], writes=[rb])
                yield
                k.op("act", lambda: nc.scalar.copy(sc[0:P, qb * 4:(qb + 1) * 4, :], b_[0:P, :].rearrange("p (h n) -> p h n", h=4)),
                     writes=[rb, r_sc])
            yield
            for hp in range(16):
                if hp % 2 == 0:
                    yield
                k.op("dve", lambda: nc.vector.max(vals[0:P, hp, 0:8], sc[0:P, hp, :]), reads=[r_sc], writes=[r_vals])
                k.op("dve", lambda: nc.vector.max_index(idxu[0:P, hp, 0:8], vals[0:P, hp, 0:8], sc[0:P, hp, :]),
                     reads=[r_sc, r_vals], writes=[r_idxu])
                k.op("dve", lambda: nc.vector.match_replace(sc2[0:P, hp, :], vals[0:P, hp, 0:8], sc[0:P, hp, :], -1e30),
                     reads=[r_sc, r_vals], writes=[r_sc2])
                k.op("dve", lambda: nc.vector.max(vals[0:P, hp, 8:16], sc2[0:P, hp, :]), reads=[r_sc2], writes=[r_vals])
                k.op("dve", lambda: nc.vector.max_index(idxu[0:P, hp, 8:16], vals[0:P, hp, 8:16], sc2[0:P, hp, :]),
                     reads=[r_sc2, r_vals], writes=[r_idxu])
            yield
            k.op("dve", lambda: nc.vector.tensor_copy(idxf[0:P], idxu[0:P]), reads=[r_idxu], writes=[r_idxf])
            v4 = vals[0:P].rearrange("p (h two) j -> p h two j", two=2)
            k.op("dve", lambda: nc.vector.tensor_tensor(cand[0:P].rearrange("p h (a b) -> p h a b", a=16),
                                                        v4[:, :, 0, :].unsqueeze(3).to_broadcast([P, 8, 16, 16]),
                                                        v4[:, :, 1, :].unsqueeze(2).to_broadcast([P, 8, 16, 16]), ALU.add),
                 reads=[r_vals], writes=[r_cand])
            for h in range(8):
                yield
                k.op("dve", lambda: nc.vector.max(tv[0:P, h, 0:8], cand[0:P, h, :]), reads=[r_cand], writes=[r_tv])
                k.op("dve", lambda: nc.vector.max_index(posu[0:P, h, 0:8], tv[0:P, h, 0:8], cand[0:P, h, :]),
                     reads=[r_cand, r_tv], writes=[r_posu])
                k.op("dve", lambda: nc.vector.match_replace(cand2[0:P, h, :], tv[0:P, h, 0:8], cand[0:P, h, :], -1e30),
                     reads=[r_cand, r_tv], writes=[r_cand2])
                k.op("dve", lambda: nc.vector.max(tv[0:P, h, 8:16], cand2[0:P, h, :]), reads=[r_cand2], writes=[r_tv])
                k.op("dve", lambda: nc.vector.max_index(posu[0:P, h, 8:16], tv[0:P, h, 8:16], cand2[0:P, h, :]),
                     reads=[r_cand2, r_tv], writes=[r_posu])
            yield
            k.op("dve", lambda: nc.vector.tensor_single_scalar(pa_u[0:P], posu[0:P], 4, ALU.logical_shift_right), reads=[r_posu], writes=[r_pau])
            k.op("dve", lambda: nc.vector.tensor_copy(pa_f[0:P, 0], pa_u[0:P]), reads=[r_pau], writes=[r_paf])
            k.op("dve", lambda: nc.vector.tensor_single_scalar(pa_u[0:P], posu[0:P], 15, ALU.bitwise_and), reads=[r_posu], writes=[r_pau])
            k.op("dve", lambda: nc.vector.tensor_copy(pa_f[0:P, 1], pa_u[0:P]), reads=[r_pau], writes=[r_paf])
            i4 = idxf[0:P].rearrange("p (h two) j -> p h two j", two=2)
            for w_ in range(2):
                yield
                ohv = oh[0:P].rearrange("p h (j a) -> p h j a", j=16)
                k.op("dve", lambda: nc.vector.tensor_tensor(ohv, pa_f[0:P, w_].unsqueeze(3).to_broadcast([P, 8, 16, 16]),
                                                            iota16[0:P, :].unsqueeze(1).unsqueeze(1).to_broadcast([P, 8, 16, 16]), ALU.is_equal),
                     reads=[r_paf, r_W], writes=[r_oh])
                k.op("dve", lambda: nc.vector.tensor_tensor(ohv, ohv, i4[:, :, w_, :].unsqueeze(2).to_broadcast([P, 8, 16, 16]), ALU.mult),
                     reads=[r_idxf], writes=[r_oh])
                k.op("dve", lambda: nc.vector.tensor_reduce(isel[0:P, w_], ohv, AX.X, ALU.add), reads=[r_oh], writes=[r_isel])
            k.op("dve", lambda: nc.vector.scalar_tensor_tensor(eid_f[0:P, :], isel[0:P, 0].rearrange("p h j -> p (h j)"), 128.0,
                                                               isel[0:P, 1].rearrange("p h j -> p (h j)"), ALU.mult, ALU.add),
                 reads=[r_isel], writes=[r_eidf])
            k.op("dve", lambda: nc.vector.tensor_copy(eid[0:P, :], eid_f[0:P, :]), reads=[r_eidf], writes=[r_eid])
            yield
            k.op("dve", lambda: nc.vector.tensor_tensor(gat[0:P], tv[0:P], tv[0:P, :, 0:1].to_broadcast([P, 8, 16]), ALU.subtract),
                 reads=[r_tv], writes=[r_gat])
            yield
            k.op("act", lambda: nc.scalar.activation(gat[0:P], gat[0:P], AF.Exp), writes=[r_gat])
            yield
            k.op("dve", lambda: nc.vector.tensor_reduce(gsm[0:P, :], gat[0:P], AX.X, ALU.add), reads=[r_gat], writes=[r_rl])
            k.op("dve", lambda: nc.vector.reciprocal(gsm[0:P, :], gsm[0:P, :]), writes=[r_rl])
            k.op("dve", lambda: nc.vector.tensor_tensor(gat[0:P], gat[0:P], gsm[0:P, :].unsqueeze(2).to_broadcast([P, 8, 16]), ALU.mult),
                 reads=[r_rl], writes=[r_gat])
        def gather(idx, gen):
            s, t = tiles[idx]
            P = 128 if s < 2 else NS
            i = idx % 2
            pp = idx % 2
            x1, r_x1 = x1s[pp], r_x1s[pp]
            h2b, r_h2b = h2bs[pp], r_h2bs[pp]
            eid, r_eid = eids[pp], r_eids[pp]
            gat, r_gat = gats[pp], r_gats[pp]
            G2 = G2s[s % 2]
            py = [(psf[5], r_psf[5]), (psf[6], r_psf[6])]
            gatf = gat[0:P].rearrange("p h j -> p (h j)")
            for grp in range(32):
                gs = slice(grp * 4, grp * 4 + 4)
                rd, rc = r_dotg[grp], r_coefg[grp]
                for j in range(grp * 4, grp * 4 + 4):
                    bi = j % NUV
                    k.dma("pool", uvb[bi][0:P, :], UV, reads=[r_eid, r_uv], writes=[r_uvb[bi]],
                          indirect=bass.IndirectOffsetOnAxis(ap=eid[0:P, j:j + 1], axis=0))
                    k.op("dve", lambda: nc.vector.scalar_tensor_tensor(jb[0:P, :], uvb[bi][0:P, 0:D], 1.0, h2b[0:P, :], ALU.mult, ALU.mult,
                                                                       accum_out=dots[0:P, j:j + 1]),
                         reads=[r_uvb[bi], r_h2b], writes=[rd])
                if gen is not None:
                    next(gen, None)
                k.op("dve", lambda: nc.vector.tensor_tensor(coef[0:P, gs], dots[0:P, gs], dots[0:P, gs], ALU.mult), reads=[rd], writes=[rc])
                k.op("dve", lambda: nc.vector.tensor_scalar(coef[0:P, gs], coef[0:P, gs], 0.044715, 1.0, ALU.mult, ALU.add), writes=[rc])
                k.op("dve", lambda: nc.vector.tensor_tensor(coef[0:P, gs], coef[0:P, gs], dots[0:P, gs], ALU.mult), reads=[rd], writes=[rc])
                k.op("act", lambda: nc.scalar.activation(coef[0:P, gs], coef[0:P, gs], AF.Sigmoid, scale=1.5957691216057308), writes=[rc])
                k.op("dve", lambda: nc.vector.tensor_tensor(coef[0:P, gs], coef[0:P, gs], dots[0:P, gs], ALU.mult), reads=[rd], writes=[rc])
                k.op("dve", lambda: nc.vector.tensor_tensor(coef[0:P, gs], coef[0:P, gs], gatf[:, gs], ALU.mult), reads=[r_gat], writes=[rc])
                for j in range(grp * 4, grp * 4 + 4):
                    bi = j % NUV
                    di_ = j % 4
                    k.op("act", lambda: nc.scalar.mul(dg[di_][0:P, 0:P], ident_b[0:P, 0:P], coef[0:P, j:j + 1]),
                         reads=[rc, r_idb], writes=[r_dg[di_]])
                    for hb in range(2):
                        b_, rb = py[hb]
                        k.op("pe", lambda: nc.tensor.matmul(b_[0:P, :], dg[di_][0:P, 0:P], uvb[bi][0:P, D + hb * 512:D + (hb + 1) * 512],
                                                            start=(j == 0), stop=(j == 127)),
                             reads=[r_dg[di_], r_uvb[bi]], writes=[rb])
                if gen is not None:
                    next(gen, None)
                    next(gen, None)
            io = idx % 2
            for hb in range(2):
                b_, rb = py[hb]
                k.op("dve", lambda: nc.vector.tensor_tensor(yo[io][0:P, hb * 512:(hb + 1) * 512], b_[0:P, :], G2[0:P, hb * 512:(hb + 1) * 512], ALU.mult),
                     reads=[r_M], writes=[rb, r_yo[io]])
            k.op("dve", lambda: nc.vector.tensor_tensor(yo[io][0:P, :], yo[io][0:P, :], x1[0:P, :], ALU.add), reads=[r_x1], writes=[r_yo[io]])
            if DEBUG and (idx == 0 or s == 2):
                di = 0 if idx == 0 else 1
                k.dma("pool", dbg[di, 0, 0:P, 0:512], att_b[0:P, :], reads=[r_attb])
                k.dma("pool", dbg[di, 2, 0:P, :], yb[0:P, :], reads=[r_yb])
                k.dma("sp", dbg[di, 3, 0:P, :], x1[0:P, :], reads=[r_x1])
                k.dma("pool", dbg[di, 4, 0:P, :], h2b[0:P, :], reads=[r_h2b])
                k.dma("sp", dbg[di, 5, 0:P, 0:128], coef[0:P, :], reads=r_coefg)
                k.dma("sp", dbg[di, 6, 0:P, 0:128], eid_f[0:P, :], reads=[r_eidf])
                k.dma("sp", dbg[di, 7, 0:P, 0:128], dots[0:P, :], reads=r_dotg)
                k.dma("sp", dbg[di, 8, 0:P, 0:128], gat[0:P].rearrange("p h j -> p (h j)"), reads=[r_gat])
                k.dma("sp", dbg[di, 9, 0:P, 0:256], vals[0:P].rearrange("p a b -> p (a b)"), reads=[r_vals])
            k.dma("sp", y_p[s, t * 128:(t + 1) * 128, :] if s < 2 else y_s[:, :], yo[io][0:P, :], reads=[r_yo[io]])
        g_ = front(0)
        for _ in g_:
            pass
        for idx in range(len(tiles)):
            g_ = front(idx + 1) if idx + 1 < len(tiles) else None
            gather(idx, g_)
            if g_ is not None:
                for _ in g_:
                    pass
        k.flush()
    k.finish()


def _consts():
    c = np.zeros((128, 1024), np.float32)
    j = np.arange(128)[:, None]
    i = np.arange(128)[None, :]
    c[:, 0:128] = np.eye(128, dtype=np.float32)
    c[:, 128:256] = (j <= i)
    c[:, 256:384] = (j >= i)
    same = (j // 64) == (i // 64)
    c[:, 384:512] = same * ((j <= i).astype(np.float32) - ((j % 64) <= 31).astype(np.float32))
    c[:, 512:640] = same * (j > i)
    s_ = np.arange(128)
    c[:, 640] = s_ < 64
    c[:, 641] = s_ >= 64
    c[:, 642] = (s_ < 64) & (s_ % 64 <= 31)
    c[:, 643] = (s_ >= 64) & (s_ % 64 <= 31)
    c[:, 768:896] = same * (j <= i)
    return c


def _selc():
    c = np.zeros((NS, 2064), np.float32)
    for b in range(NS):
        c[b, b * 128:(b + 1) * 128] = 1.0
    c[:, 2048:2064] = np.arange(16, dtype=np.float32)[None, :]
    return c


_CACHE = {}


def kernel(x_prompt, x_sample, cache_kv_w128, cache_kv_w512, cache_kv_w2048, state_hgrn, c_prompt, c_sample,
           w_ada, b_ada, norm1_w, norm2_w, w_in, q_norm_w, k_norm_w, hg_lb_logits, hg_norm_w, w_br_a, w_br_b,
           w_o, w_peer_q, peer_subkeys, peer_u, peer_v, _stop_after=99):
    f = lambda a: np.ascontiguousarray(np.asarray(a, dtype=np.float32))
    key = ("nc", _stop_after)
    if key not in _CACHE:
        _CACHE[key] = build_program(_stop_after)
    nc = _CACHE[key]
    caches = [f(cache_kv_w128)[0], f(cache_kv_w512)[0], f(cache_kv_w2048)[0]]
    shared = {
        "w_ada": f(w_ada)[0], "b_ada": f(b_ada).reshape(1, -1), "norm1_w": f(norm1_w).reshape(1, -1),
        "norm2_w": f(norm2_w).reshape(1, -1), "w_in": f(w_in)[0],
        "qk_w": np.ascontiguousarray(np.stack([np.tile(f(q_norm_w)[0], 8), np.tile(f(k_norm_w)[0], 8)])),
        "lb_log": f(hg_lb_logits), "hgn_w": np.ascontiguousarray(np.tile(f(hg_norm_w)[0], 8).reshape(1, -1)),
        "w_br_a": f(w_br_a)[0], "w_br_b": f(w_br_b)[0], "w_o": f(w_o)[0], "w_pq": f(w_peer_q)[0],
        "skT": np.ascontiguousarray(f(peer_subkeys)[0].transpose(0, 2, 1)),
        "peer_u": f(peer_u)[0], "peer_v": f(peer_v)[0], "cst": _consts(), "selc": _selc(),
    }
    xpf, xsf = f(x_prompt), f(x_sample)
    cp, cs = f(c_prompt), f(c_sample)
    st = f(state_hgrn)[0]
    in_maps = []
    for c in range(NCORES):
        m = dict(shared)
        m["xp"] = xpf[c * NSEQ:(c + 1) * NSEQ]
        m["xs"] = np.ascontiguousarray(xsf[c * NS:(c + 1) * NS, 0, :])
        m["cT"] = np.ascontiguousarray(np.concatenate([cp[c * NSEQ:(c + 1) * NSEQ], cs[c * NS:(c + 1) * NS]], 0).T)
        for g in range(3):
            m["ck%d" % g] = np.ascontiguousarray(
                caches[g][c * NS:(c + 1) * NS, 0::GROUPS[g][1]][:, :128].reshape(NS, 128, 2, 512))
        m["st_in"] = st[c * NS:(c + 1) * NS]
        in_maps.append(m)
    res = run_bass_kernel_spmd(nc, in_maps, core_ids=list(range(NCORES)))
    R = res.results
    cat = lambda n: np.concatenate([np.asarray(r[n]) for r in R], 0)
    y_prompt = cat("y_p")
    y_sample = cat("y_s").reshape(NCORES * NS, 1, D)
    outs = [y_prompt, y_sample]
    for g in range(3):
        outs.append(cat("kv%d_p" % g).reshape(1, NCORES * NSEQ, GROUPS[g][0], 2, 8, 64))
    outs.append(cat("hg_p")[None])
    for g in range(3):
        outs.append(cat("kv%d_s" % g).reshape(1, NCORES * NS, 1, 2, 8, 64))
    outs.append(cat("hg_s")[None])
    if DEBUG:
        global _DBG
        _DBG = np.asarray(R[0]["dbg"])
    return tuple(np.ascontiguousarray(o, dtype=np.float32) for o in outs)
```

```python
import contextlib
import numpy as np
import concourse.bass as bass
import concourse.mybir as mybir
from concourse.bass_utils import run_bass_kernel_spmd

F32 = mybir.dt.float32
BF16 = mybir.dt.bfloat16
I32 = mybir.dt.int32
U32 = mybir.dt.uint32
ALU = mybir.AluOpType
AF = mybir.ActivationFunctionType
AX = mybir.AxisListType

NCORES = 8
D = 1024
SEQ = 4096
NSEQ = 2
NS = 16
INW = 10752
GROUPS = ((128, 1), (512, 4), (2048, 16))
EPS = 1e-6
NT = SEQ // 128
NROWS = NSEQ + NS
DEBUG = False


class Res:
    __slots__ = ("w", "rs")

    def __init__(self):
        self.w = {}
        self.rs = {}


class K:
    def __init__(self, nc, es):
        self.nc = nc
        self.es = es
        self.eng = {"pe": nc.tensor, "act": nc.scalar, "dve": nc.vector, "pool": nc.gpsimd, "sp": nc.sync}
        self.sem = {}
        self.cnt = {}
        for e in ("pe", "act", "dve", "pool"):
            self.sem[e] = es.enter_context(nc.semaphore("c_" + e))
            self.cnt[e] = 0
        self.known = {e: {} for e in self.eng}
        self.dsem = {}
        self.dpos = {}
        for q, n in (("sp", 24), ("pool", 16), ("act", 8)):
            self.dsem[q] = [[es.enter_context(nc.semaphore("d_%s%d" % (q, i))), 0] for i in range(n)]
            self.dpos[q] = 0
        self.deferred = []

    def _wait(self, e, tok):
        s, v = tok
        if v <= 0:
            return
        if e == "pe" and s is self.sem["pe"]:
            return
        kn = self.known[e]
        if kn.get(id(s), 0) >= v:
            return
        self.eng[e].wait_ge(s, v)
        kn[id(s)] = v

    def _deps(self, e, reads, writes):
        for r in reads:
            for tok in r.w.values():
                self._wait(e, tok)
        for r in writes:
            for tok in r.w.values():
                self._wait(e, tok)
            for tok in r.rs.values():
                self._wait(e, tok)

    def _mark(self, tok, reads, writes, accum):
        s, v = tok
        for r in reads:
            r.rs[id(s)] = tok
        for r in writes:
            r.w = {id(s): tok}
            r.rs = {}
        for r in accum:
            r.w[id(s)] = tok

    def op(self, e, fn, reads=(), writes=(), accum=()):
        self._deps(e, reads, writes)
        ins = fn()
        self.cnt[e] += 1
        ins.then_inc(self.sem[e], 1)
        self._mark((self.sem[e], self.cnt[e]), reads, writes, accum)

    def dma(self, q, out, in_, reads=(), writes=(), accum=(), indirect=None):
        self._deps(q, reads, writes)
        slot = self.dsem[q][self.dpos[q] % len(self.dsem[q])]
        self.dpos[q] += 1
        self._wait(q, (slot[0], slot[1]))
        if indirect is not None:
            ins = self.eng[q].indirect_dma_start(out=out, out_offset=None, in_=in_, in_offset=indirect)
        else:
            ins = self.eng[q].dma_start(out=out, in_=in_)
        slot[1] += 16
        ins.then_inc(slot[0], 16)
        self._mark((slot[0], slot[1]), reads, writes, accum)

    def barrier(self):
        self.flush()
        toks = [(self.sem[e], self.cnt[e]) for e in self.sem]
        for q in self.dsem:
            toks += [(s, v) for s, v in self.dsem[q]]
        for e in self.eng:
            for tok in toks:
                if e != "pe" or tok[0] is not self.sem["pe"]:
                    self._wait(e, tok)

    def defer(self, fn):
        self.deferred.append(fn)

    def flush(self):
        d, self.deferred = self.deferred, []
        for fn in d:
            fn()

    def finish(self):
        self.flush()
        for q in self.dsem:
            for s, v in self.dsem[q]:
                self._wait("sp", (s, v))


def build_program(stop_after=99):
    nc = bass.Bass("TRN2", target_bir_lowering=False)
    es = contextlib.ExitStack()
    with es:
        _emit(nc, es, stop_after)
    return nc


def _emit(nc, es, stop_after):
    k = K(nc, es)

    def din(name, shape, dt=F32):
        return nc.dram_tensor(name, list(shape), dt, kind="ExternalInput").ap()

    def dout(name, shape, dt=F32):
        return nc.dram_tensor(name, list(shape), dt, kind="ExternalOutput").ap()

    def dscr(name, shape, dt):
        return nc.dram_tensor(name, list(shape), dt, kind="Internal").ap()

    def sb(name, shape, dt):
        return es.enter_context(nc.sbuf_tensor(name, list(shape), dt))

    xp = din("xp", [NSEQ, SEQ, D])
    xs = din("xs", [NS, D])
    cT = din("cT", [D, NROWS])
    ck = [din("ck%d" % g, [NS, 128, 2, 512]) for g in range(3)]
    st_in = din("st_in", [NS, 8, 128, 128])
    w_ada = din("w_ada", [D, 6 * D])
    b_ada = din("b_ada", [1, 6 * D])
    norm1_w = din("norm1_w", [1, D])
    norm2_w = din("norm2_w", [1, D])
    w_in = din("w_in", [D, INW])
    qk_w = din("qk_w", [2, 512])
    lb_log = din("lb_log", [2, D])
    hgn_w = din("hgn_w", [1, D])
    w_br_a = din("w_br_a", [512, D])
    w_br_b = din("w_br_b", [D, D])
    w_o = din("w_o", [D, D])
    w_pq = din("w_pq", [D, 2048])
    skT = din("skT", [2, 128, 128])
    peer_u = din("peer_u", [16384, D])
    peer_v = din("peer_v", [16384, D])
    cst = din("cst", [128, 128 * 8])
    selc = din("selc", [NS, 2064])

    y_p = dout("y_p", [NSEQ, SEQ, D])
    y_s = dout("y_s", [NS, D])
    kv_p = [dout("kv%d_p" % g, [NSEQ, GROUPS[g][0], 2, 512]) for g in range(3)]
    hg_p = dout("hg_p", [NSEQ, 8, 128, 128])
    kv_s = [dout("kv%d_s" % g, [NS, 2, 512]) for g in range(3)]
    hg_s = dout("hg_s", [NS, 8, 128, 128])
    dbg = dout("dbg", [2, 10, 128, D]) if DEBUG else None

    MODS = dscr("MODS", [NROWS, 6 * D], F32)
    NTOK = NSEQ * SEQ + 128
    QS = dscr("QS", [NTOK, 3, 512], BF16)
    KS = dscr("KS", [NTOK, 3, 512], BF16)
    VS = dscr("VS", [NTOK, 3, 520], BF16)

    cst_f = sb("cst_f", [128, 1024], F32)
    ident_b = sb("ident_b", [128, 128], BF16)
    r_cst = Res()
    k.dma("sp", cst_f[:], cst, writes=[r_cst])
    r_idb = Res()
    k.op("dve", lambda: nc.vector.tensor_copy(ident_b[:], cst_f[:, 0:128]), reads=[r_cst], writes=[r_idb])
    ident_f = cst_f[:, 0:128]

    psf = [es.enter_context(nc.psum_tensor("psf%d" % i, [128, 512], F32)) for i in range(7)]
    r_psf = [Res() for _ in range(7)]
    psb = es.enter_context(nc.psum_tensor("psb", [128, 1024], BF16))
    r_psb = Res()

    def rstd_from_ss(P, ss, n_elem, tmp):
        t, r = ss
        k.op("dve", lambda: nc.vector.tensor_scalar(t, t, 1.0 / n_elem, EPS, ALU.mult, ALU.add), writes=[r])
        k.op("act", lambda: nc.scalar.activation(t, t, AF.Sqrt), writes=[r])
        k.op("dve", lambda: nc.vector.reciprocal(t, t), writes=[r])

    with contextlib.ExitStack() as ph:
        def psb_(name, shape, dt):
            return ph.enter_context(nc.sbuf_tensor(name, list(shape), dt))
        cT_f = psb_("cT_f", [128, 8, NROWS], F32)
        cT_b = psb_("cT_b", [128, 8, NROWS], BF16)
        r_cT = Res()
        k.dma("sp", cT_f[:], cT.rearrange("(kc p) n -> p kc n", p=128), writes=[r_cT])
        k.op("act", lambda: nc.scalar.activation(cT_b[:], cT_f[:], AF.Silu), reads=[r_cT], writes=[r_cT])
        wa = [psb_("wa%d" % i, [128, 8, 512], BF16) for i in range(2)]
        r_wa = [Res(), Res()]
        ba = [psb_("ba%d" % i, [NROWS, 512], F32) for i in range(2)]
        r_ba = [Res(), Res()]
        mo = [psb_("mo%d" % i, [NROWS, 512], F32) for i in range(2)]
        r_mo = [Res(), Res()]
        r_MODS = Res()
        for c in range(12):
            i = c % 2
            cs = slice(c * 512, (c + 1) * 512)
            k.dma("pool", wa[i][:], w_ada[:, cs].rearrange("(kc p) n -> p kc n", p=128), writes=[r_wa[i]])
            k.dma("sp", ba[i][:], b_ada[:, cs].partition_broadcast(NROWS), writes=[r_ba[i]])
            for kc in range(8):
                k.op("pe", lambda kc=kc: nc.tensor.matmul(psf[i][0:NROWS, :], cT_b[:, kc, :], wa[i][:, kc, :],
                                                          start=(kc == 0), stop=(kc == 7)),
                     reads=[r_cT, r_wa[i]], writes=[r_psf[i]])
            k.op("dve", lambda: nc.vector.tensor_tensor(mo[i][:], psf[i][0:NROWS, :], ba[i][:], ALU.add),
                 reads=[r_ba[i]], writes=[r_psf[i], r_mo[i]])
            k.dma("sp", MODS[:, cs], mo[i][:], reads=[r_mo[i]], accum=[r_MODS])
        k.barrier()
    if stop_after <= 0:
        k.finish()
        return

    def tok0(s):
        return s * SEQ

    NTOKX = NSEQ * SEQ + 128
    GG = dscr("GG", [NTOKX, 3072], BF16)
    HGS = dscr("HGS", [NTOKX, D], BF16)
    OACC = dscr("OACC", [3, NTOKX, 520], F32)
    UV = dscr("UV", [16384, 2 * D], BF16)
    r_uv = Res()
    r_scr = Res()
    r_gg = Res()
    r_hgs = Res()
    r_oacc = Res()
    bank_ctr = [0]

    nbank_rot = [7]

    def nb():
        i = bank_ctr[0] % nbank_rot[0]
        bank_ctr[0] += 1
        return psf[i], r_psf[i]

    maskc_b = sb("maskc_b", [128, 4, 128], BF16)
    maskp_b = sb("maskp_b", [128, 4, 128], BF16)
    r_mask = Res()
    for hh in range(4):
        k.op("dve", lambda hh=hh: nc.vector.tensor_copy(maskc_b[:, hh, :], cst_f[:, 128:256]), reads=[r_cst], writes=[r_mask])
        k.op("dve", lambda hh=hh: nc.vector.tensor_copy(maskp_b[:, hh, :], cst_f[:, 256:384]), reads=[r_cst], writes=[r_mask])
    L1 = cst_f[:, 384:512]
    L3 = cst_f[:, 512:640]
    R4 = cst_f[:, 640:644]
    MA = cst_f[:, 768:896]

    def proj(P, hT, ts_, W, c0, ncol512, r_hT, r_W):
        out = []
        for j in range(ncol512):
            b_, rb = nb()
            for kc in range(8):
                k.op("pe", lambda kc=kc: nc.tensor.matmul(b_[0:P, :], hT[:, kc, ts_], W[:, kc, c0 + j * 512:c0 + (j + 1) * 512],
                                                          start=(kc == 0), stop=(kc == 7)),
                     reads=[r_hT, r_W], writes=[rb])
            out.append((b_, rb))
        return out

    with contextlib.ExitStack() as seqscope:
        hT = seqscope.enter_context(nc.sbuf_tensor("hT", [128, 8, SEQ], BF16))
        r_hT = Res()
        for s in range(3):
            P = 128 if s < 2 else NS
            ntile = NT if s < 2 else 1

            def xsrc(t):
                return xp[s, t * 128:(t + 1) * 128, :] if s < 2 else xs[:, :]

            with contextlib.ExitStack() as ph:
                def psb_(name, shape, dt):
                    return ph.enter_context(nc.sbuf_tensor(name + "_s%d" % s, list(shape), dt))
                S1 = psb_("S1", [128, D], F32)
                SH1 = psb_("SH1", [128, D], F32)
                n1w = psb_("n1w", [128, D], F32)
                r_S1 = Res()
                r_n1w = Res()
                k.dma("sp", n1w[:], norm1_w.partition_broadcast(128), writes=[r_n1w])
                qkw = psb_("qkw", [128, 2, 512], F32)
                r_qkw = Res()
                k.dma("sp", qkw[:, 0, :], qk_w[0:1, :].partition_broadcast(128), writes=[r_qkw])
                k.dma("sp", qkw[:, 1, :], qk_w[1:2, :].partition_broadcast(128), writes=[r_qkw])
                k.op("dve", lambda: nc.vector.tensor_scalar(qkw[:, 0, :], qkw[:, 0, :], 0.125, None, ALU.mult), writes=[r_qkw])
                xt = [psb_("xt%d" % i, [128, D], F32) for i in range(2)]
                r_xt = [Res(), Res()]
                xm = psb_("xm", [128, D], F32)
                xb = psb_("xb", [128, D], BF16)
                r_xm = Res()
                r_xb = Res()
                junk = psb_("junk", [128, D], F32)
                r_junk = Res()
                ss1 = psb_("ss1", [128, 1], F32)
                r_ss1 = Res()
                Wg = psb_("Wg", [128, 8, 1536], BF16)
                r_Wg = Res()
                ss8 = psb_("ss8", [128, 8], F32)
                r_ss8 = Res()
                qn_b = [psb_("qn_b%d" % i, [128, 512], BF16) for i in range(2)]
                r_qn = [Res(), Res()]
                kn32 = [psb_("kn32%d" % i, [128, 512], F32) for i in range(2)]
                r_kn32 = [Res(), Res()]
                kn_b = [psb_("kn_b%d" % i, [128, 512], BF16) for i in range(2)]
                r_knb = [Res(), Res()]
                v32 = [psb_("v32%d" % i, [128, 512], F32) for i in range(2)]
                r_v32 = [Res(), Res()]
                vaug = [psb_("vaug%d" % i, [128, 8, 65], BF16) for i in range(2)]
                r_vaug = [Res(), Res()]
                for i in range(2):
                    k.op("pool", lambda i=i: nc.gpsimd.memset(vaug[i][:], 1.0), writes=[r_vaug[i]])

                if s < 2:
                    k.dma("sp", SH1[:], MODS[s:s + 1, 0:D].partition_broadcast(128), reads=[r_MODS], writes=[r_S1])
                    k.dma("sp", S1[:], MODS[s:s + 1, D:2 * D].partition_broadcast(128), reads=[r_MODS], writes=[r_S1])
                else:
                    k.dma("sp", SH1[0:NS, :], MODS[2:2 + NS, 0:D], reads=[r_MODS], writes=[r_S1])
                    k.dma("sp", S1[0:NS, :], MODS[2:2 + NS, D:2 * D], reads=[r_MODS], writes=[r_S1])
                k.op("dve", lambda: nc.vector.scalar_tensor_tensor(S1[0:P, :], S1[0:P, :], 1.0, n1w[0:P, :], ALU.add, ALU.mult),
                     reads=[r_n1w], writes=[r_S1])

                k.dma("sp", xt[0][0:P, :], xsrc(0), writes=[r_xt[0]])
                for t in range(ntile):
                    i = t % 2
                    if t + 1 < ntile:
                        k.dma("sp", xt[1 - i][0:P, :], xsrc(t + 1), writes=[r_xt[1 - i]])
                    k.op("act", lambda: nc.scalar.activation(junk[0:P, :], xt[i][0:P, :], AF.Square, accum_out=ss1[0:P, :]),
                         reads=[r_xt[i]], writes=[r_junk, r_ss1])
                    rstd_from_ss(P, (ss1[0:P, :], r_ss1), D, None)
                    k.op("dve", lambda: nc.vector.scalar_tensor_tensor(xm[0:P, :], xt[i][0:P, :], ss1[0:P, 0:1], S1[0:P, :],
                                                                       ALU.mult, ALU.mult),
                         reads=[r_xt[i], r_ss1, r_S1], writes=[r_xm])
                    k.op("dve", lambda: nc.vector.tensor_tensor(xb[0:P, :], xm[0:P, :], SH1[0:P, :], ALU.add),
                         reads=[r_xm, r_S1], writes=[r_xb])
                    for kc in range(8):
                        k.op("pe", lambda kc=kc: nc.tensor.transpose(psb[:, kc * 128:kc * 128 + P], xb[0:P, kc * 128:(kc + 1) * 128],
                                                                     ident_b[0:P, 0:P]),
                             reads=[r_xb, r_idb], writes=[r_psb])
                    k.op("act", lambda: nc.scalar.copy(hT[:, :, t * 128:t * 128 + P],
                                                       psb[:].rearrange("p (kc n) -> p kc n", kc=8)[:, :, 0:P]),
                         writes=[r_psb, r_hT])

                for g in range(3):
                    win = GROUPS[g][0]
                    for part in range(3):
                        c0 = part * 1536 + g * 512
                        k.dma("pool", Wg[:, :, part * 512:(part + 1) * 512],
                              w_in[:, c0:c0 + 512].rearrange("(kc p) n -> p kc n", p=128), writes=[r_Wg])
                    for t in range(ntile):
                        i = t % 2
                        ts_ = slice(t * 128, t * 128 + P)
                        g0 = tok0(s) + t * 128
                        pr = proj(P, hT, ts_, Wg, 0, 3, r_hT, r_Wg)
                        bank = [p_[0] for p_ in pr]
                        rbank = [p_[1] for p_ in pr]
                        for part in range(2):
                            ps_ = bank[part]
                            k.op("act", lambda: nc.scalar.activation(junk[0:P, 0:512], ps_[0:P, :], AF.Square),
                                 writes=[rbank[part], r_junk])
                            k.op("dve", lambda: nc.vector.tensor_reduce(ss8[0:P, :], junk[0:P, 0:512].rearrange("p (h e) -> p h e", h=8),
                                                                        AX.X, ALU.add),
                                 reads=[r_junk], writes=[r_ss8])
                            rstd_from_ss(P, (ss8[0:P, :], r_ss8), 64, None)
                            dst32 = junk if part == 0 else kn32[i]
                            rdst = r_junk if part == 0 else r_kn32[i]
                            k.op("dve", lambda: nc.vector.tensor_tensor(
                                dst32[0:P, 0:512].rearrange("p (h e) -> p h e", h=8),
                                ps_[0:P, :].rearrange("p (h e) -> p h e", h=8),
                                ss8[0:P, :].unsqueeze(2).to_broadcast([P, 8, 64]), ALU.mult),
                                reads=[r_ss8], writes=[rbank[part], rdst])
                            if part == 0:
                                k.op("dve", lambda: nc.vector.tensor_tensor(qn_b[i][0:P, :], junk[0:P, 0:512], qkw[0:P, 0, :], ALU.mult),
                                     reads=[r_junk, r_qkw], writes=[r_qn[i]])
                            else:
                                k.op("dve", lambda: nc.vector.tensor_tensor(kn32[i][0:P, :], kn32[i][0:P, :], qkw[0:P, 1, :], ALU.mult),
                                     reads=[r_qkw], writes=[r_kn32[i]])
                                k.op("pool", lambda: nc.gpsimd.tensor_copy(kn_b[i][0:P, :], kn32[i][0:P, :]),
                                     reads=[r_kn32[i]], writes=[r_knb[i]])
                        k.op("act", lambda: nc.scalar.copy(v32[i][0:P, :], bank[2][0:P, :]), writes=[rbank[2], r_v32[i]])
                        k.op("pool", lambda: nc.gpsimd.tensor_copy(vaug[i][0:P, :, 0:64],
                                                                    v32[i][0:P, :].rearrange("p (h e) -> p h e", h=8)),
                             reads=[r_v32[i]], writes=[r_vaug[i]])
                        k.flush()

                        def stores(i=i, g=g, g0=g0, t=t, P=P, s=s, win=win):
                            k.dma("sp", QS[g0:g0 + P, g, :], qn_b[i][0:P, :], reads=[r_qn[i]], accum=[r_scr])
                            k.dma("sp", KS[g0:g0 + P, g, :], kn_b[i][0:P, :], reads=[r_knb[i]], accum=[r_scr])
                            k.dma("sp", VS[g0:g0 + P, g, :], vaug[i][0:P, :, :].rearrange("p h e -> p (h e)"),
                                  reads=[r_vaug[i]], accum=[r_scr])
                            if s < 2:
                                r0 = t * 128 - (SEQ - win)
                                if r0 >= 0:
                                    k.dma("sp", kv_p[g][s, r0:r0 + 128, 0, :], kn32[i][:], reads=[r_kn32[i]])
                                    k.dma("sp", kv_p[g][s, r0:r0 + 128, 1, :], v32[i][:], reads=[r_v32[i]])
                            else:
                                k.dma("sp", kv_s[g][:, 0, :], kn32[i][0:P, :], reads=[r_kn32[i]])
                                k.dma("sp", kv_s[g][:, 1, :], v32[i][0:P, :], reads=[r_v32[i]])
                        k.defer(stores)
                    k.flush()
                k.barrier()
            if stop_after <= 1:
                continue

            with contextlib.ExitStack() as ph:
                def psb_(name, shape, dt):
                    return ph.enter_context(nc.sbuf_tensor(name + "_s%d" % s, list(shape), dt))
                Wt = psb_("Wt", [128, 8, 3072], BF16)
                r_Wt = Res()
                for j, c0 in enumerate((7680, 8704, 9728)):
                    k.dma("pool", Wt[:, :, j * 1024:(j + 1) * 1024],
                          w_in[:, c0:c0 + 1024].rearrange("(kc p) n -> p kc n", p=128), writes=[r_Wt])
                ggb = [psb_("ggb%d" % i, [128, 3072], BF16) for i in range(2)]
                r_ggb = [Res(), Res()]
                for t in range(ntile):
                    i = t % 2
                    ts_ = slice(t * 128, t * 128 + P)
                    g0 = tok0(s) + t * 128
                    for j in range(6):
                        pr = proj(P, hT, ts_, Wt, j * 512, 1, r_hT, r_Wt)
                        b_, rb = pr[0]
                        fn_ = AF.Silu if j < 2 else AF.Sigmoid
                        k.op("act", lambda: nc.scalar.activation(ggb[i][0:P, j * 512:(j + 1) * 512], b_[0:P, :], fn_),
                             writes=[rb, r_ggb[i]])
                    k.flush()
                    k.defer(lambda i=i, g0=g0, P=P: k.dma("sp", GG[g0:g0 + P, :], ggb[i][0:P, :], reads=[r_ggb[i]], accum=[r_gg]))
                k.barrier()
            if stop_after <= 2:
                continue

            with contextlib.ExitStack() as ph:
                def psb_(name, shape, dt):
                    return ph.enter_context(nc.sbuf_tensor(name + "_s%d" % s, list(shape), dt))
                Wh = psb_("Wh", [128, 8, 3072], BF16)
                r_Wh = Res()
                for j in range(3):
                    c0 = 4608 + j * 1024
                    k.dma("pool", Wh[:, :, j * 1024:(j + 1) * 1024],
                          w_in[:, c0:c0 + 1024].rearrange("(kc p) n -> p kc n", p=128), writes=[r_Wh])
                lbt = psb_("lbt", [128, D], F32)
                omlt = psb_("omlt", [128, D], F32)
                hgw = psb_("hgw", [128, D], F32)
                r_lb = Res()
                k.dma("sp", lbt[:], lb_log[0:1, :].partition_broadcast(128), writes=[r_lb])
                k.dma("sp", omlt[:], lb_log[1:2, :].partition_broadcast(128), writes=[r_lb])
                k.dma("sp", hgw[:], hgn_w.partition_broadcast(128), writes=[r_lb])
                k.op("dve", lambda: nc.vector.tensor_tensor(lbt[:], lbt[:], omlt[:], ALU.subtract), writes=[r_lb])
                k.op("act", lambda: nc.scalar.activation(lbt[:], lbt[:], AF.Sigmoid), writes=[r_lb])
                k.op("dve", lambda: nc.vector.tensor_scalar(omlt[:], lbt[:], -1.0, 1.0, ALU.mult, ALU.add), writes=[r_lb])
                logf = psb_("logf", [128, D], F32)
                kk = psb_("kk", [128, D], F32)
                et = psb_("et", [128, D], F32)
                t2 = psb_("t2", [128, D], F32)
                r_logf, r_kk, r_et, r_t2 = Res(), Res(), Res(), Res()
                kt_b = psb_("kt_b", [128, D], BF16)
                kh_b = psb_("kh_b", [128, D], BF16)
                qt_b = psb_("qt_b", [128, D], BF16)
                v_b = psb_("v_b", [128, D], BF16)
                r_ktb, r_khb, r_qtb, r_vb = Res(), Res(), Res(), Res()
                gt_b = psb_("gt_b", [128, D], BF16)
                r_gtb = Res()
                hg_b = [psb_("hg_b%d" % i, [128, D], BF16) for i in range(2)]
                r_hgb = [Res(), Res()]
                ss8 = psb_("hss8", [128, 8], F32)
                r_ss8 = Res()
                if s < 2:
                    qT = psb_("qT", [128, 8, 128], BF16)
                    kT = psb_("kT", [128, 8, 128], BF16)
                    r_qT, r_kT = Res(), Res()
                    Abd = psb_("Abd", [128, 8, 128], BF16)
                    r_Abd = Res()
                    k.op("pool", lambda: nc.gpsimd.memset(Abd[:], 0.0), writes=[r_Abd])
                    Sm = psb_("Sm", [128, 8, 128], F32)
                    St = psb_("St", [128, 8, 128], F32)
                    Sb = [psb_("Sb%d" % i, [128, 8, 128], BF16) for i in range(2)]
                    r_Sm, r_St = Res(), Res()
                    r_Sb = [Res(), Res()]
                    eb = psb_("eb", [128, 8, 4], F32)
                    r_eb = Res()
                    k.op("pool", lambda: nc.gpsimd.memset(Sm[:], 0.0), writes=[r_Sm])
                    k.op("pool", lambda: nc.gpsimd.memset(Sb[0][:], 0.0), writes=[r_Sb[0]])
                else:
                    selc_sb = psb_("selc_sb", [NS, 2048], F32)
                    k.dma("sp", selc_sb[:], selc[:, 0:2048], writes=[r_cst])
                    fT = psb_("fT", [128, 3, 8, NS], F32)
                    r_fT = Res()
                    v32s = psb_("v32s", [NS, D], F32)
                    r_v32s = Res()
                    QZ = psb_("QZ", [128, 8, NS * NS], F32)
                    r_QZ = Res()
                    k.op("pool", lambda: nc.gpsimd.memset(QZ[:], 0.0), writes=[r_QZ])
                    S0b = [psb_("S0b%d" % i, [128, 8, 128], F32) for i in range(2)]
                    r_S0b = [Res(), Res()]
                    Sn = [psb_("Sn%d" % i, [128, 8, 128], F32) for i in range(2)]
                    r_Sn = [Res(), Res()]

                def hg_epilogue(P, po, i, g0):
                    for hb in range(2):
                        b_, rb = po[hb]
                        k.op("act", lambda: nc.scalar.activation(t2[0:P, hb * 512:(hb + 1) * 512], b_[0:P, :], AF.Square),
                             writes=[rb, r_t2])
                    k.op("dve", lambda: nc.vector.tensor_reduce(ss8[0:P, :], t2[0:P, :].rearrange("p (h e) -> p h e", h=8), AX.X, ALU.add),
                         reads=[r_t2], writes=[r_ss8])
                    rstd_from_ss(P, (ss8[0:P, :], r_ss8), 128, None)
                    for hb in range(2):
                        b_, rb = po[hb]
                        k.op("dve", lambda: nc.vector.tensor_tensor(
                            t2[0:P, hb * 512:(hb + 1) * 512].rearrange("p (h e) -> p h e", h=4),
                            b_[0:P, :].rearrange("p (h e) -> p h e", h=4),
                            ss8[0:P, hb * 4:(hb + 1) * 4].unsqueeze(2).to_broadcast([P, 4, 128]), ALU.mult),
                            reads=[r_ss8], writes=[rb, r_t2])
                    k.op("dve", lambda: nc.vector.tensor_tensor(t2[0:P, :], t2[0:P, :], gt_b[0:P, :], ALU.mult),
                         reads=[r_gtb], writes=[r_t2])
                    k.op("dve", lambda: nc.vector.tensor_tensor(hg_b[i][0:P, :], t2[0:P, :], hgw[0:P, :], ALU.mult),
                         reads=[r_t2, r_lb], writes=[r_hgb[i]])
                    k.flush()
                    k.defer(lambda: k.dma("sp", HGS[g0:g0 + P, :], hg_b[i][0:P, :], reads=[r_hgb[i]], accum=[r_hgs]))

                for t in range(ntile):
                    i = t % 2
                    ts_ = slice(t * 128, t * 128 + P)
                    g0 = tok0(s) + t * 128
                    k.dma("sp", gt_b[0:P, :], GG[g0:g0 + P, 0:D], reads=[r_gg], writes=[r_gtb])
                    pr = proj(P, hT, ts_, Wh, 1024, 2, r_hT, r_Wh)
                    for hb in range(2):
                        b_, rb = pr[hb]
                        k.op("act", lambda: nc.scalar.activation(logf[0:P, hb * 512:(hb + 1) * 512], b_[0:P, :], AF.Sigmoid),
                             writes=[rb, r_logf])
                    k.op("dve", lambda: nc.vector.tensor_tensor(logf[0:P, :], logf[0:P, :], omlt[0:P, :], ALU.mult), reads=[r_lb], writes=[r_logf])
                    k.op("dve", lambda: nc.vector.tensor_tensor(logf[0:P, :], logf[0:P, :], lbt[0:P, :], ALU.add), reads=[r_lb], writes=[r_logf])
                    k.op("dve", lambda: nc.vector.tensor_scalar(kk[0:P, :], logf[0:P, :], -1.0, 1.0, ALU.mult, ALU.add),
                         reads=[r_logf], writes=[r_kk])
                    if s < 2:
                        k.op("act", lambda: nc.scalar.activation(logf[0:P, :], logf[0:P, :], AF.Ln), reads=[r_kk], writes=[r_logf])
                    if s == 2:
                        k.op("pool", lambda: nc.gpsimd.tensor_copy(et[0:P, :], logf[0:P, :]), reads=[r_logf], writes=[r_et])
                        pq = proj(P, hT, ts_, Wh, 0, 2, r_hT, r_Wh)
                        for hb in range(2):
                            b_, rb = pq[hb]
                            k.op("act", lambda: nc.scalar.activation(t2[0:P, hb * 512:(hb + 1) * 512], b_[0:P, :], AF.Silu),
                                 writes=[rb, r_t2])
                        pv = proj(P, hT, ts_, Wh, 2048, 2, r_hT, r_Wh)
                        for hb in range(2):
                            b_, rb = pv[hb]
                            k.op("act", lambda: nc.scalar.copy(v32s[0:P, hb * 512:(hb + 1) * 512], b_[0:P, :]), writes=[rb, r_v32s])
                        for qi, (src_, rs_) in enumerate(((et, r_et), (kk, r_kk), (t2, r_t2))):
                            b_, rb = nb()
                            for h in range(8):
                                k.op("pe", lambda h=h: nc.tensor.transpose(b_[:, h * NS:(h + 1) * NS], src_[0:P, h * 128:(h + 1) * 128],
                                                                            ident_f[0:P, 0:P]),
                                     reads=[rs_, r_cst], writes=[rb])
                            k.op("act", lambda: nc.scalar.copy(fT[:, qi, :, :], b_[:, 0:8 * NS].rearrange("p (h b) -> p h b", h=8)),
                                 writes=[rb, r_fT])
                        k.op("dve", lambda: nc.vector.tensor_copy(QZ[:, :, 0:NS * NS:NS + 1], fT[:, 2, :, :]), reads=[r_fT], writes=[r_QZ])
                        po = [(psf[5], r_psf[5]), (psf[6], r_psf[6])]
                        for b in range(NS):
                            ib = b % 2
                            k.dma("sp", S0b[ib][:], st_in[b].rearrange("h k v -> k h v"), writes=[r_S0b[ib]])
                            pvb = [(psf[2 * ib], r_psf[2 * ib]), (psf[2 * ib + 1], r_psf[2 * ib + 1])]
                            for hb in range(2):
                                b_, rb = pvb[hb]
                                k.op("pe", lambda: nc.tensor.matmul(b_[:, :], selc_sb[0:NS, b * 128:(b + 1) * 128], v32s[0:NS, hb * 512:(hb + 1) * 512],
                                                                    start=True, stop=True),
                                     reads=[r_v32s, r_cst], writes=[rb])
                            k.op("dve", lambda: nc.vector.tensor_tensor(Sn[ib][:], S0b[ib][:],
                                                                        fT[:, 0, :, b:b + 1].to_broadcast([128, 8, 128]), ALU.mult),
                                 reads=[r_S0b[ib], r_fT], writes=[r_Sn[ib]])
                            for hb in range(2):
                                b_, rb = pvb[hb]
                                k.op("dve", lambda: nc.vector.tensor_tensor(
                                    S0b[ib][:, hb * 4:(hb + 1) * 4, :], b_[:, :].rearrange("p (h v) -> p h v", h=4),
                                    fT[:, 1, hb * 4:(hb + 1) * 4, b:b + 1].to_broadcast([128, 4, 128]), ALU.mult),
                                    reads=[r_fT], writes=[rb, r_S0b[ib]])
                            k.op("dve", lambda: nc.vector.tensor_tensor(Sn[ib][:], Sn[ib][:], S0b[ib][:], ALU.add),
                                 reads=[r_S0b[ib]], writes=[r_Sn[ib]])
                            k.dma("sp", hg_s[b].rearrange("h k v -> k h v"), Sn[ib][:], reads=[r_Sn[ib]])
                            for h in range(8):
                                b_, rb = po[h // 4]
                                k.op("pe", lambda h=h: nc.tensor.matmul(b_[0:NS, (h % 4) * 128:(h % 4 + 1) * 128],
                                                                        QZ[:, h, b * NS:(b + 1) * NS], Sn[ib][:, h, :],
                                                                        start=(b == 0 and h % 4 == 0), stop=(b == NS - 1),
                                                                        skip_group_check=True),
                                     reads=[r_QZ, r_Sn[ib]], writes=[rb])
                        hg_epilogue(P, po, i, g0)
                        continue
                    d1 = [nb(), nb()]
                    d3 = [nb(), nb()]
                    for hb in range(2):
                        k.op("pe", lambda: nc.tensor.matmul(d1[hb][0][:, :], L1, logf[:, hb * 512:(hb + 1) * 512], start=True, stop=True),
                             reads=[r_logf, r_cst], writes=[d1[hb][1]])
                        k.op("pe", lambda: nc.tensor.matmul(d3[hb][0][:, :], L3, logf[:, hb * 512:(hb + 1) * 512], start=True, stop=True),
                             reads=[r_logf, r_cst], writes=[d3[hb][1]])
                    for hb in range(2):
                        k.op("act", lambda: nc.scalar.activation(et[:, hb * 512:(hb + 1) * 512], d1[hb][0][:, :], AF.Exp, scale=-1.0),
                             writes=[d1[hb][1], r_et])
                    k.op("dve", lambda: nc.vector.tensor_tensor(kt_b[:], kk[:], et[:], ALU.mult), reads=[r_kk, r_et], writes=[r_ktb])
                    for hb in range(2):
                        k.op("act", lambda: nc.scalar.activation(et[:, hb * 512:(hb + 1) * 512], d3[hb][0][:, :], AF.Exp),
                             writes=[d3[hb][1], r_et])
                    k.op("dve", lambda: nc.vector.tensor_tensor(kh_b[:], kk[:], et[:], ALU.mult), reads=[r_kk, r_et], writes=[r_khb])
                    for hb in range(2):
                        k.op("act", lambda: nc.scalar.activation(et[:, hb * 512:(hb + 1) * 512], d1[hb][0][:, :], AF.Exp),
                             writes=[d1[hb][1], r_et])
                    pq = proj(P, hT, ts_, Wh, 0, 2, r_hT, r_Wh)
                    for hb in range(2):
                        b_, rb = pq[hb]
                        k.op("act", lambda: nc.scalar.activation(t2[:, hb * 512:(hb + 1) * 512], b_[:, :], AF.Silu), writes=[rb, r_t2])
                    k.op("dve", lambda: nc.vector.tensor_tensor(qt_b[:], t2[:], et[:], ALU.mult), reads=[r_t2, r_et], writes=[r_qtb])
                    pv = proj(P, hT, ts_, Wh, 2048, 2, r_hT, r_Wh)
                    for hb in range(2):
                        b_, rb = pv[hb]
                        k.op("act", lambda: nc.scalar.copy(v_b[:, hb * 512:(hb + 1) * 512], b_[:, :]), writes=[rb, r_vb])
                    be, rbe = nb()
                    for h in range(8):
                        k.op("pe", lambda h=h: nc.tensor.matmul(be[:, h * 4:(h + 1) * 4], logf[:, h * 128:(h + 1) * 128], R4,
                                                                start=(h == 0), stop=(h == 7), skip_group_check=True),
                             reads=[r_logf, r_cst], writes=[rbe])
                    k.op("act", lambda: nc.scalar.activation(eb[:], be[:, 0:32].rearrange("p (h c) -> p h c", h=8), AF.Exp),
                         writes=[rbe, r_eb])
                    for src_, rs_, dst_, rd_ in ((qt_b, r_qtb, qT, r_qT), (kt_b, r_ktb, kT, r_kT)):
                        for h in range(8):
                            k.op("pe", lambda h=h: nc.tensor.transpose(psb[:, h * 128:(h + 1) * 128], src_[:, h * 128:(h + 1) * 128], ident_b[:]),
                                 reads=[rs_, r_idb], writes=[r_psb])
                        k.op("act", lambda: nc.scalar.copy(dst_[:], psb[:].rearrange("p (h n) -> p h n", h=8)), writes=[r_psb, rd_])
                    pa = [nb(), nb()]
                    for h in range(8):
                        b_, rb = pa[h // 4]
                        c_ = (h % 4) * 128
                        k.op("pe", lambda h=h: nc.tensor.matmul(b_[0:64, c_:c_ + 64], kT[:, h, 0:64], qT[:, h, 0:64], start=True, stop=True,
                                                                skip_group_check=True),
                             reads=[r_kT, r_qT], writes=[rb])
                        k.op("pe", lambda h=h: nc.tensor.matmul(b_[:, c_ + 64:c_ + 128], kT[:, h, :], qT[:, h, 64:128], start=True, stop=True,
                                                                skip_group_check=True),
                             reads=[r_kT, r_qT], writes=[rb])
                    for hb in range(2):
                        b_, rb = pa[hb]
                        bv = b_[:, :].rearrange("p (h t) -> p h t", h=4)
                        k.op("dve", lambda: nc.vector.tensor_tensor(Abd[0:64, hb * 4:(hb + 1) * 4, 0:64], bv[0:64, :, 0:64],
                                                                    MA[0:64, 0:64].unsqueeze(1).to_broadcast([64, 4, 64]), ALU.mult),
                             reads=[r_cst], writes=[rb, r_Abd])
                        k.op("dve", lambda: nc.vector.tensor_tensor(Abd[64:128, hb * 4:(hb + 1) * 4, 64:128], bv[64:128, :, 64:128],
                                                                    MA[64:128, 64:128].unsqueeze(1).to_broadcast([64, 4, 64]), ALU.mult),
                             reads=[r_cst], writes=[rb, r_Abd])
                    k.op("dve", lambda: nc.vector.tensor_tensor(Sb[0][:], Sm[:], eb[:, :, 2:3].to_broadcast([128, 8, 128]), ALU.mult),
                         reads=[r_Sm, r_eb], writes=[r_Sb[0]])
                    for c in range(2):
                        src_S, rsrc = (Sm, r_Sm) if c == 0 else (St, r_St)
                        dst_S, rdst = (St, r_St) if c == 0 else (Sm, r_Sm)
                        psn = [nb(), nb()]
                        for h in range(8):
                            b_, rb = psn[h // 4]
                            c_ = (h % 4) * 128
                            k.op("pe", lambda h=h: nc.tensor.matmul(b_[:, c_:c_ + 128], kh_b[c * 64:(c + 1) * 64, h * 128:(h + 1) * 128],
                                                                    v_b[c * 64:(c + 1) * 64, h * 128:(h + 1) * 128], start=True, stop=True,
                                                                    skip_group_check=True),
                                 reads=[r_khb, r_vb], writes=[rb])
                        k.op("dve", lambda: nc.vector.tensor_tensor(dst_S[:], src_S[:], eb[:, :, c:c + 1].to_broadcast([128, 8, 128]), ALU.mult),
                             reads=[rsrc, r_eb], writes=[rdst])
                        for hb in range(2):
                            b_, rb = psn[hb]
                            k.op("dve", lambda: nc.vector.tensor_tensor(dst_S[:, hb * 4:(hb + 1) * 4, :], dst_S[:, hb * 4:(hb + 1) * 4, :],
                                                                        b_[:, :].rearrange("p (h v) -> p h v", h=4), ALU.add),
                                 writes=[rb, rdst])
                        if c == 0:
                            k.op("dve", lambda: nc.vector.tensor_tensor(Sb[1][:], St[:], eb[:, :, 3:4].to_broadcast([128, 8, 128]), ALU.mult),
                                 reads=[r_St, r_eb], writes=[r_Sb[1]])
                    po = [nb(), nb()]
                    for h in range(8):
                        b_, rb = po[h // 4]
                        c_ = (h % 4) * 128
                        k.op("pe", lambda h=h: nc.tensor.matmul(b_[:, c_:c_ + 128], Abd[:, h, :], v_b[:, h * 128:(h + 1) * 128],
                                                                start=True, stop=False, skip_group_check=True),
                             reads=[r_Abd, r_vb], writes=[rb])
                        k.op("pe", lambda h=h: nc.tensor.matmul(b_[0:64, c_:c_ + 128], qT[:, h, 0:64], Sb[0][:, h, :],
                                                                start=False, stop=False, skip_group_check=True),
                             reads=[r_qT, r_Sb[0]], writes=[rb])
                        k.op("pe", lambda h=h: nc.tensor.matmul(b_[64:128, c_:c_ + 128], qT[:, h, 64:128], Sb[1][:, h, :],
                                                                start=False, stop=True, skip_group_check=True),
                             reads=[r_qT, r_Sb[1]], writes=[rb])
                    hg_epilogue(P, po, i, g0)
                k.flush()
                if s < 2:
                    k.dma("sp", hg_p[s].rearrange("h k v -> k h v"), Sm[:], reads=[r_Sm])
                k.barrier()
    if stop_after <= 3:
        k.finish()
        return


    with contextlib.ExitStack() as ph:
        def psb_(name, shape, dt):
            return ph.enter_context(nc.sbuf_tensor(name, list(shape), dt))
        qblk = [psb_("qblk%d" % i, [128, 512], BF16) for i in range(2)]
        kblk = [psb_("kblk%d" % i, [128, 512], BF16) for i in range(2)]
        vblk = [psb_("vblk%d" % i, [128, 8, 65], BF16) for i in range(2)]
        r_qblk, r_kblk, r_vblk = [Res(), Res()], [Res(), Res()], [Res(), Res()]
        qTa = psb_("qTa", [128, 4, 128], BF16)
        kTa = [psb_("kTa%d" % i, [128, 4, 128], BF16) for i in range(2)]
        r_qTa = Res()
        r_kTa = [Res(), Res()]
        pT = [psb_("pT%d" % i, [128, 4, 128], BF16) for i in range(4)]
        r_pT = [Res() for _ in range(4)]
        oac = [psb_("oac%d" % i, [128, 520], F32) for i in range(2)]
        r_oac = [Res(), Res()]
        for i in range(2):
            k.op("pool", lambda i=i: nc.gpsimd.memset(qblk[i][:], 0.0), writes=[r_qblk[i]])
            k.op("pool", lambda i=i: nc.gpsimd.memset(kblk[i][:], 0.0), writes=[r_kblk[i]])
            k.op("pool", lambda i=i: nc.gpsimd.memset(vblk[i][:], 1.0), writes=[r_vblk[i]])
        blk_ctr = [0]
        for c_ in range(8):
            rs_ = slice(c_ * 2048, (c_ + 1) * 2048)
            k.dma("pool", UV[rs_, 0:D], peer_u[rs_, :], accum=[r_uv])
            k.dma("pool", UV[rs_, D:2 * D], peer_v[rs_, :], accum=[r_uv])

        def attn_block(load_cur, has_prev, ip, store):
            n_ = blk_ctr[0]
            blk_ctr[0] += 1
            ic = 1 - ip
            load_cur(ic)
            for src_, rs_, dst_, rd_ in ((qblk[ic], r_qblk[ic], qTa, r_qTa), (kblk[ic], r_kblk[ic], kTa[ic], r_kTa[ic])):
                for hp in range(4):
                    k.op("pe", lambda hp=hp: nc.tensor.transpose(psb[:, hp * 128:(hp + 1) * 128], src_[:, hp * 128:(hp + 1) * 128], ident_b[:]),
                         reads=[rs_, r_idb], writes=[r_psb])
                k.op("act", lambda: nc.scalar.copy(dst_[:], psb[:, 0:512].rearrange("p (h n) -> p h n", h=4)), writes=[r_psb, rd_])
            srcs = [(ic, maskc_b)] + ([(ip, maskp_b)] if has_prev else [])
            pts = []
            for si, (ib, msk) in enumerate(srcs):
                for hb in range(2):
                    b_, rb = nb()
                    for hh in range(4):
                        h = 2 * hh + hb
                        po_ = hb * 64
                        k.op("pe", lambda: nc.tensor.matmul(b_[:, hh * 128:(hh + 1) * 128], kTa[ib][po_:po_ + 64, h // 2, :],
                                                            qTa[po_:po_ + 64, h // 2, :], start=True, stop=True, skip_group_check=True),
                             reads=[r_kTa[ib], r_qTa], writes=[rb])
                    pi = si * 2 + hb
                    k.op("act", lambda: nc.scalar.activation(pT[pi][:], b_[:, :].rearrange("p (h n) -> p h n", h=4), AF.Exp),
                         writes=[rb, r_pT[pi]])
                    k.op("dve", lambda: nc.vector.tensor_tensor(pT[pi][:], pT[pi][:], msk[:], ALU.mult), reads=[r_mask], writes=[r_pT[pi]])
                    pts.append((pi, ib))
            io = n_ % 2
            for hb in range(2):
                b_, rb = nb()
                for hh in range(4):
                    h = hb * 4 + hh
                    for si, (ib, msk) in enumerate(srcs):
                        pi = si * 2 + (h % 2)
                        k.op("pe", lambda: nc.tensor.matmul(b_[:, hh * 65:(hh + 1) * 65], pT[pi][:, h // 2, :], vblk[ib][:, h, :],
                                                            start=(si == 0), stop=(si == len(srcs) - 1), skip_group_check=True),
                             reads=[r_pT[pi], r_vblk[ib]], writes=[rb])
                k.op("act", lambda: nc.scalar.copy(oac[io][:, hb * 260:(hb + 1) * 260], b_[:, 0:260]), writes=[rb, r_oac[io]])
            k.flush()
            k.defer(lambda io=io, store=store: store(oac[io], r_oac[io]))
            return ic

        for s in range(2):
            for g in range(3):
                d = GROUPS[g][1]
                nblk = SEQ // (128 * d)

                def view(T, width):
                    return T[tok0(s):tok0(s) + SEQ, g, :].rearrange("(n j dd) c -> dd n j c", dd=d, j=128)
                Qv, Kv, Vv = view(QS, 512), view(KS, 512), view(VS, 520)
                Ov = OACC[g, tok0(s):tok0(s) + SEQ, :].rearrange("(n j dd) c -> dd n j c", dd=d, j=128)
                for r in range(d):
                    ip = 0
                    for n in range(nblk):
                        def load_cur(ic, r=r, n=n, Qv=Qv, Kv=Kv, Vv=Vv):
                            k.dma("sp", qblk[ic][:], Qv[r, n], reads=[r_scr], writes=[r_qblk[ic]])
                            k.dma("sp", kblk[ic][:], Kv[r, n], reads=[r_scr], writes=[r_kblk[ic]])
                            k.dma("sp", vblk[ic][:].rearrange("p h e -> p (h e)"), Vv[r, n], reads=[r_scr], writes=[r_vblk[ic]])

                        def store(o_, ro_, r=r, n=n, Ov=Ov):
                            k.dma("sp", Ov[r, n], o_[:], reads=[ro_], accum=[r_oacc])
                        ip = attn_block(load_cur, n > 0, ip, store)
        for b in range(NS):
            for g in range(3):
                tokb = tok0(2) + b
                ip = 0
                k.dma("pool", kblk[ip][:], ck[g][b, :, 0, :], writes=[r_kblk[ip]])
                k.dma("pool", vblk[ip][:, :, 0:64], ck[g][b, :, 1, :].rearrange("j (h e) -> j h e", h=8), writes=[r_vblk[ip]])
                for hp in range(4):
                    k.op("pe", lambda hp=hp: nc.tensor.transpose(psb[:, hp * 128:(hp + 1) * 128], kblk[ip][:, hp * 128:(hp + 1) * 128], ident_b[:]),
                         reads=[r_kblk[ip], r_idb], writes=[r_psb])
                k.op("act", lambda: nc.scalar.copy(kTa[ip][:], psb[:, 0:512].rearrange("p (h n) -> p h n", h=4)), writes=[r_psb, r_kTa[ip]])

                def load_cur(ic, g=g, tokb=tokb):
                    k.dma("sp", qblk[ic][0:1, :], QS[tokb:tokb + 1, g, :], reads=[r_scr], writes=[r_qblk[ic]])
                    k.dma("sp", kblk[ic][0:1, :], KS[tokb:tokb + 1, g, :], reads=[r_scr], writes=[r_kblk[ic]])
                    k.dma("sp", vblk[ic][0:1, :, :].rearrange("p h e -> p (h e)"), VS[tokb:tokb + 1, g, :], reads=[r_scr], writes=[r_vblk[ic]])

                def store(o_, ro_, g=g, tokb=tokb):
                    k.dma("sp", OACC[g, tokb:tokb + 1, :], o_[0:1, :], reads=[ro_], accum=[r_oacc])
                attn_block(load_cur, True, ip, store)
        k.barrier()
    if stop_after <= 4:
        k.finish()
        return

    with contextlib.ExitStack() as ph:
        def psb_(name, shape, dt):
            return ph.enter_context(nc.sbuf_tensor(name, list(shape), dt))
        Wa = psb_("Wa", [128, 4, D], BF16)
        Wb = psb_("Wb", [128, 8, D], BF16)
        Wo = psb_("Wo", [128, 8, D], BF16)
        Wq = psb_("Wq", [128, 8, 2048], BF16)
        skt = psb_("skt", [128, 2, 128], F32)
        r_W = Res()
        k.dma("pool", Wa[:], w_br_a.rearrange("(kc p) n -> p kc n", p=128), writes=[r_W])
        k.dma("pool", Wb[:], w_br_b.rearrange("(kc p) n -> p kc n", p=128), writes=[r_W])
        k.dma("pool", Wo[:], w_o.rearrange("(kc p) n -> p kc n", p=128), writes=[r_W])
        k.dma("pool", Wq[:], w_pq.rearrange("(kc p) n -> p kc n", p=128), writes=[r_W])
        k.dma("sp", skt[:], skT.rearrange("t e c -> e t c"), writes=[r_W])
        n2w = psb_("n2w", [128, D], F32)
        k.dma("sp", n2w[:], norm2_w.partition_broadcast(128), writes=[r_W])
        iota16 = psb_("iota16", [128, 16], F32)
        k.dma("sp", iota16[:], selc[0:1, 2048:2064].partition_broadcast(128), writes=[r_W])
        G1 = psb_("G1", [128, D], F32)
        S2 = psb_("S2", [128, D], F32)
        SH2 = psb_("SH2", [128, D], F32)
        G2s = [psb_("G2_%d" % i, [128, D], F32) for i in range(2)]
        r_M = Res()
        xin = [psb_("xin0", [128, D], F32)] * 2
        r_xin = [Res()] * 2
        oin = [psb_("oin0", [128, 3, 520], F32)] * 2
        r_oin = [Res()] * 2
        hgin = [psb_("hgin0", [128, D], BF16)] * 2
        r_hgin = [Res()] * 2
        ggin = [psb_("ggin0", [128, 2048], BF16)] * 2
        r_ggin = [Res()] * 2
        rl = psb_("rl", [128, 8], F32)
        r_rl = Res()
        att_b = psb_("att_b", [128, 512], BF16)
        r_attb = Res()
        TT = psb_("TT", [128, 8, 128], BF16)
        r_TT = Res()
        f1 = psb_("f1", [128, D], F32)
        r_f1 = Res()
        yb = psb_("yb", [128, D], BF16)
        r_yb = Res()
        x1s = [psb_("x1_%d" % i, [128, D], F32) for i in range(2)]
        r_x1s = [Res(), Res()]
        h2bs = [psb_("h2b_%d" % i, [128, D], BF16) for i in range(2)]
        r_h2bs = [Res(), Res()]
        ssq = psb_("ssq", [128, 1], F32)
        r_ssq = Res()
        sc = psb_("sc", [128, 16, 128], F32)
        sc2 = psb_("sc2", [128, 16, 128], F32)
        r_sc, r_sc2 = Res(), Res()
        q2T = sc2
        r_q2T = r_sc2
        vals = psb_("vals", [128, 16, 16], F32)
        idxu = psb_("idxu", [128, 16, 16], U32)
        idxf = psb_("idxf", [128, 16, 16], F32)
        r_vals, r_idxu, r_idxf = Res(), Res(), Res()
        cand = sc[:].rearrange("p a b -> p (a b)").rearrange("p (h x) -> p h x", h=8)
        cand2 = sc2[:].rearrange("p a b -> p (a b)").rearrange("p (h x) -> p h x", h=8)
        r_cand, r_cand2 = r_sc, r_sc2
        tv = psb_("tv", [128, 8, 16], F32)
        posu = psb_("posu", [128, 8, 16], U32)
        pa_u = psb_("pa_u", [128, 8, 16], U32)
        pa_f = psb_("pa_f", [128, 2, 8, 16], F32)
        r_tv, r_posu, r_pau, r_paf = Res(), Res(), Res(), Res()
        oh = cand2
        r_oh = r_sc2
        isel = psb_("isel", [128, 2, 8, 16], F32)
        r_isel = Res()
        eid_f = psb_("eid_f", [128, 128], F32)
        eids = [psb_("eid%d" % i, [128, 128], I32) for i in range(2)]
        r_eids = [Res(), Res()]
        r_eidf = Res()
        gsm = psb_("gsm", [128, 8], F32)
        gats = [psb_("gat%d" % i, [128, 8, 16], F32) for i in range(2)]
        r_gats = [Res(), Res()]
        dots = psb_("dots", [128, 128], F32)
        r_dots = Res()
        coef = psb_("coef", [128, 128], F32)
        r_coef = Res()
        NUV = 8
        uvb = [psb_("uvb%d" % i, [128, 2 * D], BF16) for i in range(NUV)]
        r_uvb = [Res() for _ in range(NUV)]
        r_dotg = [Res() for _ in range(32)]
        r_coefg = [Res() for _ in range(32)]
        dg = [psb_("dg%d" % i, [128, 128], BF16) for i in range(4)]
        r_dg = [Res() for _ in range(4)]
        jb = psb_("jb", [128, D], BF16)
        r_jb = Res()
        yo = [psb_("yo0", [128, D], F32)] * 2
        r_yo = [Res()] * 2

        def transpose_to_TT(P, src, rsrc, nk):
            yield
            for kc in range(nk):
                k.op("pe", lambda kc=kc: nc.tensor.transpose(psb[:, kc * 128:kc * 128 + P], src[0:P, kc * 128:(kc + 1) * 128], ident_b[0:P, 0:P]),
                     reads=[rsrc, r_idb], writes=[r_psb])
            yield
            k.op("act", lambda: nc.scalar.copy(TT[:, 0:nk, 0:P], psb[:, 0:nk * 128].rearrange("p (kc n) -> p kc n", kc=nk)[:, :, 0:P]),
                 writes=[r_psb, r_TT])
            yield

        def mm_tok(P, W, nk, nbank, rW):
            out = []
            for j in range(nbank):
                b_, rb = nb()
                for kc in range(nk):
                    k.op("pe", lambda kc=kc: nc.tensor.matmul(b_[0:P, :], TT[:, kc, 0:P], W[:, kc, j * 512:(j + 1) * 512],
                                                              start=(kc == 0), stop=(kc == nk - 1)),
                         reads=[r_TT, rW], writes=[rb])
                out.append((b_, rb))
            yield
            return out

        tiles = [(s, t) for s in range(2) for t in range(NT)] + [(2, 0)]
        nbank_rot[0] = 5
        cur_s = [-1]

        def loads(idx):
            s, t = tiles[idx]
            P = 128 if s < 2 else NS
            i = idx % 2
            g0 = tok0(s) + t * 128
            k.dma("sp", xin[i][0:P, :], xp[s, t * 128:(t + 1) * 128, :] if s < 2 else xs[:, :], writes=[r_xin[i]])
            for g in range(3):
                k.dma("sp", oin[i][0:P, g, :], OACC[g, g0:g0 + P, :], reads=[r_oacc], writes=[r_oin[i]])
            k.dma("sp", hgin[i][0:P, :], HGS[g0:g0 + P, :], reads=[r_hgs], writes=[r_hgin[i]])
            k.dma("sp", ggin[i][0:P, :], GG[g0:g0 + P, 1024:3072], reads=[r_gg], writes=[r_ggin[i]])

        loads(0)
        def front(idx):
            s, t = tiles[idx]
            P = 128 if s < 2 else NS
            i = idx % 2
            pp = idx % 2
            x1, r_x1 = x1s[pp], r_x1s[pp]
            h2b, r_h2b = h2bs[pp], r_h2bs[pp]
            eid, r_eid = eids[pp], r_eids[pp]
            gat, r_gat = gats[pp], r_gats[pp]
            G2 = G2s[s % 2]
            yield
            if s != cur_s[0]:
                cur_s[0] = s
                for dst_, c0 in ((G1, 2 * D), (SH2, 3 * D), (S2, 4 * D), (G2, 5 * D)):
                    if s < 2:
                        k.dma("sp", dst_[:], MODS[s:s + 1, c0:c0 + D].partition_broadcast(128), reads=[r_MODS], writes=[r_M])
                    else:
                        k.dma("sp", dst_[0:NS, :], MODS[2:2 + NS, c0:c0 + D], reads=[r_MODS], writes=[r_M])
                k.op("dve", lambda: nc.vector.scalar_tensor_tensor(S2[0:P, :], S2[0:P, :], 1.0, n2w[0:P, :], ALU.add, ALU.mult),
                     reads=[r_W], writes=[r_M])
            yield
            o0 = oin[i]
            k.op("dve", lambda: nc.vector.tensor_tensor(o0[0:P, 0, :], o0[0:P, 0, :], o0[0:P, 1, :], ALU.add), writes=[r_oin[i]])
            k.op("dve", lambda: nc.vector.tensor_tensor(o0[0:P, 0, :], o0[0:P, 0, :], o0[0:P, 2, :], ALU.add), writes=[r_oin[i]])
            ov = o0[0:P, 0, :].rearrange("p (h e) -> p h e", h=8)
            k.op("dve", lambda: nc.vector.reciprocal(rl[0:P, :], ov[:, :, 64]), reads=[r_oin[i]], writes=[r_rl])
            k.op("dve", lambda: nc.vector.tensor_tensor(att_b[0:P, :].rearrange("p (h e) -> p h e", h=8), ov[:, :, 0:64],
                                                        rl[0:P, :].unsqueeze(2).to_broadcast([P, 8, 64]), ALU.mult),
                 reads=[r_oin[i], r_rl], writes=[r_attb])
            yield from transpose_to_TT(P, att_b, r_attb, 4)
            pA = yield from mm_tok(P, Wa, 4, 2, r_W)
            for hb in range(2):
                b_, rb = pA[hb]
                k.op("dve", lambda: nc.vector.tensor_tensor(f1[0:P, hb * 512:(hb + 1) * 512], b_[0:P, :], ggin[i][0:P, hb * 512:(hb + 1) * 512], ALU.mult),
                     reads=[r_ggin[i]], writes=[rb, r_f1])
            yield
            yield from transpose_to_TT(P, hgin[i], r_hgin[i], 8)
            pB = yield from mm_tok(P, Wb, 8, 2, r_W)
            for hb in range(2):
                b_, rb = pB[hb]
                k.op("dve", lambda: nc.vector.tensor_tensor(x1[0:P, hb * 512:(hb + 1) * 512], b_[0:P, :],
                                                            ggin[i][0:P, 1024 + hb * 512:1024 + (hb + 1) * 512], ALU.mult),
                     reads=[r_ggin[i]], writes=[rb, r_x1])
            k.op("dve", lambda: nc.vector.tensor_tensor(yb[0:P, :], f1[0:P, :], x1[0:P, :], ALU.add), reads=[r_f1, r_x1], writes=[r_yb])
            yield
            yield from transpose_to_TT(P, yb, r_yb, 8)
            pZ = yield from mm_tok(P, Wo, 8, 2, r_W)
            for hb in range(2):
                b_, rb = pZ[hb]
                k.op("dve", lambda: nc.vector.tensor_tensor(x1[0:P, hb * 512:(hb + 1) * 512], b_[0:P, :], G1[0:P, hb * 512:(hb + 1) * 512], ALU.mult),
                     reads=[r_M], writes=[rb, r_x1])
            k.op("dve", lambda: nc.vector.tensor_tensor(x1[0:P, :], x1[0:P, :], xin[i][0:P, :], ALU.add), reads=[r_xin[i]], writes=[r_x1])
            yield
            if DEBUG and (idx == 0 or s == 2):
                k.dma("pool", dbg[0 if idx == 0 else 1, 1, 0:P, :], hgin[i][0:P, :], reads=[r_hgin[i]])
            if idx + 1 < len(tiles):
                loads(idx + 1)
            k.op("act", lambda: nc.scalar.activation(f1[0:P, :], x1[0:P, :], AF.Square, accum_out=ssq[0:P, :]),
                 reads=[r_x1], writes=[r_f1, r_ssq])
            yield
            k.op("dve", lambda: nc.vector.tensor_scalar(ssq[0:P, :], ssq[0:P, :], 1.0 / D, EPS, ALU.mult, ALU.add), writes=[r_ssq])
            yield
            k.op("act", lambda: nc.scalar.activation(ssq[0:P, :], ssq[0:P, :], AF.Sqrt), writes=[r_ssq])
            yield
            k.op("dve", lambda: nc.vector.reciprocal(ssq[0:P, :], ssq[0:P, :]), writes=[r_ssq])
            k.op("dve", lambda: nc.vector.scalar_tensor_tensor(f1[0:P, :], x1[0:P, :], ssq[0:P, 0:1], S2[0:P, :], ALU.mult, ALU.mult),
                 reads=[r_x1, r_ssq, r_M], writes=[r_f1])
            k.op("dve", lambda: nc.vector.tensor_tensor(h2b[0:P, :], f1[0:P, :], SH2[0:P, :], ALU.add), reads=[r_f1, r_M], writes=[r_h2b])
            yield from transpose_to_TT(P, h2b, r_h2b, 8)
            yield
            for qb in range(4):
                yield
                b_, rb = nb()
                for hh in range(4):
                    hp = qb * 4 + hh
                    for kc in range(8):
                        k.op("pe", lambda kc=kc: nc.tensor.matmul(b_[:, hh * 128:hh * 128 + P], Wq[:, kc, hp * 128:(hp + 1) * 128], TT[:, kc, 0:P],
                                                                  start=(kc == 0), stop=(kc == 7), skip_group_check=True),
                             reads=[r_TT, r_W], writes=[rb])
                yield
                k.op("act", lambda: nc.scalar.copy(q2T[:, qb * 4:(qb + 1) * 4, 0:P], b_[:, :].rearrange("p (h n) -> p h n", h=4)[:, :, 0:P]),
                     writes=[rb, r_q2T])
            yield
            for qb in range(4):
                b_, rb = nb()
                for hh in range(4):
                    hp = qb * 4 + hh
                    k.op("pe", lambda: nc.tensor.matmul(b_[0:P, hh * 128:(hh + 1) * 128], q2T[:, hp, 0:P], skt[:, hp % 2, :],
                                                        start=True, stop=True, skip_group_check=True),
                         reads=[r_q2T, r_W], writes=[rb])
                yield
                k.op("act", lambda: nc.scalar.copy(sc[0:P, qb * 4:(qb + 1) * 4, :], b_[0:P, :].rearrange("p (h n) -> p h n", h=4)),
                     writes=[rb, r_sc])
            yield
            for hp in range(16):
                if hp % 4 == 0:
                    yield
                k.op("dve", lambda: nc.vector.max(vals[0:P, hp, 0:8], sc[0:P, hp, :]), reads=[r_sc], writes=[r_vals])
                k.op("dve", lambda: nc.vector.max_index(idxu[0:P, hp, 0:8], vals[0:P, hp, 0:8], sc[0:P, hp, :]),
                     reads=[r_sc, r_vals], writes=[r_idxu])
                k.op("dve", lambda: nc.vector.match_replace(sc2[0:P, hp, :], vals[0:P, hp, 0:8], sc[0:P, hp, :], -1e30),
                     reads=[r_sc, r_vals], writes=[r_sc2])
                k.op("dve", lambda: nc.vector.max(vals[0:P, hp, 8:16], sc2[0:P, hp, :]), reads=[r_sc2], writes=[r_vals])
                k.op("dve", lambda: nc.vector.max_index(idxu[0:P, hp, 8:16], vals[0:P, hp, 8:16], sc2[0:P, hp, :]),
                     reads=[r_sc2, r_vals], writes=[r_idxu])
            yield
            k.op("dve", lambda: nc.vector.tensor_copy(idxf[0:P], idxu[0:P]), reads=[r_idxu], writes=[r_idxf])
            v4 = vals[0:P].rearrange("p (h two) j -> p h two j", two=2)
            k.op("dve", lambda: nc.vector.tensor_tensor(cand[0:P].rearrange("p h (a b) -> p h a b", a=16),
                                                        v4[:, :, 0, :].unsqueeze(3).to_broadcast([P, 8, 16, 16]),
                                                        v4[:, :, 1, :].unsqueeze(2).to_broadcast([P, 8, 16, 16]), ALU.add),
                 reads=[r_vals], writes=[r_cand])
            for h in range(8):
                if h % 2 == 0:
                    yield
                k.op("dve", lambda: nc.vector.max(tv[0:P, h, 0:8], cand[0:P, h, :]), reads=[r_cand], writes=[r_tv])
                k.op("dve", lambda: nc.vector.max_index(posu[0:P, h, 0:8], tv[0:P, h, 0:8], cand[0:P, h, :]),
                     reads=[r_cand, r_tv], writes=[r_posu])
                k.op("dve", lambda: nc.vector.match_replace(cand2[0:P, h, :], tv[0:P, h, 0:8], cand[0:P, h, :], -1e30),
                     reads=[r_cand, r_tv], writes=[r_cand2])
                k.op("dve", lambda: nc.vector.max(tv[0:P, h, 8:16], cand2[0:P, h, :]), reads=[r_cand2], writes=[r_tv])
                k.op("dve", lambda: nc.vector.max_index(posu[0:P, h, 8:16], tv[0:P, h, 8:16], cand2[0:P, h, :]),
                     reads=[r_cand2, r_tv], writes=[r_posu])
            yield
            k.op("dve", lambda: nc.vector.tensor_single_scalar(pa_u[0:P], posu[0:P], 4, ALU.logical_shift_right), reads=[r_posu], writes=[r_pau])
            k.op("dve", lambda: nc.vector.tensor_copy(pa_f[0:P, 0], pa_u[0:P]), reads=[r_pau], writes=[r_paf])
            k.op("dve", lambda: nc.vector.tensor_single_scalar(pa_u[0:P], posu[0:P], 15, ALU.bitwise_and), reads=[r_posu], writes=[r_pau])
            k.op("dve", lambda: nc.vector.tensor_copy(pa_f[0:P, 1], pa_u[0:P]), reads=[r_pau], writes=[r_paf])
            i4 = idxf[0:P].rearrange("p (h two) j -> p h two j", two=2)
            for w_ in range(2):
                yield
                ohv = oh[0:P].rearrange("p h (j a) -> p h j a", j=16)
                k.op("dve", lambda: nc.vector.tensor_tensor(ohv, pa_f[0:P, w_].unsqueeze(3).to_broadcast([P, 8, 16, 16]),
                                                            iota16[0:P, :].unsqueeze(1).unsqueeze(1).to_broadcast([P, 8, 16, 16]), ALU.is_equal),
                     reads=[r_paf, r_W], writes=[r_oh])
                k.op("dve", lambda: nc.vector.tensor_tensor(ohv, ohv, i4[:, :, w_, :].unsqueeze(2).to_broadcast([P, 8, 16, 16]), ALU.mult),
                     reads=[r_idxf], writes=[r_oh])
                k.op("dve", lambda: nc.vector.tensor_reduce(isel[0:P, w_], ohv, AX.X, ALU.add), reads=[r_oh], writes=[r_isel])
            k.op("dve", lambda: nc.vector.scalar_tensor_tensor(eid_f[0:P, :], isel[0:P, 0].rearrange("p h j -> p (h j)"), 128.0,
                                                               isel[0:P, 1].rearrange("p h j -> p (h j)"), ALU.mult, ALU.add),
                 reads=[r_isel], writes=[r_eidf])
            k.op("dve", lambda: nc.vector.tensor_copy(eid[0:P, :], eid_f[0:P, :]), reads=[r_eidf], writes=[r_eid])
            yield
            k.op("dve", lambda: nc.vector.tensor_tensor(gat[0:P], tv[0:P], tv[0:P, :, 0:1].to_broadcast([P, 8, 16]), ALU.subtract),
                 reads=[r_tv], writes=[r_gat])
            yield
            k.op("act", lambda: nc.scalar.activation(gat[0:P], gat[0:P], AF.Exp), writes=[r_gat])
            yield
            k.op("dve", lambda: nc.vector.tensor_reduce(gsm[0:P, :], gat[0:P], AX.X, ALU.add), reads=[r_gat], writes=[r_rl])
            k.op("dve", lambda: nc.vector.reciprocal(gsm[0:P, :], gsm[0:P, :]), writes=[r_rl])
            k.op("dve", lambda: nc.vector.tensor_tensor(gat[0:P], gat[0:P], gsm[0:P, :].unsqueeze(2).to_broadcast([P, 8, 16]), ALU.mult),
                 reads=[r_rl], writes=[r_gat])
        def gather(idx, gen):
            s, t = tiles[idx]
            P = 128 if s < 2 else NS
            i = idx % 2
            pp = idx % 2
            x1, r_x1 = x1s[pp], r_x1s[pp]
            h2b, r_h2b = h2bs[pp], r_h2bs[pp]
            eid, r_eid = eids[pp], r_eids[pp]
            gat, r_gat = gats[pp], r_gats[pp]
            G2 = G2s[s % 2]
            py = [(psf[5], r_psf[5]), (psf[6], r_psf[6])]
            gatf = gat[0:P].rearrange("p h j -> p (h j)")
            for grp in range(32):
                gs = slice(grp * 4, grp * 4 + 4)
                rd, rc = r_dotg[grp], r_coefg[grp]
                for j in range(grp * 4, grp * 4 + 4):
                    bi = j % NUV
                    k.dma("pool", uvb[bi][0:P, :], UV, reads=[r_eid, r_uv], writes=[r_uvb[bi]],
                          indirect=bass.IndirectOffsetOnAxis(ap=eid[0:P, j:j + 1], axis=0))
                    k.op("dve", lambda: nc.vector.scalar_tensor_tensor(jb[0:P, :], uvb[bi][0:P, 0:D], 1.0, h2b[0:P, :], ALU.mult, ALU.mult,
                                                                       accum_out=dots[0:P, j:j + 1]),
                         reads=[r_uvb[bi], r_h2b], writes=[rd])
                if gen is not None:
                    next(gen, None)
                k.op("dve", lambda: nc.vector.tensor_tensor(coef[0:P, gs], dots[0:P, gs], dots[0:P, gs], ALU.mult), reads=[rd], writes=[rc])
                k.op("dve", lambda: nc.vector.tensor_scalar(coef[0:P, gs], coef[0:P, gs], 0.044715, 1.0, ALU.mult, ALU.add), writes=[rc])
                k.op("dve", lambda: nc.vector.tensor_tensor(coef[0:P, gs], coef[0:P, gs], dots[0:P, gs], ALU.mult), reads=[rd], writes=[rc])
                k.op("act", lambda: nc.scalar.activation(coef[0:P, gs], coef[0:P, gs], AF.Sigmoid, scale=1.5957691216057308), writes=[rc])
                k.op("dve", lambda: nc.vector.tensor_tensor(coef[0:P, gs], coef[0:P, gs], dots[0:P, gs], ALU.mult), reads=[rd], writes=[rc])
                k.op("dve", lambda: nc.vector.tensor_tensor(coef[0:P, gs], coef[0:P, gs], gatf[:, gs], ALU.mult), reads=[r_gat], writes=[rc])
                for j in range(grp * 4, grp * 4 + 4):
                    bi = j % NUV
                    di_ = j % 4
                    k.op("act", lambda: nc.scalar.mul(dg[di_][0:P, 0:P], ident_b[0:P, 0:P], coef[0:P, j:j + 1]),
                         reads=[rc, r_idb], writes=[r_dg[di_]])
                    for hb in range(2):
                        b_, rb = py[hb]
                        k.op("pe", lambda: nc.tensor.matmul(b_[0:P, :], dg[di_][0:P, 0:P], uvb[bi][0:P, D + hb * 512:D + (hb + 1) * 512],
                                                            start=(j == 0), stop=(j == 127)),
                             reads=[r_dg[di_], r_uvb[bi]], writes=[rb])
                if gen is not None:
                    next(gen, None)
            io = idx % 2
            for hb in range(2):
                b_, rb = py[hb]
                k.op("dve", lambda: nc.vector.tensor_tensor(yo[io][0:P, hb * 512:(hb + 1) * 512], b_[0:P, :], G2[0:P, hb * 512:(hb + 1) * 512], ALU.mult),
                     reads=[r_M], writes=[rb, r_yo[io]])
            k.op("dve", lambda: nc.vector.tensor_tensor(yo[io][0:P, :], yo[io][0:P, :], x1[0:P, :], ALU.add), reads=[r_x1], writes=[r_yo[io]])
            if DEBUG and (idx == 0 or s == 2):
                di = 0 if idx == 0 else 1
                k.dma("pool", dbg[di, 0, 0:P, 0:512], att_b[0:P, :], reads=[r_attb])
                k.dma("pool", dbg[di, 2, 0:P, :], yb[0:P, :], reads=[r_yb])
                k.dma("sp", dbg[di, 3, 0:P, :], x1[0:P, :], reads=[r_x1])
                k.dma("pool", dbg[di, 4, 0:P, :], h2b[0:P, :], reads=[r_h2b])
                k.dma("sp", dbg[di, 5, 0:P, 0:128], coef[0:P, :], reads=r_coefg)
                k.dma("sp", dbg[di, 6, 0:P, 0:128], eid_f[0:P, :], reads=[r_eidf])
                k.dma("sp", dbg[di, 7, 0:P, 0:128], dots[0:P, :], reads=r_dotg)
                k.dma("sp", dbg[di, 8, 0:P, 0:128], gat[0:P].rearrange("p h j -> p (h j)"), reads=[r_gat])
                k.dma("sp", dbg[di, 9, 0:P, 0:256], vals[0:P].rearrange("p a b -> p (a b)"), reads=[r_vals])
            k.dma("sp", y_p[s, t * 128:(t + 1) * 128, :] if s < 2 else y_s[:, :], yo[io][0:P, :], reads=[r_yo[io]])
        g_ = front(0)
        for _ in g_:
            pass
        for idx in range(len(tiles)):
            g_ = front(idx + 1) if idx + 1 < len(tiles) else None
            gather(idx, g_)
            if g_ is not None:
                for _ in g_:
                    pass
        k.flush()
    k.finish()


def _consts():
    c = np.zeros((128, 1024), np.float32)
    j = np.arange(128)[:, None]
    i = np.arange(128)[None, :]
    c[:, 0:128] = np.eye(128, dtype=np.float32)
    c[:, 128:256] = (j <= i)
    c[:, 256:384] = (j >= i)
    same = (j // 64) == (i // 64)
    c[:, 384:512] = same * ((j <= i).astype(np.float32) - ((j % 64) <= 31).astype(np.float32))
    c[:, 512:640] = same * (j > i)
    s_ = np.arange(128)
    c[:, 640] = s_ < 64
    c[:, 641] = s_ >= 64
    c[:, 642] = (s_ < 64) & (s_ % 64 <= 31)
    c[:, 643] = (s_ >= 64) & (s_ % 64 <= 31)
    c[:, 768:896] = same * (j <= i)
    return c


def _selc():
    c = np.zeros((NS, 2064), np.float32)
    for b in range(NS):
        c[b, b * 128:(b + 1) * 128] = 1.0
    c[:, 2048:2064] = np.arange(16, dtype=np.float32)[None, :]
    return c


_CACHE = {}


def kernel(x_prompt, x_sample, cache_kv_w128, cache_kv_w512, cache_kv_w2048, state_hgrn, c_prompt, c_sample,
           w_ada, b_ada, norm1_w, norm2_w, w_in, q_norm_w, k_norm_w, hg_lb_logits, hg_norm_w, w_br_a, w_br_b,
           w_o, w_peer_q, peer_subkeys, peer_u, peer_v, _stop_after=99):
    f = lambda a: np.ascontiguousarray(np.asarray(a, dtype=np.float32))
    key = ("nc", _stop_after)
    if key not in _CACHE:
        _CACHE[key] = build_program(_stop_after)
    nc = _CACHE[key]
    caches = [f(cache_kv_w128)[0], f(cache_kv_w512)[0], f(cache_kv_w2048)[0]]
    shared = {
        "w_ada": f(w_ada)[0], "b_ada": f(b_ada).reshape(1, -1), "norm1_w": f(norm1_w).reshape(1, -1),
        "norm2_w": f(norm2_w).reshape(1, -1), "w_in": f(w_in)[0],
        "qk_w": np.ascontiguousarray(np.stack([np.tile(f(q_norm_w)[0], 8), np.tile(f(k_norm_w)[0], 8)])),
        "lb_log": f(hg_lb_logits), "hgn_w": np.ascontiguousarray(np.tile(f(hg_norm_w)[0], 8).reshape(1, -1)),
        "w_br_a": f(w_br_a)[0], "w_br_b": f(w_br_b)[0], "w_o": f(w_o)[0], "w_pq": f(w_peer_q)[0],
        "skT": np.ascontiguousarray(f(peer_subkeys)[0].transpose(0, 2, 1)),
        "peer_u": f(peer_u)[0], "peer_v": f(peer_v)[0], "cst": _consts(), "selc": _selc(),
    }
    xpf, xsf = f(x_prompt), f(x_sample)
    cp, cs = f(c_prompt), f(c_sample)
    st = f(state_hgrn)[0]
    in_maps = []
    for c in range(NCORES):
        m = dict(shared)
        m["xp"] = xpf[c * NSEQ:(c + 1) * NSEQ]
        m["xs"] = np.ascontiguousarray(xsf[c * NS:(c + 1) * NS, 0, :])
        m["cT"] = np.ascontiguousarray(np.concatenate([cp[c * NSEQ:(c + 1) * NSEQ], cs[c * NS:(c + 1) * NS]], 0).T)
        for g in range(3):
            m["ck%d" % g] = np.ascontiguousarray(
                caches[g][c * NS:(c + 1) * NS, 0::GROUPS[g][1]][:, :128].reshape(NS, 128, 2, 512))
        m["st_in"] = st[c * NS:(c + 1) * NS]
        in_maps.append(m)
    res = run_bass_kernel_spmd(nc, in_maps, core_ids=list(range(NCORES)))
    R = res.results
    cat = lambda n: np.concatenate([np.asarray(r[n]) for r in R], 0)
    y_prompt = cat("y_p")
    y_sample = cat("y_s").reshape(NCORES * NS, 1, D)
    outs = [y_prompt, y_sample]
    for g in range(3):
        outs.append(cat("kv%d_p" % g).reshape(1, NCORES * NSEQ, GROUPS[g][0], 2, 8, 64))
    outs.append(cat("hg_p")[None])
    for g in range(3):
        outs.append(cat("kv%d_s" % g).reshape(1, NCORES * NS, 1, 2, 8, 64))
    outs.append(cat("hg_s")[None])
    if DEBUG:
        global _DBG
        _DBG = np.asarray(R[0]["dbg"])
    return tuple(np.ascontiguousarray(o, dtype=np.float32) for o in outs)
```

```python
import contextlib
import numpy as np
import concourse.bass as bass
import concourse.mybir as mybir
from concourse.bass_utils import run_bass_kernel_spmd

F32 = mybir.dt.float32
BF16 = mybir.dt.bfloat16
I32 = mybir.dt.int32
U32 = mybir.dt.uint32
ALU = mybir.AluOpType
AF = mybir.ActivationFunctionType
AX = mybir.AxisListType

NCORES = 8
D = 1024
SEQ = 4096
NSEQ = 2
NS = 16
INW = 10752
GROUPS = ((128, 1), (512, 4), (2048, 16))
EPS = 1e-6
NT = SEQ // 128
NROWS = NSEQ + NS
DEBUG = False


class Res:
    __slots__ = ("w", "rs")

    def __init__(self):
        self.w = {}
        self.rs = {}


class K:
    def __init__(self, nc, es):
        self.nc = nc
        self.es = es
        self.eng = {"pe": nc.tensor, "act": nc.scalar, "dve": nc.vector, "pool": nc.gpsimd, "sp": nc.sync}
        self.sem = {}
        self.cnt = {}
        for e in ("pe", "act", "dve", "pool"):
            self.sem[e] = es.enter_context(nc.semaphore("c_" + e))
            self.cnt[e] = 0
        self.known = {e: {} for e in self.eng}
        self.dsem = {}
        self.dpos = {}
        for q, n in (("sp", 24), ("pool", 16), ("act", 8)):
            self.dsem[q] = [[es.enter_context(nc.semaphore("d_%s%d" % (q, i))), 0] for i in range(n)]
            self.dpos[q] = 0
        self.deferred = []

    def _wait(self, e, tok):
        s, v = tok
        if v <= 0:
            return
        if e == "pe" and s is self.sem["pe"]:
            return
        kn = self.known[e]
        if kn.get(id(s), 0) >= v:
            return
        self.eng[e].wait_ge(s, v)
        kn[id(s)] = v

    def _deps(self, e, reads, writes):
        for r in reads:
            for tok in r.w.values():
                self._wait(e, tok)
        for r in writes:
            for tok in r.w.values():
                self._wait(e, tok)
            for tok in r.rs.values():
                self._wait(e, tok)

    def _mark(self, tok, reads, writes, accum):
        s, v = tok
        for r in reads:
            r.rs[id(s)] = tok
        for r in writes:
            r.w = {id(s): tok}
            r.rs = {}
        for r in accum:
            r.w[id(s)] = tok

    def op(self, e, fn, reads=(), writes=(), accum=()):
        self._deps(e, reads, writes)
        ins = fn()
        self.cnt[e] += 1
        ins.then_inc(self.sem[e], 1)
        self._mark((self.sem[e], self.cnt[e]), reads, writes, accum)

    def dma(self, q, out, in_, reads=(), writes=(), accum=(), indirect=None):
        self._deps(q, reads, writes)
        slot = self.dsem[q][self.dpos[q] % len(self.dsem[q])]
        self.dpos[q] += 1
        self._wait(q, (slot[0], slot[1]))
        if indirect is not None:
            ins = self.eng[q].indirect_dma_start(out=out, out_offset=None, in_=in_, in_offset=indirect)
        else:
            ins = self.eng[q].dma_start(out=out, in_=in_)
        slot[1] += 16
        ins.then_inc(slot[0], 16)
        self._mark((slot[0], slot[1]), reads, writes, accum)

    def barrier(self):
        self.flush()
        toks = [(self.sem[e], self.cnt[e]) for e in self.sem]
        for q in self.dsem:
            toks += [(s, v) for s, v in self.dsem[q]]
        for e in self.eng:
            for tok in toks:
                if e != "pe" or tok[0] is not self.sem["pe"]:
                    self._wait(e, tok)

    def defer(self, fn):
        self.deferred.append(fn)

    def flush(self):
        d, self.deferred = self.deferred, []
        for fn in d:
            fn()

    def finish(self):
        self.flush()
        for q in self.dsem:
            for s, v in self.dsem[q]:
                self._wait("sp", (s, v))


def build_program(stop_after=99):
    nc = bass.Bass("TRN2", target_bir_lowering=False)
    es = contextlib.ExitStack()
    with es:
        _emit(nc, es, stop_after)
    return nc


def _emit(nc, es, stop_after):
    k = K(nc, es)

    def din(name, shape, dt=F32):
        return nc.dram_tensor(name, list(shape), dt, kind="ExternalInput").ap()

    def dout(name, shape, dt=F32):
        return nc.dram_tensor(name, list(shape), dt, kind="ExternalOutput").ap()

    def dscr(name, shape, dt):
        return nc.dram_tensor(name, list(shape), dt, kind="Internal").ap()

    def sb(name, shape, dt):
        return es.enter_context(nc.sbuf_tensor(name, list(shape), dt))

    xp = din("xp", [NSEQ, SEQ, D])
    xs = din("xs", [NS, D])
    cT = din("cT", [D, NROWS])
    ck = [din("ck%d" % g, [NS, 128, 2, 512]) for g in range(3)]
    st_in = din("st_in", [NS, 8, 128, 128])
    w_ada = din("w_ada", [D, 6 * D])
    b_ada = din("b_ada", [1, 6 * D])
    norm1_w = din("norm1_w", [1, D])
    norm2_w = din("norm2_w", [1, D])
    w_in = din("w_in", [D, INW])
    qk_w = din("qk_w", [2, 512])
    lb_log = din("lb_log", [2, D])
    hgn_w = din("hgn_w", [1, D])
    w_br_a = din("w_br_a", [512, D])
    w_br_b = din("w_br_b", [D, D])
    w_o = din("w_o", [D, D])
    w_pq = din("w_pq", [D, 2048])
    skT = din("skT", [2, 128, 128])
    peer_u = din("peer_u", [16384, D])
    peer_v = din("peer_v", [16384, D])
    cst = din("cst", [128, 128 * 8])
    selc = din("selc", [NS, 2064])

    y_p = dout("y_p", [NSEQ, SEQ, D])
    y_s = dout("y_s", [NS, D])
    kv_p = [dout("kv%d_p" % g, [NSEQ, GROUPS[g][0], 2, 512]) for g in range(3)]
    hg_p = dout("hg_p", [NSEQ, 8, 128, 128])
    kv_s = [dout("kv%d_s" % g, [NS, 2, 512]) for g in range(3)]
    hg_s = dout("hg_s", [NS, 8, 128, 128])
    dbg = dout("dbg", [2, 10, 128, D]) if DEBUG else None

    MODS = dscr("MODS", [NROWS, 6 * D], F32)
    NTOK = NSEQ * SEQ + 128
    QS = dscr("QS", [NTOK, 3, 512], BF16)
    KS = dscr("KS", [NTOK, 3, 512], BF16)
    VS = dscr("VS", [NTOK, 3, 520], BF16)

    cst_f = sb("cst_f", [128, 1024], F32)
    ident_b = sb("ident_b", [128, 128], BF16)
    r_cst = Res()
    k.dma("sp", cst_f[:], cst, writes=[r_cst])
    r_idb = Res()
    k.op("dve", lambda: nc.vector.tensor_copy(ident_b[:], cst_f[:, 0:128]), reads=[r_cst], writes=[r_idb])
    ident_f = cst_f[:, 0:128]

    psf = [es.enter_context(nc.psum_tensor("psf%d" % i, [128, 512], F32)) for i in range(7)]
    r_psf = [Res() for _ in range(7)]
    psb = es.enter_context(nc.psum_tensor("psb", [128, 1024], BF16))
    r_psb = Res()

    def rstd_from_ss(P, ss, n_elem, tmp):
        t, r = ss
        k.op("dve", lambda: nc.vector.tensor_scalar(t, t, 1.0 / n_elem, EPS, ALU.mult, ALU.add), writes=[r])
        k.op("act", lambda: nc.scalar.activation(t, t, AF.Sqrt), writes=[r])
        k.op("dve", lambda: nc.vector.reciprocal(t, t), writes=[r])

    with contextlib.ExitStack() as ph:
        def psb_(name, shape, dt):
            return ph.enter_context(nc.sbuf_tensor(name, list(shape), dt))
        cT_f = psb_("cT_f", [128, 8, NROWS], F32)
        cT_b = psb_("cT_b", [128, 8, NROWS], BF16)
        r_cT = Res()
        k.dma("sp", cT_f[:], cT.rearrange("(kc p) n -> p kc n", p=128), writes=[r_cT])
        k.op("act", lambda: nc.scalar.activation(cT_b[:], cT_f[:], AF.Silu), reads=[r_cT], writes=[r_cT])
        wa = [psb_("wa%d" % i, [128, 8, 512], BF16) for i in range(2)]
        r_wa = [Res(), Res()]
        ba = [psb_("ba%d" % i, [NROWS, 512], F32) for i in range(2)]
        r_ba = [Res(), Res()]
        mo = [psb_("mo%d" % i, [NROWS, 512], F32) for i in range(2)]
        r_mo = [Res(), Res()]
        r_MODS = Res()
        for c in range(12):
            i = c % 2
            cs = slice(c * 512, (c + 1) * 512)
            k.dma("pool", wa[i][:], w_ada[:, cs].rearrange("(kc p) n -> p kc n", p=128), writes=[r_wa[i]])
            k.dma("sp", ba[i][:], b_ada[:, cs].partition_broadcast(NROWS), writes=[r_ba[i]])
            for kc in range(8):
                k.op("pe", lambda kc=kc: nc.tensor.matmul(psf[i][0:NROWS, :], cT_b[:, kc, :], wa[i][:, kc, :],
                                                          start=(kc == 0), stop=(kc == 7)),
                     reads=[r_cT, r_wa[i]], writes=[r_psf[i]])
            k.op("dve", lambda: nc.vector.tensor_tensor(mo[i][:], psf[i][0:NROWS, :], ba[i][:], ALU.add),
                 reads=[r_ba[i]], writes=[r_psf[i], r_mo[i]])
            k.dma("sp", MODS[:, cs], mo[i][:], reads=[r_mo[i]], accum=[r_MODS])
        k.barrier()
    if stop_after <= 0:
        k.finish()
        return

    def tok0(s):
        return s * SEQ

    NTOKX = NSEQ * SEQ + 128
    GG = dscr("GG", [NTOKX, 3072], BF16)
    HGS = dscr("HGS", [NTOKX, D], BF16)
    OACC = dscr("OACC", [3, NTOKX, 520], F32)
    UV = dscr("UV", [16384, 2 * D], BF16)
    r_uv = Res()
    r_scr = Res()
    r_gg = Res()
    r_hgs = Res()
    r_oacc = Res()
    bank_ctr = [0]

    nbank_rot = [7]

    def nb():
        i = bank_ctr[0] % nbank_rot[0]
        bank_ctr[0] += 1
        return psf[i], r_psf[i]

    maskc_b = sb("maskc_b", [128, 4, 128], BF16)
    maskp_b = sb("maskp_b", [128, 4, 128], BF16)
    r_mask = Res()
    for hh in range(4):
        k.op("dve", lambda hh=hh: nc.vector.tensor_copy(maskc_b[:, hh, :], cst_f[:, 128:256]), reads=[r_cst], writes=[r_mask])
        k.op("dve", lambda hh=hh: nc.vector.tensor_copy(maskp_b[:, hh, :], cst_f[:, 256:384]), reads=[r_cst], writes=[r_mask])
    L1 = cst_f[:, 384:512]
    L3 = cst_f[:, 512:640]
    R4 = cst_f[:, 640:644]
    MA = cst_f[:, 768:896]

    def proj(P, hT, ts_, W, c0, ncol512, r_hT, r_W):
        out = []
        for j in range(ncol512):
            b_, rb = nb()
            for kc in range(8):
                k.op("pe", lambda kc=kc: nc.tensor.matmul(b_[0:P, :], hT[:, kc, ts_], W[:, kc, c0 + j * 512:c0 + (j + 1) * 512],
                                                          start=(kc == 0), stop=(kc == 7)),
                     reads=[r_hT, r_W], writes=[rb])
            out.append((b_, rb))
        return out

    with contextlib.ExitStack() as seqscope:
        hT = seqscope.enter_context(nc.sbuf_tensor("hT", [128, 8, SEQ], BF16))
        r_hT = Res()
        for s in range(3):
            P = 128 if s < 2 else NS
            ntile = NT if s < 2 else 1

            def xsrc(t):
                return xp[s, t * 128:(t + 1) * 128, :] if s < 2 else xs[:, :]

            with contextlib.ExitStack() as ph:
                def psb_(name, shape, dt):
                    return ph.enter_context(nc.sbuf_tensor(name + "_s%d" % s, list(shape), dt))
                S1 = psb_("S1", [128, D], F32)
                SH1 = psb_("SH1", [128, D], F32)
                n1w = psb_("n1w", [128, D], F32)
                r_S1 = Res()
                r_n1w = Res()
                k.dma("sp", n1w[:], norm1_w.partition_broadcast(128), writes=[r_n1w])
                qkw = psb_("qkw", [128, 2, 512], F32)
                r_qkw = Res()
                k.dma("sp", qkw[:, 0, :], qk_w[0:1, :].partition_broadcast(128), writes=[r_qkw])
                k.dma("sp", qkw[:, 1, :], qk_w[1:2, :].partition_broadcast(128), writes=[r_qkw])
                k.op("dve", lambda: nc.vector.tensor_scalar(qkw[:, 0, :], qkw[:, 0, :], 0.125, None, ALU.mult), writes=[r_qkw])
                xt = [psb_("xt%d" % i, [128, D], F32) for i in range(2)]
                r_xt = [Res(), Res()]
                xm = psb_("xm", [128, D], F32)
                xb = psb_("xb", [128, D], BF16)
                r_xm = Res()
                r_xb = Res()
                junk = psb_("junk", [128, D], F32)
                r_junk = Res()
                ss1 = psb_("ss1", [128, 1], F32)
                r_ss1 = Res()
                Wg = psb_("Wg", [128, 8, 1536], BF16)
                r_Wg = Res()
                ss8 = psb_("ss8", [128, 8], F32)
                r_ss8 = Res()
                qn_b = [psb_("qn_b%d" % i, [128, 512], BF16) for i in range(2)]
                r_qn = [Res(), Res()]
                kn32 = [psb_("kn32%d" % i, [128, 512], F32) for i in range(2)]
                r_kn32 = [Res(), Res()]
                kn_b = [psb_("kn_b%d" % i, [128, 512], BF16) for i in range(2)]
                r_knb = [Res(), Res()]
                v32 = [psb_("v32%d" % i, [128, 512], F32) for i in range(2)]
                r_v32 = [Res(), Res()]
                vaug = [psb_("vaug%d" % i, [128, 8, 65], BF16) for i in range(2)]
                r_vaug = [Res(), Res()]
                for i in range(2):
                    k.op("pool", lambda i=i: nc.gpsimd.memset(vaug[i][:], 1.0), writes=[r_vaug[i]])

                if s < 2:
                    k.dma("sp", SH1[:], MODS[s:s + 1, 0:D].partition_broadcast(128), reads=[r_MODS], writes=[r_S1])
                    k.dma("sp", S1[:], MODS[s:s + 1, D:2 * D].partition_broadcast(128), reads=[r_MODS], writes=[r_S1])
                else:
                    k.dma("sp", SH1[0:NS, :], MODS[2:2 + NS, 0:D], reads=[r_MODS], writes=[r_S1])
                    k.dma("sp", S1[0:NS, :], MODS[2:2 + NS, D:2 * D], reads=[r_MODS], writes=[r_S1])
                k.op("dve", lambda: nc.vector.scalar_tensor_tensor(S1[0:P, :], S1[0:P, :], 1.0, n1w[0:P, :], ALU.add, ALU.mult),
                     reads=[r_n1w], writes=[r_S1])

                k.dma("sp", xt[0][0:P, :], xsrc(0), writes=[r_xt[0]])
                for t in range(ntile):
                    i = t % 2
                    if t + 1 < ntile:
                        k.dma("sp", xt[1 - i][0:P, :], xsrc(t + 1), writes=[r_xt[1 - i]])
                    k.op("act", lambda: nc.scalar.activation(junk[0:P, :], xt[i][0:P, :], AF.Square, accum_out=ss1[0:P, :]),
                         reads=[r_xt[i]], writes=[r_junk, r_ss1])
                    rstd_from_ss(P, (ss1[0:P, :], r_ss1), D, None)
                    k.op("dve", lambda: nc.vector.scalar_tensor_tensor(xm[0:P, :], xt[i][0:P, :], ss1[0:P, 0:1], S1[0:P, :],
                                                                       ALU.mult, ALU.mult),
                         reads=[r_xt[i], r_ss1, r_S1], writes=[r_xm])
                    k.op("dve", lambda: nc.vector.tensor_tensor(xb[0:P, :], xm[0:P, :], SH1[0:P, :], ALU.add),
                         reads=[r_xm, r_S1], writes=[r_xb])
                    for kc in range(8):
                        k.op("pe", lambda kc=kc: nc.tensor.transpose(psb[:, kc * 128:kc * 128 + P], xb[0:P, kc * 128:(kc + 1) * 128],
                                                                     ident_b[0:P, 0:P]),
                             reads=[r_xb, r_idb], writes=[r_psb])
                    k.op("act", lambda: nc.scalar.copy(hT[:, :, t * 128:t * 128 + P],
                                                       psb[:].rearrange("p (kc n) -> p kc n", kc=8)[:, :, 0:P]),
                         writes=[r_psb, r_hT])

                for g in range(3):
                    win = GROUPS[g][0]
                    for part in range(3):
                        c0 = part * 1536 + g * 512
                        k.dma("pool", Wg[:, :, part * 512:(part + 1) * 512],
                              w_in[:, c0:c0 + 512].rearrange("(kc p) n -> p kc n", p=128), writes=[r_Wg])
                    for t in range(ntile):
                        i = t % 2
                        ts_ = slice(t * 128, t * 128 + P)
                        g0 = tok0(s) + t * 128
                        pr = proj(P, hT, ts_, Wg, 0, 3, r_hT, r_Wg)
                        bank = [p_[0] for p_ in pr]
                        rbank = [p_[1] for p_ in pr]
                        for part in range(2):
                            ps_ = bank[part]
                            k.op("act", lambda: nc.scalar.activation(junk[0:P, 0:512], ps_[0:P, :], AF.Square),
                                 writes=[rbank[part], r_junk])
                            k.op("dve", lambda: nc.vector.tensor_reduce(ss8[0:P, :], junk[0:P, 0:512].rearrange("p (h e) -> p h e", h=8),
                                                                        AX.X, ALU.add),
                                 reads=[r_junk], writes=[r_ss8])
                            rstd_from_ss(P, (ss8[0:P, :], r_ss8), 64, None)
                            dst32 = junk if part == 0 else kn32[i]
                            rdst = r_junk if part == 0 else r_kn32[i]
                            k.op("dve", lambda: nc.vector.tensor_tensor(
                                dst32[0:P, 0:512].rearrange("p (h e) -> p h e", h=8),
                                ps_[0:P, :].rearrange("p (h e) -> p h e", h=8),
                                ss8[0:P, :].unsqueeze(2).to_broadcast([P, 8, 64]), ALU.mult),
                                reads=[r_ss8], writes=[rbank[part], rdst])
                            if part == 0:
                                k.op("dve", lambda: nc.vector.tensor_tensor(qn_b[i][0:P, :], junk[0:P, 0:512], qkw[0:P, 0, :], ALU.mult),
                                     reads=[r_junk, r_qkw], writes=[r_qn[i]])
                            else:
                                k.op("dve", lambda: nc.vector.tensor_tensor(kn32[i][0:P, :], kn32[i][0:P, :], qkw[0:P, 1, :], ALU.mult),
                                     reads=[r_qkw], writes=[r_kn32[i]])
                                k.op("pool", lambda: nc.gpsimd.tensor_copy(kn_b[i][0:P, :], kn32[i][0:P, :]),
                                     reads=[r_kn32[i]], writes=[r_knb[i]])
                        k.op("act", lambda: nc.scalar.copy(v32[i][0:P, :], bank[2][0:P, :]), writes=[rbank[2], r_v32[i]])
                        k.op("pool", lambda: nc.gpsimd.tensor_copy(vaug[i][0:P, :, 0:64],
                                                                    v32[i][0:P, :].rearrange("p (h e) -> p h e", h=8)),
                             reads=[r_v32[i]], writes=[r_vaug[i]])
                        k.flush()

                        def stores(i=i, g=g, g0=g0, t=t, P=P, s=s, win=win):
                            k.dma("sp", QS[g0:g0 + P, g, :], qn_b[i][0:P, :], reads=[r_qn[i]], accum=[r_scr])
                            k.dma("sp", KS[g0:g0 + P, g, :], kn_b[i][0:P, :], reads=[r_knb[i]], accum=[r_scr])
                            k.dma("sp", VS[g0:g0 + P, g, :], vaug[i][0:P, :, :].rearrange("p h e -> p (h e)"),
                                  reads=[r_vaug[i]], accum=[r_scr])
                            if s < 2:
                                r0 = t * 128 - (SEQ - win)
                                if r0 >= 0:
                                    k.dma("sp", kv_p[g][s, r0:r0 + 128, 0, :], kn32[i][:], reads=[r_kn32[i]])
                                    k.dma("sp", kv_p[g][s, r0:r0 + 128, 1, :], v32[i][:], reads=[r_v32[i]])
                            else:
                                k.dma("sp", kv_s[g][:, 0, :], kn32[i][0:P, :], reads=[r_kn32[i]])
                                k.dma("sp", kv_s[g][:, 1, :], v32[i][0:P, :], reads=[r_v32[i]])
                        k.defer(stores)
                    k.flush()
                k.barrier()
            if stop_after <= 1:
                continue

            with contextlib.ExitStack() as ph:
                def psb_(name, shape, dt):
                    return ph.enter_context(nc.sbuf_tensor(name + "_s%d" % s, list(shape), dt))
                Wt = psb_("Wt", [128, 8, 3072], BF16)
                r_Wt = Res()
                for j, c0 in enumerate((7680, 8704, 9728)):
                    k.dma("pool", Wt[:, :, j * 1024:(j + 1) * 1024],
                          w_in[:, c0:c0 + 1024].rearrange("(kc p) n -> p kc n", p=128), writes=[r_Wt])
                ggb = [psb_("ggb%d" % i, [128, 3072], BF16) for i in range(2)]
                r_ggb = [Res(), Res()]
                for t in range(ntile):
                    i = t % 2
                    ts_ = slice(t * 128, t * 128 + P)
                    g0 = tok0(s) + t * 128
                    for j in range(6):
                        pr = proj(P, hT, ts_, Wt, j * 512, 1, r_hT, r_Wt)
                        b_, rb = pr[0]
                        fn_ = AF.Silu if j < 2 else AF.Sigmoid
                        k.op("act", lambda: nc.scalar.activation(ggb[i][0:P, j * 512:(j + 1) * 512], b_[0:P, :], fn_),
                             writes=[rb, r_ggb[i]])
                    k.flush()
                    k.defer(lambda i=i, g0=g0, P=P: k.dma("sp", GG[g0:g0 + P, :], ggb[i][0:P, :], reads=[r_ggb[i]], accum=[r_gg]))
                k.barrier()
            if stop_after <= 2:
                continue

            with contextlib.ExitStack() as ph:
                def psb_(name, shape, dt):
                    return ph.enter_context(nc.sbuf_tensor(name + "_s%d" % s, list(shape), dt))
                Wh = psb_("Wh", [128, 8, 3072], BF16)
                r_Wh = Res()
                for j in range(3):
                    c0 = 4608 + j * 1024
                    k.dma("pool", Wh[:, :, j * 1024:(j + 1) * 1024],
                          w_in[:, c0:c0 + 1024].rearrange("(kc p) n -> p kc n", p=128), writes=[r_Wh])
                lbt = psb_("lbt", [128, D], F32)
                omlt = psb_("omlt", [128, D], F32)
                hgw = psb_("hgw", [128, D], F32)
                r_lb = Res()
                k.dma("sp", lbt[:], lb_log[0:1, :].partition_broadcast(128), writes=[r_lb])
                k.dma("sp", omlt[:], lb_log[1:2, :].partition_broadcast(128), writes=[r_lb])
                k.dma("sp", hgw[:], hgn_w.partition_broadcast(128), writes=[r_lb])
                k.op("dve", lambda: nc.vector.tensor_tensor(lbt[:], lbt[:], omlt[:], ALU.subtract), writes=[r_lb])
                k.op("act", lambda: nc.scalar.activation(lbt[:], lbt[:], AF.Sigmoid), writes=[r_lb])
                k.op("dve", lambda: nc.vector.tensor_scalar(omlt[:], lbt[:], -1.0, 1.0, ALU.mult, ALU.add), writes=[r_lb])
                logf = psb_("logf", [128, D], F32)
                kk = psb_("kk", [128, D], F32)
                et = psb_("et", [128, D], F32)
                t2 = psb_("t2", [128, D], F32)
                r_logf, r_kk, r_et, r_t2 = Res(), Res(), Res(), Res()
                kt_b = psb_("kt_b", [128, D], BF16)
                kh_b = psb_("kh_b", [128, D], BF16)
                qt_b = psb_("qt_b", [128, D], BF16)
                v_b = psb_("v_b", [128, D], BF16)
                r_ktb, r_khb, r_qtb, r_vb = Res(), Res(), Res(), Res()
                gt_b = psb_("gt_b", [128, D], BF16)
                r_gtb = Res()
                hg_b = [psb_("hg_b%d" % i, [128, D], BF16) for i in range(2)]
                r_hgb = [Res(), Res()]
                ss8 = psb_("hss8", [128, 8], F32)
                r_ss8 = Res()
                if s < 2:
                    qT = psb_("qT", [128, 8, 128], BF16)
                    kT = psb_("kT", [128, 8, 128], BF16)
                    r_qT, r_kT = Res(), Res()
                    Abd = psb_("Abd", [128, 8, 128], BF16)
                    r_Abd = Res()
                    k.op("pool", lambda: nc.gpsimd.memset(Abd[:], 0.0), writes=[r_Abd])
                    Sm = psb_("Sm", [128, 8, 128], F32)
                    St = psb_("St", [128, 8, 128], F32)
                    Sb = [psb_("Sb%d" % i, [128, 8, 128], BF16) for i in range(2)]
                    r_Sm, r_St = Res(), Res()
                    r_Sb = [Res(), Res()]
                    eb = psb_("eb", [128, 8, 4], F32)
                    r_eb = Res()
                    k.op("pool", lambda: nc.gpsimd.memset(Sm[:], 0.0), writes=[r_Sm])
                    k.op("pool", lambda: nc.gpsimd.memset(Sb[0][:], 0.0), writes=[r_Sb[0]])
                else:
                    selc_sb = psb_("selc_sb", [NS, 2048], F32)
                    k.dma("sp", selc_sb[:], selc[:, 0:2048], writes=[r_cst])
                    fT = psb_("fT", [128, 3, 8, NS], F32)
                    r_fT = Res()
                    v32s = psb_("v32s", [NS, D], F32)
                    r_v32s = Res()
                    QZ = psb_("QZ", [128, 8, NS * NS], F32)
                    r_QZ = Res()
                    k.op("pool", lambda: nc.gpsimd.memset(QZ[:], 0.0), writes=[r_QZ])
                    S0b = [psb_("S0b%d" % i, [128, 8, 128], F32) for i in range(2)]
                    r_S0b = [Res(), Res()]
                    Sn = [psb_("Sn%d" % i, [128, 8, 128], F32) for i in range(2)]
                    r_Sn = [Res(), Res()]

                def hg_epilogue(P, po, i, g0):
                    for hb in range(2):
                        b_, rb = po[hb]
                        k.op("act", lambda: nc.scalar.activation(t2[0:P, hb * 512:(hb + 1) * 512], b_[0:P, :], AF.Square),
                             writes=[rb, r_t2])
                    k.op("dve", lambda: nc.vector.tensor_reduce(ss8[0:P, :], t2[0:P, :].rearrange("p (h e) -> p h e", h=8), AX.X, ALU.add),
                         reads=[r_t2], writes=[r_ss8])
                    rstd_from_ss(P, (ss8[0:P, :], r_ss8), 128, None)
                    for hb in range(2):
                        b_, rb = po[hb]
                        k.op("dve", lambda: nc.vector.tensor_tensor(
                            t2[0:P, hb * 512:(hb + 1) * 512].rearrange("p (h e) -> p h e", h=4),
                            b_[0:P, :].rearrange("p (h e) -> p h e", h=4),
                            ss8[0:P, hb * 4:(hb + 1) * 4].unsqueeze(2).to_broadcast([P, 4, 128]), ALU.mult),
                            reads=[r_ss8], writes=[rb, r_t2])
                    k.op("dve", lambda: nc.vector.tensor_tensor(t2[0:P, :], t2[0:P, :], gt_b[0:P, :], ALU.mult),
                         reads=[r_gtb], writes=[r_t2])
                    k.op("dve", lambda: nc.vector.tensor_tensor(hg_b[i][0:P, :], t2[0:P, :], hgw[0:P, :], ALU.mult),
                         reads=[r_t2, r_lb], writes=[r_hgb[i]])
                    k.flush()
                    k.defer(lambda: k.dma("sp", HGS[g0:g0 + P, :], hg_b[i][0:P, :], reads=[r_hgb[i]], accum=[r_hgs]))

                for t in range(ntile):
                    i = t % 2
                    ts_ = slice(t * 128, t * 128 + P)
                    g0 = tok0(s) + t * 128
                    k.dma("sp", gt_b[0:P, :], GG[g0:g0 + P, 0:D], reads=[r_gg], writes=[r_gtb])
                    pr = proj(P, hT, ts_, Wh, 1024, 2, r_hT, r_Wh)
                    for hb in range(2):
                        b_, rb = pr[hb]
                        k.op("act", lambda: nc.scalar.activation(logf[0:P, hb * 512:(hb + 1) * 512], b_[0:P, :], AF.Sigmoid),
                             writes=[rb, r_logf])
                    k.op("dve", lambda: nc.vector.tensor_tensor(logf[0:P, :], logf[0:P, :], omlt[0:P, :], ALU.mult), reads=[r_lb], writes=[r_logf])
                    k.op("dve", lambda: nc.vector.tensor_tensor(logf[0:P, :], logf[0:P, :], lbt[0:P, :], ALU.add), reads=[r_lb], writes=[r_logf])
                    k.op("dve", lambda: nc.vector.tensor_scalar(kk[0:P, :], logf[0:P, :], -1.0, 1.0, ALU.mult, ALU.add),
                         reads=[r_logf], writes=[r_kk])
                    if s < 2:
                        k.op("act", lambda: nc.scalar.activation(logf[0:P, :], logf[0:P, :], AF.Ln), reads=[r_kk], writes=[r_logf])
                    if s == 2:
                        k.op("pool", lambda: nc.gpsimd.tensor_copy(et[0:P, :], logf[0:P, :]), reads=[r_logf], writes=[r_et])
                        pq = proj(P, hT, ts_, Wh, 0, 2, r_hT, r_Wh)
                        for hb in range(2):
                            b_, rb = pq[hb]
                            k.op("act", lambda: nc.scalar.activation(t2[0:P, hb * 512:(hb + 1) * 512], b_[0:P, :], AF.Silu),
                                 writes=[rb, r_t2])
                        pv = proj(P, hT, ts_, Wh, 2048, 2, r_hT, r_Wh)
                        for hb in range(2):
                            b_, rb = pv[hb]
                            k.op("act", lambda: nc.scalar.copy(v32s[0:P, hb * 512:(hb + 1) * 512], b_[0:P, :]), writes=[rb, r_v32s])
                        for qi, (src_, rs_) in enumerate(((et, r_et), (kk, r_kk), (t2, r_t2))):
                            b_, rb = nb()
                            for h in range(8):
                                k.op("pe", lambda h=h: nc.tensor.transpose(b_[:, h * NS:(h + 1) * NS], src_[0:P, h * 128:(h + 1) * 128],
                                                                            ident_f[0:P, 0:P]),
                                     reads=[rs_, r_cst], writes=[rb])
                            k.op("act", lambda: nc.scalar.copy(fT[:, qi, :, :], b_[:, 0:8 * NS].rearrange("p (h b) -> p h b", h=8)),
                                 writes=[rb, r_fT])
                        k.op("dve", lambda: nc.vector.tensor_copy(QZ[:, :, 0:NS * NS:NS + 1], fT[:, 2, :, :]), reads=[r_fT], writes=[r_QZ])
                        po = [(psf[5], r_psf[5]), (psf[6], r_psf[6])]
                        for b in range(NS):
                            ib = b % 2
                            k.dma("sp", S0b[ib][:], st_in[b].rearrange("h k v -> k h v"), writes=[r_S0b[ib]])
                            pvb = [(psf[2 * ib], r_psf[2 * ib]), (psf[2 * ib + 1], r_psf[2 * ib + 1])]
                            for hb in range(2):
                                b_, rb = pvb[hb]
                                k.op("pe", lambda: nc.tensor.matmul(b_[:, :], selc_sb[0:NS, b * 128:(b + 1) * 128], v32s[0:NS, hb * 512:(hb + 1) * 512],
                                                                    start=True, stop=True),
                                     reads=[r_v32s, r_cst], writes=[rb])
                            k.op("dve", lambda: nc.vector.tensor_tensor(Sn[ib][:], S0b[ib][:],
                                                                        fT[:, 0, :, b:b + 1].to_broadcast([128, 8, 128]), ALU.mult),
                                 reads=[r_S0b[ib], r_fT], writes=[r_Sn[ib]])
                            for hb in range(2):
                                b_, rb = pvb[hb]
                                k.op("dve", lambda: nc.vector.tensor_tensor(
                                    S0b[ib][:, hb * 4:(hb + 1) * 4, :], b_[:, :].rearrange("p (h v) -> p h v", h=4),
                                    fT[:, 1, hb * 4:(hb + 1) * 4, b:b + 1].to_broadcast([128, 4, 128]), ALU.mult),
                                    reads=[r_fT], writes=[rb, r_S0b[ib]])
                            k.op("dve", lambda: nc.vector.tensor_tensor(Sn[ib][:], Sn[ib][:], S0b[ib][:], ALU.add),
                                 reads=[r_S0b[ib]], writes=[r_Sn[ib]])
                            k.dma("sp", hg_s[b].rearrange("h k v -> k h v"), Sn[ib][:], reads=[r_Sn[ib]])
                            for h in range(8):
                                b_, rb = po[h // 4]
                                k.op("pe", lambda h=h: nc.tensor.matmul(b_[0:NS, (h % 4) * 128:(h % 4 + 1) * 128],
                                                                        QZ[:, h, b * NS:(b + 1) * NS], Sn[ib][:, h, :],
                                                                        start=(b == 0 and h % 4 == 0), stop=(b == NS - 1),
                                                                        skip_group_check=True),
                                     reads=[r_QZ, r_Sn[ib]], writes=[rb])
                        hg_epilogue(P, po, i, g0)
                        continue
                    d1 = [nb(), nb()]
                    d3 = [nb(), nb()]
                    for hb in range(2):
                        k.op("pe", lambda: nc.tensor.matmul(d1[hb][0][:, :], L1, logf[:, hb * 512:(hb + 1) * 512], start=True, stop=True),
                             reads=[r_logf, r_cst], writes=[d1[hb][1]])
                        k.op("pe", lambda: nc.tensor.matmul(d3[hb][0][:, :], L3, logf[:, hb * 512:(hb + 1) * 512], start=True, stop=True),
                             reads=[r_logf, r_cst], writes=[d3[hb][1]])
                    for hb in range(2):
                        k.op("act", lambda: nc.scalar.activation(et[:, hb * 512:(hb + 1) * 512], d1[hb][0][:, :], AF.Exp, scale=-1.0),
                             writes=[d1[hb][1], r_et])
                    k.op("dve", lambda: nc.vector.tensor_tensor(kt_b[:], kk[:], et[:], ALU.mult), reads=[r_kk, r_et], writes=[r_ktb])
                    for hb in range(2):
                        k.op("act", lambda: nc.scalar.activation(et[:, hb * 512:(hb + 1) * 512], d3[hb][0][:, :], AF.Exp),
                             writes=[d3[hb][1], r_et])
                    k.op("dve", lambda: nc.vector.tensor_tensor(kh_b[:], kk[:], et[:], ALU.mult), reads=[r_kk, r_et], writes=[r_khb])
                    for hb in range(2):
                        k.op("act", lambda: nc.scalar.activation(et[:, hb * 512:(hb + 1) * 512], d1[hb][0][:, :], AF.Exp),
                             writes=[d1[hb][1], r_et])
                    pq = proj(P, hT, ts_, Wh, 0, 2, r_hT, r_Wh)
                    for hb in range(2):
                        b_, rb = pq[hb]
                        k.op("act", lambda: nc.scalar.activation(t2[:, hb * 512:(hb + 1) * 512], b_[:, :], AF.Silu), writes=[rb, r_t2])
                    k.op("dve", lambda: nc.vector.tensor_tensor(qt_b[:], t2[:], et[:], ALU.mult), reads=[r_t2, r_et], writes=[r_qtb])
                    pv = proj(P, hT, ts_, Wh, 2048, 2, r_hT, r_Wh)
                    for hb in range(2):
                        b_, rb = pv[hb]
                        k.op("act", lambda: nc.scalar.copy(v_b[:, hb * 512:(hb + 1) * 512], b_[:, :]), writes=[rb, r_vb])
                    be, rbe = nb()
                    for h in range(8):
                        k.op("pe", lambda h=h: nc.tensor.matmul(be[:, h * 4:(h + 1) * 4], logf[:, h * 128:(h + 1) * 128], R4,
                                                                start=(h == 0), stop=(h == 7), skip_group_check=True),
                             reads=[r_logf, r_cst], writes=[rbe])
                    k.op("act", lambda: nc.scalar.activation(eb[:], be[:, 0:32].rearrange("p (h c) -> p h c", h=8), AF.Exp),
                         writes=[rbe, r_eb])
                    for src_, rs_, dst_, rd_ in ((qt_b, r_qtb, qT, r_qT), (kt_b, r_ktb, kT, r_kT)):
                        for h in range(8):
                            k.op("pe", lambda h=h: nc.tensor.transpose(psb[:, h * 128:(h + 1) * 128], src_[:, h * 128:(h + 1) * 128], ident_b[:]),
                                 reads=[rs_, r_idb], writes=[r_psb])
                        k.op("act", lambda: nc.scalar.copy(dst_[:], psb[:].rearrange("p (h n) -> p h n", h=8)), writes=[r_psb, rd_])
                    pa = [nb(), nb()]
                    for h in range(8):
                        b_, rb = pa[h // 4]
                        c_ = (h % 4) * 128
                        k.op("pe", lambda h=h: nc.tensor.matmul(b_[0:64, c_:c_ + 64], kT[:, h, 0:64], qT[:, h, 0:64], start=True, stop=True,
                                                                skip_group_check=True),
                             reads=[r_kT, r_qT], writes=[rb])
                        k.op("pe", lambda h=h: nc.tensor.matmul(b_[:, c_ + 64:c_ + 128], kT[:, h, :], qT[:, h, 64:128], start=True, stop=True,
                                                                skip_group_check=True),
                             reads=[r_kT, r_qT], writes=[rb])
                    for hb in range(2):
                        b_, rb = pa[hb]
                        bv = b_[:, :].rearrange("p (h t) -> p h t", h=4)
                        k.op("dve", lambda: nc.vector.tensor_tensor(Abd[0:64, hb * 4:(hb + 1) * 4, 0:64], bv[0:64, :, 0:64],
                                                                    MA[0:64, 0:64].unsqueeze(1).to_broadcast([64, 4, 64]), ALU.mult),
                             reads=[r_cst], writes=[rb, r_Abd])
                        k.op("dve", lambda: nc.vector.tensor_tensor(Abd[64:128, hb * 4:(hb + 1) * 4, 64:128], bv[64:128, :, 64:128],
                                                                    MA[64:128, 64:128].unsqueeze(1).to_broadcast([64, 4, 64]), ALU.mult),
                             reads=[r_cst], writes=[rb, r_Abd])
                    k.op("dve", lambda: nc.vector.tensor_tensor(Sb[0][:], Sm[:], eb[:, :, 2:3].to_broadcast([128, 8, 128]), ALU.mult),
                         reads=[r_Sm, r_eb], writes=[r_Sb[0]])
                    for c in range(2):
                        src_S, rsrc = (Sm, r_Sm) if c == 0 else (St, r_St)
                        dst_S, rdst = (St, r_St) if c == 0 else (Sm, r_Sm)
                        psn = [nb(), nb()]
                        for h in range(8):
                            b_, rb = psn[h // 4]
                            c_ = (h % 4) * 128
                            k.op("pe", lambda h=h: nc.tensor.matmul(b_[:, c_:c_ + 128], kh_b[c * 64:(c + 1) * 64, h * 128:(h + 1) * 128],
                                                                    v_b[c * 64:(c + 1) * 64, h * 128:(h + 1) * 128], start=True, stop=True,
                                                                    skip_group_check=True),
                                 reads=[r_khb, r_vb], writes=[rb])
                        k.op("dve", lambda: nc.vector.tensor_tensor(dst_S[:], src_S[:], eb[:, :, c:c + 1].to_broadcast([128, 8, 128]), ALU.mult),
                             reads=[rsrc, r_eb], writes=[rdst])
                        for hb in range(2):
                            b_, rb = psn[hb]
                            k.op("dve", lambda: nc.vector.tensor_tensor(dst_S[:, hb * 4:(hb + 1) * 4, :], dst_S[:, hb * 4:(hb + 1) * 4, :],
                                                                        b_[:, :].rearrange("p (h v) -> p h v", h=4), ALU.add),
                                 writes=[rb, rdst])
                        if c == 0:
                            k.op("dve", lambda: nc.vector.tensor_tensor(Sb[1][:], St[:], eb[:, :, 3:4].to_broadcast([128, 8, 128]), ALU.mult),
                                 reads=[r_St, r_eb], writes=[r_Sb[1]])
                    po = [nb(), nb()]
                    for h in range(8):
                        b_, rb = po[h // 4]
                        c_ = (h % 4) * 128
                        k.op("pe", lambda h=h: nc.tensor.matmul(b_[:, c_:c_ + 128], Abd[:, h, :], v_b[:, h * 128:(h + 1) * 128],
                                                                start=True, stop=False, skip_group_check=True),
                             reads=[r_Abd, r_vb], writes=[rb])
                        k.op("pe", lambda h=h: nc.tensor.matmul(b_[0:64, c_:c_ + 128], qT[:, h, 0:64], Sb[0][:, h, :],
                                                                start=False, stop=False, skip_group_check=True),
                             reads=[r_qT, r_Sb[0]], writes=[rb])
                        k.op("pe", lambda h=h: nc.tensor.matmul(b_[64:128, c_:c_ + 128], qT[:, h, 64:128], Sb[1][:, h, :],
                                                                start=False, stop=True, skip_group_check=True),
                             reads=[r_qT, r_Sb[1]], writes=[rb])
                    hg_epilogue(P, po, i, g0)
                k.flush()
                if s < 2:
                    k.dma("sp", hg_p[s].rearrange("h k v -> k h v"), Sm[:], reads=[r_Sm])
                k.barrier()
    if stop_after <= 3:
        k.finish()
        return


    with contextlib.ExitStack() as ph:
        def psb_(name, shape, dt):
            return ph.enter_context(nc.sbuf_tensor(name, list(shape), dt))
        qblk = [psb_("qblk%d" % i, [128, 512], BF16) for i in range(2)]
        kblk = [psb_("kblk%d" % i, [128, 512], BF16) for i in range(2)]
        vblk = [psb_("vblk%d" % i, [128, 8, 65], BF16) for i in range(2)]
        r_qblk, r_kblk, r_vblk = [Res(), Res()], [Res(), Res()], [Res(), Res()]
        qTa = psb_("qTa", [128, 4, 128], BF16)
        kTa = [psb_("kTa%d" % i, [128, 4, 128], BF16) for i in range(2)]
        r_qTa = Res()
        r_kTa = [Res(), Res()]
        pT = [psb_("pT%d" % i, [128, 4, 128], BF16) for i in range(4)]
        r_pT = [Res() for _ in range(4)]
        oac = [psb_("oac%d" % i, [128, 520], F32) for i in range(2)]
        r_oac = [Res(), Res()]
        for i in range(2):
            k.op("pool", lambda i=i: nc.gpsimd.memset(qblk[i][:], 0.0), writes=[r_qblk[i]])
            k.op("pool", lambda i=i: nc.gpsimd.memset(kblk[i][:], 0.0), writes=[r_kblk[i]])
            k.op("pool", lambda i=i: nc.gpsimd.memset(vblk[i][:], 1.0), writes=[r_vblk[i]])
        blk_ctr = [0]
        for c_ in range(8):
            rs_ = slice(c_ * 2048, (c_ + 1) * 2048)
            k.dma("pool", UV[rs_, 0:D], peer_u[rs_, :], accum=[r_uv])
            k.dma("pool", UV[rs_, D:2 * D], peer_v[rs_, :], accum=[r_uv])

        def attn_block(load_cur, has_prev, ip, store):
            n_ = blk_ctr[0]
            blk_ctr[0] += 1
            ic = 1 - ip
            load_cur(ic)
            for src_, rs_, dst_, rd_ in ((qblk[ic], r_qblk[ic], qTa, r_qTa), (kblk[ic], r_kblk[ic], kTa[ic], r_kTa[ic])):
                for hp in range(4):
                    k.op("pe", lambda hp=hp: nc.tensor.transpose(psb[:, hp * 128:(hp + 1) * 128], src_[:, hp * 128:(hp + 1) * 128], ident_b[:]),
                         reads=[rs_, r_idb], writes=[r_psb])
                k.op("act", lambda: nc.scalar.copy(dst_[:], psb[:, 0:512].rearrange("p (h n) -> p h n", h=4)), writes=[r_psb, rd_])
            srcs = [(ic, maskc_b)] + ([(ip, maskp_b)] if has_prev else [])
            pts = []
            for si, (ib, msk) in enumerate(srcs):
                for hb in range(2):
                    b_, rb = nb()
                    for hh in range(4):
                        h = 2 * hh + hb
                        po_ = hb * 64
                        k.op("pe", lambda: nc.tensor.matmul(b_[:, hh * 128:(hh + 1) * 128], kTa[ib][po_:po_ + 64, h // 2, :],
                                                            qTa[po_:po_ + 64, h // 2, :], start=True, stop=True, skip_group_check=True),
                             reads=[r_kTa[ib], r_qTa], writes=[rb])
                    pi = si * 2 + hb
                    k.op("act", lambda: nc.scalar.activation(pT[pi][:], b_[:, :].rearrange("p (h n) -> p h n", h=4), AF.Exp),
                         writes=[rb, r_pT[pi]])
                    k.op("dve", lambda: nc.vector.tensor_tensor(pT[pi][:], pT[pi][:], msk[:], ALU.mult), reads=[r_mask], writes=[r_pT[pi]])
                    pts.append((pi, ib))
            io = n_ % 2
            for hb in range(2):
                b_, rb = nb()
                for hh in range(4):
                    h = hb * 4 + hh
                    for si, (ib, msk) in enumerate(srcs):
                        pi = si * 2 + (h % 2)
                        k.op("pe", lambda: nc.tensor.matmul(b_[:, hh * 65:(hh + 1) * 65], pT[pi][:, h // 2, :], vblk[ib][:, h, :],
                                                            start=(si == 0), stop=(si == len(srcs) - 1), skip_group_check=True),
                             reads=[r_pT[pi], r_vblk[ib]], writes=[rb])
                k.op("act", lambda: nc.scalar.copy(oac[io][:, hb * 260:(hb + 1) * 260], b_[:, 0:260]), writes=[rb, r_oac[io]])
            k.flush()
            k.defer(lambda io=io, store=store: store(oac[io], r_oac[io]))
            return ic

        for s in range(2):
            for g in range(3):
                d = GROUPS[g][1]
                nblk = SEQ // (128 * d)

                def view(T, width):
                    return T[tok0(s):tok0(s) + SEQ, g, :].rearrange("(n j dd) c -> dd n j c", dd=d, j=128)
                Qv, Kv, Vv = view(QS, 512), view(KS, 512), view(VS, 520)
                Ov = OACC[g, tok0(s):tok0(s) + SEQ, :].rearrange("(n j dd) c -> dd n j c", dd=d, j=128)
                for r in range(d):
                    ip = 0
                    for n in range(nblk):
                        def load_cur(ic, r=r, n=n, Qv=Qv, Kv=Kv, Vv=Vv):
                            k.dma("sp", qblk[ic][:], Qv[r, n], reads=[r_scr], writes=[r_qblk[ic]])
                            k.dma("sp", kblk[ic][:], Kv[r, n], reads=[r_scr], writes=[r_kblk[ic]])
                            k.dma("sp", vblk[ic][:].rearrange("p h e -> p (h e)"), Vv[r, n], reads=[r_scr], writes=[r_vblk[ic]])

                        def store(o_, ro_, r=r, n=n, Ov=Ov):
                            k.dma("sp", Ov[r, n], o_[:], reads=[ro_], accum=[r_oacc])
                        ip = attn_block(load_cur, n > 0, ip, store)
        for b in range(NS):
            for g in range(3):
                tokb = tok0(2) + b
                ip = 0
                k.dma("pool", kblk[ip][:], ck[g][b, :, 0, :], writes=[r_kblk[ip]])
                k.dma("pool", vblk[ip][:, :, 0:64], ck[g][b, :, 1, :].rearrange("j (h e) -> j h e", h=8), writes=[r_vblk[ip]])
                for hp in range(4):
                    k.op("pe", lambda hp=hp: nc.tensor.transpose(psb[:, hp * 128:(hp + 1) * 128], kblk[ip][:, hp * 128:(hp + 1) * 128], ident_b[:]),
                         reads=[r_kblk[ip], r_idb], writes=[r_psb])
                k.op("act", lambda: nc.scalar.copy(kTa[ip][:], psb[:, 0:512].rearrange("p (h n) -> p h n", h=4)), writes=[r_psb, r_kTa[ip]])

                def load_cur(ic, g=g, tokb=tokb):
                    k.dma("sp", qblk[ic][0:1, :], QS[tokb:tokb + 1, g, :], reads=[r_scr], writes=[r_qblk[ic]])
                    k.dma("sp", kblk[ic][0:1, :], KS[tokb:tokb + 1, g, :], reads=[r_scr], writes=[r_kblk[ic]])
                    k.dma("sp", vblk[ic][0:1, :, :].rearrange("p h e -> p (h e)"), VS[tokb:tokb + 1, g, :], reads=[r_scr], writes=[r_vblk[ic]])

                def store(o_, ro_, g=g, tokb=tokb):
                    k.dma("sp", OACC[g, tokb:tokb + 1, :], o_[0:1, :], reads=[ro_], accum=[r_oacc])
                attn_block(load_cur, True, ip, store)
        k.barrier()
    if stop_after <= 4:
        k.finish()
        return

    with contextlib.ExitStack() as ph:
        def psb_(name, shape, dt):
            return ph.enter_context(nc.sbuf_tensor(name, list(shape), dt))
        Wa = psb_("Wa", [128, 4, D], BF16)
        Wb = psb_("Wb", [128, 8, D], BF16)
        Wo = psb_("Wo", [128, 8, D], BF16)
        Wq = psb_("Wq", [128, 8, 2048], BF16)
        skt = psb_("skt", [128, 2, 128], F32)
        r_W = Res()
        k.dma("pool", Wa[:], w_br_a.rearrange("(kc p) n -> p kc n", p=128), writes=[r_W])
        k.dma("pool", Wb[:], w_br_b.rearrange("(kc p) n -> p kc n", p=128), writes=[r_W])
        k.dma("pool", Wo[:], w_o.rearrange("(kc p) n -> p kc n", p=128), writes=[r_W])
        k.dma("pool", Wq[:], w_pq.rearrange("(kc p) n -> p kc n", p=128), writes=[r_W])
        k.dma("sp", skt[:], skT.rearrange("t e c -> e t c"), writes=[r_W])
        n2w = psb_("n2w", [128, D], F32)
        k.dma("sp", n2w[:], norm2_w.partition_broadcast(128), writes=[r_W])
        iota16 = psb_("iota16", [128, 16], F32)
        k.dma("sp", iota16[:], selc[0:1, 2048:2064].partition_broadcast(128), writes=[r_W])
        G1 = psb_("G1", [128, D], F32)
        S2 = psb_("S2", [128, D], F32)
        SH2 = psb_("SH2", [128, D], F32)
        G2s = [psb_("G2_%d" % i, [128, D], F32) for i in range(2)]
        r_M = Res()
        xin = [psb_("xin0", [128, D], F32)] * 2
        r_xin = [Res()] * 2
        oin = [psb_("oin0", [128, 3, 520], F32)] * 2
        r_oin = [Res()] * 2
        hgin = [psb_("hgin0", [128, D], BF16)] * 2
        r_hgin = [Res()] * 2
        ggin = [psb_("ggin0", [128, 2048], BF16)] * 2
        r_ggin = [Res()] * 2
        rl = psb_("rl", [128, 8], F32)
        r_rl = Res()
        att_b = psb_("att_b", [128, 512], BF16)
        r_attb = Res()
        TT = psb_("TT", [128, 8, 128], BF16)
        r_TT = Res()
        f1 = psb_("f1", [128, D], F32)
        r_f1 = Res()
        yb = psb_("yb", [128, D], BF16)
        r_yb = Res()
        x1s = [psb_("x1_%d" % i, [128, D], F32) for i in range(2)]
        r_x1s = [Res(), Res()]
        h2bs = [psb_("h2b_%d" % i, [128, D], BF16) for i in range(2)]
        r_h2bs = [Res(), Res()]
        ssq = psb_("ssq", [128, 1], F32)
        r_ssq = Res()
        sc = psb_("sc", [128, 16, 128], F32)
        sc2 = psb_("sc2", [128, 16, 128], F32)
        r_sc, r_sc2 = Res(), Res()
        q2T = sc2
        r_q2T = r_sc2
        vals = psb_("vals", [128, 16, 16], F32)
        idxu = psb_("idxu", [128, 16, 16], U32)
        idxf = psb_("idxf", [128, 16, 16], F32)
        r_vals, r_idxu, r_idxf = Res(), Res(), Res()
        r_valsH = [Res() for _ in range(16)]
        r_idxuH = [Res() for _ in range(16)]
        r_sc2H = [Res() for _ in range(16)]
        cand = sc[:].rearrange("p a b -> p (a b)").rearrange("p (h x) -> p h x", h=8)
        cand2 = sc2[:].rearrange("p a b -> p (a b)").rearrange("p (h x) -> p h x", h=8)
        r_cand, r_cand2 = r_sc, r_sc2
        tv = psb_("tv", [128, 8, 16], F32)
        posu = psb_("posu", [128, 8, 16], U32)
        pa_u = psb_("pa_u", [128, 8, 16], U32)
        pa_f = psb_("pa_f", [128, 2, 8, 16], F32)
        r_tv, r_posu, r_pau, r_paf = Res(), Res(), Res(), Res()
        oh = cand2
        r_oh = r_sc2
        isel = psb_("isel", [128, 2, 8, 16], F32)
        r_isel = Res()
        eid_f = psb_("eid_f", [128, 128], F32)
        eids = [psb_("eid%d" % i, [128, 128], I32) for i in range(2)]
        r_eids = [Res(), Res()]
        r_eidf = Res()
        gsm = psb_("gsm", [128, 8], F32)
        gats = [psb_("gat%d" % i, [128, 8, 16], F32) for i in range(2)]
        r_gats = [Res(), Res()]
        dots = psb_("dots", [128, 128], F32)
        r_dots = Res()
        coef = psb_("coef", [128, 128], F32)
        r_coef = Res()
        NUV = 8
        uvb = [psb_("uvb%d" % i, [128, 2 * D], BF16) for i in range(NUV)]
        r_uvb = [Res() for _ in range(NUV)]
        r_dotg = [Res() for _ in range(32)]
        r_coefg = [Res() for _ in range(32)]
        dg = [psb_("dg%d" % i, [128, 128], BF16) for i in range(4)]
        r_dg = [Res() for _ in range(4)]
        jb = psb_("jb", [128, D], BF16)
        r_jb = Res()
        yo = [psb_("yo0", [128, D], F32)] * 2
        r_yo = [Res()] * 2

        def transpose_to_TT(P, src, rsrc, nk):
            yield
            for kc in range(nk):
                k.op("pe", lambda kc=kc: nc.tensor.transpose(psb[:, kc * 128:kc * 128 + P], src[0:P, kc * 128:(kc + 1) * 128], ident_b[0:P, 0:P]),
                     reads=[rsrc, r_idb], writes=[r_psb])
            yield
            k.op("act", lambda: nc.scalar.copy(TT[:, 0:nk, 0:P], psb[:, 0:nk * 128].rearrange("p (kc n) -> p kc n", kc=nk)[:, :, 0:P]),
                 writes=[r_psb, r_TT])
            yield

        def mm_tok(P, W, nk, nbank, rW):
            out = []
            for j in range(nbank):
                b_, rb = nb()
                for kc in range(nk):
                    k.op("pe", lambda kc=kc: nc.tensor.matmul(b_[0:P, :], TT[:, kc, 0:P], W[:, kc, j * 512:(j + 1) * 512],
                                                              start=(kc == 0), stop=(kc == nk - 1)),
                         reads=[r_TT, rW], writes=[rb])
                out.append((b_, rb))
            yield
            return out

        tiles = [(s, t) for s in range(2) for t in range(NT)] + [(2, 0)]
        nbank_rot[0] = 5
        cur_s = [-1]

        def loads(idx):
            s, t = tiles[idx]
            P = 128 if s < 2 else NS
            i = idx % 2
            g0 = tok0(s) + t * 128
            k.dma("sp", xin[i][0:P, :], xp[s, t * 128:(t + 1) * 128, :] if s < 2 else xs[:, :], writes=[r_xin[i]])
            for g in range(3):
                k.dma("sp", oin[i][0:P, g, :], OACC[g, g0:g0 + P, :], reads=[r_oacc], writes=[r_oin[i]])
            k.dma("sp", hgin[i][0:P, :], HGS[g0:g0 + P, :], reads=[r_hgs], writes=[r_hgin[i]])
            k.dma("sp", ggin[i][0:P, :], GG[g0:g0 + P, 1024:3072], reads=[r_gg], writes=[r_ggin[i]])

        loads(0)
        def front(idx):
            s, t = tiles[idx]
            P = 128 if s < 2 else NS
            i = idx % 2
            pp = idx % 2
            x1, r_x1 = x1s[pp], r_x1s[pp]
            h2b, r_h2b = h2bs[pp], r_h2bs[pp]
            eid, r_eid = eids[pp], r_eids[pp]
            gat, r_gat = gats[pp], r_gats[pp]
            G2 = G2s[s % 2]
            yield
            if s != cur_s[0]:
                cur_s[0] = s
                for dst_, c0 in ((G1, 2 * D), (SH2, 3 * D), (S2, 4 * D), (G2, 5 * D)):
                    if s < 2:
                        k.dma("sp", dst_[:], MODS[s:s + 1, c0:c0 + D].partition_broadcast(128), reads=[r_MODS], writes=[r_M])
                    else:
                        k.dma("sp", dst_[0:NS, :], MODS[2:2 + NS, c0:c0 + D], reads=[r_MODS], writes=[r_M])
                k.op("dve", lambda: nc.vector.scalar_tensor_tensor(S2[0:P, :], S2[0:P, :], 1.0, n2w[0:P, :], ALU.add, ALU.mult),
                     reads=[r_W], writes=[r_M])
            yield
            o0 = oin[i]
            k.op("dve", lambda: nc.vector.tensor_tensor(o0[0:P, 0, :], o0[0:P, 0, :], o0[0:P, 1, :], ALU.add), writes=[r_oin[i]])
            k.op("dve", lambda: nc.vector.tensor_tensor(o0[0:P, 0, :], o0[0:P, 0, :], o0[0:P, 2, :], ALU.add), writes=[r_oin[i]])
            ov = o0[0:P, 0, :].rearrange("p (h e) -> p h e", h=8)
            k.op("dve", lambda: nc.vector.reciprocal(rl[0:P, :], ov[:, :, 64]), reads=[r_oin[i]], writes=[r_rl])
            k.op("dve", lambda: nc.vector.tensor_tensor(att_b[0:P, :].rearrange("p (h e) -> p h e", h=8), ov[:, :, 0:64],
                                                        rl[0:P, :].unsqueeze(2).to_broadcast([P, 8, 64]), ALU.mult),
                 reads=[r_oin[i], r_rl], writes=[r_attb])
            yield from transpose_to_TT(P, att_b, r_attb, 4)
            pA = yield from mm_tok(P, Wa, 4, 2, r_W)
            for hb in range(2):
                b_, rb = pA[hb]
                k.op("dve", lambda: nc.vector.tensor_tensor(f1[0:P, hb * 512:(hb + 1) * 512], b_[0:P, :], ggin[i][0:P, hb * 512:(hb + 1) * 512], ALU.mult),
                     reads=[r_ggin[i]], writes=[rb, r_f1])
            yield
            yield from transpose_to_TT(P, hgin[i], r_hgin[i], 8)
            pB = yield from mm_tok(P, Wb, 8, 2, r_W)
            for hb in range(2):
                b_, rb = pB[hb]
                k.op("dve", lambda: nc.vector.tensor_tensor(x1[0:P, hb * 512:(hb + 1) * 512], b_[0:P, :],
                                                            ggin[i][0:P, 1024 + hb * 512:1024 + (hb + 1) * 512], ALU.mult),
                     reads=[r_ggin[i]], writes=[rb, r_x1])
            k.op("dve", lambda: nc.vector.tensor_tensor(yb[0:P, :], f1[0:P, :], x1[0:P, :], ALU.add), reads=[r_f1, r_x1], writes=[r_yb])
            yield
            yield from transpose_to_TT(P, yb, r_yb, 8)
            pZ = yield from mm_tok(P, Wo, 8, 2, r_W)
            for hb in range(2):
                b_, rb = pZ[hb]
                k.op("dve", lambda: nc.vector.tensor_tensor(x1[0:P, hb * 512:(hb + 1) * 512], b_[0:P, :], G1[0:P, hb * 512:(hb + 1) * 512], ALU.mult),
                     reads=[r_M], writes=[rb, r_x1])
            k.op("dve", lambda: nc.vector.tensor_tensor(x1[0:P, :], x1[0:P, :], xin[i][0:P, :], ALU.add), reads=[r_xin[i]], writes=[r_x1])
            yield
            if DEBUG and (idx == 0 or s == 2):
                k.dma("pool", dbg[0 if idx == 0 else 1, 1, 0:P, :], hgin[i][0:P, :], reads=[r_hgin[i]])
            if idx + 1 < len(tiles):
                loads(idx + 1)
            k.op("act", lambda: nc.scalar.activation(f1[0:P, :], x1[0:P, :], AF.Square, accum_out=ssq[0:P, :]),
                 reads=[r_x1], writes=[r_f1, r_ssq])
            yield
            k.op("dve", lambda: nc.vector.tensor_scalar(ssq[0:P, :], ssq[0:P, :], 1.0 / D, EPS, ALU.mult, ALU.add), writes=[r_ssq])
            yield
            k.op("act", lambda: nc.scalar.activation(ssq[0:P, :], ssq[0:P, :], AF.Sqrt), writes=[r_ssq])
            yield
            k.op("dve", lambda: nc.vector.reciprocal(ssq[0:P, :], ssq[0:P, :]), writes=[r_ssq])
            k.op("dve", lambda: nc.vector.scalar_tensor_tensor(f1[0:P, :], x1[0:P, :], ssq[0:P, 0:1], S2[0:P, :], ALU.mult, ALU.mult),
                 reads=[r_x1, r_ssq, r_M], writes=[r_f1])
            k.op("dve", lambda: nc.vector.tensor_tensor(h2b[0:P, :], f1[0:P, :], SH2[0:P, :], ALU.add), reads=[r_f1, r_M], writes=[r_h2b])
            yield from transpose_to_TT(P, h2b, r_h2b, 8)
            yield
            for qb in range(4):
                yield
                b_, rb = nb()
                for hh in range(4):
                    hp = qb * 4 + hh
                    for kc in range(8):
                        k.op("pe", lambda kc=kc: nc.tensor.matmul(b_[:, hh * 128:hh * 128 + P], Wq[:, kc, hp * 128:(hp + 1) * 128], TT[:, kc, 0:P],
                                                                  start=(kc == 0), stop=(kc == 7), skip_group_check=True),
                             reads=[r_TT, r_W], writes=[rb])
                yield
                k.op("act", lambda: nc.scalar.copy(q2T[:, qb * 4:(qb + 1) * 4, 0:P], b_[:, :].rearrange("p (h n) -> p h n", h=4)[:, :, 0:P]),
                     writes=[rb, r_q2T])
            yield
            for qb in range(4):
                b_, rb = nb()
                for hh in range(4):
                    hp = qb * 4 + hh
                    k.op("pe", lambda: nc.tensor.matmul(b_[0:P, hh * 128:(hh + 1) * 128], q2T[:, hp, 0:P], skt[:, hp % 2, :],
                                                        start=True, stop=True, skip_group_check=True),
                         reads=[r_q2T, r_W], writes=[rb])
                yield
                k.op("act", lambda: nc.scalar.copy(sc[0:P, qb * 4:(qb + 1) * 4, :], b_[0:P, :].rearrange("p (h n) -> p h n", h=4)),
                     writes=[rb, r_sc])
            yield
            for hp in range(16):
                if hp % 4 == 0:
                    yield
                k.op("dve", lambda: nc.vector.max(vals[0:P, hp, 0:8], sc[0:P, hp, :]), reads=[r_sc, r_sc2], writes=[r_valsH[hp]])
                k.op("dve", lambda: nc.vector.max_index(idxu[0:P, hp, 0:8], vals[0:P, hp, 0:8], sc[0:P, hp, :]),
                     reads=[r_sc, r_valsH[hp]], writes=[r_idxuH[hp]])
                k.op("dve", lambda: nc.vector.match_replace(sc2[0:P, hp, :], vals[0:P, hp, 0:8], sc[0:P, hp, :], -1e30),
                     reads=[r_sc, r_valsH[hp]], writes=[r_sc2H[hp]])
                k.op("dve", lambda: nc.vector.max(vals[0:P, hp, 8:16], sc2[0:P, hp, :]), reads=[r_sc2H[hp]], writes=[r_valsH[hp]])
                k.op("dve", lambda: nc.vector.max_index(idxu[0:P, hp, 8:16], vals[0:P, hp, 8:16], sc2[0:P, hp, :]),
                     reads=[r_sc2H[hp], r_valsH[hp]], writes=[r_idxuH[hp]])
            yield
            k.op("dve", lambda: nc.vector.tensor_copy(idxf[0:P], idxu[0:P]), reads=r_idxuH, writes=[r_idxf])
            v4 = vals[0:P].rearrange("p (h two) j -> p h two j", two=2)
            k.op("dve", lambda: nc.vector.tensor_tensor(cand[0:P].rearrange("p h (a b) -> p h a b", a=16),
                                                        v4[:, :, 0, :].unsqueeze(3).to_broadcast([P, 8, 16, 16]),
                                                        v4[:, :, 1, :].unsqueeze(2).to_broadcast([P, 8, 16, 16]), ALU.add),
                 reads=r_valsH + r_sc2H, writes=[r_cand, r_sc2])
            for h in range(8):
                if h % 2 == 0:
                    yield
                k.op("dve", lambda: nc.vector.max(tv[0:P, h, 0:8], cand[0:P, h, :]), reads=[r_cand], writes=[r_tv])
                k.op("dve", lambda: nc.vector.max_index(posu[0:P, h, 0:8], tv[0:P, h, 0:8], cand[0:P, h, :]),
                     reads=[r_cand, r_tv], writes=[r_posu])
                k.op("dve", lambda: nc.vector.match_replace(cand2[0:P, h, :], tv[0:P, h, 0:8], cand[0:P, h, :], -1e30),
                     reads=[r_cand, r_tv], writes=[r_cand2])
                k.op("dve", lambda: nc.vector.max(tv[0:P, h, 8:16], cand2[0:P, h, :]), reads=[r_cand2], writes=[r_tv])
                k.op("dve", lambda: nc.vector.max_index(posu[0:P, h, 8:16], tv[0:P, h, 8:16], cand2[0:P, h, :]),
                     reads=[r_cand2, r_tv], writes=[r_posu])
            yield
            k.op("dve", lambda: nc.vector.tensor_single_scalar(pa_u[0:P], posu[0:P], 4, ALU.logical_shift_right), reads=[r_posu], writes=[r_pau])
            k.op("dve", lambda: nc.vector.tensor_copy(pa_f[0:P, 0], pa_u[0:P]), reads=[r_pau], writes=[r_paf])
            k.op("dve", lambda: nc.vector.tensor_single_scalar(pa_u[0:P], posu[0:P], 15, ALU.bitwise_and), reads=[r_posu], writes=[r_pau])
            k.op("dve", lambda: nc.vector.tensor_copy(pa_f[0:P, 1], pa_u[0:P]), reads=[r_pau], writes=[r_paf])
            i4 = idxf[0:P].rearrange("p (h two) j -> p h two j", two=2)
            for w_ in range(2):
                yield
                ohv = oh[0:P].rearrange("p h (j a) -> p h j a", j=16)
                k.op("dve", lambda: nc.vector.tensor_tensor(ohv, pa_f[0:P, w_].unsqueeze(3).to_broadcast([P, 8, 16, 16]),
                                                            iota16[0:P, :].unsqueeze(1).unsqueeze(1).to_broadcast([P, 8, 16, 16]), ALU.is_equal),
                     reads=[r_paf, r_W], writes=[r_oh])
                k.op("dve", lambda: nc.vector.tensor_tensor(ohv, ohv, i4[:, :, w_, :].unsqueeze(2).to_broadcast([P, 8, 16, 16]), ALU.mult),
                     reads=[r_idxf], writes=[r_oh])
                k.op("dve", lambda: nc.vector.tensor_reduce(isel[0:P, w_], ohv, AX.X, ALU.add), reads=[r_oh], writes=[r_isel])
            k.op("dve", lambda: nc.vector.scalar_tensor_tensor(eid_f[0:P, :], isel[0:P, 0].rearrange("p h j -> p (h j)"), 128.0,
                                                               isel[0:P, 1].rearrange("p h j -> p (h j)"), ALU.mult, ALU.add),
                 reads=[r_isel], writes=[r_eidf])
            k.op("dve", lambda: nc.vector.tensor_copy(eid[0:P, :], eid_f[0:P, :]), reads=[r_eidf], writes=[r_eid])
            yield
            k.op("dve", lambda: nc.vector.tensor_tensor(gat[0:P], tv[0:P], tv[0:P, :, 0:1].to_broadcast([P, 8, 16]), ALU.subtract),
                 reads=[r_tv], writes=[r_gat])
            yield
            k.op("act", lambda: nc.scalar.activation(gat[0:P], gat[0:P], AF.Exp), writes=[r_gat])
            yield
            k.op("dve", lambda: nc.vector.tensor_reduce(gsm[0:P, :], gat[0:P], AX.X, ALU.add), reads=[r_gat], writes=[r_rl])
            k.op("dve", lambda: nc.vector.reciprocal(gsm[0:P, :], gsm[0:P, :]), writes=[r_rl])
            k.op("dve", lambda: nc.vector.tensor_tensor(gat[0:P], gat[0:P], gsm[0:P, :].unsqueeze(2).to_broadcast([P, 8, 16]), ALU.mult),
                 reads=[r_rl], writes=[r_gat])
        def gather(idx, gen):
            s, t = tiles[idx]
            P = 128 if s < 2 else NS
            i = idx % 2
            pp = idx % 2
            x1, r_x1 = x1s[pp], r_x1s[pp]
            h2b, r_h2b = h2bs[pp], r_h2bs[pp]
            eid, r_eid = eids[pp], r_eids[pp]
            gat, r_gat = gats[pp], r_gats[pp]
            G2 = G2s[s % 2]
            py = [(psf[5], r_psf[5]), (psf[6], r_psf[6])]
            gatf = gat[0:P].rearrange("p h j -> p (h j)")
            for grp in range(32):
                gs = slice(grp * 4, grp * 4 + 4)
                rd, rc = r_dotg[grp], r_coefg[grp]
                for j in range(grp * 4, grp * 4 + 4):
                    bi = j % NUV
                    k.dma("pool", uvb[bi][0:P, :], UV, reads=[r_eid, r_uv], writes=[r_uvb[bi]],
                          indirect=bass.IndirectOffsetOnAxis(ap=eid[0:P, j:j + 1], axis=0))
                    k.op("dve", lambda: nc.vector.scalar_tensor_tensor(jb[0:P, :], uvb[bi][0:P, 0:D], 1.0, h2b[0:P, :], ALU.mult, ALU.mult,
                                                                       accum_out=dots[0:P, j:j + 1]),
                         reads=[r_uvb[bi], r_h2b], writes=[rd])
                if gen is not None:
                    next(gen, None)
                k.op("dve", lambda: nc.vector.tensor_tensor(coef[0:P, gs], dots[0:P, gs], dots[0:P, gs], ALU.mult), reads=[rd], writes=[rc])
                k.op("dve", lambda: nc.vector.tensor_scalar(coef[0:P, gs], coef[0:P, gs], 0.044715, 1.0, ALU.mult, ALU.add), writes=[rc])
                k.op("dve", lambda: nc.vector.tensor_tensor(coef[0:P, gs], coef[0:P, gs], dots[0:P, gs], ALU.mult), reads=[rd], writes=[rc])
                k.op("act", lambda: nc.scalar.activation(coef[0:P, gs], coef[0:P, gs], AF.Sigmoid, scale=1.5957691216057308), writes=[rc])
                k.op("dve", lambda: nc.vector.tensor_tensor(coef[0:P, gs], coef[0:P, gs], dots[0:P, gs], ALU.mult), reads=[rd], writes=[rc])
                k.op("dve", lambda: nc.vector.tensor_tensor(coef[0:P, gs], coef[0:P, gs], gatf[:, gs], ALU.mult), reads=[r_gat], writes=[rc])
                for j in range(grp * 4, grp * 4 + 4):
                    bi = j % NUV
                    di_ = j % 4
                    k.op("act", lambda: nc.scalar.mul(dg[di_][0:P, 0:P], ident_b[0:P, 0:P], coef[0:P, j:j + 1]),
                         reads=[rc, r_idb], writes=[r_dg[di_]])
                    for hb in range(2):
                        b_, rb = py[hb]
                        k.op("pe", lambda: nc.tensor.matmul(b_[0:P, :], dg[di_][0:P, 0:P], uvb[bi][0:P, D + hb * 512:D + (hb + 1) * 512],
                                                            start=(j == 0), stop=(j == 127)),
                             reads=[r_dg[di_], r_uvb[bi]], writes=[rb])
                if gen is not None:
                    next(gen, None)
            io = idx % 2
            for hb in range(2):
                b_, rb = py[hb]
                k.op("dve", lambda: nc.vector.tensor_tensor(yo[io][0:P, hb * 512:(hb + 1) * 512], b_[0:P, :], G2[0:P, hb * 512:(hb + 1) * 512], ALU.mult),
                     reads=[r_M], writes=[rb, r_yo[io]])
            k.op("dve", lambda: nc.vector.tensor_tensor(yo[io][0:P, :], yo[io][0:P, :], x1[0:P, :], ALU.add), reads=[r_x1], writes=[r_yo[io]])
            if DEBUG and (idx == 0 or s == 2):
                di = 0 if idx == 0 else 1
                k.dma("pool", dbg[di, 0, 0:P, 0:512], att_b[0:P, :], reads=[r_attb])
                k.dma("pool", dbg[di, 2, 0:P, :], yb[0:P, :], reads=[r_yb])
                k.dma("sp", dbg[di, 3, 0:P, :], x1[0:P, :], reads=[r_x1])
                k.dma("pool", dbg[di, 4, 0:P, :], h2b[0:P, :], reads=[r_h2b])
                k.dma("sp", dbg[di, 5, 0:P, 0:128], coef[0:P, :], reads=r_coefg)
                k.dma("sp", dbg[di, 6, 0:P, 0:128], eid_f[0:P, :], reads=[r_eidf])
                k.dma("sp", dbg[di, 7, 0:P, 0:128], dots[0:P, :], reads=r_dotg)
                k.dma("sp", dbg[di, 8, 0:P, 0:128], gat[0:P].rearrange("p h j -> p (h j)"), reads=[r_gat])
                k.dma("sp", dbg[di, 9, 0:P, 0:256], vals[0:P].rearrange("p a b -> p (a b)"), reads=r_valsH)
            k.dma("sp", y_p[s, t * 128:(t + 1) * 128, :] if s < 2 else y_s[:, :], yo[io][0:P, :], reads=[r_yo[io]])
        g_ = front(0)
        for _ in g_:
            pass
        for idx in range(len(tiles)):
            g_ = front(idx + 1) if idx + 1 < len(tiles) else None
            gather(idx, g_)
            if g_ is not None:
                for _ in g_:
                    pass
        k.flush()
    k.finish()


def _consts():
    c = np.zeros((128, 1024), np.float32)
    j = np.arange(128)[:, None]
    i = np.arange(128)[None, :]
    c[:, 0:128] = np.eye(128, dtype=np.float32)
    c[:, 128:256] = (j <= i)
    c[:, 256:384] = (j >= i)
    same = (j // 64) == (i // 64)
    c[:, 384:512] = same * ((j <= i).astype(np.float32) - ((j % 64) <= 31).astype(np.float32))
    c[:, 512:640] = same * (j > i)
    s_ = np.arange(128)
    c[:, 640] = s_ < 64
    c[:, 641] = s_ >= 64
    c[:, 642] = (s_ < 64) & (s_ % 64 <= 31)
    c[:, 643] = (s_ >= 64) & (s_ % 64 <= 31)
    c[:, 768:896] = same * (j <= i)
    return c


def _selc():
    c = np.zeros((NS, 2064), np.float32)
    for b in range(NS):
        c[b, b * 128:(b + 1) * 128] = 1.0
    c[:, 2048:2064] = np.arange(16, dtype=np.float32)[None, :]
    return c


_CACHE = {}


def kernel(x_prompt, x_sample, cache_kv_w128, cache_kv_w512, cache_kv_w2048, state_hgrn, c_prompt, c_sample,
           w_ada, b_ada, norm1_w, norm2_w, w_in, q_norm_w, k_norm_w, hg_lb_logits, hg_norm_w, w_br_a, w_br_b,
           w_o, w_peer_q, peer_subkeys, peer_u, peer_v, _stop_after=99):
    f = lambda a: np.ascontiguousarray(np.asarray(a, dtype=np.float32))
    key = ("nc", _stop_after)
    if key not in _CACHE:
        _CACHE[key] = build_program(_stop_after)
    nc = _CACHE[key]
    caches = [f(cache_kv_w128)[0], f(cache_kv_w512)[0], f(cache_kv_w2048)[0]]
    shared = {
        "w_ada": f(w_ada)[0], "b_ada": f(b_ada).reshape(1, -1), "norm1_w": f(norm1_w).reshape(1, -1),
        "norm2_w": f(norm2_w).reshape(1, -1), "w_in": f(w_in)[0],
        "qk_w": np.ascontiguousarray(np.stack([np.tile(f(q_norm_w)[0], 8), np.tile(f(k_norm_w)[0], 8)])),
        "lb_log": f(hg_lb_logits), "hgn_w": np.ascontiguousarray(np.tile(f(hg_norm_w)[0], 8).reshape(1, -1)),
        "w_br_a": f(w_br_a)[0], "w_br_b": f(w_br_b)[0], "w_o": f(w_o)[0], "w_pq": f(w_peer_q)[0],
        "skT": np.ascontiguousarray(f(peer_subkeys)[0].transpose(0, 2, 1)),
        "peer_u": f(peer_u)[0], "peer_v": f(peer_v)[0], "cst": _consts(), "selc": _selc(),
    }
    xpf, xsf = f(x_prompt), f(x_sample)
    cp, cs = f(c_prompt), f(c_sample)
    st = f(state_hgrn)[0]
    in_maps = []
    for c in range(NCORES):
        m = dict(shared)
        m["xp"] = xpf[c * NSEQ:(c + 1) * NSEQ]
        m["xs"] = np.ascontiguousarray(xsf[c * NS:(c + 1) * NS, 0, :])
        m["cT"] = np.ascontiguousarray(np.concatenate([cp[c * NSEQ:(c + 1) * NSEQ], cs[c * NS:(c + 1) * NS]], 0).T)
        for g in range(3):
            m["ck%d" % g] = np.ascontiguousarray(
                caches[g][c * NS:(c + 1) * NS, 0::GROUPS[g][1]][:, :128].reshape(NS, 128, 2, 512))
        m["st_in"] = st[c * NS:(c + 1) * NS]
        in_maps.append(m)
    res = run_bass_kernel_spmd(nc, in_maps, core_ids=list(range(NCORES)))
    R = res.results
    cat = lambda n: np.concatenate([np.asarray(r[n]) for r in R], 0)
    y_prompt = cat("y_p")
    y_sample = cat("y_s").reshape(NCORES * NS, 1, D)
    outs = [y_prompt, y_sample]
    for g in range(3):
        outs.append(cat("kv%d_p" % g).reshape(1, NCORES * NSEQ, GROUPS[g][0], 2, 8, 64))
    outs.append(cat("hg_p")[None])
    for g in range(3):
        outs.append(cat("kv%d_s" % g).reshape(1, NCORES * NS, 1, 2, 8, 64))
    outs.append(cat("hg_s")[None])
    if DEBUG:
        global _DBG
        _DBG = np.asarray(R[0]["dbg"])
    return tuple(np.ascontiguousarray(o, dtype=np.float32) for o in outs)
```
